# Optimizing a Trainium2 kernel written in Bass

```python
import math
import numpy as np
import jax
import jax.numpy as jnp
from jax import lax

D_MODEL = 1024
BATCH = 8
SEQ = 4096
DEPTH = 4

GRID_W = 64
CTX_LEN = 256
Q_BLOCK = 128
EPS = 1e-6
ROPE_BASE = 10000.0
N_MOD = 6

N_BRANCHES = 4
BRANCH_WIDTH = 256

MLA_HEADS = 4
MLA_Q_RANK = 256
MLA_KV_RANK = 128
MLA_NOPE = 64
MLA_ROPE = 32
MLA_V = 64

NA_HEADS = 4
NA_HEAD_DIM = 64
NA_WIN_ROWS = 8
NA_WIN_COLS = 16

DIFF_HEADS = 4
DIFF_HALF = 32
DIFF_V = 2 * DIFF_HALF

GQA_Q_HEADS = 4
GQA_KV_HEADS = 2
GQA_HEAD_DIM = 64

PEER_HEADS = 8
PEER_NKEYS = 128
PEER_EXPERTS = PEER_NKEYS * PEER_NKEYS
PEER_QDIM = 256
PEER_TOPK = 16
PEER_CHUNK = 128

IN_SIZES = (
    MLA_Q_RANK, MLA_KV_RANK, MLA_ROPE,
    NA_HEADS * NA_HEAD_DIM, NA_HEADS * NA_HEAD_DIM, NA_HEADS * NA_HEAD_DIM,
    DIFF_HEADS * 2 * DIFF_HALF, DIFF_HEADS * 2 * DIFF_HALF, DIFF_HEADS * DIFF_V,
    GQA_Q_HEADS * GQA_HEAD_DIM, GQA_KV_HEADS * GQA_HEAD_DIM, GQA_KV_HEADS * GQA_HEAD_DIM,
)
IN_COLS = sum(IN_SIZES)

kernel_name = 'hybrid_latent_peer_trunk'


def rmsnorm(x, g):
    x32 = x.astype(jnp.float32)
    y = x32 * lax.rsqrt(jnp.mean(x32 * x32, axis=-1, keepdims=True) + EPS)
    return (y * g.astype(jnp.float32)).astype(x.dtype)


def modulate(x, shift, scale):
    return x * (1 + scale) + shift


def split_heads(t, n_heads):
    b, l, _ = t.shape
    return t.reshape(b, l, n_heads, -1).transpose(0, 2, 1, 3)


def merge_heads(t):
    b, h, l, d = t.shape
    return t.transpose(0, 2, 1, 3).reshape(b, l, h * d)


def split_cols(p):
    return jnp.split(p, np.cumsum(IN_SIZES)[:-1].tolist(), axis=-1)


def rope_1d(t, pos):
    d = t.shape[-1]
    inv_freq = jnp.power(ROPE_BASE, -jnp.arange(0, d, 2, dtype=jnp.float32) / d)
    ang = pos[:, None] * inv_freq[None, :]
    cos = jnp.cos(ang).astype(t.dtype)
    sin = jnp.sin(ang).astype(t.dtype)
    t1, t2 = t[..., : d // 2], t[..., d // 2:]
    return jnp.concatenate([t1 * cos - t2 * sin, t1 * sin + t2 * cos], axis=-1)


def axial_rope(t, rows, cols):
    h = t.shape[-1] // 2
    return jnp.concatenate([rope_1d(t[..., :h], rows), rope_1d(t[..., h:], cols)], axis=-1)


def sweep_query_blocks(f, *qs):
    l = qs[0].shape[-2]
    nb = l // Q_BLOCK

    def to_blocks(a):
        a = a.reshape(a.shape[:-2] + (nb, Q_BLOCK, a.shape[-1]))
        return jnp.moveaxis(a, -3, 0)

    out = lax.map(lambda blk: f(*blk), tuple(to_blocks(a) for a in qs))
    out = jnp.moveaxis(out, 0, -3)
    return out.reshape(out.shape[:-3] + (l, out.shape[-1]))


def attend(q, k, v, scale):
    s = jnp.einsum('bkgqd,bksd->bkgqs', q, k).astype(jnp.float32) * scale
    p = jax.nn.softmax(s, axis=-1).astype(v.dtype)
    return jnp.einsum('bkgqs,bksd->bkgqd', p, v)


def attend_latent_and_context(q_lat, k_lat, v_lat, q_ctx, k_ctx, v_ctx, scale):
    k_all = jnp.concatenate([k_lat, k_ctx], axis=2)
    v_all = jnp.concatenate([v_lat, v_ctx], axis=2)
    o_lat = sweep_query_blocks(lambda qb: attend(qb, k_all, v_all, scale), q_lat)
    o_ctx = None if q_ctx is None else attend(q_ctx, k_ctx, v_ctx, scale)
    return o_lat, o_ctx


def mla_mixer(pl, pc, rows, cols, q_norm, kv_norm, w_uq, w_ukv, with_ctx):
    def queries(q_c, rotate):
        q = split_heads(rmsnorm(q_c, q_norm) @ w_uq, MLA_HEADS)
        q_rope = q[..., MLA_NOPE:]
        if rotate:
            q_rope = axial_rope(q_rope, rows, cols)
        return jnp.concatenate([q[..., :MLA_NOPE], q_rope], axis=-1)[:, :, None]

    def keys_values(kv_c, k_r, rotate):
        kv = split_heads(rmsnorm(kv_c, kv_norm) @ w_ukv, MLA_HEADS)
        k_nope, v = kv[..., :MLA_NOPE], kv[..., MLA_NOPE:]
        k_rope = k_r[:, None]
        if rotate:
            k_rope = axial_rope(k_rope, rows, cols)
        k_rope = jnp.broadcast_to(k_rope, k_nope.shape[:-1] + (MLA_ROPE,))
        return jnp.concatenate([k_nope, k_rope], axis=-1), v

    q_lat = queries(pl[0], True)
    k_lat, v_lat = keys_values(pl[1], pl[2], True)
    k_ctx, v_ctx = keys_values(pc[1], pc[2], False)
    q_ctx = queries(pc[0], False) if with_ctx else None
    scale = (MLA_NOPE + MLA_ROPE) ** -0.5
    o_lat, o_ctx = attend_latent_and_context(q_lat, k_lat, v_lat, q_ctx, k_ctx, v_ctx, scale)
    o_ctx = merge_heads(o_ctx[:, :, 0]) if with_ctx else None
    return merge_heads(o_lat[:, :, 0]), o_ctx


def na_mixer(pl, pc, rpb, with_ctx):
    q, k, v = [split_heads(t, NA_HEADS) for t in pl]
    k_ctx, v_ctx = split_heads(pc[1], NA_HEADS), split_heads(pc[2], NA_HEADS)
    b, h, l, d = q.shape
    n_rows = l // GRID_W
    kr = min(NA_WIN_ROWS, n_rows)
    nc = NA_WIN_COLS
    scale = d ** -0.5
    q_g = q.reshape(b, h, n_rows, GRID_W, d)
    k_g = k.reshape(b, h, n_rows, GRID_W, d)
    v_g = v.reshape(b, h, n_rows, GRID_W, d)
    col_q = np.arange(GRID_W)
    col_start = np.clip(col_q - nc // 2, 0, GRID_W - nc)
    col_idx = col_start[:, None] + np.arange(nc)[None, :]
    col_off = col_idx - col_q[:, None] + (NA_WIN_COLS - 1)

    def row_block(args):
        r, q_r = args
        rs = jnp.clip(r - kr // 2, 0, n_rows - kr)
        k_rows = lax.dynamic_slice_in_dim(k_g, rs, kr, axis=2)
        v_rows = lax.dynamic_slice_in_dim(v_g, rs, kr, axis=2)
        k_win = k_rows[:, :, :, col_idx]
        v_win = v_rows[:, :, :, col_idx]
        row_off = rs + jnp.arange(kr) - r + (NA_WIN_ROWS - 1)
        bias = rpb[:, row_off[None, :, None], col_off[:, None, :]]
        s_loc = jnp.einsum('bhcd,bhicjd->bhcij', q_r, k_win).astype(jnp.float32) * scale
        s_loc = s_loc + bias.astype(jnp.float32)
        s_ctx = jnp.einsum('bhcd,bhsd->bhcs', q_r, k_ctx).astype(jnp.float32) * scale
        s = jnp.concatenate([s_loc.reshape(b, h, GRID_W, kr * nc), s_ctx], axis=-1)
        p = jax.nn.softmax(s, axis=-1).astype(v.dtype)
        p_loc = p[..., : kr * nc].reshape(b, h, GRID_W, kr, nc)
        p_ctx = p[..., kr * nc:]
        return (jnp.einsum('bhcij,bhicjd->bhcd', p_loc, v_win)
                + jnp.einsum('bhcs,bhsd->bhcd', p_ctx, v_ctx))

    o = lax.map(row_block, (jnp.arange(n_rows), jnp.moveaxis(q_g, 2, 0)))
    o_lat = merge_heads(jnp.moveaxis(o, 0, 2).reshape(b, h, l, d))
    o_ctx = None
    if with_ctx:
        q_ctx = split_heads(pc[0], NA_HEADS)[:, :, None]
        o_ctx = merge_heads(attend(q_ctx, k_ctx, v_ctx, scale)[:, :, 0])
    return o_lat, o_ctx


def diff_attend(q1, q2, k1, k2, v, lam, scale):
    s1 = jnp.einsum('bhqd,bhsd->bhqs', q1, k1).astype(jnp.float32) * scale
    s2 = jnp.einsum('bhqd,bhsd->bhqs', q2, k2).astype(jnp.float32) * scale
    p = jax.nn.softmax(s1, axis=-1) - lam * jax.nn.softmax(s2, axis=-1)
    return jnp.einsum('bhqs,bhsd->bhqd', p.astype(v.dtype), v)


def diff_mixer(pl, pc, rows, cols, lq1, lk1, lq2, lk2, subln, lam_init, with_ctx):
    q, k, v = [split_heads(t, DIFF_HEADS) for t in pl]
    q1 = axial_rope(q[..., :DIFF_HALF], rows, cols)
    q2 = axial_rope(q[..., DIFF_HALF:], rows, cols)
    k1 = axial_rope(k[..., :DIFF_HALF], rows, cols)
    k2 = axial_rope(k[..., DIFF_HALF:], rows, cols)
    kc = split_heads(pc[1], DIFF_HEADS)
    vc = split_heads(pc[2], DIFF_HEADS)
    kc1, kc2 = kc[..., :DIFF_HALF], kc[..., DIFF_HALF:]
    f32 = jnp.float32
    lam = (jnp.exp(jnp.sum(lq1.astype(f32) * lk1.astype(f32)))
           - jnp.exp(jnp.sum(lq2.astype(f32) * lk2.astype(f32))) + lam_init)
    scale = DIFF_HALF ** -0.5
    k1_all = jnp.concatenate([k1, kc1], axis=2)
    k2_all = jnp.concatenate([k2, kc2], axis=2)
    v_all = jnp.concatenate([v, vc], axis=2)
    o_lat = sweep_query_blocks(
        lambda a, b_: diff_attend(a, b_, k1_all, k2_all, v_all, lam, scale), q1, q2)

    def finish(o):
        return merge_heads(rmsnorm(o, subln) * (1 - lam_init))

    o_ctx = None
    if with_ctx:
        qc = split_heads(pc[0], DIFF_HEADS)
        o_ctx = finish(diff_attend(qc[..., :DIFF_HALF], qc[..., DIFF_HALF:], kc1, kc2, vc, lam, scale))
    return finish(o_lat), o_ctx


def gqa_mixer(pl, pc, rows, cols, q_norm, k_norm, with_ctx):
    group = GQA_Q_HEADS // GQA_KV_HEADS

    def group_q(q):
        b, h, l, d = q.shape
        return q.reshape(b, GQA_KV_HEADS, group, l, d)

    def ungroup(o):
        b, hk, g, l, d = o.shape
        return merge_heads(o.reshape(b, hk * g, l, d))

    q = axial_rope(rmsnorm(split_heads(pl[0], GQA_Q_HEADS), q_norm), rows, cols)
    k = axial_rope(rmsnorm(split_heads(pl[1], GQA_KV_HEADS), k_norm), rows, cols)
    v = split_heads(pl[2], GQA_KV_HEADS)
    k_ctx = rmsnorm(split_heads(pc[1], GQA_KV_HEADS), k_norm)
    v_ctx = split_heads(pc[2], GQA_KV_HEADS)
    q_ctx = group_q(rmsnorm(split_heads(pc[0], GQA_Q_HEADS), q_norm)) if with_ctx else None
    o_lat, o_ctx = attend_latent_and_context(group_q(q), k, v, q_ctx, k_ctx, v_ctx,
                                             GQA_HEAD_DIM ** -0.5)
    return ungroup(o_lat), (ungroup(o_ctx) if with_ctx else None)


def merge_branches(h, outs, w_branch, w_gate, b_gate, w_out):
    terms = [jax.nn.sigmoid(h @ w_gate[i] + b_gate[i]) * (o @ w_branch[i]) for i, o in enumerate(outs)]
    merged = terms[0]
    for t in terms[1:]:
        merged = merged + t
    return merged @ w_out


def peer_ffn(h, w_q, subkeys, u, v):
    b, l, d = h.shape
    tokens = h.reshape(-1, PEER_CHUNK, d)
    kk = PEER_TOPK

    def chunk(x_t):
        t = x_t.shape[0]
        q = (x_t @ w_q).reshape(t, PEER_HEADS, 2, PEER_QDIM // 2)
        s = jnp.einsum('thpd,hpnd->thpn', q, subkeys).astype(jnp.float32)
        s_top, i_top = lax.top_k(s, kk)
        cand = (s_top[:, :, 0, :, None] + s_top[:, :, 1, None, :]).reshape(t, PEER_HEADS, kk * kk)
        cand_idx = (i_top[:, :, 0, :, None] * PEER_NKEYS
                    + i_top[:, :, 1, None, :]).reshape(t, PEER_HEADS, kk * kk)
        best, pos = lax.top_k(cand, kk)
        experts = jnp.take_along_axis(cand_idx, pos, axis=-1)
        g = jax.nn.softmax(best, axis=-1).astype(x_t.dtype)
        u_sel = jnp.take(u, experts, axis=0)
        v_sel = jnp.take(v, experts, axis=0)
        a = jax.nn.gelu(jnp.einsum('td,thkd->thk', x_t, u_sel), approximate=False)
        return jnp.einsum('thk,thkd->td', g * a, v_sel)

    return lax.map(chunk, tokens).reshape(b, l, d)


def setup_inputs(seed: int = 0) -> dict:
    key = jax.random.key(seed)
    ks = iter(jax.random.split(key, 40))
    D = D_MODEL

    def nrm(shape, scale):
        return jax.random.normal(next(ks), shape, jnp.float32) * scale

    def gain(shape):
        return 1.0 + nrm(shape, 0.02)

    return {
        'x': nrm((BATCH, SEQ, D), 1.0),
        'c': nrm((BATCH, D), 1.0),
        'ctx': nrm((BATCH, CTX_LEN, D), 1.0),
        'c_ctx': nrm((D,), 1.0),
        'w_mod': nrm((DEPTH, D, N_MOD * D), 0.5 * D ** -0.5),
        'b_mod': nrm((DEPTH, N_MOD * D), 0.02),
        'norm_mix': gain((DEPTH, D)),
        'norm_ffn': gain((DEPTH, D)),
        'w_in': nrm((DEPTH, D, IN_COLS), D ** -0.5),
        'mla_q_norm': gain((DEPTH, MLA_Q_RANK)),
        'mla_kv_norm': gain((DEPTH, MLA_KV_RANK)),
        'mla_w_uq': nrm((DEPTH, MLA_Q_RANK, MLA_HEADS * (MLA_NOPE + MLA_ROPE)), MLA_Q_RANK ** -0.5),
        'mla_w_ukv': nrm((DEPTH, MLA_KV_RANK, MLA_HEADS * (MLA_NOPE + MLA_V)), MLA_KV_RANK ** -0.5),
        'na_rpb': nrm((DEPTH, NA_HEADS, 2 * NA_WIN_ROWS - 1, 2 * NA_WIN_COLS - 1), 0.5),
        'diff_lam_q1': nrm((DEPTH, DIFF_HALF), 0.1),
        'diff_lam_k1': nrm((DEPTH, DIFF_HALF), 0.1),
        'diff_lam_q2': nrm((DEPTH, DIFF_HALF), 0.1),
        'diff_lam_k2': nrm((DEPTH, DIFF_HALF), 0.1),
        'diff_subln': gain((DEPTH, DIFF_V)),
        'gqa_q_norm': gain((DEPTH, GQA_HEAD_DIM)),
        'gqa_k_norm': gain((DEPTH, GQA_HEAD_DIM)),
        'w_branch': nrm((DEPTH, N_BRANCHES, BRANCH_WIDTH, D), BRANCH_WIDTH ** -0.5),
        'w_gate': nrm((DEPTH, N_BRANCHES, D, D), D ** -0.5),
        'b_gate': nrm((DEPTH, N_BRANCHES, D), 0.02),
        'w_out': nrm((DEPTH, D, D), D ** -0.5),
        'peer_w_q': nrm((DEPTH, D, PEER_HEADS * PEER_QDIM), D ** -0.5),
        'peer_subkeys': nrm((DEPTH, PEER_HEADS, 2, PEER_NKEYS, PEER_QDIM // 2), (PEER_QDIM // 2) ** -0.5),
        'peer_u': nrm((DEPTH, PEER_EXPERTS, D), D ** -0.5),
        'peer_v': nrm((DEPTH, PEER_EXPERTS, D), 0.3),
        'final_norm': gain((D,)),
    }


def reference(x, c, ctx, c_ctx, w_mod, b_mod, norm_mix, norm_ffn, w_in,
              mla_q_norm, mla_kv_norm, mla_w_uq, mla_w_ukv, na_rpb,
              diff_lam_q1, diff_lam_k1, diff_lam_q2, diff_lam_k2, diff_subln,
              gqa_q_norm, gqa_k_norm, w_branch, w_gate, b_gate, w_out,
              peer_w_q, peer_subkeys, peer_u, peer_v, final_norm):
    l_lat = x.shape[1]
    pos = jnp.arange(l_lat)
    rows = (pos // GRID_W).astype(jnp.float32)
    cols = (pos % GRID_W).astype(jnp.float32)
    for l in range(DEPTH):
        with_ctx = l < DEPTH - 1
        mod_lat = (jax.nn.silu(c) @ w_mod[l] + b_mod[l])[:, None, :]
        mod_ctx = jax.nn.silu(c_ctx) @ w_mod[l] + b_mod[l]
        sh1, sc1, g1, sh2, sc2, g2 = jnp.split(mod_lat, N_MOD, axis=-1)
        csh1, csc1, cg1, csh2, csc2, cg2 = jnp.split(mod_ctx, N_MOD, axis=-1)

        h = modulate(rmsnorm(x, norm_mix[l]), sh1, sc1)
        hc = modulate(rmsnorm(ctx, norm_mix[l]), csh1, csc1)
        pl = split_cols(h @ w_in[l])
        pc = split_cols(hc @ w_in[l])
        outs = [
            mla_mixer(pl[0:3], pc[0:3], rows, cols, mla_q_norm[l], mla_kv_norm[l],
                      mla_w_uq[l], mla_w_ukv[l], with_ctx),
            na_mixer(pl[3:6], pc[3:6], na_rpb[l], with_ctx),
            diff_mixer(pl[6:9], pc[6:9], rows, cols, diff_lam_q1[l], diff_lam_k1[l],
                       diff_lam_q2[l], diff_lam_k2[l], diff_subln[l],
                       0.8 - 0.6 * math.exp(-0.3 * l), with_ctx),
            gqa_mixer(pl[9:12], pc[9:12], rows, cols, gqa_q_norm[l], gqa_k_norm[l], with_ctx),
        ]
        x = x + g1 * merge_branches(h, [o[0] for o in outs], w_branch[l], w_gate[l], b_gate[l], w_out[l])

        h = modulate(rmsnorm(x, norm_ffn[l]), sh2, sc2)
        x = x + g2 * peer_ffn(h, peer_w_q[l], peer_subkeys[l], peer_u[l], peer_v[l])

        if with_ctx:
            ctx = ctx + cg1 * merge_branches(hc, [o[1] for o in outs], w_branch[l], w_gate[l],
                                             b_gate[l], w_out[l])
            hc = modulate(rmsnorm(ctx, norm_ffn[l]), csh2, csc2)
            ctx = ctx + cg2 * peer_ffn(hc, peer_w_q[l], peer_subkeys[l], peer_u[l], peer_v[l])
    return rmsnorm(x, final_norm)
```

```python
import math
import contextlib
import numpy as np
import concourse.bass as bass
import concourse.mybir as mybir
from concourse.bass_utils import run_bass_kernel_spmd

F32 = mybir.dt.float32
BF16 = mybir.dt.bfloat16
AF = mybir.ActivationFunctionType
ALU = mybir.AluOpType
AX = mybir.AxisListType

D = 1024
KC = 8
NLAT = 4096
NCTX = 256
NT = NLAT + NCTX
NTILE = NT // 128
DEPTH = 4
EPS = 1e-6
NEG = -30000.0
BLOCKS = [(i * 512, 512, 0) for i in range(8)] + [(4096, 256, 1)]
PBLOCKS = [(i * 256, 256, 0) for i in range(16)] + [(4096, 256, 1)]


class Ev:
    __slots__ = ("key", "val", "clk")

    def __init__(s, key, val, clk):
        s.key = key
        s.val = val
        s.clk = clk


class TB:
    __slots__ = ("name", "w", "r", "excl")

    def __init__(s, name="", excl=False):
        s.name = name
        s.w = None
        s.r = {}
        s.excl = excl


class KB:
    NS = {"sp": 24, "pool": 12, "act": 4}

    def __init__(s, nc, es):
        s.nc = nc
        s.eng = {"pe": nc.tensor, "act": nc.scalar, "dve": nc.vector, "pool": nc.gpsimd, "sp": nc.sync}
        s.semobj = {}
        s.ccnt = {}
        for e in ["pe", "act", "dve", "pool"]:
            s.semobj[e] = es.enter_context(nc.semaphore("cs_" + e))
            s.ccnt[e] = 0
        s.known = {e: {} for e in s.eng}
        s.dcnt = {}
        for q, n in s.NS.items():
            s.dcnt[q] = 0
            for j in range(n):
                s.semobj[(q, j)] = es.enter_context(nc.semaphore("ds_%s%d" % (q, j)))
        s.last_dma = {}
        s.ninstr = 0
        s.dead = False

    def _wait(s, e, deps):
        k = s.known[e]
        changed = False
        for ev in deps:
            if ev is None:
                continue
            if k.get(ev.key, 0) >= ev.val:
                continue
            s.eng[e].wait_ge(s.semobj[ev.key], ev.val)
            if not changed:
                k = dict(k)
                changed = True
            for kk, vv in ev.clk.items():
                if k.get(kk, 0) < vv:
                    k[kk] = vv
            k[ev.key] = ev.val
        if changed:
            s.known[e] = k

    def _deps(s, reads, writes):
        deps = []
        for b in reads:
            if b.w is not None:
                deps.append(b.w)
        for b in writes:
            if b.w is not None:
                deps.append(b.w)
            deps.extend(b.r.values())
        return deps

    def _upd(s, ev, reads, writes):
        for b in reads:
            o = b.r.get(ev.key)
            if o is None or o.val < ev.val:
                b.r[ev.key] = ev
        for b in writes:
            b.w = ev
            b.r = {}

    def op(s, e, fn, reads=(), writes=()):
        if s.dead:
            return None
        ex = [b for b in reads if b.excl]
        if ex:
            reads = [b for b in reads if not b.excl]
            writes = list(writes) + ex
        deps = s._deps(reads, writes)
        if e == "pe":
            deps = [d for d in deps if d.key != "pe"]
        s._wait(e, deps)
        ins = fn()
        s.ccnt[e] += 1
        ins.then_inc(s.semobj[e], 1)
        ev = Ev(e, s.ccnt[e], s.known[e])
        s._upd(ev, reads, writes)
        s.ninstr += 1
        return ev

    def dma(s, q, out, in_, reads=(), writes=()):
        if s.dead:
            return None
        i = s.dcnt[q]
        n = s.NS[q]
        j = i % n
        key = (q, j)
        prev = 16 * (i // n)
        val = prev + 16
        deps = s._deps(reads, writes)
        if prev > 0:
            deps.append(Ev(key, prev, {}))
        s._wait(q, deps)
        ins = s.eng[q].dma_start(out=out, in_=in_)
        ins.then_inc(s.semobj[key], 16)
        s.dcnt[q] += 1
        ev = Ev(key, val, s.known[q])
        s.last_dma[key] = ev
        s._upd(ev, reads, writes)
        s.ninstr += 1
        return ev

    def barrier(s, engines=("pe", "act", "dve", "pool", "sp")):
        if s.dead:
            return
        evs = [Ev(e, s.ccnt[e], {}) for e in ["pe", "act", "dve", "pool"] if s.ccnt[e] > 0]
        evs += list(s.last_dma.values())
        for e in engines:
            s._wait(e, evs)


class StopBuild(Exception):
    pass


class T:
    def __init__(s, t, name, excl=False):
        s.t = t
        s.b = TB(name, excl)

    def __getitem__(s, k):
        return s.t[k]


def build(NL=DEPTH, stop=None, dbg=()):
    nc = bass.Bass("TRN2", target_bir_lowering=False)

    def din(name, shape, dt=F32):
        return nc.dram_tensor(name, list(shape), dt, kind="ExternalInput").ap()

    def dscr(name, shape, dt):
        kind = "ExternalOutput" if name in dbg else "Internal"
        return nc.dram_tensor(name, list(shape), dt, kind=kind).ap()

    L = DEPTH
    xin = din("xin", [NT, D])
    cc = din("cc", [128, 16])
    w_mod = din("w_mod", [L, D, 6 * D])
    b_modT = din("b_modT", [L, 128, 48])
    nmixT = din("nmixT", [L, 128, 8])
    nffnT = din("nffnT", [L, 128, 8])
    fnormT = din("fnormT", [128, 8])
    w_in = din("w_in", [L, D, 2464])
    g_mlaq = din("g_mlaq", [L, 256])
    g_mlakv = din("g_mlakv", [L, 128])
    w_uq = din("w_uq", [L, 256, 384])
    w_ukv = din("w_ukv", [L, 128, 512])
    nab = din("nab", [L, 5, 128, 4, 5, 128])
    lamv = din("lamv", [L, 4, 32])
    g_subln = din("g_subln", [L, 64])
    g_gq = din("g_gq", [L, 64])
    g_gk = din("g_gk", [L, 64])
    w_branch = din("w_branch", [L, 4, 256, D])
    w_gate = din("w_gate", [L, 4, D, D])
    b_gateT = din("b_gateT", [L, 128, 4, 8])
    w_out = din("w_out", [L, D, D])
    w_pq = din("w_pq", [L, D, 2048])
    skT = din("skT", [L, 128, 16, 128])
    uT = din("uT", [L, D, 16384])
    pv = din("pv", [L, 16384, D])
    rope32 = din("rope32", [NT, 32])
    rope64 = din("rope64", [NT, 64])
    out = nc.dram_tensor("out", [NLAT, D], F32, kind="ExternalOutput").ap()

    xa = dscr("xa", [D, NT], F32)
    hta = dscr("hta", [D, NT], BF16)
    ota = dscr("ota", [D, NT], BF16)
    qt_mla = dscr("qt_mla", [4 * 96, NT], BF16)
    kt_mla = dscr("kt_mla", [4 * 96, NT], BF16)
    qt_na = dscr("qt_na", [256, NT], BF16)
    kt_na = dscr("kt_na", [256, NT], BF16)
    qt_df = dscr("qt_df", [256, NT], BF16)
    kt_df = dscr("kt_df", [256, NT], BF16)
    qt_gq = dscr("qt_gq", [256, NT], BF16)
    kt_gq = dscr("kt_gq", [128, NT], BF16)
    v1a = dscr("v1a", [NT, 14 * 65], BF16)
    ub = dscr("ub", [D, 16384], BF16)
    vb = dscr("vb", [16384, D], BF16)

    def tiles_tb(name):
        return [TB("%s%d" % (name, i)) for i in range(NTILE)]

    xa_b = tiles_tb("xa")
    hta_b = tiles_tb("hta")
    ota_b = tiles_tb("ota")
    qk_b = tiles_tb("qk")
    ub_b = TB("ub")
    vb_b = TB("vb")

    def tr(bl, start, n):
        return bl[start // 128:(start + n) // 128]

    es_top = contextlib.ExitStack()
    with es_top:
        kb = KB(nc, es_top)

        uid = [0]

        def sbuf(es, name, shape, dt):
            uid[0] += 1
            name = "%s_%d" % (name, uid[0])
            return T(es.enter_context(nc.sbuf_tensor(name, list(shape), dt)), name)

        def psum(es, name, shape, dt=F32):
            uid[0] += 1
            name = "%s_%d" % (name, uid[0])
            return T(es.enter_context(nc.psum_tensor(name, list(shape), dt)), name, True)

        def mm(o, oap, lhsT, rhs, reads, start=True, stop=True):
            kb.op("pe", lambda: nc.tensor.matmul(oap, lhsT=lhsT, rhs=rhs, start=start, stop=stop),
                  reads=[r.b for r in reads], writes=[o.b])

        def tp(o, oap, iap, ident, reads):
            kb.op("pe", lambda: nc.tensor.transpose(oap, iap, ident), reads=[r.b for r in reads], writes=[o.b])

        def act(o, oap, iap, func, reads, bias=None, scale=None):
            kw = {}
            if bias is not None:
                kw["bias"] = bias
            if scale is not None:
                kw["scale"] = scale
            kb.op("act", lambda: nc.scalar.activation(out=oap, in_=iap, func=func, **kw),
                  reads=[r.b for r in reads], writes=[o.b])

        def tt(e, o, oap, a, b, op, reads):
            en = nc.vector if e == "dve" else nc.gpsimd
            kb.op(e, lambda: en.tensor_tensor(out=oap, in0=a, in1=b, op=op), reads=[r.b for r in reads], writes=[o.b])

        def ts(e, o, oap, a, s1, s2, op0, op1, reads):
            en = nc.vector if e == "dve" else nc.gpsimd
            if op1 is None:
                kb.op(e, lambda: en.tensor_scalar(out=oap, in0=a, scalar1=s1, scalar2=None, op0=op0),
                      reads=[r.b for r in reads], writes=[o.b])
            else:
                kb.op(e, lambda: en.tensor_scalar(out=oap, in0=a, scalar1=s1, scalar2=s2, op0=op0, op1=op1),
                      reads=[r.b for r in reads], writes=[o.b])

        def stt(o, oap, a, sc, b, op0, op1, reads):
            kb.op("dve", lambda: nc.vector.scalar_tensor_tensor(out=oap, in0=a, scalar=sc, in1=b, op0=op0, op1=op1),
                  reads=[r.b for r in reads], writes=[o.b])

        def cp(e, o, oap, iap, reads):
            if e == "act":
                act(o, oap, iap, AF.Copy, reads)
            else:
                en = nc.vector if e == "dve" else nc.gpsimd
                kb.op(e, lambda: en.tensor_copy(out=oap, in_=iap), reads=[r.b for r in reads], writes=[o.b])

        def red(o, oap, iap, op, reads):
            kb.op("dve", lambda: nc.vector.tensor_reduce(out=oap, in_=iap, axis=AX.X, op=op),
                  reads=[r.b for r in reads], writes=[o.b])

        def recip(o, oap, iap, reads):
            kb.op("dve", lambda: nc.vector.reciprocal(out=oap, in_=iap), reads=[r.b for r in reads], writes=[o.b])

        def mset(e, o, oap, val):
            en = nc.vector if e == "dve" else nc.gpsimd
            kb.op(e, lambda: en.memset(oap, val), writes=[o.b])

        def ck(name):
            if stop == name:
                kb.dead = True

        def rstd_from_ss(o, oap, ssap, n, reads):
            ts("dve", o, oap, ssap, 1.0 / n, EPS, ALU.mult, ALU.add, reads)
            act(o, oap, oap, AF.Sqrt, [o])
            recip(o, oap, oap, [o])

        ident_f = sbuf(es_top, "ident_f", [128, 128], F32)
        ident_b = sbuf(es_top, "ident_b", [128, 128], BF16)
        ones_f = sbuf(es_top, "ones_f", [128, 128], F32)
        modT = sbuf(es_top, "modT", [128, 48, 2], F32)
        A1 = sbuf(es_top, "A1", [128, 8, 2], F32)
        A2 = sbuf(es_top, "A2", [128, 8, 2], F32)
        mset("pool", ident_f, ident_f[:], 1.0)
        kb.op("pool", lambda: nc.gpsimd.affine_select(out=ident_f[:], in_=ident_f[:], pattern=[[-1, 128]],
                                                      compare_op=ALU.is_equal, fill=0.0, base=0,
                                                      channel_multiplier=1),
              reads=[ident_f.b], writes=[ident_f.b])
        cp("dve", ident_b, ident_b[:], ident_f[:], [ident_f])
        mset("pool", ones_f, ones_f[:], 1.0)

        def xa_view(c0, n):
            return xa.rearrange("(k p) n -> p k n", p=128)[:, :, c0:c0 + n]

        def hta_view(c0, n):
            return hta.rearrange("(k p) n -> p k n", p=128)[:, :, c0:c0 + n]

        def ota_view(c0, n):
            return ota.rearrange("(k p) n -> p k n", p=128)[:, :, c0:c0 + n]

        with contextlib.ExitStack() as es:
            xt = [sbuf(es, "i_xt%d" % i, [128, D], F32) for i in range(2)]
            xo = [sbuf(es, "i_xo%d" % i, [128, 8, 128], F32) for i in range(2)]
            pp = [psum(es, "i_pp%d" % i, [128, 1024], F32) for i in range(2)]
            for t in range(NTILE):
                a = xt[t % 2]
                o = xo[t % 2]
                p = pp[t % 2]
                kb.dma("sp", a[:], xin[t * 128:(t + 1) * 128, :], writes=[a.b])
                for k in range(8):
                    tp(p, p[:, k * 128:(k + 1) * 128], a[:, k * 128:(k + 1) * 128], ident_f[:], [a, ident_f])
                cp("act" if t % 2 else "dve", o, o[:].rearrange("p k n -> p (k n)"), p[:], [p])
                kb.dma("pool", xa_view(t * 128, 128), o[:], reads=[o.b], writes=[xa_b[t]])
        kb.barrier()
        if stop == "I":
            NL = 0

        def norm_mod(xb, hb, sqk, rstd, tmpk, ssp, Tn, Acol, Bcol):
            for k in range(8):
                q_ = sqk[k % 2]
                act(q_, q_[:, :Tn], xb[:, k, :Tn], AF.Square, [xb])
                mm(ssp, ssp[:, :Tn], ones_f[:], q_[:, :Tn], [ones_f, q_], start=(k == 0), stop=(k == 7))
            rstd_from_ss(rstd, rstd[:, :Tn], ssp[:, :Tn], float(D), [ssp])
            for k in range(8):
                t_ = tmpk[k % 2]
                tt("dve", t_, t_[:, :Tn], xb[:, k, :Tn], rstd[:, :Tn], ALU.mult, [xb, rstd])
                act(hb, hb[:, k, :Tn], t_[:, :Tn], AF.Identity, [t_, modT, A1, A2], bias=Bcol(k), scale=Acol(k))

        for l in range(NL):
          try:
            lam_init = 0.8 - 0.6 * math.exp(-0.3 * l)
            with contextlib.ExitStack() as es:
                cct = sbuf(es, "m_cc", [128, 16], F32)
                sc = sbuf(es, "m_sc", [128, 16], F32)
                wm = [sbuf(es, "m_w%d" % i, [128, 8, 768], F32) for i in range(2)]
                bm = sbuf(es, "m_b", [128, 48], F32)
                nm = sbuf(es, "m_nm", [128, 8], F32)
                nf = sbuf(es, "m_nf", [128, 8], F32)
                pm = psum(es, "m_p", [128, 96], F32)
                kb.dma("sp", cct[:], cc[:, :], writes=[cct.b])
                kb.dma("sp", bm[:], b_modT[l], writes=[bm.b])
                kb.dma("sp", nm[:], nmixT[l], writes=[nm.b])
                kb.dma("sp", nf[:], nffnT[l], writes=[nf.b])
                act(sc, sc[:], cct[:], AF.Silu, [cct])
                for blk in range(8):
                    w = wm[blk % 2]
                    for k in range(8):
                        kb.dma("sp", w[:, k, :], w_mod[l, k * 128:(k + 1) * 128, blk * 768:(blk + 1) * 768], writes=[w.b])
                    for cl in range(6):
                        c = blk * 6 + cl
                        for k in range(8):
                            mm(pm, pm[:, c * 2:c * 2 + 2], w[:, k, cl * 128:(cl + 1) * 128], sc[:, k * 2:k * 2 + 2], [w, sc],
                               start=(k == 0), stop=(k == 7))
                tt("dve", modT, modT[:], pm[:].rearrange("p (c j) -> p c j", j=2),
                   bm[:].unsqueeze(2).to_broadcast([128, 48, 2]), ALU.add, [pm, bm])
                stt(A1, A1[:], modT[:, 8:16, :], 1.0, nm[:].unsqueeze(2).to_broadcast([128, 8, 2]), ALU.add, ALU.mult, [modT, nm])
                stt(A2, A2[:], modT[:, 32:40, :], 1.0, nf[:].unsqueeze(2).to_broadcast([128, 8, 2]), ALU.add, ALU.mult, [modT, nf])
            kb.barrier()
            if stop == "mod":
                break

            with contextlib.ExitStack() as es:
                win = sbuf(es, "a_win", [128, 8, 2464], BF16)
                wuq = sbuf(es, "a_wuq", [128, 2, 384], BF16)
                wukv = sbuf(es, "a_wukv", [128, 512], BF16)
                gq = sbuf(es, "a_gq", [128, 256], F32)
                gkv = sbuf(es, "a_gkv", [128, 128], F32)
                ggq = sbuf(es, "a_ggq", [128, 6, 64], F32)
                r32 = sbuf(es, "a_r32", [128, NTILE, 32], F32)
                r64 = sbuf(es, "a_r64", [128, NTILE, 64], F32)
                xb_ = [sbuf(es, "a_xb%d" % i, [128, 8, 512], F32) for i in range(2)]
                hb_ = [sbuf(es, "a_hb%d" % i, [128, 8, 512], BF16) for i in range(2)]
                sq = [sbuf(es, "a_sq%d" % i, [128, 512], F32) for i in range(2)]
                tmpn = [sbuf(es, "a_tmpn%d" % i, [128, 512], F32) for i in range(2)]
                rstd = sbuf(es, "a_rstd", [128, 512], F32)
                sqs = sbuf(es, "a_sqs", [128, 384], F32)
                st = sbuf(es, "a_st", [128, 8], F32)
                cn = sbuf(es, "a_cn", [128, 384], BF16)
                cnT = sbuf(es, "a_cnT", [128, 3, 128], BF16)
                qf = sbuf(es, "a_qf", [128, 4, 96], BF16)
                kf = sbuf(es, "a_kf", [128, 4, 96], BF16)
                krr = sbuf(es, "a_krr", [128, 32], F32)
                ra = sbuf(es, "a_ra", [128, 512], F32)
                rb = sbuf(es, "a_rb", [128, 512], F32)
                qkb = sbuf(es, "a_qkb", [128, 512], BF16)
                gtmp = sbuf(es, "a_gtmp", [128, 384], F32)
                gtmp2 = sbuf(es, "a_gtmp2", [128, 384], F32)
                v1 = [sbuf(es, "a_v1_%d" % i, [128, 14, 65], BF16) for i in range(2)]
                tq = [sbuf(es, "a_tq%d" % i, [128, 4, 128], BF16) for i in range(4)]
                ssp = psum(es, "a_ssp", [128, 512], F32)
                pA = psum(es, "a_pA", [128, 512], F32)
                pB = psum(es, "a_pB", [128, 1024], F32)
                pC = psum(es, "a_pC", [128, 512], F32)
                pU1 = psum(es, "a_pU1", [128, 512], F32)
                pU2 = psum(es, "a_pU2", [128, 512], F32)
                pT = psum(es, "a_pT", [128, 8, 128], BF16)

                for k in range(8):
                    kb.dma("pool", win[:, k, :], w_in[l, k * 128:(k + 1) * 128, :], writes=[win.b])
                for k in range(2):
                    kb.dma("pool", wuq[:, k, :], w_uq[l, k * 128:(k + 1) * 128, :], writes=[wuq.b])
                kb.dma("pool", wukv[:], w_ukv[l], writes=[wukv.b])
                kb.dma("sp", gq[:], g_mlaq[l].partition_broadcast(128), writes=[gq.b])
                kb.dma("sp", gkv[:], g_mlakv[l].partition_broadcast(128), writes=[gkv.b])
                for h in range(4):
                    kb.dma("sp", ggq[:, h, :], g_gq[l].partition_broadcast(128), writes=[ggq.b])
                for h in range(2):
                    kb.dma("sp", ggq[:, 4 + h, :], g_gk[l].partition_broadcast(128), writes=[ggq.b])
                kb.dma("sp", r32[:], rope32.rearrange("(t p) c -> p t c", p=128), writes=[r32.b])
                kb.dma("sp", r64[:], rope64.rearrange("(t p) c -> p t c", p=128), writes=[r64.b])
                for i in range(2):
                    mset("pool", v1[i], v1[i][:], 1.0)

                tqi = [0]

                def next_tq():
                    tqi[0] += 1
                    return tq[tqi[0] % 4]

                def rope(src5, dst5, rt, t, G, Fq, reads, dstT):
                    tab = rt[:, t, :].rearrange("p (a b f) -> p a b f", a=2, b=2)
                    C = tab[:, 0].unsqueeze(1).to_broadcast([128, G, 2, Fq])
                    S = tab[:, 1].unsqueeze(1).to_broadcast([128, G, 2, Fq])
                    n = G * 2 * Fq
                    rav = ra[:, 0:n].rearrange("p (g b f) -> p g b f", g=G, b=2)
                    rbv = rb[:, 0:n].rearrange("p (g b f) -> p g b f", g=G, b=2)
                    t1 = src5[:, :, :, 0, :]
                    t2 = src5[:, :, :, 1, :]
                    tt("dve", ra, rav, t1, C, ALU.mult, reads + [rt])
                    tt("dve", rb, rbv, t2, S, ALU.mult, reads + [rt])
                    tt("pool", dstT, dst5[:, :, :, 0, :], rav, rbv, ALU.subtract, [ra, rb])
                    tt("dve", ra, rav, t1, S, ALU.mult, reads + [rt])
                    tt("dve", rb, rbv, t2, C, ALU.mult, reads + [rt])
                    tt("pool", dstT, dst5[:, :, :, 1, :], rav, rbv, ALU.add, [ra, rb])

                def r5(ap, G, Fq):
                    if len(ap.shape) == 2:
                        return ap.rearrange("p (g a b f) -> p g a b f", g=G, a=2, b=2)
                    return ap.rearrange("p g (a b f) -> p g a b f", a=2, b=2)

                for bi, (c0, Tn, j) in enumerate(BLOCKS):
                    xb = xb_[bi % 2]
                    hb = hb_[bi % 2]
                    kb.dma("sp", xb[:, :, :Tn], xa_view(c0, Tn), reads=tr(xa_b, c0, Tn), writes=[xb.b])
                    norm_mod(xb, hb, sq, rstd, tmpn, ssp, Tn,
                             lambda k: A1[:, k, j:j + 1], lambda k: modT[:, 0 + k, j:j + 1])
                    kb.dma("pool", hta_view(c0, Tn), hb[:, :, :Tn], reads=[hb.b], writes=tr(hta_b, c0, Tn))
                    ck("A0")
                    for ti in range(Tn // 128):
                        t = c0 // 128 + ti
                        ts_ = slice(ti * 128, (ti + 1) * 128)
                        vv = v1[t % 2]
                        for k in range(8):
                            mm(pA, pA[:, 0:416], hb[:, k, ts_], win[:, k, 0:416], [hb, win], start=(k == 0), stop=(k == 7))
                        act(sqs, sqs[:, 0:384], pA[:, 0:384], AF.Square, [pA])
                        red(st, st[:, 0:1], sqs[:, 0:256], ALU.add, [sqs])
                        red(st, st[:, 1:2], sqs[:, 256:384], ALU.add, [sqs])
                        ts("dve", st, st[:, 0:1], st[:, 0:1], 1.0 / 256, EPS, ALU.mult, ALU.add, [st])
                        ts("dve", st, st[:, 1:2], st[:, 1:2], 1.0 / 128, EPS, ALU.mult, ALU.add, [st])
                        act(st, st[:, 0:2], st[:, 0:2], AF.Sqrt, [st])
                        recip(st, st[:, 0:2], st[:, 0:2], [st])
                        stt(cn, cn[:, 0:256], pA[:, 0:256], st[:, 0:1], gq[:], ALU.mult, ALU.mult, [pA, st, gq])
                        stt(cn, cn[:, 256:384], pA[:, 256:384], st[:, 1:2], gkv[:], ALU.mult, ALU.mult, [pA, st, gkv])
                        ck("A1a")
                        for k in range(3):
                            tp(pT, pT[:, k, :], cn[:, k * 128:(k + 1) * 128], ident_b[:], [cn, ident_b])
                        cp("act", cnT, cnT[:], pT[:, 0:3, :], [pT])
                        ck("A1b")
                        for k in range(2):
                            mm(pU1, pU1[:, 0:384], cnT[:, k, :], wuq[:, k, :], [cnT, wuq], start=(k == 0), stop=(k == 1))
                        mm(pU2, pU2[:, 0:512], cnT[:, 2, :], wukv[:], [cnT, wukv])
                        u1 = pU1[:, 0:384].rearrange("p (h d) -> p h d", h=4)
                        u2 = pU2[:, 0:512].rearrange("p (h d) -> p h d", h=4)
                        ck("A1c")
                        cp("act", qf, qf[:, :, 0:64], u1[:, :, 0:64], [pU1])
                        rope(r5(u1[:, :, 64:96], 4, 8), r5(qf[:, :, 64:96], 4, 8), r32, t, 4, 8, [pU1], qf)
                        ck("A1c1")
                        cp("act", kf, kf[:, :, 0:64], u2[:, :, 0:64], [pU2])
                        ck("A1c2")
                        rope(r5(pA[:, 384:416], 1, 8), r5(krr[:, :], 1, 8), r32, t, 1, 8, [pA], krr)
                        ck("A1c3")
                        cp("pool", kf, kf[:, :, 64:96], krr[:].unsqueeze(1).to_broadcast([128, 4, 32]), [krr])
                        ck("A1c4")
                        cp("dve", vv, vv[:, 0:4, 0:64], u2[:, :, 64:128], [pU2])
                        ck("A1d")
                        for h in range(4):
                            tp(pT, pT[0:96, h, :], qf[:, h, :], ident_b[:], [qf, ident_b])
                        for h in range(4):
                            tp(pT, pT[0:96, 4 + h, :], kf[:, h, :], ident_b[:], [kf, ident_b])
                        o1 = next_tq()
                        o2 = next_tq()
                        cp("act", o1, o1[0:96, :, :], pT[0:96, 0:4, :], [pT])
                        cp("dve", o2, o2[0:96, :, :], pT[0:96, 4:8, :], [pT])
                        cols = slice(t * 128, (t + 1) * 128)
                        ck("A1e")
                        kb.dma("pool", qt_mla.rearrange("(m p) n -> p m n", p=96)[:, :, cols], o1[0:96, :, :], reads=[o1.b], writes=[qk_b[t]])
                        kb.dma("pool", kt_mla.rearrange("(m p) n -> p m n", p=96)[:, :, cols], o2[0:96, :, :], reads=[o2.b], writes=[qk_b[t]])
                        ck("A1")
                        for k in range(8):
                            mm(pB, pB[:, 0:512], hb[:, k, ts_], win[:, k, 416:928], [hb, win], start=(k == 0), stop=(k == 7))
                        for k in range(8):
                            mm(pB, pB[:, 512:768], hb[:, k, ts_], win[:, k, 928:1184], [hb, win], start=(k == 0), stop=(k == 7))
                        cp("act", qkb, qkb[:], pB[:, 0:512], [pB])
                        cp("dve", vv, vv[:, 4:8, 0:64], pB[:, 512:768].rearrange("p (h d) -> p h d", h=4), [pB])
                        for k in range(4):
                            tp(pT, pT[:, k, :], qkb[:, k * 128:(k + 1) * 128], ident_b[:], [qkb, ident_b])
                        o1 = next_tq()
                        cp("act", o1, o1[:], pT[:, 0:4, :], [pT])
                        kb.dma("pool", qt_na.rearrange("(m p) n -> p m n", p=128)[:, :, cols], o1[:, 0:2, :], reads=[o1.b], writes=[qk_b[t]])
                        kb.dma("pool", kt_na.rearrange("(m p) n -> p m n", p=128)[:, :, cols], o1[:, 2:4, :], reads=[o1.b], writes=[qk_b[t]])
                        ck("A2")
                        for k in range(8):
                            mm(pB, pB[:, 0:512], hb[:, k, ts_], win[:, k, 1184:1696], [hb, win], start=(k == 0), stop=(k == 7))
                        for k in range(8):
                            mm(pB, pB[:, 512:768], hb[:, k, ts_], win[:, k, 1696:1952], [hb, win], start=(k == 0), stop=(k == 7))
                        for half in range(2):
                            rope(r5(pB[:, half * 256:(half + 1) * 256], 8, 8), r5(qkb[:, half * 256:(half + 1) * 256], 8, 8),
                                 r32, t, 8, 8, [pB], qkb)
                        cp("dve", vv, vv[:, 8:12, 0:64], pB[:, 512:768].rearrange("p (h d) -> p h d", h=4), [pB])
                        for k in range(4):
                            tp(pT, pT[:, k, :], qkb[:, k * 128:(k + 1) * 128], ident_b[:], [qkb, ident_b])
                        o1 = next_tq()
                        cp("act", o1, o1[:], pT[:, 0:4, :], [pT])
                        kb.dma("pool", qt_df.rearrange("(m p) n -> p m n", p=128)[:, :, cols], o1[:, 0:2, :], reads=[o1.b], writes=[qk_b[t]])
                        kb.dma("pool", kt_df.rearrange("(m p) n -> p m n", p=128)[:, :, cols], o1[:, 2:4, :], reads=[o1.b], writes=[qk_b[t]])
                        ck("A3")
                        for k in range(8):
                            mm(pC, pC[:, 0:512], hb[:, k, ts_], win[:, k, 1952:2464], [hb, win], start=(k == 0), stop=(k == 7))
                        act(sqs, sqs[:, 0:384], pC[:, 0:384], AF.Square, [pC])
                        red(st, st[:, 2:8], sqs[:, 0:384].rearrange("p (h d) -> p h d", h=6), ALU.add, [sqs])
                        rstd_from_ss(st, st[:, 2:8], st[:, 2:8], 64.0, [st])
                        g3 = gtmp[:, 0:384].rearrange("p (h d) -> p h d", h=6)
                        g32 = gtmp2[:, 0:384].rearrange("p (h d) -> p h d", h=6)
                        tt("dve", gtmp, g3, pC[:, 0:384].rearrange("p (h d) -> p h d", h=6),
                           st[:, 2:8].unsqueeze(2).to_broadcast([128, 6, 64]), ALU.mult, [pC, st])
                        tt("pool", gtmp2, g32, g3, ggq[:], ALU.mult, [gtmp, ggq])
                        rope(r5(gtmp2[:, 0:384], 6, 16), r5(qkb[:, 0:384], 6, 16), r64, t, 6, 16, [gtmp2], qkb)
                        cp("dve", vv, vv[:, 12:14, 0:64], pC[:, 384:512].rearrange("p (h d) -> p h d", h=2), [pC])
                        for k in range(3):
                            tp(pT, pT[:, k, :], qkb[:, k * 128:(k + 1) * 128], ident_b[:], [qkb, ident_b])
                        o1 = next_tq()
                        cp("act", o1, o1[:, 0:3, :], pT[:, 0:3, :], [pT])
                        kb.dma("pool", qt_gq.rearrange("(m p) n -> p m n", p=128)[:, :, cols], o1[:, 0:2, :], reads=[o1.b], writes=[qk_b[t]])
                        kb.dma("pool", kt_gq.rearrange("(m p) n -> p m n", p=128)[:, :, cols], o1[:, 2:3, :], reads=[o1.b], writes=[qk_b[t]])
                        kb.dma("pool", v1a[t * 128:(t + 1) * 128, :], vv[:].rearrange("p a b -> p (a b)"), reads=[vv.b], writes=[qk_b[t]])
            kb.barrier()
            if stop == "A":
                break
            cntB = [0]

            def attn_std(mixer, qt_d, kt_d, dk, nq, nk, kmap, vbase, nv, vmap, scale, diff=False):
                with contextlib.ExitStack() as es:
                    KT = sbuf(es, "b_KT", [128, nk, NT], BF16)
                    V1 = sbuf(es, "b_V1", [128, NTILE, nv, 65], BF16)
                    QT = [sbuf(es, "b_QT%d" % i, [128, nq, 512], BF16) for i in range(2)]
                    PT = [sbuf(es, "b_PT%d" % i, [128, 512], BF16) for i in range(3)]
                    osb = sbuf(es, "b_osb", [128, 4, nq, 64], F32)
                    osbb = sbuf(es, "b_osbb", [128, 4, 256], BF16)
                    rs = sbuf(es, "b_rs", [128, 4], F32)
                    otb = [sbuf(es, "b_otb%d" % i, [128, 2, 512], BF16) for i in range(2)]
                    stp = [psum(es, "b_st%d" % i, [128, 512]) for i in range(2)]
                    ops = [psum(es, "b_o%d" % i, [128, 512]) for i in range(4)]
                    tpp = psum(es, "b_tp", [128, 8, 128], BF16)
                    for m in range(nk):
                        kb.dma("sp", KT[0:dk, m, :], kt_d[m * dk:(m + 1) * dk, :], reads=qk_b, writes=[KT.b])
                    v4 = v1a.rearrange("(t p) (a b) -> p t a b", p=128, b=65)
                    for t0 in range(0, NTILE, 9):
                        t1 = min(NTILE, t0 + 9)
                        kb.dma("sp", V1[:, t0:t1, :, :], v4[:, t0:t1, vbase:vbase + nv, :], reads=qk_b, writes=[V1.b])
                    if diff:
                        lamt = sbuf(es, "b_lamt", [128, 4, 32], F32)
                        lp = sbuf(es, "b_lp", [128, 2, 32], F32)
                        ls = sbuf(es, "b_ls", [128, 2], F32)
                        neglam = sbuf(es, "b_neglam", [128, 1], F32)
                        gsub = sbuf(es, "b_gsub", [128, 64], F32)
                        dsb = sbuf(es, "b_dsb", [128, 4, 64], F32)
                        dsq = sbuf(es, "b_dsq", [128, 4, 64], F32)
                        dst = sbuf(es, "b_dst", [128, 4], F32)
                        for i in range(4):
                            kb.dma("sp", lamt[:, i, :], lamv[l, i].partition_broadcast(128), writes=[lamt.b])
                        kb.dma("sp", gsub[:], g_subln[l].partition_broadcast(128), writes=[gsub.b])
                        tt("dve", lp, lp[:, 0, :], lamt[:, 0, :], lamt[:, 1, :], ALU.mult, [lamt])
                        tt("dve", lp, lp[:, 1, :], lamt[:, 2, :], lamt[:, 3, :], ALU.mult, [lamt])
                        red(ls, ls[:, 0:2], lp[:], ALU.add, [lp])
                        act(ls, ls[:], ls[:], AF.Exp, [ls])
                        tt("dve", neglam, neglam[:, 0:1], ls[:, 1:2], ls[:, 0:1], ALU.subtract, [ls])
                        ts("dve", neglam, neglam[:], neglam[:], -lam_init, None, ALU.add, None, [neglam])
                        ts("dve", gsub, gsub[:], gsub[:], 1.0 - lam_init, None, ALU.mult, None, [gsub])
                    for bi, (c0, Tn, j) in enumerate(BLOCKS):
                        QTb = QT[bi % 2]
                        kb.dma("sp", QTb[0:dk, :, :Tn], qt_d.rearrange("(m p) n -> p m n", p=dk)[:, :, c0:c0 + Tn],
                               reads=qk_b, writes=[QTb.b])
                        kchunks = list(range(NTILE)) if j == 0 else [32, 33]
                        nqs = Tn // 128
                        for m in range(nq):
                            pend = None
                            for ci, kc in enumerate(kchunks):
                                sp_ = stp[cntB[0] % 2]
                                pt_ = PT[cntB[0] % 3]
                                cntB[0] += 1
                                mm(sp_, sp_[:, :Tn], KT[0:dk, kmap(m), kc * 128:(kc + 1) * 128], QTb[0:dk, m, :Tn], [KT, QTb])
                                if pend is not None:
                                    pend()
                                act(pt_, pt_[:, :Tn], sp_[:, :Tn], AF.Exp, [sp_], scale=scale)

                                def mk(ci=ci, kc=kc, pt_=pt_, m=m):
                                    def f():
                                        for qs in range(nqs):
                                            mm(ops[qs], ops[qs][:, 0:65], pt_[:, qs * 128:(qs + 1) * 128], V1[:, kc, vmap(m), :], [pt_, V1],
                                               start=(ci == 0), stop=(ci == len(kchunks) - 1))
                                    return f
                                pend = mk()
                            pend()
                            for qs in range(nqs):
                                recip(rs, rs[:, qs:qs + 1], ops[qs][:, 64:65], [ops[qs]])
                                if diff:
                                    ts("dve", osb, osb[:, qs, m, :], ops[qs][:, 0:64], rs[:, qs:qs + 1], None, ALU.mult, None, [ops[qs], rs])
                                else:
                                    ts("dve", osbb, osbb[:, qs, m * 64:(m + 1) * 64], ops[qs][:, 0:64], rs[:, qs:qs + 1], None, ALU.mult, None,
                                       [ops[qs], rs])
                        if diff:
                            for qs in range(nqs):
                                ov = osb[:, qs].rearrange("p (h i) d -> p h i d", i=2)
                                stt(dsb, dsb[:], ov[:, :, 1, :], neglam[:, 0:1], ov[:, :, 0, :], ALU.mult, ALU.add, [osb, neglam])
                                tt("pool", dsq, dsq[:], dsb[:], dsb[:], ALU.mult, [dsb])
                                red(dst, dst[:, 0:4], dsq[:], ALU.add, [dsq])
                                rstd_from_ss(dst, dst[:, 0:4], dst[:, 0:4], 64.0, [dst])
                                tt("dve", dsb, dsb[:], dsb[:], dst[:, 0:4].unsqueeze(2).to_broadcast([128, 4, 64]), ALU.mult, [dsb, dst])
                                tt("pool", osbb, osbb[:, qs, :].rearrange("p (h d) -> p h d", h=4), dsb[:],
                                   gsub[:].unsqueeze(1).to_broadcast([128, 4, 64]), ALU.mult, [dsb, gsub])
                        ot_ = otb[bi % 2]
                        for qs in range(nqs):
                            for c in range(2):
                                tp(tpp, tpp[:, qs * 2 + c, :], osbb[:, qs, c * 128:(c + 1) * 128], ident_b[:], [osbb, ident_b])
                        cp("act", ot_, ot_[:, :, :Tn].rearrange("p c (q n) -> p q c n", n=128),
                           tpp[:, 0:2 * nqs, :].rearrange("p (q c) n -> p q c n", c=2), [tpp])
                        kb.dma("pool", ota_view(c0, Tn)[:, 2 * mixer:2 * mixer + 2, :], ot_[:, :, :Tn], reads=[ot_.b],
                               writes=tr(ota_b, c0, Tn))
                kb.barrier()

            def attn_na():
                scale = 64.0 ** -0.5
                with contextlib.ExitStack() as es:
                    KT = sbuf(es, "n_KT", [128, 4, NT], BF16)
                    QT = sbuf(es, "n_QT", [128, 4, NT], BF16)
                    V1 = sbuf(es, "n_V1", [128, NTILE, 4, 65], BF16)
                    nb = [sbuf(es, "n_nb%d" % i, [128, 4, 5, 128], F32) for i in range(5)]
                    PT = [sbuf(es, "n_PT%d" % i, [128, 128], BF16) for i in range(3)]
                    tmpb = [sbuf(es, "n_tmp%d" % i, [128, 128], F32) for i in range(2)]
                    osbb = sbuf(es, "n_osbb", [128, 256], BF16)
                    rs = sbuf(es, "n_rs", [128, 1], F32)
                    otb = [sbuf(es, "n_otb%d" % i, [128, 2, 128], BF16) for i in range(2)]
                    stp = [psum(es, "n_st%d" % i, [128, 512]) for i in range(2)]
                    ops = [psum(es, "n_o%d" % i, [128, 512]) for i in range(2)]
                    tpp = psum(es, "n_tp", [128, 8, 128], BF16)
                    for m in range(4):
                        kb.dma("sp", KT[0:64, m, :], kt_na[m * 64:(m + 1) * 64, :], reads=qk_b, writes=[KT.b])
                        kb.dma("sp", QT[0:64, m, :], qt_na[m * 64:(m + 1) * 64, :], reads=qk_b, writes=[QT.b])
                    v4 = v1a.rearrange("(t p) (a b) -> p t a b", p=128, b=65)
                    for t0 in range(0, NTILE, 9):
                        t1 = min(NTILE, t0 + 9)
                        kb.dma("sp", V1[:, t0:t1, :, :], v4[:, t0:t1, 4:8, :], reads=qk_b, writes=[V1.b])
                    for cs in range(5):
                        kb.dma("sp", nb[cs][:].rearrange("p a b c -> p (a b c)"), nab[l, cs].rearrange("p a b c -> p (a b c)"),
                               writes=[nb[cs].b])
                    cnt = 0
                    for m in range(NTILE):
                        if m < 32:
                            case = {0: 0, 1: 1, 30: 3, 31: 4}.get(m, 2)
                            k0 = min(max(m - 2, 0), 27)
                            chunks = [(k0 + i, i) for i in range(5)] + [(32, None), (33, None)]
                        else:
                            chunks = [(32, None), (33, None)]
                        for h in range(4):
                            o_ = ops[(m * 4 + h) % 2]
                            for ci, (kc, li) in enumerate(chunks):
                                sp_ = stp[cnt % 2]
                                pt_ = PT[cnt % 3]
                                tb_ = tmpb[cnt % 2]
                                cnt += 1
                                mm(sp_, sp_[:, 0:128], KT[0:64, h, kc * 128:(kc + 1) * 128], QT[0:64, h, m * 128:(m + 1) * 128], [KT, QT])
                                if li is not None:
                                    stt(tb_, tb_[:], sp_[:, 0:128], scale, nb[case][:, h, li, :], ALU.mult, ALU.add, [sp_, nb[case]])
                                    act(pt_, pt_[:], tb_[:], AF.Exp, [tb_])
                                else:
                                    act(pt_, pt_[:], sp_[:, 0:128], AF.Exp, [sp_], scale=scale)
                                mm(o_, o_[:, 0:65], pt_[:], V1[:, kc, h, :], [pt_, V1], start=(ci == 0), stop=(ci == len(chunks) - 1))
                            recip(rs, rs[:, 0:1], o_[:, 64:65], [o_])
                            ts("dve", osbb, osbb[:, h * 64:(h + 1) * 64], o_[:, 0:64], rs[:, 0:1], None, ALU.mult, None, [o_, rs])
                        ot_ = otb[m % 2]
                        for c in range(2):
                            tp(tpp, tpp[:, c, :], osbb[:, c * 128:(c + 1) * 128], ident_b[:], [osbb, ident_b])
                        cp("act", ot_, ot_[:], tpp[:, 0:2, :], [tpp])
                        kb.dma("pool", ota_view(m * 128, 128)[:, 2:4, :], ot_[:], reads=[ot_.b], writes=[ota_b[m]])
                kb.barrier()

            attn_std(0, qt_mla, kt_mla, 96, 4, 4, lambda m: m, 0, 4, lambda m: m, 96.0 ** -0.5)
            ck("B0")
            attn_na()
            ck("B1")
            attn_std(2, qt_df, kt_df, 32, 8, 8, lambda m: m, 8, 4, lambda m: m // 2, 32.0 ** -0.5, diff=True)
            ck("B2")
            attn_std(3, qt_gq, kt_gq, 64, 4, 2, lambda m: m // 2, 12, 2, lambda m: m // 2, 64.0 ** -0.5)
            if stop == "B":
                break
            with contextlib.ExitStack() as es:
                wg = sbuf(es, "c_wg", [128, 4, 8, D], BF16)
                wbr = sbuf(es, "c_wbr", [128, 4, 2, D], BF16)
                wo = sbuf(es, "c_wo", [128, 8, D], BF16)
                bg = sbuf(es, "c_bg", [128, 4, 8], F32)
                hb = sbuf(es, "c_hb", [128, 8, 512], BF16)
                ob = sbuf(es, "c_ob", [128, 8, 512], BF16)
                xb = sbuf(es, "c_xb", [128, 8, 512], F32)
                sig = [sbuf(es, "c_sig%d" % i, [128, 512], BF16) for i in range(2)]
                macc = [sbuf(es, "c_macc%d" % i, [128, 512], F32) for i in range(2)]
                tmpm = [sbuf(es, "c_tmpm%d" % i, [128, 512], F32) for i in range(2)]
                mrg = sbuf(es, "c_mrg", [128, 8, 512], BF16)
                pg = [psum(es, "c_pg%d" % i, [128, 512]) for i in range(2)]
                pb = [psum(es, "c_pb%d" % i, [128, 512]) for i in range(2)]
                py = [psum(es, "c_py%d" % i, [128, 512]) for i in range(2)]
                for i in range(4):
                    for k in range(8):
                        kb.dma("pool", wg[:, i, k, :], w_gate[l, i, k * 128:(k + 1) * 128, :], writes=[wg.b])
                    for k in range(2):
                        kb.dma("pool", wbr[:, i, k, :], w_branch[l, i, k * 128:(k + 1) * 128, :], writes=[wbr.b])
                for k in range(8):
                    kb.dma("pool", wo[:, k, :], w_out[l, k * 128:(k + 1) * 128, :], writes=[wo.b])
                kb.dma("sp", bg[:], b_gateT[l], writes=[bg.b])
                cntC = 0
                for bi, (c0, Tn, j) in enumerate(BLOCKS):
                    kb.dma("sp", hb[:, :, :Tn], hta_view(c0, Tn), reads=tr(hta_b, c0, Tn), writes=[hb.b])
                    kb.dma("sp", ob[:, :, :Tn], ota_view(c0, Tn), reads=tr(ota_b, c0, Tn), writes=[ob.b])
                    kb.dma("sp", xb[:, :, :Tn], xa_view(c0, Tn), reads=tr(xa_b, c0, Tn), writes=[xb.b])
                    for oc in range(8):
                        ocs = slice(oc * 128, (oc + 1) * 128)
                        ma = macc[oc % 2]
                        for i in range(4):
                            g_ = pg[cntC % 2]
                            b_ = pb[cntC % 2]
                            s_ = sig[cntC % 2]
                            t_ = tmpm[cntC % 2]
                            cntC += 1
                            for k in range(8):
                                mm(g_, g_[:, :Tn], wg[:, i, k, ocs], hb[:, k, :Tn], [wg, hb], start=(k == 0), stop=(k == 7))
                            act(s_, s_[:, :Tn], g_[:, :Tn], AF.Sigmoid, [g_, bg], bias=bg[:, i, oc:oc + 1])
                            for k in range(2):
                                mm(b_, b_[:, :Tn], wbr[:, i, k, ocs], ob[:, 2 * i + k, :Tn], [wbr, ob], start=(k == 0), stop=(k == 1))
                            if i == 0:
                                tt("dve", ma, ma[:, :Tn], b_[:, :Tn], s_[:, :Tn], ALU.mult, [b_, s_])
                            else:
                                tt("dve", t_, t_[:, :Tn], b_[:, :Tn], s_[:, :Tn], ALU.mult, [b_, s_])
                                if i < 3:
                                    tt("pool", ma, ma[:, :Tn], ma[:, :Tn], t_[:, :Tn], ALU.add, [ma, t_])
                                else:
                                    tt("pool", mrg, mrg[:, oc, :Tn], ma[:, :Tn], t_[:, :Tn], ALU.add, [ma, t_])
                    for oc in range(8):
                        ocs = slice(oc * 128, (oc + 1) * 128)
                        y_ = py[oc % 2]
                        for k in range(8):
                            mm(y_, y_[:, :Tn], wo[:, k, ocs], mrg[:, k, :Tn], [wo, mrg], start=(k == 0), stop=(k == 7))
                        stt(xb, xb[:, oc, :Tn], y_[:, :Tn], modT[:, 16 + oc, j:j + 1], xb[:, oc, :Tn], ALU.mult, ALU.add, [y_, modT, xb])
                    kb.dma("pool", xa_view(c0, Tn), xb[:, :, :Tn], reads=[xb.b], writes=tr(xa_b, c0, Tn))
            kb.barrier()
            if stop == "C":
                break

            with contextlib.ExitStack() as es:
                cb = [sbuf(es, "d_cb%d" % i, [128, 8192], BF16) for i in range(2)]
                n_ = 0
                for k in range(8):
                    for hf in range(2):
                        b = cb[n_ % 2]
                        n_ += 1
                        kb.dma("pool", b[:], uT[l, k * 128:(k + 1) * 128, hf * 8192:(hf + 1) * 8192], writes=[b.b])
                        kb.dma("sp", ub[k * 128:(k + 1) * 128, hf * 8192:(hf + 1) * 8192], b[:], reads=[b.b], writes=[ub_b])
                pvv = pv[l].rearrange("(a p) d -> p a d", p=128)
                vbv = vb.rearrange("(a p) d -> p a d", p=128)
                for g in range(16):
                    b = cb[n_ % 2]
                    n_ += 1
                    kb.dma("pool", b[:].rearrange("p (a d) -> p a d", d=D), pvv[:, g * 8:(g + 1) * 8, :], writes=[b.b])
                    kb.dma("sp", vbv[:, g * 8:(g + 1) * 8, :], b[:].rearrange("p (a d) -> p a d", d=D), reads=[b.b], writes=[vb_b])
            kb.barrier()
            with contextlib.ExitStack() as es:
                wq = sbuf(es, "d_wq", [128, 8, 2048], BF16)
                sk = sbuf(es, "d_sk", [128, 16, 128], F32)
                xb = sbuf(es, "d_xb", [128, 8, 256], F32)
                hb = sbuf(es, "d_hb", [128, 8, 256], BF16)
                sqk = [sbuf(es, "d_sq%d" % i, [128, 256], F32) for i in range(2)]
                tmpk = [sbuf(es, "d_tk%d" % i, [128, 256], F32) for i in range(2)]
                rstd = sbuf(es, "d_rstd", [128, 256], F32)
                qpc = [sbuf(es, "d_qpc%d" % i, [128, 256], F32) for i in range(2)]
                s_sb = sbuf(es, "d_s", [128, 2, 16, 128], F32)
                top16 = sbuf(es, "d_top", [128, 16, 16], F32)
                mr = sbuf(es, "d_mr", [128, 256], F32)
                best = sbuf(es, "d_best", [128, 8, 16], F32)
                eb = sbuf(es, "d_eb", [128, 8, 16], F32)
                sm = sbuf(es, "d_sm", [128, 6, 8], F32)
                tau = sbuf(es, "d_tau", [128, 2, 8], F32)
                nbias = sbuf(es, "d_nbias", [128, 2, 8], F32)
                cf = [sbuf(es, "d_cf%d" % i, [128, 16, 128], F32) for i in range(4)]
                cand = T(cf[0].t[:].rearrange("p (h a) (b c) -> p h a (b c)", h=8, c=16).rearrange("p h a (b c) -> p h (a b) c", c=16), "cand_alias")
                cand.b = cf[0].b
                pr = [sbuf(es, "d_pr%d" % i, [128, 16, 128], BF16) for i in range(4)]
                tw = [sbuf(es, "d_tw%d" % i, [128, 16, 128], BF16) for i in range(2)]
                Ws = [sbuf(es, "d_Ws%d" % i, [128, 2, 16, 128], BF16) for i in range(2)]
                uch = [sbuf(es, "d_uch%d" % i, [128, 8, 512], BF16) for i in range(2)]
                vch = [sbuf(es, "d_vch%d" % i, [128, 4, D], BF16) for i in range(2)]
                wts = [sbuf(es, "d_wts%d" % i, [128, 4, 256], BF16) for i in range(2)]
                gel = [sbuf(es, "d_gel%d" % i, [128, 256], BF16) for i in range(2)]
                cT = [sbuf(es, "d_cT%d" % i, [128, 256], BF16) for i in range(4)]
                yps = [psum(es, "d_y%d" % i, [128, 512]) for i in range(4)]
                pm1 = psum(es, "d_pm1", [128, 512])
                apsl = [psum(es, "d_a%d" % i, [128, 512]) for i in range(2)]
                wtp = psum(es, "d_wt", [128, 4, 256], BF16)
                for k in range(8):
                    kb.dma("pool", wq[:, k, :], w_pq[l, k * 128:(k + 1) * 128, :], writes=[wq.b])
                kb.dma("sp", sk[:].rearrange("p a b -> p (a b)"), skT[l].rearrange("p a b -> p (a b)"), writes=[sk.b])
                ubv = ub.rearrange("(k p) e -> p k e", p=128)
                cDl = [0]
                cUl = [0]
                for bi, (c0, Tn, j) in enumerate(PBLOCKS):
                    kb.dma("sp", xb[:], xa_view(c0, 256), reads=tr(xa_b, c0, 256), writes=[xb.b])
                    norm_mod(xb, hb, sqk, rstd, tmpk, pm1, 256,
                             lambda k: A2[:, k, j:j + 1], lambda k: modT[:, 24 + k, j:j + 1])
                    for c in range(16):
                        q_ = qpc[c % 2]
                        for k in range(8):
                            mm(pm1, pm1[:, 0:256], wq[:, k, c * 128:(c + 1) * 128], hb[:, k, :], [wq, hb], start=(k == 0), stop=(k == 7))
                        cp("act", q_, q_[:], pm1[:, 0:256], [pm1])
                        for tl in range(2):
                            col = 256 + tl * 128
                            mm(pm1, pm1[:, col:col + 128], q_[:, tl * 128:(tl + 1) * 128], sk[:, c, :], [q_, sk])
                        cp("dve", s_sb, s_sb[:, :, c, :], pm1[:, 256:512].rearrange("p (t n) -> p t n", t=2), [pm1])
                    for tl in range(2):
                        for c in range(16):
                            kb.op("dve", lambda: nc.vector.max(out=top16[:, c, 0:8], in_=s_sb[:, tl, c, :]), reads=[s_sb.b], writes=[top16.b])
                            kb.op("dve", lambda: nc.vector.match_replace(out=mr[:, 0:128], in_to_replace=top16[:, c, 0:8],
                                                                          in_values=s_sb[:, tl, c, :], imm_value=-1e30),
                                  reads=[s_sb.b, top16.b], writes=[mr.b])
                            kb.op("dve", lambda: nc.vector.max(out=top16[:, c, 8:16], in_=mr[:, 0:128]), reads=[mr.b], writes=[top16.b])
                        t4 = top16[:].rearrange("p (h a) k -> p h a k", a=2)
                        tt("dve", cand, cand[:], t4[:, :, 0, :].unsqueeze(3).to_broadcast([128, 8, 16, 16]),
                           t4[:, :, 1, :].unsqueeze(2).to_broadcast([128, 8, 16, 16]), ALU.add, [top16])
                        for h in range(8):
                            ch = cand[:, h].rearrange("p a b -> p (a b)")
                            kb.op("dve", lambda: nc.vector.max(out=best[:, h, 0:8], in_=ch), reads=[cand.b], writes=[best.b])
                            kb.op("dve", lambda: nc.vector.match_replace(out=mr[:, 0:256], in_to_replace=best[:, h, 0:8], in_values=ch,
                                                                          imm_value=-1e30),
                                  reads=[cand.b, best.b], writes=[mr.b])
                            kb.op("dve", lambda: nc.vector.max(out=best[:, h, 8:16], in_=mr[:, 0:256]), reads=[mr.b], writes=[best.b])
                        ts("dve", sm, sm[:, 0, :], best[:, :, 0], -1.0, None, ALU.mult, None, [best])
                        cp("dve", tau, tau[:, tl, :], best[:, :, 15], [best])
                        for h in range(8):
                            act(eb, eb[:, h, :], best[:, h, :], AF.Exp, [best, sm], bias=sm[:, 0, h:h + 1])
                        red(sm, sm[:, 1, :], eb[:], ALU.add, [eb])
                        act(sm, sm[:, 1, :], sm[:, 1, :], AF.Ln, [sm])
                        ts("dve", sm, sm[:, 2, :], t4[:, :, 0, 0], -1.0, None, ALU.mult, None, [top16])
                        stt(sm, sm[:, 3, :], t4[:, :, 1, 0], -1.0, sm[:, 1, :], ALU.mult, ALU.subtract, [top16, sm])
                        tt("dve", nbias, nbias[:, tl, :], sm[:, 0, :], sm[:, 1, :], ALU.subtract, [sm])
                    units = [(ig, tl, h) for ig in range(8) for tl in range(2) for h in range(8)]

                    def cand_of(n):
                        ig, tl, h = units[n]
                        isl = slice(ig * 16, (ig + 1) * 16)
                        c_ = cf[n % 4]
                        tt("pool", c_, c_[:], s_sb[:, tl, 2 * h, isl].unsqueeze(2).to_broadcast([128, 16, 128]),
                           s_sb[:, tl, 2 * h + 1, :].unsqueeze(1).to_broadcast([128, 16, 128]), ALU.add, [s_sb])

                    def exp_of(n):
                        ig, tl, h = units[n]
                        act(pr[n % 4], pr[n % 4][:], cf[n % 4][:], AF.Exp, [cf[n % 4], nbias], bias=nbias[:, tl, h:h + 1])

                    def acc_of(n):
                        ig, tl, h = units[n]
                        W_ = Ws[ig % 2]
                        c_ = cf[n % 4]
                        p_ = pr[n % 4]
                        w_ = tw[n % 2]
                        if h == 0:
                            stt(W_, W_[:, tl], c_[:], tau[:, tl, h:h + 1], p_[:], ALU.is_ge, ALU.mult, [c_, p_, tau])
                        else:
                            stt(w_, w_[:], c_[:], tau[:, tl, h:h + 1], p_[:], ALU.is_ge, ALU.mult, [c_, p_, tau])
                            tt("dve", W_, W_[:, tl], W_[:, tl], w_[:], ALU.add, [W_, w_])

                    cand_of(0)
                    cand_of(1)
                    pend = None
                    grp = None
                    for s_ in range(64 + 8):
                        if s_ + 1 < 64:
                            cand_of(2 * s_ + 2)
                            cand_of(2 * s_ + 3)
                        if s_ < 64:
                            exp_of(2 * s_)
                            exp_of(2 * s_ + 1)
                            acc_of(2 * s_)
                            acc_of(2 * s_ + 1)
                        if s_ >= 8:
                            cpair = s_ - 8
                            i_a = 2 * cpair
                            ig = i_a // 16
                            il = i_a % 16
                            W_ = Ws[ig % 2]
                            if i_a % 4 == 0:
                                u_ = uch[cUl[0] % 2]
                                v_ = vch[cUl[0] % 2]
                                ws_ = wts[cUl[0] % 2]
                                cUl[0] += 1
                                kb.dma("sp", u_[:], ubv[:, :, i_a * 128:(i_a + 4) * 128], reads=[ub_b], writes=[u_.b])
                                kb.dma("sp", v_[:], vb[i_a * 128:(i_a + 4) * 128, :].rearrange("(a p) d -> p a d", p=128), reads=[vb_b],
                                       writes=[v_.b])
                                for k4 in range(4):
                                    for tl in range(2):
                                        tp(wtp, wtp[:, k4, tl * 128:(tl + 1) * 128], W_[:, tl, il + k4, :], ident_b[:], [W_, ident_b])
                                cp("dve", ws_, ws_[:], wtp[:], [wtp])
                                grp = (u_, v_, ws_)
                            u_, v_, ws_ = grp
                            for i in (i_a, i_a + 1):
                                ii = i % 4
                                a_ = apsl[i % 2]
                                for k in range(8):
                                    mm(a_, a_[:, 0:256], u_[:, k, ii * 128:(ii + 1) * 128], hb[:, k, :], [u_, hb], start=(k == 0), stop=(k == 7))
                            if pend is not None:
                                pend()
                            for i in (i_a, i_a + 1):
                                act(gel[i % 2], gel[i % 2][:], apsl[i % 2][:, 0:256], AF.Gelu, [apsl[i % 2]])
                            for i in (i_a, i_a + 1):
                                tt("dve", cT[i % 4], cT[i % 4][:], gel[i % 2][:], ws_[:, i % 4, :], ALU.mult, [gel[i % 2], ws_])

                            def mkv(i_a=i_a, v_=v_):
                                def f():
                                    for i in (i_a, i_a + 1):
                                        c2 = cT[i % 4]
                                        for oc in range(8):
                                            y_ = yps[oc // 2]
                                            mm(y_, y_[:, (oc % 2) * 256:(oc % 2) * 256 + 256], v_[:, i % 4, oc * 128:(oc + 1) * 128], c2[:],
                                               [v_, c2], start=(i == 0 and oc % 2 == 0), stop=(i == 127))
                                return f
                            pend = mkv()
                    pend()
                    for oc in range(8):
                        y_ = yps[oc // 2]
                        stt(xb, xb[:, oc, :], y_[:, (oc % 2) * 256:(oc % 2) * 256 + 256], modT[:, 40 + oc, j:j + 1], xb[:, oc, :],
                            ALU.mult, ALU.add, [y_, modT, xb])
                    kb.dma("sp", xa_view(c0, 256), xb[:], reads=[xb.b], writes=tr(xa_b, c0, 256))
                    ck("D0")
            kb.barrier()
            if stop == "D":
                break
          except StopBuild:
            break

        if stop is None:
            with contextlib.ExitStack() as es:
                fn = sbuf(es, "f_fn", [128, 8], F32)
                xb_ = [sbuf(es, "f_xb%d" % i, [128, 8, 512], F32) for i in range(2)]
                sqk = [sbuf(es, "f_sq%d" % i, [128, 512], F32) for i in range(2)]
                xn = sbuf(es, "f_xn", [128, 8, 512], F32)
                rstd = sbuf(es, "f_rstd", [128, 512], F32)
                yo = [sbuf(es, "f_yo%d" % i, [128, D], F32) for i in range(2)]
                ssp = psum(es, "f_ssp", [128, 512])
                pp = [psum(es, "f_pp%d" % i, [128, 1024]) for i in range(2)]
                kb.dma("sp", fn[:], fnormT[:, :], writes=[fn.b])
                for bi in range(8):
                    c0 = bi * 512
                    xb = xb_[bi % 2]
                    kb.dma("sp", xb[:], xa_view(c0, 512), reads=tr(xa_b, c0, 512), writes=[xb.b])
                    for k in range(8):
                        q_ = sqk[k % 2]
                        act(q_, q_[:], xb[:, k, :], AF.Square, [xb])
                        mm(ssp, ssp[:], ones_f[:], q_[:], [ones_f, q_], start=(k == 0), stop=(k == 7))
                    rstd_from_ss(rstd, rstd[:], ssp[:], float(D), [ssp])
                    for k in range(8):
                        stt(xn, xn[:, k, :], xb[:, k, :], fn[:, k:k + 1], rstd[:], ALU.mult, ALU.mult, [xb, fn, rstd])
                    for ti in range(4):
                        t = bi * 4 + ti
                        p = pp[t % 2]
                        o = yo[t % 2]
                        for k in range(8):
                            tp(p, p[:, k * 128:(k + 1) * 128], xn[:, k, ti * 128:(ti + 1) * 128], ident_f[:], [xn, ident_f])
                        cp("act" if t % 2 else "dve", o, o[:], p[:], [p])
                        kb.dma("sp", out[t * 128:(t + 1) * 128, :], o[:], reads=[o.b])

        kb.dead = False
        kb.barrier()
    return nc


def rope_tables():
    pos = np.arange(NLAT)
    rows = (pos // 64).astype(np.float32)
    cols = (pos % 64).astype(np.float32)

    def tab(dh):
        inv = np.power(np.float32(10000.0), -np.arange(0, dh, 2, dtype=np.float32) / np.float32(dh)).astype(np.float32)
        out = np.zeros((NT, 2, 2, dh // 2), np.float32)
        out[:, 0] = 1.0
        for a, p_ in enumerate((rows, cols)):
            ang = (p_[:, None] * inv[None, :]).astype(np.float32)
            out[:NLAT, 0, a] = np.cos(ang)
            out[:NLAT, 1, a] = np.sin(ang)
        return out.reshape(NT, -1)

    return tab(16), tab(32)


def na_bias_tables(rpb):
    Lr = rpb.shape[0]
    out = np.full((Lr, 5, 128, 4, 5, 128), NEG, np.float32)
    for case, m in enumerate((0, 1, 2, 30, 31)):
        k0 = min(max(m - 2, 0), 27)
        q = m * 128 + np.arange(128)
        qr, qc = q // 64, q % 64
        rs = np.clip(qr - 4, 0, 56)
        cs = np.clip(qc - 8, 0, 48)
        for i in range(5):
            key = (k0 + i) * 128 + np.arange(128)
            kr, kc_ = key // 64, key % 64
            inw = ((kr[:, None] >= rs[None, :]) & (kr[:, None] < rs[None, :] + 8)
                   & (kc_[:, None] >= cs[None, :]) & (kc_[:, None] < cs[None, :] + 16))
            ro = np.clip(kr[:, None] - qr[None, :] + 7, 0, 14)
            co = np.clip(kc_[:, None] - qc[None, :] + 15, 0, 30)
            for h in range(4):
                g = rpb[:, h][:, ro, co]
                out[:, case, :, h, i, :] = np.where(inw[None], g, np.float32(NEG))
    return out


def prep_inputs(inp):
    f = lambda a: np.ascontiguousarray(np.asarray(a, dtype=np.float32))
    L = DEPTH
    r32, r64 = rope_tables()
    shared = {
        "w_mod": f(inp["w_mod"]),
        "b_modT": f(inp["b_mod"].reshape(L, 48, 128).transpose(0, 2, 1)),
        "nmixT": f(inp["norm_mix"].reshape(L, 8, 128).transpose(0, 2, 1)),
        "nffnT": f(inp["norm_ffn"].reshape(L, 8, 128).transpose(0, 2, 1)),
        "fnormT": f(inp["final_norm"].reshape(8, 128).T),
        "w_in": f(inp["w_in"]),
        "g_mlaq": f(inp["mla_q_norm"]),
        "g_mlakv": f(inp["mla_kv_norm"]),
        "w_uq": f(inp["mla_w_uq"]),
        "w_ukv": f(inp["mla_w_ukv"]),
        "nab": f(na_bias_tables(np.asarray(inp["na_rpb"], np.float32))),
        "lamv": f(np.stack([inp["diff_lam_q1"], inp["diff_lam_k1"], inp["diff_lam_q2"], inp["diff_lam_k2"]], axis=1)),
        "g_subln": f(inp["diff_subln"]),
        "g_gq": f(inp["gqa_q_norm"]),
        "g_gk": f(inp["gqa_k_norm"]),
        "w_branch": f(inp["w_branch"]),
        "w_gate": f(inp["w_gate"]),
        "b_gateT": f(inp["b_gate"].reshape(L, 4, 8, 128).transpose(0, 3, 1, 2)),
        "w_out": f(inp["w_out"]),
        "w_pq": f(inp["peer_w_q"]),
        "skT": f(inp["peer_subkeys"].reshape(L, 16, 128, 128).transpose(0, 3, 1, 2)),
        "uT": f(np.asarray(inp["peer_u"]).transpose(0, 2, 1)),
        "pv": f(inp["peer_v"]),
        "rope32": f(r32),
        "rope64": f(r64),
    }
    maps = []
    for b in range(8):
        m = dict(shared)
        m["xin"] = f(np.concatenate([inp["x"][b], inp["ctx"][b]], axis=0))
        cc = np.stack([inp["c"][b], inp["c_ctx"]], axis=0).reshape(2, 8, 128).transpose(2, 1, 0).reshape(128, 16)
        m["cc"] = f(cc)
        maps.append(m)
    return maps


def kernel(**inputs):
    maps = prep_inputs(inputs)
    nc = build()
    res = run_bass_kernel_spmd(nc, maps, core_ids=list(range(8)))
    return np.stack([np.asarray(r["out"], dtype=np.float32) for r in res.results], axis=0)
```

```python
import math
import contextlib
import numpy as np
import concourse.bass as bass
import concourse.mybir as mybir
from concourse.bass_utils import run_bass_kernel_spmd

F32 = mybir.dt.float32
BF16 = mybir.dt.bfloat16
AF = mybir.ActivationFunctionType
ALU = mybir.AluOpType
AX = mybir.AxisListType

D = 1024
KC = 8
NLAT = 4096
NCTX = 256
NT = NLAT + NCTX
NTILE = NT // 128
DEPTH = 4
EPS = 1e-6
NEG = -30000.0
BLOCKS = [(i * 512, 512, 0) for i in range(8)] + [(4096, 256, 1)]
PBLOCKS = [(i * 256, 256, 0) for i in range(16)] + [(4096, 256, 1)]


class Ev:
    __slots__ = ("key", "val", "clk")

    def __init__(s, key, val, clk):
        s.key = key
        s.val = val
        s.clk = clk


class TB:
    __slots__ = ("name", "w", "r", "excl")

    def __init__(s, name="", excl=False):
        s.name = name
        s.w = None
        s.r = {}
        s.excl = excl


class KB:
    NS = {"sp": 24, "pool": 40, "act": 2}

    def __init__(s, nc, es):
        s.nc = nc
        s.eng = {"pe": nc.tensor, "act": nc.scalar, "dve": nc.vector, "pool": nc.gpsimd, "sp": nc.sync}
        s.semobj = {}
        s.ccnt = {}
        for e in ["pe", "act", "dve", "pool"]:
            s.semobj[e] = es.enter_context(nc.semaphore("cs_" + e))
            s.ccnt[e] = 0
        s.known = {e: {} for e in s.eng}
        s.dcnt = {}
        for q, n in s.NS.items():
            s.dcnt[q] = 0
            for j in range(n):
                s.semobj[(q, j)] = es.enter_context(nc.semaphore("ds_%s%d" % (q, j)))
        s.last_dma = {}
        s.ninstr = 0
        s.dead = False

    def _wait(s, e, deps):
        k = s.known[e]
        changed = False
        for ev in deps:
            if ev is None:
                continue
            if k.get(ev.key, 0) >= ev.val:
                continue
            s.eng[e].wait_ge(s.semobj[ev.key], ev.val)
            if not changed:
                k = dict(k)
                changed = True
            for kk, vv in ev.clk.items():
                if k.get(kk, 0) < vv:
                    k[kk] = vv
            k[ev.key] = ev.val
        if changed:
            s.known[e] = k

    def _deps(s, reads, writes):
        deps = []
        for b in reads:
            if b.w is not None:
                deps.append(b.w)
        for b in writes:
            if b.w is not None:
                deps.append(b.w)
            deps.extend(b.r.values())
        return deps

    def _upd(s, ev, reads, writes):
        for b in reads:
            o = b.r.get(ev.key)
            if o is None or o.val < ev.val:
                b.r[ev.key] = ev
        for b in writes:
            b.w = ev
            b.r = {}

    def op(s, e, fn, reads=(), writes=()):
        if s.dead:
            return None
        ex = [b for b in reads if b.excl]
        if ex:
            reads = [b for b in reads if not b.excl]
            writes = list(writes) + ex
        deps = s._deps(reads, writes)
        if e == "pe":
            deps = [d for d in deps if d.key != "pe"]
        s._wait(e, deps)
        ins = fn()
        s.ccnt[e] += 1
        ins.then_inc(s.semobj[e], 1)
        ev = Ev(e, s.ccnt[e], s.known[e])
        s._upd(ev, reads, writes)
        s.ninstr += 1
        return ev

    def dma(s, q, out, in_, reads=(), writes=()):
        if s.dead:
            return None
        i = s.dcnt[q]
        n = s.NS[q]
        j = i % n
        key = (q, j)
        prev = 16 * (i // n)
        val = prev + 16
        deps = s._deps(reads, writes)
        if prev > 0:
            deps.append(Ev(key, prev, {}))
        s._wait(q, deps)
        ins = s.eng[q].dma_start(out=out, in_=in_)
        ins.then_inc(s.semobj[key], 16)
        s.dcnt[q] += 1
        ev = Ev(key, val, s.known[q])
        s.last_dma[key] = ev
        s._upd(ev, reads, writes)
        s.ninstr += 1
        return ev

    def barrier(s, engines=("pe", "act", "dve", "pool", "sp")):
        if s.dead:
            return
        evs = [Ev(e, s.ccnt[e], {}) for e in ["pe", "act", "dve", "pool"] if s.ccnt[e] > 0]
        evs += list(s.last_dma.values())
        for e in engines:
            s._wait(e, evs)


class StopBuild(Exception):
    pass


class T:
    def __init__(s, t, name, excl=False):
        s.t = t
        s.b = TB(name, excl)

    def __getitem__(s, k):
        return s.t[k]


def build(NL=DEPTH, stop=None, dbg=()):
    nc = bass.Bass("TRN2", target_bir_lowering=False)

    def din(name, shape, dt=F32):
        return nc.dram_tensor(name, list(shape), dt, kind="ExternalInput").ap()

    def dscr(name, shape, dt):
        kind = "ExternalOutput" if name in dbg else "Internal"
        return nc.dram_tensor(name, list(shape), dt, kind=kind).ap()

    L = DEPTH
    xin = din("xin", [NT, D])
    cc = din("cc", [128, 16])
    w_mod = din("w_mod", [L, D, 6 * D])
    b_modT = din("b_modT", [L, 128, 48])
    nmixT = din("nmixT", [L, 128, 8])
    nffnT = din("nffnT", [L, 128, 8])
    fnormT = din("fnormT", [128, 8])
    w_in = din("w_in", [L, D, 2464])
    g_mlaq = din("g_mlaq", [L, 256])
    g_mlakv = din("g_mlakv", [L, 128])
    w_uq = din("w_uq", [L, 256, 384])
    w_ukv = din("w_ukv", [L, 128, 512])
    nab = din("nab", [L, 5, 128, 4, 5, 128])
    lamv = din("lamv", [L, 4, 32])
    g_subln = din("g_subln", [L, 64])
    g_gq = din("g_gq", [L, 64])
    g_gk = din("g_gk", [L, 64])
    w_branch = din("w_branch", [L, 4, 256, D])
    w_gate = din("w_gate", [L, 4, D, D])
    b_gateT = din("b_gateT", [L, 128, 4, 8])
    w_out = din("w_out", [L, D, D])
    w_pq = din("w_pq", [L, D, 2048])
    skT = din("skT", [L, 128, 16, 128])
    uT = din("uT", [L, D, 16384])
    pv = din("pv", [L, 16384, D])
    rope32 = din("rope32", [NT, 32])
    rope64 = din("rope64", [NT, 64])
    out = nc.dram_tensor("out", [NLAT, D], F32, kind="ExternalOutput").ap()

    xa = dscr("xa", [D, NT], F32)
    hta = dscr("hta", [D, NT], BF16)
    ota = dscr("ota", [D, NT], BF16)
    qt_mla = dscr("qt_mla", [4 * 96, NT], BF16)
    kt_mla = dscr("kt_mla", [4 * 96, NT], BF16)
    qt_na = dscr("qt_na", [256, NT], BF16)
    kt_na = dscr("kt_na", [256, NT], BF16)
    qt_df = dscr("qt_df", [256, NT], BF16)
    kt_df = dscr("kt_df", [256, NT], BF16)
    qt_gq = dscr("qt_gq", [256, NT], BF16)
    kt_gq = dscr("kt_gq", [128, NT], BF16)
    v1a = dscr("v1a", [NT, 14 * 65], BF16)
    ub2 = [dscr("ub%d" % i, [D, 16384], BF16) for i in range(2)]
    vb2 = [dscr("vb%d" % i, [16384, D], BF16) for i in range(2)]

    def tiles_tb(name):
        return [TB("%s%d" % (name, i)) for i in range(NTILE)]

    xa_b = tiles_tb("xa")
    hta_b = tiles_tb("hta")
    ota_b = tiles_tb("ota")
    qk_b = tiles_tb("qk")
    ub_b2 = [TB("ub0"), TB("ub1")]
    vb_b2 = [TB("vb0"), TB("vb1")]

    def tr(bl, start, n):
        return bl[start // 128:(start + n) // 128]

    es_top = contextlib.ExitStack()
    with es_top:
        kb = KB(nc, es_top)

        uid = [0]

        def sbuf(es, name, shape, dt):
            uid[0] += 1
            name = "%s_%d" % (name, uid[0])
            return T(es.enter_context(nc.sbuf_tensor(name, list(shape), dt)), name)

        def psum(es, name, shape, dt=F32):
            uid[0] += 1
            name = "%s_%d" % (name, uid[0])
            return T(es.enter_context(nc.psum_tensor(name, list(shape), dt)), name, True)

        def mm(o, oap, lhsT, rhs, reads, start=True, stop=True):
            kb.op("pe", lambda: nc.tensor.matmul(oap, lhsT=lhsT, rhs=rhs, start=start, stop=stop),
                  reads=[r.b for r in reads], writes=[o.b])

        def tp(o, oap, iap, ident, reads):
            kb.op("pe", lambda: nc.tensor.transpose(oap, iap, ident), reads=[r.b for r in reads], writes=[o.b])

        def act(o, oap, iap, func, reads, bias=None, scale=None):
            kw = {}
            if bias is not None:
                kw["bias"] = bias
            if scale is not None:
                kw["scale"] = scale
            kb.op("act", lambda: nc.scalar.activation(out=oap, in_=iap, func=func, **kw),
                  reads=[r.b for r in reads], writes=[o.b])

        def tt(e, o, oap, a, b, op, reads):
            en = nc.vector if e == "dve" else nc.gpsimd
            kb.op(e, lambda: en.tensor_tensor(out=oap, in0=a, in1=b, op=op), reads=[r.b for r in reads], writes=[o.b])

        def ts(e, o, oap, a, s1, s2, op0, op1, reads):
            en = nc.vector if e == "dve" else nc.gpsimd
            if op1 is None:
                kb.op(e, lambda: en.tensor_scalar(out=oap, in0=a, scalar1=s1, scalar2=None, op0=op0),
                      reads=[r.b for r in reads], writes=[o.b])
            else:
                kb.op(e, lambda: en.tensor_scalar(out=oap, in0=a, scalar1=s1, scalar2=s2, op0=op0, op1=op1),
                      reads=[r.b for r in reads], writes=[o.b])

        def stt(o, oap, a, sc, b, op0, op1, reads):
            kb.op("dve", lambda: nc.vector.scalar_tensor_tensor(out=oap, in0=a, scalar=sc, in1=b, op0=op0, op1=op1),
                  reads=[r.b for r in reads], writes=[o.b])

        def cp(e, o, oap, iap, reads):
            if e == "act":
                act(o, oap, iap, AF.Copy, reads)
            else:
                en = nc.vector if e == "dve" else nc.gpsimd
                kb.op(e, lambda: en.tensor_copy(out=oap, in_=iap), reads=[r.b for r in reads], writes=[o.b])

        def red(o, oap, iap, op, reads):
            kb.op("dve", lambda: nc.vector.tensor_reduce(out=oap, in_=iap, axis=AX.X, op=op),
                  reads=[r.b for r in reads], writes=[o.b])

        def recip(o, oap, iap, reads):
            kb.op("dve", lambda: nc.vector.reciprocal(out=oap, in_=iap), reads=[r.b for r in reads], writes=[o.b])

        def mset(e, o, oap, val):
            en = nc.vector if e == "dve" else nc.gpsimd
            kb.op(e, lambda: en.memset(oap, val), writes=[o.b])

        def ck(name):
            if stop == name:
                kb.dead = True

        def rstd_from_ss(o, oap, ssap, n, reads):
            ts("dve", o, oap, ssap, 1.0 / n, EPS, ALU.mult, ALU.add, reads)
            act(o, oap, oap, AF.Sqrt, [o])
            recip(o, oap, oap, [o])

        ident_f = sbuf(es_top, "ident_f", [128, 128], F32)
        ident_b = sbuf(es_top, "ident_b", [128, 128], BF16)
        ones_f = sbuf(es_top, "ones_f", [128, 128], F32)
        modT = sbuf(es_top, "modT", [128, 48, 2], F32)
        A1 = sbuf(es_top, "A1", [128, 8, 2], F32)
        A2 = sbuf(es_top, "A2", [128, 8, 2], F32)
        mset("pool", ident_f, ident_f[:], 1.0)
        kb.op("pool", lambda: nc.gpsimd.affine_select(out=ident_f[:], in_=ident_f[:], pattern=[[-1, 128]],
                                                      compare_op=ALU.is_equal, fill=0.0, base=0,
                                                      channel_multiplier=1),
              reads=[ident_f.b], writes=[ident_f.b])
        cp("dve", ident_b, ident_b[:], ident_f[:], [ident_f])
        mset("pool", ones_f, ones_f[:], 1.0)

        def convert_uv(lc):
            sset = lc % 2
            for k in range(8):
                for hf in range(2):
                    kb.dma("pool", ub2[sset][k * 128:(k + 1) * 128, hf * 8192:(hf + 1) * 8192],
                           uT[lc, k * 128:(k + 1) * 128, hf * 8192:(hf + 1) * 8192], writes=[ub_b2[sset]])
            src = pv[lc].rearrange("(g p r) d -> g p (r d)", p=128, r=8)
            dst = vb2[sset].rearrange("(g p r) d -> g p (r d)", p=128, r=8)
            for g in range(16):
                kb.dma("pool", dst[g], src[g], writes=[vb_b2[sset]])

        def xa_view(c0, n):
            return xa.rearrange("(k p) n -> p k n", p=128)[:, :, c0:c0 + n]

        def hta_view(c0, n):
            return hta.rearrange("(k p) n -> p k n", p=128)[:, :, c0:c0 + n]

        def ota_view(c0, n):
            return ota.rearrange("(k p) n -> p k n", p=128)[:, :, c0:c0 + n]

        if NL > 0:
            convert_uv(0)
        with contextlib.ExitStack() as es:
            xt = [sbuf(es, "i_xt%d" % i, [128, D], F32) for i in range(2)]
            xo = [sbuf(es, "i_xo%d" % i, [128, 8, 128], F32) for i in range(2)]
            pp = [psum(es, "i_pp%d" % i, [128, 1024], F32) for i in range(2)]
            for t in range(NTILE):
                a = xt[t % 2]
                o = xo[t % 2]
                p = pp[t % 2]
                kb.dma("sp", a[:], xin[t * 128:(t + 1) * 128, :], writes=[a.b])
                for k in range(8):
                    tp(p, p[:, k * 128:(k + 1) * 128], a[:, k * 128:(k + 1) * 128], ident_f[:], [a, ident_f])
                cp("act" if t % 2 else "dve", o, o[:].rearrange("p k n -> p (k n)"), p[:], [p])
                kb.dma("pool", xa_view(t * 128, 128), o[:], reads=[o.b], writes=[xa_b[t]])
        kb.barrier()
        if stop == "I":
            NL = 0

        def norm_mod(xb, hb, sqk, rstd, tmpk, ssp, Tn, Acol, Bcol):
            for k in range(8):
                q_ = sqk[k % 2]
                act(q_, q_[:, :Tn], xb[:, k, :Tn], AF.Square, [xb])
                mm(ssp, ssp[:, :Tn], ones_f[:], q_[:, :Tn], [ones_f, q_], start=(k == 0), stop=(k == 7))
            rstd_from_ss(rstd, rstd[:, :Tn], ssp[:, :Tn], float(D), [ssp])
            for k in range(8):
                t_ = tmpk[k % 2]
                tt("dve", t_, t_[:, :Tn], xb[:, k, :Tn], rstd[:, :Tn], ALU.mult, [xb, rstd])
                act(hb, hb[:, k, :Tn], t_[:, :Tn], AF.Identity, [t_, modT, A1, A2], bias=Bcol(k), scale=Acol(k))

        for l in range(NL):
          try:
            lam_init = 0.8 - 0.6 * math.exp(-0.3 * l)
            with contextlib.ExitStack() as es:
                cct = sbuf(es, "m_cc", [128, 16], F32)
                sc = sbuf(es, "m_sc", [128, 16], F32)
                wm = [sbuf(es, "m_w%d" % i, [128, 8, 768], F32) for i in range(2)]
                bm = sbuf(es, "m_b", [128, 48], F32)
                nm = sbuf(es, "m_nm", [128, 8], F32)
                nf = sbuf(es, "m_nf", [128, 8], F32)
                pm = psum(es, "m_p", [128, 96], F32)
                kb.dma("sp", cct[:], cc[:, :], writes=[cct.b])
                kb.dma("sp", bm[:], b_modT[l], writes=[bm.b])
                kb.dma("sp", nm[:], nmixT[l], writes=[nm.b])
                kb.dma("sp", nf[:], nffnT[l], writes=[nf.b])
                act(sc, sc[:], cct[:], AF.Silu, [cct])
                for blk in range(8):
                    w = wm[blk % 2]
                    for k in range(8):
                        kb.dma("sp", w[:, k, :], w_mod[l, k * 128:(k + 1) * 128, blk * 768:(blk + 1) * 768], writes=[w.b])
                    for cl in range(6):
                        c = blk * 6 + cl
                        for k in range(8):
                            mm(pm, pm[:, c * 2:c * 2 + 2], w[:, k, cl * 128:(cl + 1) * 128], sc[:, k * 2:k * 2 + 2], [w, sc],
                               start=(k == 0), stop=(k == 7))
                tt("dve", modT, modT[:], pm[:].rearrange("p (c j) -> p c j", j=2),
                   bm[:].unsqueeze(2).to_broadcast([128, 48, 2]), ALU.add, [pm, bm])
                stt(A1, A1[:], modT[:, 8:16, :], 1.0, nm[:].unsqueeze(2).to_broadcast([128, 8, 2]), ALU.add, ALU.mult, [modT, nm])
                stt(A2, A2[:], modT[:, 32:40, :], 1.0, nf[:].unsqueeze(2).to_broadcast([128, 8, 2]), ALU.add, ALU.mult, [modT, nf])
            kb.barrier()
            if stop == "mod":
                break

            with contextlib.ExitStack() as es:
                win = sbuf(es, "a_win", [128, 8, 2464], BF16)
                wuq = sbuf(es, "a_wuq", [128, 2, 384], BF16)
                wukv = sbuf(es, "a_wukv", [128, 512], BF16)
                gq = sbuf(es, "a_gq", [128, 256], F32)
                gkv = sbuf(es, "a_gkv", [128, 128], F32)
                ggq = sbuf(es, "a_ggq", [128, 6, 64], F32)
                r32 = sbuf(es, "a_r32", [128, NTILE, 32], F32)
                r64 = sbuf(es, "a_r64", [128, NTILE, 64], F32)
                xb_ = [sbuf(es, "a_xb%d" % i, [128, 8, 512], F32) for i in range(2)]
                hb_ = [sbuf(es, "a_hb%d" % i, [128, 8, 512], BF16) for i in range(2)]
                sq = [sbuf(es, "a_sq%d" % i, [128, 512], F32) for i in range(2)]
                tmpn = [sbuf(es, "a_tmpn%d" % i, [128, 512], F32) for i in range(2)]
                rstd = sbuf(es, "a_rstd", [128, 512], F32)
                sqs = sbuf(es, "a_sqs", [128, 384], F32)
                st = sbuf(es, "a_st", [128, 8], F32)
                cn = sbuf(es, "a_cn", [128, 384], BF16)
                cnT = sbuf(es, "a_cnT", [128, 3, 128], BF16)
                qf = sbuf(es, "a_qf", [128, 4, 96], BF16)
                kf = sbuf(es, "a_kf", [128, 4, 96], BF16)
                krr = sbuf(es, "a_krr", [128, 32], F32)
                ra = sbuf(es, "a_ra", [128, 512], F32)
                rb = sbuf(es, "a_rb", [128, 512], F32)
                qkb = sbuf(es, "a_qkb", [128, 512], BF16)
                gtmp = sbuf(es, "a_gtmp", [128, 384], F32)
                gtmp2 = sbuf(es, "a_gtmp2", [128, 384], F32)
                v1 = [sbuf(es, "a_v1_%d" % i, [128, 14, 65], BF16) for i in range(2)]
                tq = [sbuf(es, "a_tq%d" % i, [128, 4, 128], BF16) for i in range(4)]
                ssp = psum(es, "a_ssp", [128, 512], F32)
                pA = psum(es, "a_pA", [128, 512], F32)
                pB = psum(es, "a_pB", [128, 1024], F32)
                pC = psum(es, "a_pC", [128, 512], F32)
                pU1 = psum(es, "a_pU1", [128, 512], F32)
                pU2 = psum(es, "a_pU2", [128, 512], F32)
                pT = psum(es, "a_pT", [128, 8, 128], BF16)

                for k in range(8):
                    kb.dma("pool", win[:, k, :], w_in[l, k * 128:(k + 1) * 128, :], writes=[win.b])
                for k in range(2):
                    kb.dma("pool", wuq[:, k, :], w_uq[l, k * 128:(k + 1) * 128, :], writes=[wuq.b])
                kb.dma("pool", wukv[:], w_ukv[l], writes=[wukv.b])
                kb.dma("sp", gq[:], g_mlaq[l].partition_broadcast(128), writes=[gq.b])
                kb.dma("sp", gkv[:], g_mlakv[l].partition_broadcast(128), writes=[gkv.b])
                for h in range(4):
                    kb.dma("sp", ggq[:, h, :], g_gq[l].partition_broadcast(128), writes=[ggq.b])
                for h in range(2):
                    kb.dma("sp", ggq[:, 4 + h, :], g_gk[l].partition_broadcast(128), writes=[ggq.b])
                kb.dma("sp", r32[:], rope32.rearrange("(t p) c -> p t c", p=128), writes=[r32.b])
                kb.dma("sp", r64[:], rope64.rearrange("(t p) c -> p t c", p=128), writes=[r64.b])
                for i in range(2):
                    mset("pool", v1[i], v1[i][:], 1.0)

                tqi = [0]

                def next_tq():
                    tqi[0] += 1
                    return tq[tqi[0] % 4]

                def rope(src5, dst5, rt, t, G, Fq, reads, dstT):
                    tab = rt[:, t, :].rearrange("p (a b f) -> p a b f", a=2, b=2)
                    C = tab[:, 0].unsqueeze(1).to_broadcast([128, G, 2, Fq])
                    S = tab[:, 1].unsqueeze(1).to_broadcast([128, G, 2, Fq])
                    n = G * 2 * Fq
                    rav = ra[:, 0:n].rearrange("p (g b f) -> p g b f", g=G, b=2)
                    rbv = rb[:, 0:n].rearrange("p (g b f) -> p g b f", g=G, b=2)
                    t1 = src5[:, :, :, 0, :]
                    t2 = src5[:, :, :, 1, :]
                    tt("dve", ra, rav, t1, C, ALU.mult, reads + [rt])
                    tt("dve", rb, rbv, t2, S, ALU.mult, reads + [rt])
                    tt("pool", dstT, dst5[:, :, :, 0, :], rav, rbv, ALU.subtract, [ra, rb])
                    tt("dve", ra, rav, t1, S, ALU.mult, reads + [rt])
                    tt("dve", rb, rbv, t2, C, ALU.mult, reads + [rt])
                    tt("pool", dstT, dst5[:, :, :, 1, :], rav, rbv, ALU.add, [ra, rb])

                def r5(ap, G, Fq):
                    if len(ap.shape) == 2:
                        return ap.rearrange("p (g a b f) -> p g a b f", g=G, a=2, b=2)
                    return ap.rearrange("p g (a b f) -> p g a b f", a=2, b=2)

                for bi, (c0, Tn, j) in enumerate(BLOCKS):
                    xb = xb_[bi % 2]
                    hb = hb_[bi % 2]
                    kb.dma("sp", xb[:, :, :Tn], xa_view(c0, Tn), reads=tr(xa_b, c0, Tn), writes=[xb.b])
                    norm_mod(xb, hb, sq, rstd, tmpn, ssp, Tn,
                             lambda k: A1[:, k, j:j + 1], lambda k: modT[:, 0 + k, j:j + 1])
                    kb.dma("pool", hta_view(c0, Tn), hb[:, :, :Tn], reads=[hb.b], writes=tr(hta_b, c0, Tn))
                    ck("A0")
                    for ti in range(Tn // 128):
                        t = c0 // 128 + ti
                        ts_ = slice(ti * 128, (ti + 1) * 128)
                        vv = v1[t % 2]
                        for k in range(8):
                            mm(pA, pA[:, 0:416], hb[:, k, ts_], win[:, k, 0:416], [hb, win], start=(k == 0), stop=(k == 7))
                        act(sqs, sqs[:, 0:384], pA[:, 0:384], AF.Square, [pA])
                        red(st, st[:, 0:1], sqs[:, 0:256], ALU.add, [sqs])
                        red(st, st[:, 1:2], sqs[:, 256:384], ALU.add, [sqs])
                        ts("dve", st, st[:, 0:1], st[:, 0:1], 1.0 / 256, EPS, ALU.mult, ALU.add, [st])
                        ts("dve", st, st[:, 1:2], st[:, 1:2], 1.0 / 128, EPS, ALU.mult, ALU.add, [st])
                        act(st, st[:, 0:2], st[:, 0:2], AF.Sqrt, [st])
                        recip(st, st[:, 0:2], st[:, 0:2], [st])
                        stt(cn, cn[:, 0:256], pA[:, 0:256], st[:, 0:1], gq[:], ALU.mult, ALU.mult, [pA, st, gq])
                        stt(cn, cn[:, 256:384], pA[:, 256:384], st[:, 1:2], gkv[:], ALU.mult, ALU.mult, [pA, st, gkv])
                        ck("A1a")
                        for k in range(3):
                            tp(pT, pT[:, k, :], cn[:, k * 128:(k + 1) * 128], ident_b[:], [cn, ident_b])
                        cp("act", cnT, cnT[:], pT[:, 0:3, :], [pT])
                        ck("A1b")
                        for k in range(2):
                            mm(pU1, pU1[:, 0:384], cnT[:, k, :], wuq[:, k, :], [cnT, wuq], start=(k == 0), stop=(k == 1))
                        mm(pU2, pU2[:, 0:512], cnT[:, 2, :], wukv[:], [cnT, wukv])
                        u1 = pU1[:, 0:384].rearrange("p (h d) -> p h d", h=4)
                        u2 = pU2[:, 0:512].rearrange("p (h d) -> p h d", h=4)
                        ck("A1c")
                        cp("act", qf, qf[:, :, 0:64], u1[:, :, 0:64], [pU1])
                        rope(r5(u1[:, :, 64:96], 4, 8), r5(qf[:, :, 64:96], 4, 8), r32, t, 4, 8, [pU1], qf)
                        ck("A1c1")
                        cp("act", kf, kf[:, :, 0:64], u2[:, :, 0:64], [pU2])
                        ck("A1c2")
                        rope(r5(pA[:, 384:416], 1, 8), r5(krr[:, :], 1, 8), r32, t, 1, 8, [pA], krr)
                        ck("A1c3")
                        cp("pool", kf, kf[:, :, 64:96], krr[:].unsqueeze(1).to_broadcast([128, 4, 32]), [krr])
                        ck("A1c4")
                        cp("dve", vv, vv[:, 0:4, 0:64], u2[:, :, 64:128], [pU2])
                        ck("A1d")
                        for h in range(4):
                            tp(pT, pT[0:96, h, :], qf[:, h, :], ident_b[:], [qf, ident_b])
                        for h in range(4):
                            tp(pT, pT[0:96, 4 + h, :], kf[:, h, :], ident_b[:], [kf, ident_b])
                        o1 = next_tq()
                        o2 = next_tq()
                        cp("act", o1, o1[0:96, :, :], pT[0:96, 0:4, :], [pT])
                        cp("dve", o2, o2[0:96, :, :], pT[0:96, 4:8, :], [pT])
                        cols = slice(t * 128, (t + 1) * 128)
                        ck("A1e")
                        kb.dma("pool", qt_mla.rearrange("(m p) n -> p m n", p=96)[:, :, cols], o1[0:96, :, :], reads=[o1.b], writes=[qk_b[t]])
                        kb.dma("pool", kt_mla.rearrange("(m p) n -> p m n", p=96)[:, :, cols], o2[0:96, :, :], reads=[o2.b], writes=[qk_b[t]])
                        ck("A1")
                        for k in range(8):
                            mm(pB, pB[:, 0:512], hb[:, k, ts_], win[:, k, 416:928], [hb, win], start=(k == 0), stop=(k == 7))
                        for k in range(8):
                            mm(pB, pB[:, 512:768], hb[:, k, ts_], win[:, k, 928:1184], [hb, win], start=(k == 0), stop=(k == 7))
                        cp("act", qkb, qkb[:], pB[:, 0:512], [pB])
                        cp("dve", vv, vv[:, 4:8, 0:64], pB[:, 512:768].rearrange("p (h d) -> p h d", h=4), [pB])
                        for k in range(4):
                            tp(pT, pT[:, k, :], qkb[:, k * 128:(k + 1) * 128], ident_b[:], [qkb, ident_b])
                        o1 = next_tq()
                        cp("act", o1, o1[:], pT[:, 0:4, :], [pT])
                        kb.dma("pool", qt_na.rearrange("(m p) n -> p m n", p=128)[:, :, cols], o1[:, 0:2, :], reads=[o1.b], writes=[qk_b[t]])
                        kb.dma("pool", kt_na.rearrange("(m p) n -> p m n", p=128)[:, :, cols], o1[:, 2:4, :], reads=[o1.b], writes=[qk_b[t]])
                        ck("A2")
                        for k in range(8):
                            mm(pB, pB[:, 0:512], hb[:, k, ts_], win[:, k, 1184:1696], [hb, win], start=(k == 0), stop=(k == 7))
                        for k in range(8):
                            mm(pB, pB[:, 512:768], hb[:, k, ts_], win[:, k, 1696:1952], [hb, win], start=(k == 0), stop=(k == 7))
                        for half in range(2):
                            rope(r5(pB[:, half * 256:(half + 1) * 256], 8, 8), r5(qkb[:, half * 256:(half + 1) * 256], 8, 8),
                                 r32, t, 8, 8, [pB], qkb)
                        cp("dve", vv, vv[:, 8:12, 0:64], pB[:, 512:768].rearrange("p (h d) -> p h d", h=4), [pB])
                        for k in range(4):
                            tp(pT, pT[:, k, :], qkb[:, k * 128:(k + 1) * 128], ident_b[:], [qkb, ident_b])
                        o1 = next_tq()
                        cp("act", o1, o1[:], pT[:, 0:4, :], [pT])
                        kb.dma("pool", qt_df.rearrange("(m p) n -> p m n", p=128)[:, :, cols], o1[:, 0:2, :], reads=[o1.b], writes=[qk_b[t]])
                        kb.dma("pool", kt_df.rearrange("(m p) n -> p m n", p=128)[:, :, cols], o1[:, 2:4, :], reads=[o1.b], writes=[qk_b[t]])
                        ck("A3")
                        for k in range(8):
                            mm(pC, pC[:, 0:512], hb[:, k, ts_], win[:, k, 1952:2464], [hb, win], start=(k == 0), stop=(k == 7))
                        act(sqs, sqs[:, 0:384], pC[:, 0:384], AF.Square, [pC])
                        red(st, st[:, 2:8], sqs[:, 0:384].rearrange("p (h d) -> p h d", h=6), ALU.add, [sqs])
                        rstd_from_ss(st, st[:, 2:8], st[:, 2:8], 64.0, [st])
                        g3 = gtmp[:, 0:384].rearrange("p (h d) -> p h d", h=6)
                        g32 = gtmp2[:, 0:384].rearrange("p (h d) -> p h d", h=6)
                        tt("dve", gtmp, g3, pC[:, 0:384].rearrange("p (h d) -> p h d", h=6),
                           st[:, 2:8].unsqueeze(2).to_broadcast([128, 6, 64]), ALU.mult, [pC, st])
                        tt("pool", gtmp2, g32, g3, ggq[:], ALU.mult, [gtmp, ggq])
                        rope(r5(gtmp2[:, 0:384], 6, 16), r5(qkb[:, 0:384], 6, 16), r64, t, 6, 16, [gtmp2], qkb)
                        cp("dve", vv, vv[:, 12:14, 0:64], pC[:, 384:512].rearrange("p (h d) -> p h d", h=2), [pC])
                        for k in range(3):
                            tp(pT, pT[:, k, :], qkb[:, k * 128:(k + 1) * 128], ident_b[:], [qkb, ident_b])
                        o1 = next_tq()
                        cp("act", o1, o1[:, 0:3, :], pT[:, 0:3, :], [pT])
                        kb.dma("pool", qt_gq.rearrange("(m p) n -> p m n", p=128)[:, :, cols], o1[:, 0:2, :], reads=[o1.b], writes=[qk_b[t]])
                        kb.dma("pool", kt_gq.rearrange("(m p) n -> p m n", p=128)[:, :, cols], o1[:, 2:3, :], reads=[o1.b], writes=[qk_b[t]])
                        kb.dma("pool", v1a[t * 128:(t + 1) * 128, :], vv[:].rearrange("p a b -> p (a b)"), reads=[vv.b], writes=[qk_b[t]])
            kb.barrier()
            if stop == "A":
                break
            cntB = [0]

            def attn_std(mixer, qt_d, kt_d, dk, nq, nk, kmap, vbase, nv, vmap, scale, diff=False):
                with contextlib.ExitStack() as es:
                    KT = sbuf(es, "b_KT", [128, nk, NT], BF16)
                    V1 = sbuf(es, "b_V1", [128, NTILE, nv, 65], BF16)
                    QT = [sbuf(es, "b_QT%d" % i, [128, nq, 512], BF16) for i in range(2)]
                    PT = [sbuf(es, "b_PT%d" % i, [128, 512], BF16) for i in range(3)]
                    osb = sbuf(es, "b_osb", [128, 4, nq, 64], F32)
                    osbb = sbuf(es, "b_osbb", [128, 4, 256], BF16)
                    rs = sbuf(es, "b_rs", [128, 4], F32)
                    otb = [sbuf(es, "b_otb%d" % i, [128, 2, 512], BF16) for i in range(2)]
                    stp = [psum(es, "b_st%d" % i, [128, 512]) for i in range(2)]
                    ops = [psum(es, "b_o%d" % i, [128, 512]) for i in range(4)]
                    tpp = psum(es, "b_tp", [128, 8, 128], BF16)
                    for m in range(nk):
                        kb.dma("sp", KT[0:dk, m, :], kt_d[m * dk:(m + 1) * dk, :], reads=qk_b, writes=[KT.b])
                    v4 = v1a.rearrange("(t p) (a b) -> p t a b", p=128, b=65)
                    for t0 in range(0, NTILE, 9):
                        t1 = min(NTILE, t0 + 9)
                        kb.dma("sp", V1[:, t0:t1, :, :], v4[:, t0:t1, vbase:vbase + nv, :], reads=qk_b, writes=[V1.b])
                    if diff:
                        lamt = sbuf(es, "b_lamt", [128, 4, 32], F32)
                        lp = sbuf(es, "b_lp", [128, 2, 32], F32)
                        ls = sbuf(es, "b_ls", [128, 2], F32)
                        neglam = sbuf(es, "b_neglam", [128, 1], F32)
                        gsub = sbuf(es, "b_gsub", [128, 64], F32)
                        dsb = sbuf(es, "b_dsb", [128, 4, 64], F32)
                        dsq = sbuf(es, "b_dsq", [128, 4, 64], F32)
                        dst = sbuf(es, "b_dst", [128, 4], F32)
                        for i in range(4):
                            kb.dma("sp", lamt[:, i, :], lamv[l, i].partition_broadcast(128), writes=[lamt.b])
                        kb.dma("sp", gsub[:], g_subln[l].partition_broadcast(128), writes=[gsub.b])
                        tt("dve", lp, lp[:, 0, :], lamt[:, 0, :], lamt[:, 1, :], ALU.mult, [lamt])
                        tt("dve", lp, lp[:, 1, :], lamt[:, 2, :], lamt[:, 3, :], ALU.mult, [lamt])
                        red(ls, ls[:, 0:2], lp[:], ALU.add, [lp])
                        act(ls, ls[:], ls[:], AF.Exp, [ls])
                        tt("dve", neglam, neglam[:, 0:1], ls[:, 1:2], ls[:, 0:1], ALU.subtract, [ls])
                        ts("dve", neglam, neglam[:], neglam[:], -lam_init, None, ALU.add, None, [neglam])
                        ts("dve", gsub, gsub[:], gsub[:], 1.0 - lam_init, None, ALU.mult, None, [gsub])
                    for bi, (c0, Tn, j) in enumerate(BLOCKS):
                        QTb = QT[bi % 2]
                        kb.dma("sp", QTb[0:dk, :, :Tn], qt_d.rearrange("(m p) n -> p m n", p=dk)[:, :, c0:c0 + Tn],
                               reads=qk_b, writes=[QTb.b])
                        kchunks = list(range(NTILE)) if j == 0 else [32, 33]
                        nqs = Tn // 128
                        for m in range(nq):
                            pend = None
                            for ci, kc in enumerate(kchunks):
                                sp_ = stp[cntB[0] % 2]
                                pt_ = PT[cntB[0] % 3]
                                cntB[0] += 1
                                mm(sp_, sp_[:, :Tn], KT[0:dk, kmap(m), kc * 128:(kc + 1) * 128], QTb[0:dk, m, :Tn], [KT, QTb])
                                if pend is not None:
                                    pend()
                                act(pt_, pt_[:, :Tn], sp_[:, :Tn], AF.Exp, [sp_], scale=scale)

                                def mk(ci=ci, kc=kc, pt_=pt_, m=m):
                                    def f():
                                        for qs in range(nqs):
                                            mm(ops[qs], ops[qs][:, 0:65], pt_[:, qs * 128:(qs + 1) * 128], V1[:, kc, vmap(m), :], [pt_, V1],
                                               start=(ci == 0), stop=(ci == len(kchunks) - 1))
                                    return f
                                pend = mk()
                            pend()
                            for qs in range(nqs):
                                recip(rs, rs[:, qs:qs + 1], ops[qs][:, 64:65], [ops[qs]])
                                if diff:
                                    ts("dve", osb, osb[:, qs, m, :], ops[qs][:, 0:64], rs[:, qs:qs + 1], None, ALU.mult, None, [ops[qs], rs])
                                else:
                                    ts("dve", osbb, osbb[:, qs, m * 64:(m + 1) * 64], ops[qs][:, 0:64], rs[:, qs:qs + 1], None, ALU.mult, None,
                                       [ops[qs], rs])
                        if diff:
                            for qs in range(nqs):
                                ov = osb[:, qs].rearrange("p (h i) d -> p h i d", i=2)
                                stt(dsb, dsb[:], ov[:, :, 1, :], neglam[:, 0:1], ov[:, :, 0, :], ALU.mult, ALU.add, [osb, neglam])
                                tt("pool", dsq, dsq[:], dsb[:], dsb[:], ALU.mult, [dsb])
                                red(dst, dst[:, 0:4], dsq[:], ALU.add, [dsq])
                                rstd_from_ss(dst, dst[:, 0:4], dst[:, 0:4], 64.0, [dst])
                                tt("dve", dsb, dsb[:], dsb[:], dst[:, 0:4].unsqueeze(2).to_broadcast([128, 4, 64]), ALU.mult, [dsb, dst])
                                tt("pool", osbb, osbb[:, qs, :].rearrange("p (h d) -> p h d", h=4), dsb[:],
                                   gsub[:].unsqueeze(1).to_broadcast([128, 4, 64]), ALU.mult, [dsb, gsub])
                        ot_ = otb[bi % 2]
                        for qs in range(nqs):
                            for c in range(2):
                                tp(tpp, tpp[:, qs * 2 + c, :], osbb[:, qs, c * 128:(c + 1) * 128], ident_b[:], [osbb, ident_b])
                        cp("act", ot_, ot_[:, :, :Tn].rearrange("p c (q n) -> p q c n", n=128),
                           tpp[:, 0:2 * nqs, :].rearrange("p (q c) n -> p q c n", c=2), [tpp])
                        kb.dma("pool", ota_view(c0, Tn)[:, 2 * mixer:2 * mixer + 2, :], ot_[:, :, :Tn], reads=[ot_.b],
                               writes=tr(ota_b, c0, Tn))
                kb.barrier()

            def attn_na():
                scale = 64.0 ** -0.5
                with contextlib.ExitStack() as es:
                    KT = sbuf(es, "n_KT", [128, 4, NT], BF16)
                    QT = sbuf(es, "n_QT", [128, 4, NT], BF16)
                    V1 = sbuf(es, "n_V1", [128, NTILE, 4, 65], BF16)
                    nb = [sbuf(es, "n_nb%d" % i, [128, 4, 5, 128], F32) for i in range(5)]
                    PT = [sbuf(es, "n_PT%d" % i, [128, 128], BF16) for i in range(3)]
                    tmpb = [sbuf(es, "n_tmp%d" % i, [128, 128], F32) for i in range(2)]
                    osbb = sbuf(es, "n_osbb", [128, 256], BF16)
                    rs = sbuf(es, "n_rs", [128, 1], F32)
                    otb = [sbuf(es, "n_otb%d" % i, [128, 2, 128], BF16) for i in range(2)]
                    stp = [psum(es, "n_st%d" % i, [128, 512]) for i in range(2)]
                    ops = [psum(es, "n_o%d" % i, [128, 512]) for i in range(2)]
                    tpp = psum(es, "n_tp", [128, 8, 128], BF16)
                    for m in range(4):
                        kb.dma("sp", KT[0:64, m, :], kt_na[m * 64:(m + 1) * 64, :], reads=qk_b, writes=[KT.b])
                        kb.dma("sp", QT[0:64, m, :], qt_na[m * 64:(m + 1) * 64, :], reads=qk_b, writes=[QT.b])
                    v4 = v1a.rearrange("(t p) (a b) -> p t a b", p=128, b=65)
                    for t0 in range(0, NTILE, 9):
                        t1 = min(NTILE, t0 + 9)
                        kb.dma("sp", V1[:, t0:t1, :, :], v4[:, t0:t1, 4:8, :], reads=qk_b, writes=[V1.b])
                    for cs in range(5):
                        kb.dma("sp", nb[cs][:].rearrange("p a b c -> p (a b c)"), nab[l, cs].rearrange("p a b c -> p (a b c)"),
                               writes=[nb[cs].b])
                    cnt = 0
                    for m in range(NTILE):
                        if m < 32:
                            case = {0: 0, 1: 1, 30: 3, 31: 4}.get(m, 2)
                            k0 = min(max(m - 2, 0), 27)
                            chunks = [(k0 + i, i) for i in range(5)] + [(32, None), (33, None)]
                        else:
                            chunks = [(32, None), (33, None)]
                        for h in range(4):
                            o_ = ops[(m * 4 + h) % 2]
                            pend = None
                            for ci, (kc, li) in enumerate(chunks):
                                sp_ = stp[cnt % 2]
                                pt_ = PT[cnt % 3]
                                tb_ = tmpb[cnt % 2]
                                cnt += 1
                                mm(sp_, sp_[:, 0:128], KT[0:64, h, kc * 128:(kc + 1) * 128], QT[0:64, h, m * 128:(m + 1) * 128], [KT, QT])
                                if pend is not None:
                                    pend()
                                if li is not None:
                                    stt(tb_, tb_[:], sp_[:, 0:128], scale, nb[case][:, h, li, :], ALU.mult, ALU.add, [sp_, nb[case]])
                                    act(pt_, pt_[:], tb_[:], AF.Exp, [tb_])
                                else:
                                    act(pt_, pt_[:], sp_[:, 0:128], AF.Exp, [sp_], scale=scale)

                                def mkn(ci=ci, kc=kc, pt_=pt_, h=h, o_=o_, nch=len(chunks)):
                                    def f():
                                        mm(o_, o_[:, 0:65], pt_[:], V1[:, kc, h, :], [pt_, V1], start=(ci == 0), stop=(ci == nch - 1))
                                    return f
                                pend = mkn()
                            pend()
                            recip(rs, rs[:, 0:1], o_[:, 64:65], [o_])
                            ts("dve", osbb, osbb[:, h * 64:(h + 1) * 64], o_[:, 0:64], rs[:, 0:1], None, ALU.mult, None, [o_, rs])
                        ot_ = otb[m % 2]
                        for c in range(2):
                            tp(tpp, tpp[:, c, :], osbb[:, c * 128:(c + 1) * 128], ident_b[:], [osbb, ident_b])
                        cp("act", ot_, ot_[:], tpp[:, 0:2, :], [tpp])
                        kb.dma("pool", ota_view(m * 128, 128)[:, 2:4, :], ot_[:], reads=[ot_.b], writes=[ota_b[m]])
                kb.barrier()

            attn_std(0, qt_mla, kt_mla, 96, 4, 4, lambda m: m, 0, 4, lambda m: m, 96.0 ** -0.5)
            ck("B0")
            attn_na()
            ck("B1")
            attn_std(2, qt_df, kt_df, 32, 8, 8, lambda m: m, 8, 4, lambda m: m // 2, 32.0 ** -0.5, diff=True)
            ck("B2")
            attn_std(3, qt_gq, kt_gq, 64, 4, 2, lambda m: m // 2, 12, 2, lambda m: m // 2, 64.0 ** -0.5)
            if stop == "B":
                break
            with contextlib.ExitStack() as es:
                wg = sbuf(es, "c_wg", [128, 4, 8, D], BF16)
                wbr = sbuf(es, "c_wbr", [128, 4, 2, D], BF16)
                wo = sbuf(es, "c_wo", [128, 8, D], BF16)
                bg = sbuf(es, "c_bg", [128, 4, 8], F32)
                hb = sbuf(es, "c_hb", [128, 8, 512], BF16)
                ob = sbuf(es, "c_ob", [128, 8, 512], BF16)
                xb = sbuf(es, "c_xb", [128, 8, 512], F32)
                sig = [sbuf(es, "c_sig%d" % i, [128, 512], BF16) for i in range(2)]
                macc = [sbuf(es, "c_macc%d" % i, [128, 512], F32) for i in range(2)]
                tmpm = [sbuf(es, "c_tmpm%d" % i, [128, 512], F32) for i in range(2)]
                mrg = sbuf(es, "c_mrg", [128, 8, 512], BF16)
                pg = [psum(es, "c_pg%d" % i, [128, 512]) for i in range(2)]
                pb = [psum(es, "c_pb%d" % i, [128, 512]) for i in range(2)]
                py = [psum(es, "c_py%d" % i, [128, 512]) for i in range(2)]
                for i in range(4):
                    for k in range(8):
                        kb.dma("pool", wg[:, i, k, :], w_gate[l, i, k * 128:(k + 1) * 128, :], writes=[wg.b])
                    for k in range(2):
                        kb.dma("pool", wbr[:, i, k, :], w_branch[l, i, k * 128:(k + 1) * 128, :], writes=[wbr.b])
                for k in range(8):
                    kb.dma("pool", wo[:, k, :], w_out[l, k * 128:(k + 1) * 128, :], writes=[wo.b])
                kb.dma("sp", bg[:], b_gateT[l], writes=[bg.b])
                cntC = 0
                for bi, (c0, Tn, j) in enumerate(BLOCKS):
                    kb.dma("sp", hb[:, :, :Tn], hta_view(c0, Tn), reads=tr(hta_b, c0, Tn), writes=[hb.b])
                    kb.dma("sp", ob[:, :, :Tn], ota_view(c0, Tn), reads=tr(ota_b, c0, Tn), writes=[ob.b])
                    kb.dma("sp", xb[:, :, :Tn], xa_view(c0, Tn), reads=tr(xa_b, c0, Tn), writes=[xb.b])
                    for oc in range(8):
                        ocs = slice(oc * 128, (oc + 1) * 128)
                        ma = macc[oc % 2]
                        for i in range(4):
                            g_ = pg[cntC % 2]
                            b_ = pb[cntC % 2]
                            s_ = sig[cntC % 2]
                            t_ = tmpm[cntC % 2]
                            cntC += 1
                            for k in range(8):
                                mm(g_, g_[:, :Tn], wg[:, i, k, ocs], hb[:, k, :Tn], [wg, hb], start=(k == 0), stop=(k == 7))
                            act(s_, s_[:, :Tn], g_[:, :Tn], AF.Sigmoid, [g_, bg], bias=bg[:, i, oc:oc + 1])
                            for k in range(2):
                                mm(b_, b_[:, :Tn], wbr[:, i, k, ocs], ob[:, 2 * i + k, :Tn], [wbr, ob], start=(k == 0), stop=(k == 1))
                            if i == 0:
                                tt("dve", ma, ma[:, :Tn], b_[:, :Tn], s_[:, :Tn], ALU.mult, [b_, s_])
                            else:
                                tt("dve", t_, t_[:, :Tn], b_[:, :Tn], s_[:, :Tn], ALU.mult, [b_, s_])
                                if i < 3:
                                    tt("pool", ma, ma[:, :Tn], ma[:, :Tn], t_[:, :Tn], ALU.add, [ma, t_])
                                else:
                                    tt("pool", mrg, mrg[:, oc, :Tn], ma[:, :Tn], t_[:, :Tn], ALU.add, [ma, t_])
                    for oc in range(8):
                        ocs = slice(oc * 128, (oc + 1) * 128)
                        y_ = py[oc % 2]
                        for k in range(8):
                            mm(y_, y_[:, :Tn], wo[:, k, ocs], mrg[:, k, :Tn], [wo, mrg], start=(k == 0), stop=(k == 7))
                        stt(xb, xb[:, oc, :Tn], y_[:, :Tn], modT[:, 16 + oc, j:j + 1], xb[:, oc, :Tn], ALU.mult, ALU.add, [y_, modT, xb])
                    kb.dma("pool", xa_view(c0, Tn), xb[:, :, :Tn], reads=[xb.b], writes=tr(xa_b, c0, Tn))
            kb.barrier()
            if stop == "C":
                break

            with contextlib.ExitStack() as es:
                wq = sbuf(es, "d_wq", [128, 8, 2048], BF16)
                sk = sbuf(es, "d_sk", [128, 16, 128], F32)
                xb = sbuf(es, "d_xb", [128, 8, 256], F32)
                hb = sbuf(es, "d_hb", [128, 8, 256], BF16)
                sqk = [sbuf(es, "d_sq%d" % i, [128, 256], F32) for i in range(2)]
                tmpk = [sbuf(es, "d_tk%d" % i, [128, 256], F32) for i in range(2)]
                rstd = sbuf(es, "d_rstd", [128, 256], F32)
                qpc = [sbuf(es, "d_qpc%d" % i, [128, 256], F32) for i in range(2)]
                s_sb = sbuf(es, "d_s", [128, 2, 16, 128], F32)
                top16 = sbuf(es, "d_top", [128, 16, 16], F32)
                mr = sbuf(es, "d_mr", [128, 256], F32)
                best = sbuf(es, "d_best", [128, 8, 16], F32)
                eb = sbuf(es, "d_eb", [128, 8, 16], F32)
                sm = sbuf(es, "d_sm", [128, 6, 8], F32)
                tau = sbuf(es, "d_tau", [128, 2, 8], F32)
                nbias = sbuf(es, "d_nbias", [128, 2, 8], F32)
                cf = [sbuf(es, "d_cf%d" % i, [128, 16, 128], F32) for i in range(4)]
                cand = T(cf[0].t[:].rearrange("p (h a) (b c) -> p h a (b c)", h=8, c=16).rearrange("p h a (b c) -> p h (a b) c", c=16), "cand_alias")
                cand.b = cf[0].b
                pr = [sbuf(es, "d_pr%d" % i, [128, 16, 128], BF16) for i in range(4)]
                tw = [sbuf(es, "d_tw%d" % i, [128, 16, 128], BF16) for i in range(2)]
                Ws = [sbuf(es, "d_Ws%d" % i, [128, 2, 16, 128], BF16) for i in range(2)]
                uch = [sbuf(es, "d_uch%d" % i, [128, 8, 512], BF16) for i in range(2)]
                vch = [sbuf(es, "d_vch%d" % i, [128, 4, D], BF16) for i in range(2)]
                wts = [sbuf(es, "d_wts%d" % i, [128, 4, 256], BF16) for i in range(2)]
                gel = [sbuf(es, "d_gel%d" % i, [128, 256], BF16) for i in range(2)]
                cT = [sbuf(es, "d_cT%d" % i, [128, 256], BF16) for i in range(4)]
                yps = [psum(es, "d_y%d" % i, [128, 512]) for i in range(4)]
                pm1 = psum(es, "d_pm1", [128, 512])
                apsl = [psum(es, "d_a%d" % i, [128, 512]) for i in range(2)]
                wtp = psum(es, "d_wt", [128, 4, 256], BF16)
                for k in range(8):
                    kb.dma("pool", wq[:, k, :], w_pq[l, k * 128:(k + 1) * 128, :], writes=[wq.b])
                kb.dma("sp", sk[:].rearrange("p a b -> p (a b)"), skT[l].rearrange("p a b -> p (a b)"), writes=[sk.b])
                if l + 1 < NL:
                    convert_uv(l + 1)
                ub = ub2[l % 2]
                vb = vb2[l % 2]
                ub_b = ub_b2[l % 2]
                vb_b = vb_b2[l % 2]
                ubv = ub.rearrange("(k p) e -> p k e", p=128)
                cDl = [0]
                cUl = [0]
                for bi, (c0, Tn, j) in enumerate(PBLOCKS):
                    kb.dma("sp", xb[:], xa_view(c0, 256), reads=tr(xa_b, c0, 256), writes=[xb.b])
                    norm_mod(xb, hb, sqk, rstd, tmpk, pm1, 256,
                             lambda k: A2[:, k, j:j + 1], lambda k: modT[:, 24 + k, j:j + 1])
                    for c in range(16):
                        q_ = qpc[c % 2]
                        for k in range(8):
                            mm(pm1, pm1[:, 0:256], wq[:, k, c * 128:(c + 1) * 128], hb[:, k, :], [wq, hb], start=(k == 0), stop=(k == 7))
                        cp("act", q_, q_[:], pm1[:, 0:256], [pm1])
                        for tl in range(2):
                            col = 256 + tl * 128
                            mm(pm1, pm1[:, col:col + 128], q_[:, tl * 128:(tl + 1) * 128], sk[:, c, :], [q_, sk])
                        cp("dve", s_sb, s_sb[:, :, c, :], pm1[:, 256:512].rearrange("p (t n) -> p t n", t=2), [pm1])
                    for tl in range(2):
                        for c in range(16):
                            kb.op("dve", lambda: nc.vector.max(out=top16[:, c, 0:8], in_=s_sb[:, tl, c, :]), reads=[s_sb.b], writes=[top16.b])
                            kb.op("dve", lambda: nc.vector.match_replace(out=mr[:, 0:128], in_to_replace=top16[:, c, 0:8],
                                                                          in_values=s_sb[:, tl, c, :], imm_value=-1e30),
                                  reads=[s_sb.b, top16.b], writes=[mr.b])
                            kb.op("dve", lambda: nc.vector.max(out=top16[:, c, 8:16], in_=mr[:, 0:128]), reads=[mr.b], writes=[top16.b])
                        t4 = top16[:].rearrange("p (h a) k -> p h a k", a=2)
                        tt("dve", cand, cand[:], t4[:, :, 0, :].unsqueeze(3).to_broadcast([128, 8, 16, 16]),
                           t4[:, :, 1, :].unsqueeze(2).to_broadcast([128, 8, 16, 16]), ALU.add, [top16])
                        for h in range(8):
                            ch = cand[:, h].rearrange("p a b -> p (a b)")
                            kb.op("dve", lambda: nc.vector.max(out=best[:, h, 0:8], in_=ch), reads=[cand.b], writes=[best.b])
                            kb.op("dve", lambda: nc.vector.match_replace(out=mr[:, 0:256], in_to_replace=best[:, h, 0:8], in_values=ch,
                                                                          imm_value=-1e30),
                                  reads=[cand.b, best.b], writes=[mr.b])
                            kb.op("dve", lambda: nc.vector.max(out=best[:, h, 8:16], in_=mr[:, 0:256]), reads=[mr.b], writes=[best.b])
                        ts("dve", sm, sm[:, 0, :], best[:, :, 0], -1.0, None, ALU.mult, None, [best])
                        cp("dve", tau, tau[:, tl, :], best[:, :, 15], [best])
                        for h in range(8):
                            act(eb, eb[:, h, :], best[:, h, :], AF.Exp, [best, sm], bias=sm[:, 0, h:h + 1])
                        red(sm, sm[:, 1, :], eb[:], ALU.add, [eb])
                        act(sm, sm[:, 1, :], sm[:, 1, :], AF.Ln, [sm])
                        ts("dve", sm, sm[:, 2, :], t4[:, :, 0, 0], -1.0, None, ALU.mult, None, [top16])
                        stt(sm, sm[:, 3, :], t4[:, :, 1, 0], -1.0, sm[:, 1, :], ALU.mult, ALU.subtract, [top16, sm])
                        tt("dve", nbias, nbias[:, tl, :], sm[:, 0, :], sm[:, 1, :], ALU.subtract, [sm])
                    units = [(ig, tl, h) for ig in range(8) for tl in range(2) for h in range(8)]

                    def cand_of(n):
                        ig, tl, h = units[n]
                        isl = slice(ig * 16, (ig + 1) * 16)
                        c_ = cf[n % 4]
                        tt("pool", c_, c_[:], s_sb[:, tl, 2 * h, isl].unsqueeze(2).to_broadcast([128, 16, 128]),
                           s_sb[:, tl, 2 * h + 1, :].unsqueeze(1).to_broadcast([128, 16, 128]), ALU.add, [s_sb])

                    def exp_of(n):
                        ig, tl, h = units[n]
                        act(pr[n % 4], pr[n % 4][:], cf[n % 4][:], AF.Exp, [cf[n % 4], nbias], bias=nbias[:, tl, h:h + 1])

                    def acc_of(n):
                        ig, tl, h = units[n]
                        W_ = Ws[ig % 2]
                        c_ = cf[n % 4]
                        p_ = pr[n % 4]
                        w_ = tw[n % 2]
                        if h == 0:
                            stt(W_, W_[:, tl], c_[:], tau[:, tl, h:h + 1], p_[:], ALU.is_ge, ALU.mult, [c_, p_, tau])
                        else:
                            stt(w_, w_[:], c_[:], tau[:, tl, h:h + 1], p_[:], ALU.is_ge, ALU.mult, [c_, p_, tau])
                            tt("dve", W_, W_[:, tl], W_[:, tl], w_[:], ALU.add, [W_, w_])

                    cand_of(0)
                    cand_of(1)
                    pend = None
                    grp = None
                    for s_ in range(64 + 8):
                        if s_ + 1 < 64:
                            cand_of(2 * s_ + 2)
                            cand_of(2 * s_ + 3)
                        if s_ < 64:
                            exp_of(2 * s_)
                            exp_of(2 * s_ + 1)
                            acc_of(2 * s_)
                            acc_of(2 * s_ + 1)
                        if s_ >= 8:
                            cpair = s_ - 8
                            i_a = 2 * cpair
                            ig = i_a // 16
                            il = i_a % 16
                            W_ = Ws[ig % 2]
                            if i_a % 4 == 0:
                                u_ = uch[cUl[0] % 2]
                                v_ = vch[cUl[0] % 2]
                                ws_ = wts[cUl[0] % 2]
                                cUl[0] += 1
                                kb.dma("sp", u_[:], ubv[:, :, i_a * 128:(i_a + 4) * 128], reads=[ub_b], writes=[u_.b])
                                kb.dma("sp", v_[:], vb[i_a * 128:(i_a + 4) * 128, :].rearrange("(a p) d -> p a d", p=128), reads=[vb_b],
                                       writes=[v_.b])
                                for k4 in range(4):
                                    for tl in range(2):
                                        tp(wtp, wtp[:, k4, tl * 128:(tl + 1) * 128], W_[:, tl, il + k4, :], ident_b[:], [W_, ident_b])
                                cp("act", ws_, ws_[:], wtp[:], [wtp])
                                grp = (u_, v_, ws_)
                            u_, v_, ws_ = grp
                            for i in (i_a, i_a + 1):
                                ii = i % 4
                                a_ = apsl[i % 2]
                                for k in range(8):
                                    mm(a_, a_[:, 0:256], u_[:, k, ii * 128:(ii + 1) * 128], hb[:, k, :], [u_, hb], start=(k == 0), stop=(k == 7))
                            if pend is not None:
                                pend()
                            for i in (i_a, i_a + 1):
                                act(gel[i % 2], gel[i % 2][:], apsl[i % 2][:, 0:256], AF.Gelu, [apsl[i % 2]])
                            for i in (i_a, i_a + 1):
                                tt("dve", cT[i % 4], cT[i % 4][:], gel[i % 2][:], ws_[:, i % 4, :], ALU.mult, [gel[i % 2], ws_])

                            def mkv(i_a=i_a, v_=v_):
                                def f():
                                    for i in (i_a, i_a + 1):
                                        c2 = cT[i % 4]
                                        for oc in range(8):
                                            y_ = yps[oc // 2]
                                            mm(y_, y_[:, (oc % 2) * 256:(oc % 2) * 256 + 256], v_[:, i % 4, oc * 128:(oc + 1) * 128], c2[:],
                                               [v_, c2], start=(i == 0 and oc % 2 == 0), stop=(i == 127))
                                return f
                            pend = mkv()
                    pend()
                    for oc in range(8):
                        y_ = yps[oc // 2]
                        stt(xb, xb[:, oc, :], y_[:, (oc % 2) * 256:(oc % 2) * 256 + 256], modT[:, 40 + oc, j:j + 1], xb[:, oc, :],
                            ALU.mult, ALU.add, [y_, modT, xb])
                    kb.dma("sp", xa_view(c0, 256), xb[:], reads=[xb.b], writes=tr(xa_b, c0, 256))
                    ck("D0")
            kb.barrier()
            if stop == "D":
                break
          except StopBuild:
            break

        if stop is None:
            with contextlib.ExitStack() as es:
                fn = sbuf(es, "f_fn", [128, 8], F32)
                xb_ = [sbuf(es, "f_xb%d" % i, [128, 8, 512], F32) for i in range(2)]
                sqk = [sbuf(es, "f_sq%d" % i, [128, 512], F32) for i in range(2)]
                xn = sbuf(es, "f_xn", [128, 8, 512], F32)
                rstd = sbuf(es, "f_rstd", [128, 512], F32)
                yo = [sbuf(es, "f_yo%d" % i, [128, D], F32) for i in range(2)]
                ssp = psum(es, "f_ssp", [128, 512])
                pp = [psum(es, "f_pp%d" % i, [128, 1024]) for i in range(2)]
                kb.dma("sp", fn[:], fnormT[:, :], writes=[fn.b])
                for bi in range(8):
                    c0 = bi * 512
                    xb = xb_[bi % 2]
                    kb.dma("sp", xb[:], xa_view(c0, 512), reads=tr(xa_b, c0, 512), writes=[xb.b])
                    for k in range(8):
                        q_ = sqk[k % 2]
                        act(q_, q_[:], xb[:, k, :], AF.Square, [xb])
                        mm(ssp, ssp[:], ones_f[:], q_[:], [ones_f, q_], start=(k == 0), stop=(k == 7))
                    rstd_from_ss(rstd, rstd[:], ssp[:], float(D), [ssp])
                    for k in range(8):
                        stt(xn, xn[:, k, :], xb[:, k, :], fn[:, k:k + 1], rstd[:], ALU.mult, ALU.mult, [xb, fn, rstd])
                    for ti in range(4):
                        t = bi * 4 + ti
                        p = pp[t % 2]
                        o = yo[t % 2]
                        for k in range(8):
                            tp(p, p[:, k * 128:(k + 1) * 128], xn[:, k, ti * 128:(ti + 1) * 128], ident_f[:], [xn, ident_f])
                        cp("act" if t % 2 else "dve", o, o[:], p[:], [p])
                        kb.dma("sp", out[t * 128:(t + 1) * 128, :], o[:], reads=[o.b])

        kb.dead = False
        kb.barrier()
    return nc


def rope_tables():
    pos = np.arange(NLAT)
    rows = (pos // 64).astype(np.float32)
    cols = (pos % 64).astype(np.float32)

    def tab(dh):
        inv = np.power(np.float32(10000.0), -np.arange(0, dh, 2, dtype=np.float32) / np.float32(dh)).astype(np.float32)
        out = np.zeros((NT, 2, 2, dh // 2), np.float32)
        out[:, 0] = 1.0
        for a, p_ in enumerate((rows, cols)):
            ang = (p_[:, None] * inv[None, :]).astype(np.float32)
            out[:NLAT, 0, a] = np.cos(ang)
            out[:NLAT, 1, a] = np.sin(ang)
        return out.reshape(NT, -1)

    return tab(16), tab(32)


def na_bias_tables(rpb):
    Lr = rpb.shape[0]
    out = np.full((Lr, 5, 128, 4, 5, 128), NEG, np.float32)
    for case, m in enumerate((0, 1, 2, 30, 31)):
        k0 = min(max(m - 2, 0), 27)
        q = m * 128 + np.arange(128)
        qr, qc = q // 64, q % 64
        rs = np.clip(qr - 4, 0, 56)
        cs = np.clip(qc - 8, 0, 48)
        for i in range(5):
            key = (k0 + i) * 128 + np.arange(128)
            kr, kc_ = key // 64, key % 64
            inw = ((kr[:, None] >= rs[None, :]) & (kr[:, None] < rs[None, :] + 8)
                   & (kc_[:, None] >= cs[None, :]) & (kc_[:, None] < cs[None, :] + 16))
            ro = np.clip(kr[:, None] - qr[None, :] + 7, 0, 14)
            co = np.clip(kc_[:, None] - qc[None, :] + 15, 0, 30)
            for h in range(4):
                g = rpb[:, h][:, ro, co]
                out[:, case, :, h, i, :] = np.where(inw[None], g, np.float32(NEG))
    return out


def prep_inputs(inp):
    f = lambda a: np.ascontiguousarray(np.asarray(a, dtype=np.float32))
    L = DEPTH
    r32, r64 = rope_tables()
    shared = {
        "w_mod": f(inp["w_mod"]),
        "b_modT": f(inp["b_mod"].reshape(L, 48, 128).transpose(0, 2, 1)),
        "nmixT": f(inp["norm_mix"].reshape(L, 8, 128).transpose(0, 2, 1)),
        "nffnT": f(inp["norm_ffn"].reshape(L, 8, 128).transpose(0, 2, 1)),
        "fnormT": f(inp["final_norm"].reshape(8, 128).T),
        "w_in": f(inp["w_in"]),
        "g_mlaq": f(inp["mla_q_norm"]),
        "g_mlakv": f(inp["mla_kv_norm"]),
        "w_uq": f(inp["mla_w_uq"]),
        "w_ukv": f(inp["mla_w_ukv"]),
        "nab": f(na_bias_tables(np.asarray(inp["na_rpb"], np.float32))),
        "lamv": f(np.stack([inp["diff_lam_q1"], inp["diff_lam_k1"], inp["diff_lam_q2"], inp["diff_lam_k2"]], axis=1)),
        "g_subln": f(inp["diff_subln"]),
        "g_gq": f(inp["gqa_q_norm"]),
        "g_gk": f(inp["gqa_k_norm"]),
        "w_branch": f(inp["w_branch"]),
        "w_gate": f(inp["w_gate"]),
        "b_gateT": f(inp["b_gate"].reshape(L, 4, 8, 128).transpose(0, 3, 1, 2)),
        "w_out": f(inp["w_out"]),
        "w_pq": f(inp["peer_w_q"]),
        "skT": f(inp["peer_subkeys"].reshape(L, 16, 128, 128).transpose(0, 3, 1, 2)),
        "uT": f(np.asarray(inp["peer_u"]).transpose(0, 2, 1)),
        "pv": f(inp["peer_v"]),
        "rope32": f(r32),
        "rope64": f(r64),
    }
    maps = []
    for b in range(8):
        m = dict(shared)
        m["xin"] = f(np.concatenate([inp["x"][b], inp["ctx"][b]], axis=0))
        cc = np.stack([inp["c"][b], inp["c_ctx"]], axis=0).reshape(2, 8, 128).transpose(2, 1, 0).reshape(128, 16)
        m["cc"] = f(cc)
        maps.append(m)
    return maps


def kernel(**inputs):
    maps = prep_inputs(inputs)
    nc = build()
    res = run_bass_kernel_spmd(nc, maps, core_ids=list(range(8)))
    return np.stack([np.asarray(r["out"], dtype=np.float32) for r in res.results], axis=0)
```

```python
import math
import contextlib
import numpy as np
import concourse.bass as bass
import concourse.mybir as mybir
from concourse.bass_utils import run_bass_kernel_spmd

F32 = mybir.dt.float32
BF16 = mybir.dt.bfloat16
AF = mybir.ActivationFunctionType
ALU = mybir.AluOpType
AX = mybir.AxisListType

D = 1024
KC = 8
NLAT = 4096
NCTX = 256
NT = NLAT + NCTX
NTILE = NT // 128
DEPTH = 4
EPS = 1e-6
NEG = -30000.0
BLOCKS = [(i * 512, 512, 0) for i in range(8)] + [(4096, 256, 1)]
PBLOCKS = [(i * 256, 256, 0) for i in range(16)] + [(4096, 256, 1)]


class Ev:
    __slots__ = ("key", "val", "clk")

    def __init__(s, key, val, clk):
        s.key = key
        s.val = val
        s.clk = clk


class TB:
    __slots__ = ("name", "w", "r", "excl")

    def __init__(s, name="", excl=False):
        s.name = name
        s.w = None
        s.r = {}
        s.excl = excl


class KB:
    NS = {"sp": 24, "pool": 40, "act": 2}

    def __init__(s, nc, es):
        s.nc = nc
        s.eng = {"pe": nc.tensor, "act": nc.scalar, "dve": nc.vector, "pool": nc.gpsimd, "sp": nc.sync}
        s.semobj = {}
        s.ccnt = {}
        for e in ["pe", "act", "dve", "pool"]:
            s.semobj[e] = es.enter_context(nc.semaphore("cs_" + e))
            s.ccnt[e] = 0
        s.known = {e: {} for e in s.eng}
        s.dcnt = {}
        for q, n in s.NS.items():
            s.dcnt[q] = 0
            for j in range(n):
                s.semobj[(q, j)] = es.enter_context(nc.semaphore("ds_%s%d" % (q, j)))
        s.last_dma = {}
        s.ninstr = 0
        s.dead = False

    def _wait(s, e, deps):
        k = s.known[e]
        changed = False
        for ev in deps:
            if ev is None:
                continue
            if k.get(ev.key, 0) >= ev.val:
                continue
            s.eng[e].wait_ge(s.semobj[ev.key], ev.val)
            if not changed:
                k = dict(k)
                changed = True
            for kk, vv in ev.clk.items():
                if k.get(kk, 0) < vv:
                    k[kk] = vv
            k[ev.key] = ev.val
        if changed:
            s.known[e] = k

    def _deps(s, reads, writes):
        deps = []
        for b in reads:
            if b.w is not None:
                deps.append(b.w)
        for b in writes:
            if b.w is not None:
                deps.append(b.w)
            deps.extend(b.r.values())
        return deps

    def _upd(s, ev, reads, writes):
        for b in reads:
            o = b.r.get(ev.key)
            if o is None or o.val < ev.val:
                b.r[ev.key] = ev
        for b in writes:
            b.w = ev
            b.r = {}

    def op(s, e, fn, reads=(), writes=()):
        if s.dead:
            return None
        ex = [b for b in reads if b.excl]
        if ex:
            reads = [b for b in reads if not b.excl]
            writes = list(writes) + ex
        deps = s._deps(reads, writes)
        if e == "pe":
            deps = [d for d in deps if d.key != "pe"]
        s._wait(e, deps)
        ins = fn()
        s.ccnt[e] += 1
        ins.then_inc(s.semobj[e], 1)
        ev = Ev(e, s.ccnt[e], s.known[e])
        s._upd(ev, reads, writes)
        s.ninstr += 1
        return ev

    def dma(s, q, out, in_, reads=(), writes=(), in_barrier=True):
        if s.dead:
            return None
        i = s.dcnt[q]
        n = s.NS[q]
        j = i % n
        key = (q, j)
        prev = 16 * (i // n)
        val = prev + 16
        deps = s._deps(reads, writes)
        if prev > 0:
            deps.append(Ev(key, prev, {}))
        s._wait(q, deps)
        ins = s.eng[q].dma_start(out=out, in_=in_)
        ins.then_inc(s.semobj[key], 16)
        s.dcnt[q] += 1
        ev = Ev(key, val, s.known[q])
        if in_barrier:
            s.last_dma[key] = ev
        elif key in s.last_dma:
            del s.last_dma[key]
        s._upd(ev, reads, writes)
        s.ninstr += 1
        return ev

    def barrier(s, engines=("pe", "act", "dve", "pool", "sp")):
        if s.dead:
            return
        evs = [Ev(e, s.ccnt[e], {}) for e in ["pe", "act", "dve", "pool"] if s.ccnt[e] > 0]
        evs += list(s.last_dma.values())
        for e in engines:
            s._wait(e, evs)


class StopBuild(Exception):
    pass


class T:
    def __init__(s, t, name, excl=False):
        s.t = t
        s.b = TB(name, excl)

    def __getitem__(s, k):
        return s.t[k]


def build(NL=DEPTH, stop=None, dbg=()):
    nc = bass.Bass("TRN2", target_bir_lowering=False)

    def din(name, shape, dt=F32):
        return nc.dram_tensor(name, list(shape), dt, kind="ExternalInput").ap()

    def dscr(name, shape, dt):
        kind = "ExternalOutput" if name in dbg else "Internal"
        return nc.dram_tensor(name, list(shape), dt, kind=kind).ap()

    L = DEPTH
    xin = din("xin", [NT, D])
    cc = din("cc", [128, 16])
    w_mod = din("w_mod", [L, D, 6 * D])
    b_modT = din("b_modT", [L, 128, 48])
    nmixT = din("nmixT", [L, 128, 8])
    nffnT = din("nffnT", [L, 128, 8])
    fnormT = din("fnormT", [128, 8])
    w_in = din("w_in", [L, D, 2464])
    g_mlaq = din("g_mlaq", [L, 256])
    g_mlakv = din("g_mlakv", [L, 128])
    w_uq = din("w_uq", [L, 256, 384])
    w_ukv = din("w_ukv", [L, 128, 512])
    nab = din("nab", [L, 5, 128, 4, 5, 128])
    lamv = din("lamv", [L, 4, 32])
    g_subln = din("g_subln", [L, 64])
    g_gq = din("g_gq", [L, 64])
    g_gk = din("g_gk", [L, 64])
    w_branch = din("w_branch", [L, 4, 256, D])
    w_gate = din("w_gate", [L, 4, D, D])
    b_gateT = din("b_gateT", [L, 128, 4, 8])
    w_out = din("w_out", [L, D, D])
    w_pq = din("w_pq", [L, D, 2048])
    skT = din("skT", [L, 128, 16, 128])
    uT = din("uT", [L, D, 16384])
    pv = din("pv", [L, 16384, D])
    rope32 = din("rope32", [NT, 32])
    rope64 = din("rope64", [NT, 64])
    out = nc.dram_tensor("out", [NLAT, D], F32, kind="ExternalOutput").ap()

    xa = dscr("xa", [D, NT], F32)
    hta = dscr("hta", [D, NT], BF16)
    ota = dscr("ota", [D, NT], BF16)
    qt_mla = dscr("qt_mla", [4 * 96, NT], BF16)
    kt_mla = dscr("kt_mla", [4 * 96, NT], BF16)
    qt_na = dscr("qt_na", [256, NT], BF16)
    kt_na = dscr("kt_na", [256, NT], BF16)
    qt_df = dscr("qt_df", [256, NT], BF16)
    kt_df = dscr("kt_df", [256, NT], BF16)
    qt_gq = dscr("qt_gq", [256, NT], BF16)
    kt_gq = dscr("kt_gq", [128, NT], BF16)
    v1a = dscr("v1a", [NT, 14 * 65], BF16)
    ub2 = [dscr("ub%d" % i, [D, 16384], BF16) for i in range(2)]
    vb2 = [dscr("vb%d" % i, [16384, D], BF16) for i in range(2)]

    def tiles_tb(name):
        return [TB("%s%d" % (name, i)) for i in range(NTILE)]

    xa_b = tiles_tb("xa")
    hta_b = tiles_tb("hta")
    ota_b = tiles_tb("ota")
    qk_b = tiles_tb("qk")
    ub_b2 = [TB("ub0"), TB("ub1")]
    vb_b2 = [TB("vb0"), TB("vb1")]

    def tr(bl, start, n):
        return bl[start // 128:(start + n) // 128]

    es_top = contextlib.ExitStack()
    with es_top:
        kb = KB(nc, es_top)

        uid = [0]

        def sbuf(es, name, shape, dt):
            uid[0] += 1
            name = "%s_%d" % (name, uid[0])
            return T(es.enter_context(nc.sbuf_tensor(name, list(shape), dt)), name)

        def psum(es, name, shape, dt=F32):
            uid[0] += 1
            name = "%s_%d" % (name, uid[0])
            return T(es.enter_context(nc.psum_tensor(name, list(shape), dt)), name, True)

        def mm(o, oap, lhsT, rhs, reads, start=True, stop=True):
            kb.op("pe", lambda: nc.tensor.matmul(oap, lhsT=lhsT, rhs=rhs, start=start, stop=stop),
                  reads=[r.b for r in reads], writes=[o.b])

        def tp(o, oap, iap, ident, reads):
            kb.op("pe", lambda: nc.tensor.transpose(oap, iap, ident), reads=[r.b for r in reads], writes=[o.b])

        def act(o, oap, iap, func, reads, bias=None, scale=None):
            kw = {}
            if bias is not None:
                kw["bias"] = bias
            if scale is not None:
                kw["scale"] = scale
            kb.op("act", lambda: nc.scalar.activation(out=oap, in_=iap, func=func, **kw),
                  reads=[r.b for r in reads], writes=[o.b])

        def tt(e, o, oap, a, b, op, reads):
            en = nc.vector if e == "dve" else nc.gpsimd
            kb.op(e, lambda: en.tensor_tensor(out=oap, in0=a, in1=b, op=op), reads=[r.b for r in reads], writes=[o.b])

        def ts(e, o, oap, a, s1, s2, op0, op1, reads):
            en = nc.vector if e == "dve" else nc.gpsimd
            if op1 is None:
                kb.op(e, lambda: en.tensor_scalar(out=oap, in0=a, scalar1=s1, scalar2=None, op0=op0),
                      reads=[r.b for r in reads], writes=[o.b])
            else:
                kb.op(e, lambda: en.tensor_scalar(out=oap, in0=a, scalar1=s1, scalar2=s2, op0=op0, op1=op1),
                      reads=[r.b for r in reads], writes=[o.b])

        def stt(o, oap, a, sc, b, op0, op1, reads):
            kb.op("dve", lambda: nc.vector.scalar_tensor_tensor(out=oap, in0=a, scalar=sc, in1=b, op0=op0, op1=op1),
                  reads=[r.b for r in reads], writes=[o.b])

        def cp(e, o, oap, iap, reads):
            if e == "act":
                act(o, oap, iap, AF.Copy, reads)
            else:
                en = nc.vector if e == "dve" else nc.gpsimd
                kb.op(e, lambda: en.tensor_copy(out=oap, in_=iap), reads=[r.b for r in reads], writes=[o.b])

        def red(o, oap, iap, op, reads):
            kb.op("dve", lambda: nc.vector.tensor_reduce(out=oap, in_=iap, axis=AX.X, op=op),
                  reads=[r.b for r in reads], writes=[o.b])

        def recip(o, oap, iap, reads):
            kb.op("dve", lambda: nc.vector.reciprocal(out=oap, in_=iap), reads=[r.b for r in reads], writes=[o.b])

        def mset(e, o, oap, val):
            en = nc.vector if e == "dve" else nc.gpsimd
            kb.op(e, lambda: en.memset(oap, val), writes=[o.b])

        def ck(name):
            if stop == name:
                kb.dead = True

        def rstd_from_ss(o, oap, ssap, n, reads):
            ts("dve", o, oap, ssap, 1.0 / n, EPS, ALU.mult, ALU.add, reads)
            act(o, oap, oap, AF.Sqrt, [o])
            recip(o, oap, oap, [o])

        ident_f = sbuf(es_top, "ident_f", [128, 128], F32)
        ident_b = sbuf(es_top, "ident_b", [128, 128], BF16)
        ones_f = sbuf(es_top, "ones_f", [128, 128], F32)
        modT = sbuf(es_top, "modT", [128, 48, 2], F32)
        A1 = sbuf(es_top, "A1", [128, 8, 2], F32)
        A2 = sbuf(es_top, "A2", [128, 8, 2], F32)
        mset("pool", ident_f, ident_f[:], 1.0)
        kb.op("pool", lambda: nc.gpsimd.affine_select(out=ident_f[:], in_=ident_f[:], pattern=[[-1, 128]],
                                                      compare_op=ALU.is_equal, fill=0.0, base=0,
                                                      channel_multiplier=1),
              reads=[ident_f.b], writes=[ident_f.b])
        cp("dve", ident_b, ident_b[:], ident_f[:], [ident_f])
        mset("pool", ones_f, ones_f[:], 1.0)

        def convert_uv(lc):
            sset = lc % 2
            for k in range(8):
                for hf in range(2):
                    kb.dma("pool", ub2[sset][k * 128:(k + 1) * 128, hf * 8192:(hf + 1) * 8192],
                           uT[lc, k * 128:(k + 1) * 128, hf * 8192:(hf + 1) * 8192], writes=[ub_b2[sset]], in_barrier=False)
            src = pv[lc].rearrange("(g p r) d -> g p (r d)", p=128, r=8)
            dst = vb2[sset].rearrange("(g p r) d -> g p (r d)", p=128, r=8)
            for g in range(16):
                kb.dma("pool", dst[g], src[g], writes=[vb_b2[sset]], in_barrier=False)

        def xa_view(c0, n):
            return xa.rearrange("(k p) n -> p k n", p=128)[:, :, c0:c0 + n]

        def hta_view(c0, n):
            return hta.rearrange("(k p) n -> p k n", p=128)[:, :, c0:c0 + n]

        def ota_view(c0, n):
            return ota.rearrange("(k p) n -> p k n", p=128)[:, :, c0:c0 + n]

        if NL > 0:
            convert_uv(0)
        with contextlib.ExitStack() as es:
            xt = [sbuf(es, "i_xt%d" % i, [128, D], F32) for i in range(2)]
            xo = [sbuf(es, "i_xo%d" % i, [128, 8, 128], F32) for i in range(2)]
            pp = [psum(es, "i_pp%d" % i, [128, 1024], F32) for i in range(2)]
            for t in range(NTILE):
                a = xt[t % 2]
                o = xo[t % 2]
                p = pp[t % 2]
                kb.dma("sp", a[:], xin[t * 128:(t + 1) * 128, :], writes=[a.b])
                for k in range(8):
                    tp(p, p[:, k * 128:(k + 1) * 128], a[:, k * 128:(k + 1) * 128], ident_f[:], [a, ident_f])
                cp("act" if t % 2 else "dve", o, o[:].rearrange("p k n -> p (k n)"), p[:], [p])
                kb.dma("pool", xa_view(t * 128, 128), o[:], reads=[o.b], writes=[xa_b[t]])
        kb.barrier()
        if stop == "I":
            NL = 0

        def norm_mod(xb, hb, sqk, rstd, tmpk, ssp, Tn, Acol, Bcol):
            for k in range(8):
                q_ = sqk[k % 2]
                act(q_, q_[:, :Tn], xb[:, k, :Tn], AF.Square, [xb])
                mm(ssp, ssp[:, :Tn], ones_f[:], q_[:, :Tn], [ones_f, q_], start=(k == 0), stop=(k == 7))
            rstd_from_ss(rstd, rstd[:, :Tn], ssp[:, :Tn], float(D), [ssp])
            for k in range(8):
                t_ = tmpk[k % 2]
                tt("dve", t_, t_[:, :Tn], xb[:, k, :Tn], rstd[:, :Tn], ALU.mult, [xb, rstd])
                act(hb, hb[:, k, :Tn], t_[:, :Tn], AF.Identity, [t_, modT, A1, A2], bias=Bcol(k), scale=Acol(k))

        for l in range(NL):
          try:
            lam_init = 0.8 - 0.6 * math.exp(-0.3 * l)
            with contextlib.ExitStack() as es:
                cct = sbuf(es, "m_cc", [128, 16], F32)
                sc = sbuf(es, "m_sc", [128, 16], F32)
                wm = [sbuf(es, "m_w%d" % i, [128, 8, 768], F32) for i in range(2)]
                bm = sbuf(es, "m_b", [128, 48], F32)
                nm = sbuf(es, "m_nm", [128, 8], F32)
                nf = sbuf(es, "m_nf", [128, 8], F32)
                pm = psum(es, "m_p", [128, 96], F32)
                kb.dma("sp", cct[:], cc[:, :], writes=[cct.b])
                kb.dma("sp", bm[:], b_modT[l], writes=[bm.b])
                kb.dma("sp", nm[:], nmixT[l], writes=[nm.b])
                kb.dma("sp", nf[:], nffnT[l], writes=[nf.b])
                act(sc, sc[:], cct[:], AF.Silu, [cct])
                for blk in range(8):
                    w = wm[blk % 2]
                    for k in range(8):
                        kb.dma("sp", w[:, k, :], w_mod[l, k * 128:(k + 1) * 128, blk * 768:(blk + 1) * 768], writes=[w.b])
                    for cl in range(6):
                        c = blk * 6 + cl
                        for k in range(8):
                            mm(pm, pm[:, c * 2:c * 2 + 2], w[:, k, cl * 128:(cl + 1) * 128], sc[:, k * 2:k * 2 + 2], [w, sc],
                               start=(k == 0), stop=(k == 7))
                tt("dve", modT, modT[:], pm[:].rearrange("p (c j) -> p c j", j=2),
                   bm[:].unsqueeze(2).to_broadcast([128, 48, 2]), ALU.add, [pm, bm])
                stt(A1, A1[:], modT[:, 8:16, :], 1.0, nm[:].unsqueeze(2).to_broadcast([128, 8, 2]), ALU.add, ALU.mult, [modT, nm])
                stt(A2, A2[:], modT[:, 32:40, :], 1.0, nf[:].unsqueeze(2).to_broadcast([128, 8, 2]), ALU.add, ALU.mult, [modT, nf])
            kb.barrier()
            if stop == "mod":
                break

            with contextlib.ExitStack() as es:
                win = sbuf(es, "a_win", [128, 8, 2464], BF16)
                wuq = sbuf(es, "a_wuq", [128, 2, 384], BF16)
                wukv = sbuf(es, "a_wukv", [128, 512], BF16)
                gq = sbuf(es, "a_gq", [128, 256], F32)
                gkv = sbuf(es, "a_gkv", [128, 128], F32)
                ggq = sbuf(es, "a_ggq", [128, 6, 64], F32)
                r32 = sbuf(es, "a_r32", [128, NTILE, 32], F32)
                r64 = sbuf(es, "a_r64", [128, NTILE, 64], F32)
                xb_ = [sbuf(es, "a_xb%d" % i, [128, 8, 512], F32) for i in range(2)]
                hb_ = [sbuf(es, "a_hb%d" % i, [128, 8, 512], BF16) for i in range(2)]
                sq = [sbuf(es, "a_sq%d" % i, [128, 512], F32) for i in range(2)]
                tmpn = [sbuf(es, "a_tmpn%d" % i, [128, 512], F32) for i in range(2)]
                rstd = sbuf(es, "a_rstd", [128, 512], F32)
                sqs_2 = [sbuf(es, "a_sqs%d" % i_, [128, 384], F32) for i_ in range(2)]
                st_2 = [sbuf(es, "a_st%d" % i_, [128, 8], F32) for i_ in range(2)]
                cn_2 = [sbuf(es, "a_cn%d" % i_, [128, 384], BF16) for i_ in range(2)]
                cnT_2 = [sbuf(es, "a_cnT%d" % i_, [128, 3, 128], BF16) for i_ in range(2)]
                qf_2 = [sbuf(es, "a_qf%d" % i_, [128, 4, 96], BF16) for i_ in range(2)]
                kf_2 = [sbuf(es, "a_kf%d" % i_, [128, 4, 96], BF16) for i_ in range(2)]
                krr_2 = [sbuf(es, "a_krr%d" % i_, [128, 32], F32) for i_ in range(2)]
                ra_2 = [sbuf(es, "a_ra%d" % i_, [128, 512], F32) for i_ in range(2)]
                rb_2 = [sbuf(es, "a_rb%d" % i_, [128, 512], F32) for i_ in range(2)]
                qkb_2 = [sbuf(es, "a_qkb%d" % i_, [128, 512], BF16) for i_ in range(2)]
                gtmp_2 = [sbuf(es, "a_gtmp%d" % i_, [128, 384], F32) for i_ in range(2)]
                gtmp2_2 = [sbuf(es, "a_gtmp2%d" % i_, [128, 384], F32) for i_ in range(2)]
                v1 = [sbuf(es, "a_v1_%d" % i, [128, 14, 65], BF16) for i in range(2)]
                tq = [sbuf(es, "a_tq%d" % i, [128, 4, 128], BF16) for i in range(4)]
                ssp = psum(es, "a_ssp", [128, 512], F32)
                pA = psum(es, "a_pA", [128, 512], F32)
                pB = psum(es, "a_pB", [128, 1024], F32)
                pC = psum(es, "a_pC", [128, 512], F32)
                pU1 = psum(es, "a_pU1", [128, 512], F32)
                pU2 = psum(es, "a_pU2", [128, 512], F32)
                pT = psum(es, "a_pT", [128, 8, 128], BF16)

                for k in range(8):
                    kb.dma("pool", win[:, k, :], w_in[l, k * 128:(k + 1) * 128, :], writes=[win.b])
                for k in range(2):
                    kb.dma("pool", wuq[:, k, :], w_uq[l, k * 128:(k + 1) * 128, :], writes=[wuq.b])
                kb.dma("pool", wukv[:], w_ukv[l], writes=[wukv.b])
                kb.dma("sp", gq[:], g_mlaq[l].partition_broadcast(128), writes=[gq.b])
                kb.dma("sp", gkv[:], g_mlakv[l].partition_broadcast(128), writes=[gkv.b])
                for h in range(4):
                    kb.dma("sp", ggq[:, h, :], g_gq[l].partition_broadcast(128), writes=[ggq.b])
                for h in range(2):
                    kb.dma("sp", ggq[:, 4 + h, :], g_gk[l].partition_broadcast(128), writes=[ggq.b])
                kb.dma("sp", r32[:], rope32.rearrange("(t p) c -> p t c", p=128), writes=[r32.b])
                kb.dma("sp", r64[:], rope64.rearrange("(t p) c -> p t c", p=128), writes=[r64.b])
                for i in range(2):
                    mset("pool", v1[i], v1[i][:], 1.0)

                tqi = [0]

                def next_tq():
                    tqi[0] += 1
                    return tq[tqi[0] % 4]

                def rope(src5, dst5, rt, t, G, Fq, reads, dstT):
                    ra = ra_2[t % 2]
                    rb = rb_2[t % 2]
                    tab = rt[:, t, :].rearrange("p (a b f) -> p a b f", a=2, b=2)
                    C = tab[:, 0].unsqueeze(1).to_broadcast([128, G, 2, Fq])
                    S = tab[:, 1].unsqueeze(1).to_broadcast([128, G, 2, Fq])
                    n = G * 2 * Fq
                    rav = ra[:, 0:n].rearrange("p (g b f) -> p g b f", g=G, b=2)
                    rbv = rb[:, 0:n].rearrange("p (g b f) -> p g b f", g=G, b=2)
                    t1 = src5[:, :, :, 0, :]
                    t2 = src5[:, :, :, 1, :]
                    tt("dve", ra, rav, t1, C, ALU.mult, reads + [rt])
                    tt("dve", rb, rbv, t2, S, ALU.mult, reads + [rt])
                    tt("pool", dstT, dst5[:, :, :, 0, :], rav, rbv, ALU.subtract, [ra, rb])
                    tt("dve", ra, rav, t1, S, ALU.mult, reads + [rt])
                    tt("dve", rb, rbv, t2, C, ALU.mult, reads + [rt])
                    tt("pool", dstT, dst5[:, :, :, 1, :], rav, rbv, ALU.add, [ra, rb])

                def r5(ap, G, Fq):
                    if len(ap.shape) == 2:
                        return ap.rearrange("p (g a b f) -> p g a b f", g=G, a=2, b=2)
                    return ap.rearrange("p g (a b f) -> p g a b f", a=2, b=2)

                for bi, (c0, Tn, j) in enumerate(BLOCKS):
                    xb = xb_[bi % 2]
                    hb = hb_[bi % 2]
                    kb.dma("sp", xb[:, :, :Tn], xa_view(c0, Tn), reads=tr(xa_b, c0, Tn), writes=[xb.b])
                    norm_mod(xb, hb, sq, rstd, tmpn, ssp, Tn,
                             lambda k: A1[:, k, j:j + 1], lambda k: modT[:, 0 + k, j:j + 1])
                    kb.dma("pool", hta_view(c0, Tn), hb[:, :, :Tn], reads=[hb.b], writes=tr(hta_b, c0, Tn))
                    ck("A0")
                    for ti in range(Tn // 128):
                        t = c0 // 128 + ti
                        ts_ = slice(ti * 128, (ti + 1) * 128)
                        vv = v1[t % 2]
                        sqs, st, cn, cnT, qf, kf, krr, qkb, gtmp, gtmp2 = [x_[t % 2] for x_ in
                                                                          (sqs_2, st_2, cn_2, cnT_2, qf_2, kf_2, krr_2, qkb_2, gtmp_2, gtmp2_2)]
                        for k in range(8):
                            mm(pA, pA[:, 0:416], hb[:, k, ts_], win[:, k, 0:416], [hb, win], start=(k == 0), stop=(k == 7))
                        act(sqs, sqs[:, 0:384], pA[:, 0:384], AF.Square, [pA])
                        red(st, st[:, 0:1], sqs[:, 0:256], ALU.add, [sqs])
                        red(st, st[:, 1:2], sqs[:, 256:384], ALU.add, [sqs])
                        ts("dve", st, st[:, 0:1], st[:, 0:1], 1.0 / 256, EPS, ALU.mult, ALU.add, [st])
                        ts("dve", st, st[:, 1:2], st[:, 1:2], 1.0 / 128, EPS, ALU.mult, ALU.add, [st])
                        act(st, st[:, 0:2], st[:, 0:2], AF.Sqrt, [st])
                        recip(st, st[:, 0:2], st[:, 0:2], [st])
                        stt(cn, cn[:, 0:256], pA[:, 0:256], st[:, 0:1], gq[:], ALU.mult, ALU.mult, [pA, st, gq])
                        stt(cn, cn[:, 256:384], pA[:, 256:384], st[:, 1:2], gkv[:], ALU.mult, ALU.mult, [pA, st, gkv])
                        ck("A1a")
                        for k in range(3):
                            tp(pT, pT[:, k, :], cn[:, k * 128:(k + 1) * 128], ident_b[:], [cn, ident_b])
                        cp("act", cnT, cnT[:], pT[:, 0:3, :], [pT])
                        ck("A1b")
                        for k in range(2):
                            mm(pU1, pU1[:, 0:384], cnT[:, k, :], wuq[:, k, :], [cnT, wuq], start=(k == 0), stop=(k == 1))
                        mm(pU2, pU2[:, 0:512], cnT[:, 2, :], wukv[:], [cnT, wukv])
                        u1 = pU1[:, 0:384].rearrange("p (h d) -> p h d", h=4)
                        u2 = pU2[:, 0:512].rearrange("p (h d) -> p h d", h=4)
                        ck("A1c")
                        cp("act", qf, qf[:, :, 0:64], u1[:, :, 0:64], [pU1])
                        rope(r5(u1[:, :, 64:96], 4, 8), r5(qf[:, :, 64:96], 4, 8), r32, t, 4, 8, [pU1], qf)
                        ck("A1c1")
                        cp("act", kf, kf[:, :, 0:64], u2[:, :, 0:64], [pU2])
                        ck("A1c2")
                        rope(r5(pA[:, 384:416], 1, 8), r5(krr[:, :], 1, 8), r32, t, 1, 8, [pA], krr)
                        ck("A1c3")
                        cp("pool", kf, kf[:, :, 64:96], krr[:].unsqueeze(1).to_broadcast([128, 4, 32]), [krr])
                        ck("A1c4")
                        cp("dve", vv, vv[:, 0:4, 0:64], u2[:, :, 64:128], [pU2])
                        ck("A1d")
                        for h in range(4):
                            tp(pT, pT[0:96, h, :], qf[:, h, :], ident_b[:], [qf, ident_b])
                        for h in range(4):
                            tp(pT, pT[0:96, 4 + h, :], kf[:, h, :], ident_b[:], [kf, ident_b])
                        o1 = next_tq()
                        o2 = next_tq()
                        cp("act", o1, o1[0:96, :, :], pT[0:96, 0:4, :], [pT])
                        cp("dve", o2, o2[0:96, :, :], pT[0:96, 4:8, :], [pT])
                        cols = slice(t * 128, (t + 1) * 128)
                        ck("A1e")
                        kb.dma("pool", qt_mla.rearrange("(m p) n -> p m n", p=96)[:, :, cols], o1[0:96, :, :], reads=[o1.b], writes=[qk_b[t]])
                        kb.dma("pool", kt_mla.rearrange("(m p) n -> p m n", p=96)[:, :, cols], o2[0:96, :, :], reads=[o2.b], writes=[qk_b[t]])
                        ck("A1")
                        for k in range(8):
                            mm(pB, pB[:, 0:512], hb[:, k, ts_], win[:, k, 416:928], [hb, win], start=(k == 0), stop=(k == 7))
                        for k in range(8):
                            mm(pB, pB[:, 512:768], hb[:, k, ts_], win[:, k, 928:1184], [hb, win], start=(k == 0), stop=(k == 7))
                        cp("act", qkb, qkb[:], pB[:, 0:512], [pB])
                        cp("dve", vv, vv[:, 4:8, 0:64], pB[:, 512:768].rearrange("p (h d) -> p h d", h=4), [pB])
                        for k in range(4):
                            tp(pT, pT[:, k, :], qkb[:, k * 128:(k + 1) * 128], ident_b[:], [qkb, ident_b])
                        o1 = next_tq()
                        cp("act", o1, o1[:], pT[:, 0:4, :], [pT])
                        kb.dma("pool", qt_na.rearrange("(m p) n -> p m n", p=128)[:, :, cols], o1[:, 0:2, :], reads=[o1.b], writes=[qk_b[t]])
                        kb.dma("pool", kt_na.rearrange("(m p) n -> p m n", p=128)[:, :, cols], o1[:, 2:4, :], reads=[o1.b], writes=[qk_b[t]])
                        ck("A2")
                        for k in range(8):
                            mm(pB, pB[:, 0:512], hb[:, k, ts_], win[:, k, 1184:1696], [hb, win], start=(k == 0), stop=(k == 7))
                        for k in range(8):
                            mm(pB, pB[:, 512:768], hb[:, k, ts_], win[:, k, 1696:1952], [hb, win], start=(k == 0), stop=(k == 7))
                        for half in range(2):
                            rope(r5(pB[:, half * 256:(half + 1) * 256], 8, 8), r5(qkb[:, half * 256:(half + 1) * 256], 8, 8),
                                 r32, t, 8, 8, [pB], qkb)
                        cp("dve", vv, vv[:, 8:12, 0:64], pB[:, 512:768].rearrange("p (h d) -> p h d", h=4), [pB])
                        for k in range(4):
                            tp(pT, pT[:, k, :], qkb[:, k * 128:(k + 1) * 128], ident_b[:], [qkb, ident_b])
                        o1 = next_tq()
                        cp("act", o1, o1[:], pT[:, 0:4, :], [pT])
                        kb.dma("pool", qt_df.rearrange("(m p) n -> p m n", p=128)[:, :, cols], o1[:, 0:2, :], reads=[o1.b], writes=[qk_b[t]])
                        kb.dma("pool", kt_df.rearrange("(m p) n -> p m n", p=128)[:, :, cols], o1[:, 2:4, :], reads=[o1.b], writes=[qk_b[t]])
                        ck("A3")
                        for k in range(8):
                            mm(pC, pC[:, 0:512], hb[:, k, ts_], win[:, k, 1952:2464], [hb, win], start=(k == 0), stop=(k == 7))
                        act(sqs, sqs[:, 0:384], pC[:, 0:384], AF.Square, [pC])
                        red(st, st[:, 2:8], sqs[:, 0:384].rearrange("p (h d) -> p h d", h=6), ALU.add, [sqs])
                        rstd_from_ss(st, st[:, 2:8], st[:, 2:8], 64.0, [st])
                        g3 = gtmp[:, 0:384].rearrange("p (h d) -> p h d", h=6)
                        g32 = gtmp2[:, 0:384].rearrange("p (h d) -> p h d", h=6)
                        tt("dve", gtmp, g3, pC[:, 0:384].rearrange("p (h d) -> p h d", h=6),
                           st[:, 2:8].unsqueeze(2).to_broadcast([128, 6, 64]), ALU.mult, [pC, st])
                        tt("pool", gtmp2, g32, g3, ggq[:], ALU.mult, [gtmp, ggq])
                        rope(r5(gtmp2[:, 0:384], 6, 16), r5(qkb[:, 0:384], 6, 16), r64, t, 6, 16, [gtmp2], qkb)
                        cp("dve", vv, vv[:, 12:14, 0:64], pC[:, 384:512].rearrange("p (h d) -> p h d", h=2), [pC])
                        for k in range(3):
                            tp(pT, pT[:, k, :], qkb[:, k * 128:(k + 1) * 128], ident_b[:], [qkb, ident_b])
                        o1 = next_tq()
                        cp("act", o1, o1[:, 0:3, :], pT[:, 0:3, :], [pT])
                        kb.dma("pool", qt_gq.rearrange("(m p) n -> p m n", p=128)[:, :, cols], o1[:, 0:2, :], reads=[o1.b], writes=[qk_b[t]])
                        kb.dma("pool", kt_gq.rearrange("(m p) n -> p m n", p=128)[:, :, cols], o1[:, 2:3, :], reads=[o1.b], writes=[qk_b[t]])
                        kb.dma("pool", v1a[t * 128:(t + 1) * 128, :], vv[:].rearrange("p a b -> p (a b)"), reads=[vv.b], writes=[qk_b[t]])
            kb.barrier()
            if stop == "A":
                break
            cntB = [0]

            def attn_std(mixer, qt_d, kt_d, dk, nq, nk, kmap, vbase, nv, vmap, scale, diff=False):
                with contextlib.ExitStack() as es:
                    KT = sbuf(es, "b_KT", [128, nk, NT], BF16)
                    V1 = sbuf(es, "b_V1", [128, NTILE, nv, 65], BF16)
                    QT = [sbuf(es, "b_QT%d" % i, [128, nq, 512], BF16) for i in range(2)]
                    PT = [sbuf(es, "b_PT%d" % i, [128, 512], BF16) for i in range(3)]
                    osb = sbuf(es, "b_osb", [128, 4, nq, 64], F32)
                    osbb = sbuf(es, "b_osbb", [128, 4, 256], BF16)
                    rs = sbuf(es, "b_rs", [128, 4], F32)
                    otb = [sbuf(es, "b_otb%d" % i, [128, 2, 512], BF16) for i in range(2)]
                    stp = [psum(es, "b_st%d" % i, [128, 512]) for i in range(2)]
                    ops = [psum(es, "b_o%d" % i, [128, 512]) for i in range(4)]
                    tpp = psum(es, "b_tp", [128, 8, 128], BF16)
                    for m in range(nk):
                        kb.dma("sp", KT[0:dk, m, :], kt_d[m * dk:(m + 1) * dk, :], reads=qk_b, writes=[KT.b])
                    v4 = v1a.rearrange("(t p) (a b) -> p t a b", p=128, b=65)
                    for t0 in range(0, NTILE, 9):
                        t1 = min(NTILE, t0 + 9)
                        kb.dma("sp", V1[:, t0:t1, :, :], v4[:, t0:t1, vbase:vbase + nv, :], reads=qk_b, writes=[V1.b])
                    if diff:
                        lamt = sbuf(es, "b_lamt", [128, 4, 32], F32)
                        lp = sbuf(es, "b_lp", [128, 2, 32], F32)
                        ls = sbuf(es, "b_ls", [128, 2], F32)
                        neglam = sbuf(es, "b_neglam", [128, 1], F32)
                        gsub = sbuf(es, "b_gsub", [128, 64], F32)
                        dsb = sbuf(es, "b_dsb", [128, 4, 64], F32)
                        dsq = sbuf(es, "b_dsq", [128, 4, 64], F32)
                        dst = sbuf(es, "b_dst", [128, 4], F32)
                        for i in range(4):
                            kb.dma("sp", lamt[:, i, :], lamv[l, i].partition_broadcast(128), writes=[lamt.b])
                        kb.dma("sp", gsub[:], g_subln[l].partition_broadcast(128), writes=[gsub.b])
                        tt("dve", lp, lp[:, 0, :], lamt[:, 0, :], lamt[:, 1, :], ALU.mult, [lamt])
                        tt("dve", lp, lp[:, 1, :], lamt[:, 2, :], lamt[:, 3, :], ALU.mult, [lamt])
                        red(ls, ls[:, 0:2], lp[:], ALU.add, [lp])
                        act(ls, ls[:], ls[:], AF.Exp, [ls])
                        tt("dve", neglam, neglam[:, 0:1], ls[:, 1:2], ls[:, 0:1], ALU.subtract, [ls])
                        ts("dve", neglam, neglam[:], neglam[:], -lam_init, None, ALU.add, None, [neglam])
                        ts("dve", gsub, gsub[:], gsub[:], 1.0 - lam_init, None, ALU.mult, None, [gsub])
                    for bi, (c0, Tn, j) in enumerate(BLOCKS):
                        QTb = QT[bi % 2]
                        kb.dma("sp", QTb[0:dk, :, :Tn], qt_d.rearrange("(m p) n -> p m n", p=dk)[:, :, c0:c0 + Tn],
                               reads=qk_b, writes=[QTb.b])
                        kchunks = list(range(NTILE)) if j == 0 else [32, 33]
                        nqs = Tn // 128
                        for m in range(nq):
                            pend = None
                            for ci, kc in enumerate(kchunks):
                                sp_ = stp[cntB[0] % 2]
                                pt_ = PT[cntB[0] % 3]
                                cntB[0] += 1
                                mm(sp_, sp_[:, :Tn], KT[0:dk, kmap(m), kc * 128:(kc + 1) * 128], QTb[0:dk, m, :Tn], [KT, QTb])
                                if pend is not None:
                                    pend()
                                act(pt_, pt_[:, :Tn], sp_[:, :Tn], AF.Exp, [sp_], scale=scale)

                                def mk(ci=ci, kc=kc, pt_=pt_, m=m):
                                    def f():
                                        for qs in range(nqs):
                                            mm(ops[qs], ops[qs][:, 0:65], pt_[:, qs * 128:(qs + 1) * 128], V1[:, kc, vmap(m), :], [pt_, V1],
                                               start=(ci == 0), stop=(ci == len(kchunks) - 1))
                                    return f
                                pend = mk()
                            pend()
                            for qs in range(nqs):
                                recip(rs, rs[:, qs:qs + 1], ops[qs][:, 64:65], [ops[qs]])
                                if diff:
                                    ts("dve", osb, osb[:, qs, m, :], ops[qs][:, 0:64], rs[:, qs:qs + 1], None, ALU.mult, None, [ops[qs], rs])
                                else:
                                    ts("dve", osbb, osbb[:, qs, m * 64:(m + 1) * 64], ops[qs][:, 0:64], rs[:, qs:qs + 1], None, ALU.mult, None,
                                       [ops[qs], rs])
                        if diff:
                            for qs in range(nqs):
                                ov = osb[:, qs].rearrange("p (h i) d -> p h i d", i=2)
                                stt(dsb, dsb[:], ov[:, :, 1, :], neglam[:, 0:1], ov[:, :, 0, :], ALU.mult, ALU.add, [osb, neglam])
                                tt("pool", dsq, dsq[:], dsb[:], dsb[:], ALU.mult, [dsb])
                                red(dst, dst[:, 0:4], dsq[:], ALU.add, [dsq])
                                rstd_from_ss(dst, dst[:, 0:4], dst[:, 0:4], 64.0, [dst])
                                tt("dve", dsb, dsb[:], dsb[:], dst[:, 0:4].unsqueeze(2).to_broadcast([128, 4, 64]), ALU.mult, [dsb, dst])
                                tt("pool", osbb, osbb[:, qs, :].rearrange("p (h d) -> p h d", h=4), dsb[:],
                                   gsub[:].unsqueeze(1).to_broadcast([128, 4, 64]), ALU.mult, [dsb, gsub])
                        ot_ = otb[bi % 2]
                        for qs in range(nqs):
                            for c in range(2):
                                tp(tpp, tpp[:, qs * 2 + c, :], osbb[:, qs, c * 128:(c + 1) * 128], ident_b[:], [osbb, ident_b])
                        cp("act", ot_, ot_[:, :, :Tn].rearrange("p c (q n) -> p q c n", n=128),
                           tpp[:, 0:2 * nqs, :].rearrange("p (q c) n -> p q c n", c=2), [tpp])
                        kb.dma("pool", ota_view(c0, Tn)[:, 2 * mixer:2 * mixer + 2, :], ot_[:, :, :Tn], reads=[ot_.b],
                               writes=tr(ota_b, c0, Tn))
                kb.barrier()

            def attn_na():
                scale = 64.0 ** -0.5
                with contextlib.ExitStack() as es:
                    KT = sbuf(es, "n_KT", [128, 4, NT], BF16)
                    QT = sbuf(es, "n_QT", [128, 4, NT], BF16)
                    V1 = sbuf(es, "n_V1", [128, NTILE, 4, 65], BF16)
                    nb = [sbuf(es, "n_nb%d" % i, [128, 4, 5, 128], F32) for i in range(5)]
                    PT = [sbuf(es, "n_PT%d" % i, [128, 128], BF16) for i in range(3)]
                    tmpb = [sbuf(es, "n_tmp%d" % i, [128, 128], F32) for i in range(2)]
                    osbb = sbuf(es, "n_osbb", [128, 256], BF16)
                    rs = sbuf(es, "n_rs", [128, 1], F32)
                    otb = [sbuf(es, "n_otb%d" % i, [128, 2, 128], BF16) for i in range(2)]
                    stp = [psum(es, "n_st%d" % i, [128, 512]) for i in range(2)]
                    ops = [psum(es, "n_o%d" % i, [128, 512]) for i in range(2)]
                    tpp = psum(es, "n_tp", [128, 8, 128], BF16)
                    for m in range(4):
                        kb.dma("sp", KT[0:64, m, :], kt_na[m * 64:(m + 1) * 64, :], reads=qk_b, writes=[KT.b])
                        kb.dma("sp", QT[0:64, m, :], qt_na[m * 64:(m + 1) * 64, :], reads=qk_b, writes=[QT.b])
                    v4 = v1a.rearrange("(t p) (a b) -> p t a b", p=128, b=65)
                    for t0 in range(0, NTILE, 9):
                        t1 = min(NTILE, t0 + 9)
                        kb.dma("sp", V1[:, t0:t1, :, :], v4[:, t0:t1, 4:8, :], reads=qk_b, writes=[V1.b])
                    for cs in range(5):
                        kb.dma("sp", nb[cs][:].rearrange("p a b c -> p (a b c)"), nab[l, cs].rearrange("p a b c -> p (a b c)"),
                               writes=[nb[cs].b])
                    cnt = 0
                    for m in range(NTILE):
                        if m < 32:
                            case = {0: 0, 1: 1, 30: 3, 31: 4}.get(m, 2)
                            k0 = min(max(m - 2, 0), 27)
                            chunks = [(k0 + i, i) for i in range(5)] + [(32, None), (33, None)]
                        else:
                            chunks = [(32, None), (33, None)]
                        for h in range(4):
                            o_ = ops[(m * 4 + h) % 2]
                            pend = None
                            for ci, (kc, li) in enumerate(chunks):
                                sp_ = stp[cnt % 2]
                                pt_ = PT[cnt % 3]
                                tb_ = tmpb[cnt % 2]
                                cnt += 1
                                mm(sp_, sp_[:, 0:128], KT[0:64, h, kc * 128:(kc + 1) * 128], QT[0:64, h, m * 128:(m + 1) * 128], [KT, QT])
                                if pend is not None:
                                    pend()
                                if li is not None:
                                    stt(tb_, tb_[:], sp_[:, 0:128], scale, nb[case][:, h, li, :], ALU.mult, ALU.add, [sp_, nb[case]])
                                    act(pt_, pt_[:], tb_[:], AF.Exp, [tb_])
                                else:
                                    act(pt_, pt_[:], sp_[:, 0:128], AF.Exp, [sp_], scale=scale)

                                def mkn(ci=ci, kc=kc, pt_=pt_, h=h, o_=o_, nch=len(chunks)):
                                    def f():
                                        mm(o_, o_[:, 0:65], pt_[:], V1[:, kc, h, :], [pt_, V1], start=(ci == 0), stop=(ci == nch - 1))
                                    return f
                                pend = mkn()
                            pend()
                            recip(rs, rs[:, 0:1], o_[:, 64:65], [o_])
                            ts("dve", osbb, osbb[:, h * 64:(h + 1) * 64], o_[:, 0:64], rs[:, 0:1], None, ALU.mult, None, [o_, rs])
                        ot_ = otb[m % 2]
                        for c in range(2):
                            tp(tpp, tpp[:, c, :], osbb[:, c * 128:(c + 1) * 128], ident_b[:], [osbb, ident_b])
                        cp("act", ot_, ot_[:], tpp[:, 0:2, :], [tpp])
                        kb.dma("pool", ota_view(m * 128, 128)[:, 2:4, :], ot_[:], reads=[ot_.b], writes=[ota_b[m]])
                kb.barrier()

            attn_std(0, qt_mla, kt_mla, 96, 4, 4, lambda m: m, 0, 4, lambda m: m, 96.0 ** -0.5)
            ck("B0")
            attn_na()
            ck("B1")
            attn_std(2, qt_df, kt_df, 32, 8, 8, lambda m: m, 8, 4, lambda m: m // 2, 32.0 ** -0.5, diff=True)
            ck("B2")
            attn_std(3, qt_gq, kt_gq, 64, 4, 2, lambda m: m // 2, 12, 2, lambda m: m // 2, 64.0 ** -0.5)
            if stop == "B":
                break
            with contextlib.ExitStack() as es:
                wg = sbuf(es, "c_wg", [128, 4, 8, D], BF16)
                wbr = sbuf(es, "c_wbr", [128, 4, 2, D], BF16)
                wo = sbuf(es, "c_wo", [128, 8, D], BF16)
                bg = sbuf(es, "c_bg", [128, 4, 8], F32)
                hb = sbuf(es, "c_hb", [128, 8, 512], BF16)
                ob = sbuf(es, "c_ob", [128, 8, 512], BF16)
                xb = sbuf(es, "c_xb", [128, 8, 512], F32)
                sig = [sbuf(es, "c_sig%d" % i, [128, 512], BF16) for i in range(2)]
                macc = [sbuf(es, "c_macc%d" % i, [128, 512], F32) for i in range(2)]
                tmpm = [sbuf(es, "c_tmpm%d" % i, [128, 512], F32) for i in range(2)]
                mrg = sbuf(es, "c_mrg", [128, 8, 512], BF16)
                pg = [psum(es, "c_pg%d" % i, [128, 512]) for i in range(2)]
                pb = [psum(es, "c_pb%d" % i, [128, 512]) for i in range(2)]
                py = [psum(es, "c_py%d" % i, [128, 512]) for i in range(2)]
                for i in range(4):
                    for k in range(8):
                        kb.dma("pool", wg[:, i, k, :], w_gate[l, i, k * 128:(k + 1) * 128, :], writes=[wg.b])
                    for k in range(2):
                        kb.dma("pool", wbr[:, i, k, :], w_branch[l, i, k * 128:(k + 1) * 128, :], writes=[wbr.b])
                for k in range(8):
                    kb.dma("pool", wo[:, k, :], w_out[l, k * 128:(k + 1) * 128, :], writes=[wo.b])
                kb.dma("sp", bg[:], b_gateT[l], writes=[bg.b])
                cntC = 0
                for bi, (c0, Tn, j) in enumerate(BLOCKS):
                    kb.dma("sp", hb[:, :, :Tn], hta_view(c0, Tn), reads=tr(hta_b, c0, Tn), writes=[hb.b])
                    kb.dma("sp", ob[:, :, :Tn], ota_view(c0, Tn), reads=tr(ota_b, c0, Tn), writes=[ob.b])
                    kb.dma("sp", xb[:, :, :Tn], xa_view(c0, Tn), reads=tr(xa_b, c0, Tn), writes=[xb.b])
                    for oc in range(8):
                        ocs = slice(oc * 128, (oc + 1) * 128)
                        ma = macc[oc % 2]
                        for i in range(4):
                            g_ = pg[cntC % 2]
                            b_ = pb[cntC % 2]
                            s_ = sig[cntC % 2]
                            t_ = tmpm[cntC % 2]
                            cntC += 1
                            for k in range(8):
                                mm(g_, g_[:, :Tn], wg[:, i, k, ocs], hb[:, k, :Tn], [wg, hb], start=(k == 0), stop=(k == 7))
                            act(s_, s_[:, :Tn], g_[:, :Tn], AF.Sigmoid, [g_, bg], bias=bg[:, i, oc:oc + 1])
                            for k in range(2):
                                mm(b_, b_[:, :Tn], wbr[:, i, k, ocs], ob[:, 2 * i + k, :Tn], [wbr, ob], start=(k == 0), stop=(k == 1))
                            if i == 0:
                                tt("dve", ma, ma[:, :Tn], b_[:, :Tn], s_[:, :Tn], ALU.mult, [b_, s_])
                            else:
                                tt("dve", t_, t_[:, :Tn], b_[:, :Tn], s_[:, :Tn], ALU.mult, [b_, s_])
                                if i < 3:
                                    tt("pool", ma, ma[:, :Tn], ma[:, :Tn], t_[:, :Tn], ALU.add, [ma, t_])
                                else:
                                    tt("pool", mrg, mrg[:, oc, :Tn], ma[:, :Tn], t_[:, :Tn], ALU.add, [ma, t_])
                    for oc in range(8):
                        ocs = slice(oc * 128, (oc + 1) * 128)
                        y_ = py[oc % 2]
                        for k in range(8):
                            mm(y_, y_[:, :Tn], wo[:, k, ocs], mrg[:, k, :Tn], [wo, mrg], start=(k == 0), stop=(k == 7))
                        stt(xb, xb[:, oc, :Tn], y_[:, :Tn], modT[:, 16 + oc, j:j + 1], xb[:, oc, :Tn], ALU.mult, ALU.add, [y_, modT, xb])
                    kb.dma("pool", xa_view(c0, Tn), xb[:, :, :Tn], reads=[xb.b], writes=tr(xa_b, c0, Tn))
            kb.barrier()
            if stop == "C":
                break

            with contextlib.ExitStack() as es:
                wq = sbuf(es, "d_wq", [128, 8, 2048], BF16)
                sk = sbuf(es, "d_sk", [128, 16, 128], F32)
                xb = sbuf(es, "d_xb", [128, 8, 256], F32)
                hb = sbuf(es, "d_hb", [128, 8, 256], BF16)
                sqk = [sbuf(es, "d_sq%d" % i, [128, 256], F32) for i in range(2)]
                tmpk = [sbuf(es, "d_tk%d" % i, [128, 256], F32) for i in range(2)]
                rstd = sbuf(es, "d_rstd", [128, 256], F32)
                qpc = [sbuf(es, "d_qpc%d" % i, [128, 256], F32) for i in range(2)]
                s_sb = sbuf(es, "d_s", [128, 2, 16, 128], F32)
                top16 = sbuf(es, "d_top", [128, 16, 16], F32)
                mr = sbuf(es, "d_mr", [128, 256], F32)
                best = sbuf(es, "d_best", [128, 8, 16], F32)
                eb = sbuf(es, "d_eb", [128, 8, 16], F32)
                sm = sbuf(es, "d_sm", [128, 6, 8], F32)
                tau = sbuf(es, "d_tau", [128, 2, 8], F32)
                nbias = sbuf(es, "d_nbias", [128, 2, 8], F32)
                cf = [sbuf(es, "d_cf%d" % i, [128, 16, 128], F32) for i in range(4)]
                cand = T(cf[0].t[:].rearrange("p (h a) (b c) -> p h a (b c)", h=8, c=16).rearrange("p h a (b c) -> p h (a b) c", c=16), "cand_alias")
                cand.b = cf[0].b
                pr = [sbuf(es, "d_pr%d" % i, [128, 16, 128], BF16) for i in range(4)]
                tw = [sbuf(es, "d_tw%d" % i, [128, 16, 128], BF16) for i in range(2)]
                Ws = [sbuf(es, "d_Ws%d" % i, [128, 2, 16, 128], BF16) for i in range(2)]
                uch = [sbuf(es, "d_uch%d" % i, [128, 8, 512], BF16) for i in range(2)]
                vch = [sbuf(es, "d_vch%d" % i, [128, 4, D], BF16) for i in range(2)]
                wts = [sbuf(es, "d_wts%d" % i, [128, 4, 256], BF16) for i in range(2)]
                gel = [sbuf(es, "d_gel%d" % i, [128, 256], BF16) for i in range(2)]
                cT = [sbuf(es, "d_cT%d" % i, [128, 256], BF16) for i in range(4)]
                yps = [psum(es, "d_y%d" % i, [128, 512]) for i in range(4)]
                pm1 = psum(es, "d_pm1", [128, 512])
                apsl = [psum(es, "d_a%d" % i, [128, 512]) for i in range(2)]
                wtp = psum(es, "d_wt", [128, 4, 256], BF16)
                for k in range(8):
                    kb.dma("pool", wq[:, k, :], w_pq[l, k * 128:(k + 1) * 128, :], writes=[wq.b])
                kb.dma("sp", sk[:].rearrange("p a b -> p (a b)"), skT[l].rearrange("p a b -> p (a b)"), writes=[sk.b])
                if l + 1 < NL:
                    convert_uv(l + 1)
                ub = ub2[l % 2]
                vb = vb2[l % 2]
                ub_b = ub_b2[l % 2]
                vb_b = vb_b2[l % 2]
                ubv = ub.rearrange("(k p) e -> p k e", p=128)
                cDl = [0]
                cUl = [0]
                for bi, (c0, Tn, j) in enumerate(PBLOCKS):
                    kb.dma("sp", xb[:], xa_view(c0, 256), reads=tr(xa_b, c0, 256), writes=[xb.b])
                    norm_mod(xb, hb, sqk, rstd, tmpk, pm1, 256,
                             lambda k: A2[:, k, j:j + 1], lambda k: modT[:, 24 + k, j:j + 1])
                    for c in range(16):
                        q_ = qpc[c % 2]
                        for k in range(8):
                            mm(pm1, pm1[:, 0:256], wq[:, k, c * 128:(c + 1) * 128], hb[:, k, :], [wq, hb], start=(k == 0), stop=(k == 7))
                        cp("act", q_, q_[:], pm1[:, 0:256], [pm1])
                        for tl in range(2):
                            col = 256 + tl * 128
                            mm(pm1, pm1[:, col:col + 128], q_[:, tl * 128:(tl + 1) * 128], sk[:, c, :], [q_, sk])
                        cp("dve", s_sb, s_sb[:, :, c, :], pm1[:, 256:512].rearrange("p (t n) -> p t n", t=2), [pm1])
                    for tl in range(2):
                        for c in range(16):
                            kb.op("dve", lambda: nc.vector.max(out=top16[:, c, 0:8], in_=s_sb[:, tl, c, :]), reads=[s_sb.b], writes=[top16.b])
                            kb.op("dve", lambda: nc.vector.match_replace(out=mr[:, 0:128], in_to_replace=top16[:, c, 0:8],
                                                                          in_values=s_sb[:, tl, c, :], imm_value=-1e30),
                                  reads=[s_sb.b, top16.b], writes=[mr.b])
                            kb.op("dve", lambda: nc.vector.max(out=top16[:, c, 8:16], in_=mr[:, 0:128]), reads=[mr.b], writes=[top16.b])
                        t4 = top16[:].rearrange("p (h a) k -> p h a k", a=2)
                        tt("dve", cand, cand[:], t4[:, :, 0, :].unsqueeze(3).to_broadcast([128, 8, 16, 16]),
                           t4[:, :, 1, :].unsqueeze(2).to_broadcast([128, 8, 16, 16]), ALU.add, [top16])
                        for h in range(8):
                            ch = cand[:, h].rearrange("p a b -> p (a b)")
                            kb.op("dve", lambda: nc.vector.max(out=best[:, h, 0:8], in_=ch), reads=[cand.b], writes=[best.b])
                            kb.op("dve", lambda: nc.vector.match_replace(out=mr[:, 0:256], in_to_replace=best[:, h, 0:8], in_values=ch,
                                                                          imm_value=-1e30),
                                  reads=[cand.b, best.b], writes=[mr.b])
                            kb.op("dve", lambda: nc.vector.max(out=best[:, h, 8:16], in_=mr[:, 0:256]), reads=[mr.b], writes=[best.b])
                        ts("dve", sm, sm[:, 0, :], best[:, :, 0], -1.0, None, ALU.mult, None, [best])
                        cp("dve", tau, tau[:, tl, :], best[:, :, 15], [best])
                        for h in range(8):
                            act(eb, eb[:, h, :], best[:, h, :], AF.Exp, [best, sm], bias=sm[:, 0, h:h + 1])
                        red(sm, sm[:, 1, :], eb[:], ALU.add, [eb])
                        act(sm, sm[:, 1, :], sm[:, 1, :], AF.Ln, [sm])
                        ts("dve", sm, sm[:, 2, :], t4[:, :, 0, 0], -1.0, None, ALU.mult, None, [top16])
                        stt(sm, sm[:, 3, :], t4[:, :, 1, 0], -1.0, sm[:, 1, :], ALU.mult, ALU.subtract, [top16, sm])
                        tt("dve", nbias, nbias[:, tl, :], sm[:, 0, :], sm[:, 1, :], ALU.subtract, [sm])
                    units = [(ig, tl, h) for ig in range(8) for tl in range(2) for h in range(8)]

                    def cand_of(n):
                        ig, tl, h = units[n]
                        isl = slice(ig * 16, (ig + 1) * 16)
                        c_ = cf[n % 4]
                        tt("dve", c_, c_[:], s_sb[:, tl, 2 * h, isl].unsqueeze(2).to_broadcast([128, 16, 128]),
                           s_sb[:, tl, 2 * h + 1, :].unsqueeze(1).to_broadcast([128, 16, 128]), ALU.add, [s_sb])

                    def exp_of(n):
                        ig, tl, h = units[n]
                        act(pr[n % 4], pr[n % 4][:], cf[n % 4][:], AF.Exp, [cf[n % 4], nbias], bias=nbias[:, tl, h:h + 1])

                    def acc_of(n):
                        ig, tl, h = units[n]
                        W_ = Ws[ig % 2]
                        c_ = cf[n % 4]
                        p_ = pr[n % 4]
                        w_ = tw[n % 2]
                        if h == 0:
                            stt(W_, W_[:, tl], c_[:], tau[:, tl, h:h + 1], p_[:], ALU.is_ge, ALU.mult, [c_, p_, tau])
                        else:
                            stt(w_, w_[:], c_[:], tau[:, tl, h:h + 1], p_[:], ALU.is_ge, ALU.mult, [c_, p_, tau])
                            tt("dve", W_, W_[:, tl], W_[:, tl], w_[:], ALU.add, [W_, w_])

                    cand_of(0)
                    cand_of(1)
                    pend = None
                    grp = None
                    for s_ in range(64 + 8):
                        if s_ + 1 < 64:
                            cand_of(2 * s_ + 2)
                            cand_of(2 * s_ + 3)
                        if s_ < 64:
                            exp_of(2 * s_)
                            exp_of(2 * s_ + 1)
                            acc_of(2 * s_)
                            acc_of(2 * s_ + 1)
                        if s_ >= 8:
                            cpair = s_ - 8
                            i_a = 2 * cpair
                            ig = i_a // 16
                            il = i_a % 16
                            W_ = Ws[ig % 2]
                            if i_a % 4 == 0:
                                u_ = uch[cUl[0] % 2]
                                v_ = vch[cUl[0] % 2]
                                ws_ = wts[cUl[0] % 2]
                                cUl[0] += 1
                                kb.dma("sp", u_[:], ubv[:, :, i_a * 128:(i_a + 4) * 128], reads=[ub_b], writes=[u_.b])
                                kb.dma("sp", v_[:], vb[i_a * 128:(i_a + 4) * 128, :].rearrange("(a p) d -> p a d", p=128), reads=[vb_b],
                                       writes=[v_.b])
                                for k4 in range(4):
                                    for tl in range(2):
                                        tp(wtp, wtp[:, k4, tl * 128:(tl + 1) * 128], W_[:, tl, il + k4, :], ident_b[:], [W_, ident_b])
                                cp("act", ws_, ws_[:], wtp[:], [wtp])
                                grp = (u_, v_, ws_)
                            u_, v_, ws_ = grp
                            for i in (i_a, i_a + 1):
                                ii = i % 4
                                a_ = apsl[i % 2]
                                for k in range(8):
                                    mm(a_, a_[:, 0:256], u_[:, k, ii * 128:(ii + 1) * 128], hb[:, k, :], [u_, hb], start=(k == 0), stop=(k == 7))
                            if pend is not None:
                                pend()
                            for i in (i_a, i_a + 1):
                                act(gel[i % 2], gel[i % 2][:], apsl[i % 2][:, 0:256], AF.Gelu, [apsl[i % 2]])
                            for i in (i_a, i_a + 1):
                                tt("dve", cT[i % 4], cT[i % 4][:], gel[i % 2][:], ws_[:, i % 4, :], ALU.mult, [gel[i % 2], ws_])

                            def mkv(i_a=i_a, v_=v_):
                                def f():
                                    for i in (i_a, i_a + 1):
                                        c2 = cT[i % 4]
                                        for oc in range(8):
                                            y_ = yps[oc // 2]
                                            mm(y_, y_[:, (oc % 2) * 256:(oc % 2) * 256 + 256], v_[:, i % 4, oc * 128:(oc + 1) * 128], c2[:],
                                               [v_, c2], start=(i == 0 and oc % 2 == 0), stop=(i == 127))
                                return f
                            pend = mkv()
                    pend()
                    for oc in range(8):
                        y_ = yps[oc // 2]
                        stt(xb, xb[:, oc, :], y_[:, (oc % 2) * 256:(oc % 2) * 256 + 256], modT[:, 40 + oc, j:j + 1], xb[:, oc, :],
                            ALU.mult, ALU.add, [y_, modT, xb])
                    kb.dma("sp", xa_view(c0, 256), xb[:], reads=[xb.b], writes=tr(xa_b, c0, 256))
                    ck("D0")
            kb.barrier()
            if stop == "D":
                break
          except StopBuild:
            break

        if stop is None:
            with contextlib.ExitStack() as es:
                fn = sbuf(es, "f_fn", [128, 8], F32)
                xb_ = [sbuf(es, "f_xb%d" % i, [128, 8, 512], F32) for i in range(2)]
                sqk = [sbuf(es, "f_sq%d" % i, [128, 512], F32) for i in range(2)]
                xn = sbuf(es, "f_xn", [128, 8, 512], F32)
                rstd = sbuf(es, "f_rstd", [128, 512], F32)
                yo = [sbuf(es, "f_yo%d" % i, [128, D], F32) for i in range(2)]
                ssp = psum(es, "f_ssp", [128, 512])
                pp = [psum(es, "f_pp%d" % i, [128, 1024]) for i in range(2)]
                kb.dma("sp", fn[:], fnormT[:, :], writes=[fn.b])
                for bi in range(8):
                    c0 = bi * 512
                    xb = xb_[bi % 2]
                    kb.dma("sp", xb[:], xa_view(c0, 512), reads=tr(xa_b, c0, 512), writes=[xb.b])
                    for k in range(8):
                        q_ = sqk[k % 2]
                        act(q_, q_[:], xb[:, k, :], AF.Square, [xb])
                        mm(ssp, ssp[:], ones_f[:], q_[:], [ones_f, q_], start=(k == 0), stop=(k == 7))
                    rstd_from_ss(rstd, rstd[:], ssp[:], float(D), [ssp])
                    for k in range(8):
                        stt(xn, xn[:, k, :], xb[:, k, :], fn[:, k:k + 1], rstd[:], ALU.mult, ALU.mult, [xb, fn, rstd])
                    for ti in range(4):
                        t = bi * 4 + ti
                        p = pp[t % 2]
                        o = yo[t % 2]
                        for k in range(8):
                            tp(p, p[:, k * 128:(k + 1) * 128], xn[:, k, ti * 128:(ti + 1) * 128], ident_f[:], [xn, ident_f])
                        cp("act" if t % 2 else "dve", o, o[:], p[:], [p])
                        kb.dma("sp", out[t * 128:(t + 1) * 128, :], o[:], reads=[o.b])

        kb.dead = False
        kb.barrier()
    return nc


def rope_tables():
    pos = np.arange(NLAT)
    rows = (pos // 64).astype(np.float32)
    cols = (pos % 64).astype(np.float32)

    def tab(dh):
        inv = np.power(np.float32(10000.0), -np.arange(0, dh, 2, dtype=np.float32) / np.float32(dh)).astype(np.float32)
        out = np.zeros((NT, 2, 2, dh // 2), np.float32)
        out[:, 0] = 1.0
        for a, p_ in enumerate((rows, cols)):
            ang = (p_[:, None] * inv[None, :]).astype(np.float32)
            out[:NLAT, 0, a] = np.cos(ang)
            out[:NLAT, 1, a] = np.sin(ang)
        return out.reshape(NT, -1)

    return tab(16), tab(32)


def na_bias_tables(rpb):
    Lr = rpb.shape[0]
    out = np.full((Lr, 5, 128, 4, 5, 128), NEG, np.float32)
    for case, m in enumerate((0, 1, 2, 30, 31)):
        k0 = min(max(m - 2, 0), 27)
        q = m * 128 + np.arange(128)
        qr, qc = q // 64, q % 64
        rs = np.clip(qr - 4, 0, 56)
        cs = np.clip(qc - 8, 0, 48)
        for i in range(5):
            key = (k0 + i) * 128 + np.arange(128)
            kr, kc_ = key // 64, key % 64
            inw = ((kr[:, None] >= rs[None, :]) & (kr[:, None] < rs[None, :] + 8)
                   & (kc_[:, None] >= cs[None, :]) & (kc_[:, None] < cs[None, :] + 16))
            ro = np.clip(kr[:, None] - qr[None, :] + 7, 0, 14)
            co = np.clip(kc_[:, None] - qc[None, :] + 15, 0, 30)
            for h in range(4):
                g = rpb[:, h][:, ro, co]
                out[:, case, :, h, i, :] = np.where(inw[None], g, np.float32(NEG))
    return out


def prep_inputs(inp):
    f = lambda a: np.ascontiguousarray(np.asarray(a, dtype=np.float32))
    L = DEPTH
    r32, r64 = rope_tables()
    shared = {
        "w_mod": f(inp["w_mod"]),
        "b_modT": f(inp["b_mod"].reshape(L, 48, 128).transpose(0, 2, 1)),
        "nmixT": f(inp["norm_mix"].reshape(L, 8, 128).transpose(0, 2, 1)),
        "nffnT": f(inp["norm_ffn"].reshape(L, 8, 128).transpose(0, 2, 1)),
        "fnormT": f(inp["final_norm"].reshape(8, 128).T),
        "w_in": f(inp["w_in"]),
        "g_mlaq": f(inp["mla_q_norm"]),
        "g_mlakv": f(inp["mla_kv_norm"]),
        "w_uq": f(inp["mla_w_uq"]),
        "w_ukv": f(inp["mla_w_ukv"]),
        "nab": f(na_bias_tables(np.asarray(inp["na_rpb"], np.float32))),
        "lamv": f(np.stack([inp["diff_lam_q1"], inp["diff_lam_k1"], inp["diff_lam_q2"], inp["diff_lam_k2"]], axis=1)),
        "g_subln": f(inp["diff_subln"]),
        "g_gq": f(inp["gqa_q_norm"]),
        "g_gk": f(inp["gqa_k_norm"]),
        "w_branch": f(inp["w_branch"]),
        "w_gate": f(inp["w_gate"]),
        "b_gateT": f(inp["b_gate"].reshape(L, 4, 8, 128).transpose(0, 3, 1, 2)),
        "w_out": f(inp["w_out"]),
        "w_pq": f(inp["peer_w_q"]),
        "skT": f(inp["peer_subkeys"].reshape(L, 16, 128, 128).transpose(0, 3, 1, 2)),
        "uT": f(np.asarray(inp["peer_u"]).transpose(0, 2, 1)),
        "pv": f(inp["peer_v"]),
        "rope32": f(r32),
        "rope64": f(r64),
    }
    maps = []
    for b in range(8):
        m = dict(shared)
        m["xin"] = f(np.concatenate([inp["x"][b], inp["ctx"][b]], axis=0))
        cc = np.stack([inp["c"][b], inp["c_ctx"]], axis=0).reshape(2, 8, 128).transpose(2, 1, 0).reshape(128, 16)
        m["cc"] = f(cc)
        maps.append(m)
    return maps


def kernel(**inputs):
    maps = prep_inputs(inputs)
    nc = build()
    res = run_bass_kernel_spmd(nc, maps, core_ids=list(range(8)))
    return np.stack([np.asarray(r["out"], dtype=np.float32) for r in res.results], axis=0)
```

```python
import math
import contextlib
import numpy as np
import concourse.bass as bass
import concourse.mybir as mybir
from concourse.bass_utils import run_bass_kernel_spmd

F32 = mybir.dt.float32
BF16 = mybir.dt.bfloat16
AF = mybir.ActivationFunctionType
ALU = mybir.AluOpType
AX = mybir.AxisListType

D = 1024
KC = 8
NLAT = 4096
NCTX = 256
NT = NLAT + NCTX
NTILE = NT // 128
DEPTH = 4
EPS = 1e-6
NEG = -30000.0
BLOCKS = [(i * 512, 512, 0) for i in range(8)] + [(4096, 256, 1)]
PBLOCKS = [(i * 256, 256, 0) for i in range(16)] + [(4096, 256, 1)]


class Ev:
    __slots__ = ("key", "val", "clk")

    def __init__(s, key, val, clk):
        s.key = key
        s.val = val
        s.clk = clk


class TB:
    __slots__ = ("name", "w", "r", "excl")

    def __init__(s, name="", excl=False):
        s.name = name
        s.w = None
        s.r = {}
        s.excl = excl


class KB:
    NS = {"sp": 24, "pool": 40, "act": 2}

    def __init__(s, nc, es):
        s.nc = nc
        s.eng = {"pe": nc.tensor, "act": nc.scalar, "dve": nc.vector, "pool": nc.gpsimd, "sp": nc.sync}
        s.semobj = {}
        s.ccnt = {}
        for e in ["pe", "act", "dve", "pool"]:
            s.semobj[e] = es.enter_context(nc.semaphore("cs_" + e))
            s.ccnt[e] = 0
        s.known = {e: {} for e in s.eng}
        s.dcnt = {}
        for q, n in s.NS.items():
            s.dcnt[q] = 0
            for j in range(n):
                s.semobj[(q, j)] = es.enter_context(nc.semaphore("ds_%s%d" % (q, j)))
        s.last_dma = {}
        s.ninstr = 0
        s.dead = False

    def _wait(s, e, deps):
        k = s.known[e]
        changed = False
        for ev in deps:
            if ev is None:
                continue
            if k.get(ev.key, 0) >= ev.val:
                continue
            s.eng[e].wait_ge(s.semobj[ev.key], ev.val)
            if not changed:
                k = dict(k)
                changed = True
            for kk, vv in ev.clk.items():
                if k.get(kk, 0) < vv:
                    k[kk] = vv
            k[ev.key] = ev.val
        if changed:
            s.known[e] = k

    def _deps(s, reads, writes):
        deps = []
        for b in reads:
            if b.w is not None:
                deps.append(b.w)
        for b in writes:
            if b.w is not None:
                deps.append(b.w)
            deps.extend(b.r.values())
        return deps

    def _upd(s, ev, reads, writes):
        for b in reads:
            o = b.r.get(ev.key)
            if o is None or o.val < ev.val:
                b.r[ev.key] = ev
        for b in writes:
            b.w = ev
            b.r = {}

    def op(s, e, fn, reads=(), writes=()):
        if s.dead:
            return None
        ex = [b for b in reads if b.excl]
        if ex:
            reads = [b for b in reads if not b.excl]
            writes = list(writes) + ex
        deps = s._deps(reads, writes)
        if e == "pe":
            deps = [d for d in deps if d.key != "pe"]
        s._wait(e, deps)
        ins = fn()
        s.ccnt[e] += 1
        ins.then_inc(s.semobj[e], 1)
        ev = Ev(e, s.ccnt[e], s.known[e])
        s._upd(ev, reads, writes)
        s.ninstr += 1
        return ev

    def dma(s, q, out, in_, reads=(), writes=(), in_barrier=True):
        if s.dead:
            return None
        i = s.dcnt[q]
        n = s.NS[q]
        j = i % n
        key = (q, j)
        prev = 16 * (i // n)
        val = prev + 16
        deps = s._deps(reads, writes)
        if prev > 0:
            deps.append(Ev(key, prev, {}))
        s._wait(q, deps)
        ins = s.eng[q].dma_start(out=out, in_=in_)
        ins.then_inc(s.semobj[key], 16)
        s.dcnt[q] += 1
        ev = Ev(key, val, s.known[q])
        if in_barrier:
            s.last_dma[key] = ev
        elif key in s.last_dma:
            del s.last_dma[key]
        s._upd(ev, reads, writes)
        s.ninstr += 1
        return ev

    def barrier(s, engines=("pe", "act", "dve", "pool", "sp")):
        if s.dead:
            return
        evs = [Ev(e, s.ccnt[e], {}) for e in ["pe", "act", "dve", "pool"] if s.ccnt[e] > 0]
        evs += list(s.last_dma.values())
        for e in engines:
            s._wait(e, evs)


class StopBuild(Exception):
    pass


class T:
    def __init__(s, t, name, excl=False):
        s.t = t
        s.b = TB(name, excl)

    def __getitem__(s, k):
        return s.t[k]


def build(NL=DEPTH, stop=None, dbg=()):
    nc = bass.Bass("TRN2", target_bir_lowering=False)

    def din(name, shape, dt=F32):
        return nc.dram_tensor(name, list(shape), dt, kind="ExternalInput").ap()

    def dscr(name, shape, dt):
        kind = "ExternalOutput" if name in dbg else "Internal"
        return nc.dram_tensor(name, list(shape), dt, kind=kind).ap()

    L = DEPTH
    xin = din("xin", [NT, D])
    cc = din("cc", [128, 16])
    w_mod = din("w_mod", [L, D, 6 * D])
    b_modT = din("b_modT", [L, 128, 48])
    nmixT = din("nmixT", [L, 128, 8])
    nffnT = din("nffnT", [L, 128, 8])
    fnormT = din("fnormT", [128, 8])
    w_in = din("w_in", [L, D, 2464])
    g_mlaq = din("g_mlaq", [L, 256])
    g_mlakv = din("g_mlakv", [L, 128])
    w_uq = din("w_uq", [L, 256, 384])
    w_ukv = din("w_ukv", [L, 128, 512])
    nab = din("nab", [L, 5, 128, 4, 5, 128])
    lamv = din("lamv", [L, 4, 32])
    g_subln = din("g_subln", [L, 64])
    g_gq = din("g_gq", [L, 64])
    g_gk = din("g_gk", [L, 64])
    w_branch = din("w_branch", [L, 4, 256, D])
    w_gate = din("w_gate", [L, 4, D, D])
    b_gateT = din("b_gateT", [L, 128, 4, 8])
    w_out = din("w_out", [L, D, D])
    w_pq = din("w_pq", [L, D, 2048])
    skT = din("skT", [L, 128, 16, 128])
    uT = din("uT", [L, D, 16384])
    pv = din("pv", [L, 16384, D])
    rope32 = din("rope32", [NT, 32])
    rope64 = din("rope64", [NT, 64])
    out = nc.dram_tensor("out", [NLAT, D], F32, kind="ExternalOutput").ap()

    xa = dscr("xa", [D, NT], F32)
    hta = dscr("hta", [D, NT], BF16)
    ota = dscr("ota", [D, NT], BF16)
    qt_mla = dscr("qt_mla", [4 * 96, NT], BF16)
    kt_mla = dscr("kt_mla", [4 * 96, NT], BF16)
    qt_na = dscr("qt_na", [256, NT], BF16)
    kt_na = dscr("kt_na", [256, NT], BF16)
    qt_df = dscr("qt_df", [256, NT], BF16)
    kt_df = dscr("kt_df", [256, NT], BF16)
    qt_gq = dscr("qt_gq", [256, NT], BF16)
    kt_gq = dscr("kt_gq", [128, NT], BF16)
    v1a = dscr("v1a", [NT, 14 * 65], BF16)
    ub2 = [dscr("ub%d" % i, [D, 16384], BF16) for i in range(2)]
    vb2 = [dscr("vb%d" % i, [16384, D], BF16) for i in range(2)]

    def tiles_tb(name):
        return [TB("%s%d" % (name, i)) for i in range(NTILE)]

    xa_b = tiles_tb("xa")
    hta_b = tiles_tb("hta")
    ota_b = tiles_tb("ota")
    qk_b = tiles_tb("qk")
    ub_b2 = [TB("ub0"), TB("ub1")]
    vb_b2 = [TB("vb0"), TB("vb1")]

    def tr(bl, start, n):
        return bl[start // 128:(start + n) // 128]

    es_top = contextlib.ExitStack()
    with es_top:
        kb = KB(nc, es_top)

        uid = [0]

        def sbuf(es, name, shape, dt):
            uid[0] += 1
            name = "%s_%d" % (name, uid[0])
            return T(es.enter_context(nc.sbuf_tensor(name, list(shape), dt)), name)

        def psum(es, name, shape, dt=F32):
            uid[0] += 1
            name = "%s_%d" % (name, uid[0])
            return T(es.enter_context(nc.psum_tensor(name, list(shape), dt)), name, True)

        def mm(o, oap, lhsT, rhs, reads, start=True, stop=True):
            kb.op("pe", lambda: nc.tensor.matmul(oap, lhsT=lhsT, rhs=rhs, start=start, stop=stop),
                  reads=[r.b for r in reads], writes=[o.b])

        def tp(o, oap, iap, ident, reads):
            kb.op("pe", lambda: nc.tensor.transpose(oap, iap, ident), reads=[r.b for r in reads], writes=[o.b])

        def act(o, oap, iap, func, reads, bias=None, scale=None):
            kw = {}
            if bias is not None:
                kw["bias"] = bias
            if scale is not None:
                kw["scale"] = scale
            kb.op("act", lambda: nc.scalar.activation(out=oap, in_=iap, func=func, **kw),
                  reads=[r.b for r in reads], writes=[o.b])

        def tt(e, o, oap, a, b, op, reads):
            en = nc.vector if e == "dve" else nc.gpsimd
            kb.op(e, lambda: en.tensor_tensor(out=oap, in0=a, in1=b, op=op), reads=[r.b for r in reads], writes=[o.b])

        def ts(e, o, oap, a, s1, s2, op0, op1, reads):
            en = nc.vector if e == "dve" else nc.gpsimd
            if op1 is None:
                kb.op(e, lambda: en.tensor_scalar(out=oap, in0=a, scalar1=s1, scalar2=None, op0=op0),
                      reads=[r.b for r in reads], writes=[o.b])
            else:
                kb.op(e, lambda: en.tensor_scalar(out=oap, in0=a, scalar1=s1, scalar2=s2, op0=op0, op1=op1),
                      reads=[r.b for r in reads], writes=[o.b])

        def stt(o, oap, a, sc, b, op0, op1, reads):
            kb.op("dve", lambda: nc.vector.scalar_tensor_tensor(out=oap, in0=a, scalar=sc, in1=b, op0=op0, op1=op1),
                  reads=[r.b for r in reads], writes=[o.b])

        def cp(e, o, oap, iap, reads):
            if e == "act":
                act(o, oap, iap, AF.Copy, reads)
            else:
                en = nc.vector if e == "dve" else nc.gpsimd
                kb.op(e, lambda: en.tensor_copy(out=oap, in_=iap), reads=[r.b for r in reads], writes=[o.b])

        def red(o, oap, iap, op, reads):
            kb.op("dve", lambda: nc.vector.tensor_reduce(out=oap, in_=iap, axis=AX.X, op=op),
                  reads=[r.b for r in reads], writes=[o.b])

        def recip(o, oap, iap, reads):
            kb.op("dve", lambda: nc.vector.reciprocal(out=oap, in_=iap), reads=[r.b for r in reads], writes=[o.b])

        def mset(e, o, oap, val):
            en = nc.vector if e == "dve" else nc.gpsimd
            kb.op(e, lambda: en.memset(oap, val), writes=[o.b])

        def ck(name):
            if stop == name:
                kb.dead = True

        def rstd_from_ss(o, oap, ssap, n, reads):
            ts("dve", o, oap, ssap, 1.0 / n, EPS, ALU.mult, ALU.add, reads)
            act(o, oap, oap, AF.Sqrt, [o])
            recip(o, oap, oap, [o])

        ident_f = sbuf(es_top, "ident_f", [128, 128], F32)
        ident_b = sbuf(es_top, "ident_b", [128, 128], BF16)
        ones_f = sbuf(es_top, "ones_f", [128, 128], F32)
        modT = sbuf(es_top, "modT", [128, 48, 2], F32)
        A1 = sbuf(es_top, "A1", [128, 8, 2], F32)
        A2 = sbuf(es_top, "A2", [128, 8, 2], F32)
        mset("pool", ident_f, ident_f[:], 1.0)
        kb.op("pool", lambda: nc.gpsimd.affine_select(out=ident_f[:], in_=ident_f[:], pattern=[[-1, 128]],
                                                      compare_op=ALU.is_equal, fill=0.0, base=0,
                                                      channel_multiplier=1),
              reads=[ident_f.b], writes=[ident_f.b])
        cp("dve", ident_b, ident_b[:], ident_f[:], [ident_f])
        mset("pool", ones_f, ones_f[:], 1.0)

        def convert_uv(lc):
            sset = lc % 2
            for k in range(8):
                for hf in range(2):
                    kb.dma("pool", ub2[sset][k * 128:(k + 1) * 128, hf * 8192:(hf + 1) * 8192],
                           uT[lc, k * 128:(k + 1) * 128, hf * 8192:(hf + 1) * 8192], writes=[ub_b2[sset]], in_barrier=False)
            src = pv[lc].rearrange("(g p r) d -> g p (r d)", p=128, r=8)
            dst = vb2[sset].rearrange("(g p r) d -> g p (r d)", p=128, r=8)
            for g in range(16):
                kb.dma("pool", dst[g], src[g], writes=[vb_b2[sset]], in_barrier=False)

        def xa_view(c0, n):
            return xa.rearrange("(k p) n -> p k n", p=128)[:, :, c0:c0 + n]

        def hta_view(c0, n):
            return hta.rearrange("(k p) n -> p k n", p=128)[:, :, c0:c0 + n]

        def ota_view(c0, n):
            return ota.rearrange("(k p) n -> p k n", p=128)[:, :, c0:c0 + n]

        if NL > 0:
            convert_uv(0)
        with contextlib.ExitStack() as es:
            xt = [sbuf(es, "i_xt%d" % i, [128, D], F32) for i in range(2)]
            xo = [sbuf(es, "i_xo%d" % i, [128, 8, 128], F32) for i in range(2)]
            pp = [psum(es, "i_pp%d" % i, [128, 1024], F32) for i in range(2)]
            for t in range(NTILE):
                a = xt[t % 2]
                o = xo[t % 2]
                p = pp[t % 2]
                kb.dma("sp", a[:], xin[t * 128:(t + 1) * 128, :], writes=[a.b])
                for k in range(8):
                    tp(p, p[:, k * 128:(k + 1) * 128], a[:, k * 128:(k + 1) * 128], ident_f[:], [a, ident_f])
                cp("act" if t % 2 else "dve", o, o[:].rearrange("p k n -> p (k n)"), p[:], [p])
                kb.dma("pool", xa_view(t * 128, 128), o[:], reads=[o.b], writes=[xa_b[t]])
        kb.barrier()
        if stop == "I":
            NL = 0

        def norm_mod(xb, hb, sqk, rstd, tmpk, ssp, Tn, Acol, Bcol):
            for k in range(8):
                q_ = sqk[k % 2]
                act(q_, q_[:, :Tn], xb[:, k, :Tn], AF.Square, [xb])
                mm(ssp, ssp[:, :Tn], ones_f[:], q_[:, :Tn], [ones_f, q_], start=(k == 0), stop=(k == 7))
            rstd_from_ss(rstd, rstd[:, :Tn], ssp[:, :Tn], float(D), [ssp])
            for k in range(8):
                t_ = tmpk[k % 2]
                tt("dve", t_, t_[:, :Tn], xb[:, k, :Tn], rstd[:, :Tn], ALU.mult, [xb, rstd])
                act(hb, hb[:, k, :Tn], t_[:, :Tn], AF.Identity, [t_, modT, A1, A2], bias=Bcol(k), scale=Acol(k))

        for l in range(NL):
          try:
            lam_init = 0.8 - 0.6 * math.exp(-0.3 * l)
            with contextlib.ExitStack() as es:
                cct = sbuf(es, "m_cc", [128, 16], F32)
                sc = sbuf(es, "m_sc", [128, 16], F32)
                wm = [sbuf(es, "m_w%d" % i, [128, 8, 768], F32) for i in range(2)]
                bm = sbuf(es, "m_b", [128, 48], F32)
                nm = sbuf(es, "m_nm", [128, 8], F32)
                nf = sbuf(es, "m_nf", [128, 8], F32)
                pm = psum(es, "m_p", [128, 96], F32)
                kb.dma("sp", cct[:], cc[:, :], writes=[cct.b])
                kb.dma("sp", bm[:], b_modT[l], writes=[bm.b])
                kb.dma("sp", nm[:], nmixT[l], writes=[nm.b])
                kb.dma("sp", nf[:], nffnT[l], writes=[nf.b])
                act(sc, sc[:], cct[:], AF.Silu, [cct])
                for blk in range(8):
                    w = wm[blk % 2]
                    for k in range(8):
                        kb.dma("sp", w[:, k, :], w_mod[l, k * 128:(k + 1) * 128, blk * 768:(blk + 1) * 768], writes=[w.b])
                    for cl in range(6):
                        c = blk * 6 + cl
                        for k in range(8):
                            mm(pm, pm[:, c * 2:c * 2 + 2], w[:, k, cl * 128:(cl + 1) * 128], sc[:, k * 2:k * 2 + 2], [w, sc],
                               start=(k == 0), stop=(k == 7))
                tt("dve", modT, modT[:], pm[:].rearrange("p (c j) -> p c j", j=2),
                   bm[:].unsqueeze(2).to_broadcast([128, 48, 2]), ALU.add, [pm, bm])
                stt(A1, A1[:], modT[:, 8:16, :], 1.0, nm[:].unsqueeze(2).to_broadcast([128, 8, 2]), ALU.add, ALU.mult, [modT, nm])
                stt(A2, A2[:], modT[:, 32:40, :], 1.0, nf[:].unsqueeze(2).to_broadcast([128, 8, 2]), ALU.add, ALU.mult, [modT, nf])
            kb.barrier()
            if stop == "mod":
                break

            with contextlib.ExitStack() as es:
                win = sbuf(es, "a_win", [128, 8, 2464], BF16)
                wuq = sbuf(es, "a_wuq", [128, 2, 384], BF16)
                wukv = sbuf(es, "a_wukv", [128, 512], BF16)
                gq = sbuf(es, "a_gq", [128, 256], F32)
                gkv = sbuf(es, "a_gkv", [128, 128], F32)
                ggq = sbuf(es, "a_ggq", [128, 6, 64], F32)
                r32 = sbuf(es, "a_r32", [128, NTILE, 32], F32)
                r64 = sbuf(es, "a_r64", [128, NTILE, 64], F32)
                xb_ = [sbuf(es, "a_xb%d" % i, [128, 8, 512], F32) for i in range(2)]
                hb_ = [sbuf(es, "a_hb%d" % i, [128, 8, 512], BF16) for i in range(2)]
                sq = [sbuf(es, "a_sq%d" % i, [128, 512], F32) for i in range(2)]
                tmpn = [sbuf(es, "a_tmpn%d" % i, [128, 512], F32) for i in range(2)]
                rstd = sbuf(es, "a_rstd", [128, 512], F32)
                sqs_2 = [sbuf(es, "a_sqs%d" % i_, [128, 384], F32) for i_ in range(2)]
                st_2 = [sbuf(es, "a_st%d" % i_, [128, 8], F32) for i_ in range(2)]
                cn_2 = [sbuf(es, "a_cn%d" % i_, [128, 384], BF16) for i_ in range(2)]
                cnT_2 = [sbuf(es, "a_cnT%d" % i_, [128, 3, 128], BF16) for i_ in range(2)]
                qf_2 = [sbuf(es, "a_qf%d" % i_, [128, 4, 96], BF16) for i_ in range(2)]
                kf_2 = [sbuf(es, "a_kf%d" % i_, [128, 4, 96], BF16) for i_ in range(2)]
                krr_2 = [sbuf(es, "a_krr%d" % i_, [128, 32], F32) for i_ in range(2)]
                ra_2 = [sbuf(es, "a_ra%d" % i_, [128, 512], F32) for i_ in range(2)]
                rb_2 = [sbuf(es, "a_rb%d" % i_, [128, 512], F32) for i_ in range(2)]
                qkb_2 = [sbuf(es, "a_qkb%d" % i_, [128, 512], BF16) for i_ in range(2)]
                gtmp_2 = [sbuf(es, "a_gtmp%d" % i_, [128, 384], F32) for i_ in range(2)]
                gtmp2_2 = [sbuf(es, "a_gtmp2%d" % i_, [128, 384], F32) for i_ in range(2)]
                v1 = [sbuf(es, "a_v1_%d" % i, [128, 14, 65], BF16) for i in range(2)]
                tq = [sbuf(es, "a_tq%d" % i, [128, 4, 128], BF16) for i in range(4)]
                ssp = psum(es, "a_ssp", [128, 512], F32)
                pA = psum(es, "a_pA", [128, 512], F32)
                pB = psum(es, "a_pB", [128, 1024], F32)
                pC = psum(es, "a_pC", [128, 512], F32)
                pU1 = psum(es, "a_pU1", [128, 512], F32)
                pU2 = psum(es, "a_pU2", [128, 512], F32)
                pT = psum(es, "a_pT", [128, 8, 128], BF16)

                for k in range(8):
                    kb.dma("pool", win[:, k, :], w_in[l, k * 128:(k + 1) * 128, :], writes=[win.b])
                for k in range(2):
                    kb.dma("pool", wuq[:, k, :], w_uq[l, k * 128:(k + 1) * 128, :], writes=[wuq.b])
                kb.dma("pool", wukv[:], w_ukv[l], writes=[wukv.b])
                kb.dma("sp", gq[:], g_mlaq[l].partition_broadcast(128), writes=[gq.b])
                kb.dma("sp", gkv[:], g_mlakv[l].partition_broadcast(128), writes=[gkv.b])
                for h in range(4):
                    kb.dma("sp", ggq[:, h, :], g_gq[l].partition_broadcast(128), writes=[ggq.b])
                for h in range(2):
                    kb.dma("sp", ggq[:, 4 + h, :], g_gk[l].partition_broadcast(128), writes=[ggq.b])
                kb.dma("sp", r32[:], rope32.rearrange("(t p) c -> p t c", p=128), writes=[r32.b])
                kb.dma("sp", r64[:], rope64.rearrange("(t p) c -> p t c", p=128), writes=[r64.b])
                for i in range(2):
                    mset("pool", v1[i], v1[i][:], 1.0)

                tqi = [0]

                def next_tq():
                    tqi[0] += 1
                    return tq[tqi[0] % 4]

                def rope(src5, dst5, rt, t, G, Fq, reads, dstT):
                    ra = ra_2[t % 2]
                    rb = rb_2[t % 2]
                    tab = rt[:, t, :].rearrange("p (a b f) -> p a b f", a=2, b=2)
                    C = tab[:, 0].unsqueeze(1).to_broadcast([128, G, 2, Fq])
                    S = tab[:, 1].unsqueeze(1).to_broadcast([128, G, 2, Fq])
                    n = G * 2 * Fq
                    rav = ra[:, 0:n].rearrange("p (g b f) -> p g b f", g=G, b=2)
                    rbv = rb[:, 0:n].rearrange("p (g b f) -> p g b f", g=G, b=2)
                    t1 = src5[:, :, :, 0, :]
                    t2 = src5[:, :, :, 1, :]
                    tt("dve", ra, rav, t1, C, ALU.mult, reads + [rt])
                    tt("dve", rb, rbv, t2, S, ALU.mult, reads + [rt])
                    tt("pool", dstT, dst5[:, :, :, 0, :], rav, rbv, ALU.subtract, [ra, rb])
                    tt("dve", ra, rav, t1, S, ALU.mult, reads + [rt])
                    tt("dve", rb, rbv, t2, C, ALU.mult, reads + [rt])
                    tt("pool", dstT, dst5[:, :, :, 1, :], rav, rbv, ALU.add, [ra, rb])

                def r5(ap, G, Fq):
                    if len(ap.shape) == 2:
                        return ap.rearrange("p (g a b f) -> p g a b f", g=G, a=2, b=2)
                    return ap.rearrange("p g (a b f) -> p g a b f", a=2, b=2)

                for bi, (c0, Tn, j) in enumerate(BLOCKS):
                    xb = xb_[bi % 2]
                    hb = hb_[bi % 2]
                    kb.dma("sp", xb[:, :, :Tn], xa_view(c0, Tn), reads=tr(xa_b, c0, Tn), writes=[xb.b])
                    norm_mod(xb, hb, sq, rstd, tmpn, ssp, Tn,
                             lambda k: A1[:, k, j:j + 1], lambda k: modT[:, 0 + k, j:j + 1])
                    kb.dma("pool", hta_view(c0, Tn), hb[:, :, :Tn], reads=[hb.b], writes=tr(hta_b, c0, Tn))
                    ck("A0")
                    for ti in range(Tn // 128):
                        t = c0 // 128 + ti
                        ts_ = slice(ti * 128, (ti + 1) * 128)
                        vv = v1[t % 2]
                        sqs, st, cn, cnT, qf, kf, krr, qkb, gtmp, gtmp2 = [x_[t % 2] for x_ in
                                                                          (sqs_2, st_2, cn_2, cnT_2, qf_2, kf_2, krr_2, qkb_2, gtmp_2, gtmp2_2)]
                        for k in range(8):
                            mm(pA, pA[:, 0:416], hb[:, k, ts_], win[:, k, 0:416], [hb, win], start=(k == 0), stop=(k == 7))
                        act(sqs, sqs[:, 0:384], pA[:, 0:384], AF.Square, [pA])
                        red(st, st[:, 0:1], sqs[:, 0:256], ALU.add, [sqs])
                        red(st, st[:, 1:2], sqs[:, 256:384], ALU.add, [sqs])
                        ts("dve", st, st[:, 0:1], st[:, 0:1], 1.0 / 256, EPS, ALU.mult, ALU.add, [st])
                        ts("dve", st, st[:, 1:2], st[:, 1:2], 1.0 / 128, EPS, ALU.mult, ALU.add, [st])
                        act(st, st[:, 0:2], st[:, 0:2], AF.Sqrt, [st])
                        recip(st, st[:, 0:2], st[:, 0:2], [st])
                        stt(cn, cn[:, 0:256], pA[:, 0:256], st[:, 0:1], gq[:], ALU.mult, ALU.mult, [pA, st, gq])
                        stt(cn, cn[:, 256:384], pA[:, 256:384], st[:, 1:2], gkv[:], ALU.mult, ALU.mult, [pA, st, gkv])
                        ck("A1a")
                        for k in range(3):
                            tp(pT, pT[:, k, :], cn[:, k * 128:(k + 1) * 128], ident_b[:], [cn, ident_b])
                        cp("act", cnT, cnT[:], pT[:, 0:3, :], [pT])
                        ck("A1b")
                        for k in range(2):
                            mm(pU1, pU1[:, 0:384], cnT[:, k, :], wuq[:, k, :], [cnT, wuq], start=(k == 0), stop=(k == 1))
                        mm(pU2, pU2[:, 0:512], cnT[:, 2, :], wukv[:], [cnT, wukv])
                        u1 = pU1[:, 0:384].rearrange("p (h d) -> p h d", h=4)
                        u2 = pU2[:, 0:512].rearrange("p (h d) -> p h d", h=4)
                        ck("A1c")
                        cp("act", qf, qf[:, :, 0:64], u1[:, :, 0:64], [pU1])
                        rope(r5(u1[:, :, 64:96], 4, 8), r5(qf[:, :, 64:96], 4, 8), r32, t, 4, 8, [pU1], qf)
                        ck("A1c1")
                        cp("act", kf, kf[:, :, 0:64], u2[:, :, 0:64], [pU2])
                        ck("A1c2")
                        rope(r5(pA[:, 384:416], 1, 8), r5(krr[:, :], 1, 8), r32, t, 1, 8, [pA], krr)
                        ck("A1c3")
                        cp("pool", kf, kf[:, :, 64:96], krr[:].unsqueeze(1).to_broadcast([128, 4, 32]), [krr])
                        ck("A1c4")
                        cp("dve", vv, vv[:, 0:4, 0:64], u2[:, :, 64:128], [pU2])
                        ck("A1d")
                        for h in range(4):
                            tp(pT, pT[0:96, h, :], qf[:, h, :], ident_b[:], [qf, ident_b])
                        for h in range(4):
                            tp(pT, pT[0:96, 4 + h, :], kf[:, h, :], ident_b[:], [kf, ident_b])
                        o1 = next_tq()
                        o2 = next_tq()
                        cp("act", o1, o1[0:96, :, :], pT[0:96, 0:4, :], [pT])
                        cp("dve", o2, o2[0:96, :, :], pT[0:96, 4:8, :], [pT])
                        cols = slice(t * 128, (t + 1) * 128)
                        ck("A1e")
                        kb.dma("pool", qt_mla.rearrange("(m p) n -> p m n", p=96)[:, :, cols], o1[0:96, :, :], reads=[o1.b], writes=[qk_b[t]])
                        kb.dma("pool", kt_mla.rearrange("(m p) n -> p m n", p=96)[:, :, cols], o2[0:96, :, :], reads=[o2.b], writes=[qk_b[t]])
                        ck("A1")
                        for k in range(8):
                            mm(pB, pB[:, 0:512], hb[:, k, ts_], win[:, k, 416:928], [hb, win], start=(k == 0), stop=(k == 7))
                        for k in range(8):
                            mm(pB, pB[:, 512:768], hb[:, k, ts_], win[:, k, 928:1184], [hb, win], start=(k == 0), stop=(k == 7))
                        cp("act", qkb, qkb[:], pB[:, 0:512], [pB])
                        cp("dve", vv, vv[:, 4:8, 0:64], pB[:, 512:768].rearrange("p (h d) -> p h d", h=4), [pB])
                        for k in range(4):
                            tp(pT, pT[:, k, :], qkb[:, k * 128:(k + 1) * 128], ident_b[:], [qkb, ident_b])
                        o1 = next_tq()
                        cp("act", o1, o1[:], pT[:, 0:4, :], [pT])
                        kb.dma("pool", qt_na.rearrange("(m p) n -> p m n", p=128)[:, :, cols], o1[:, 0:2, :], reads=[o1.b], writes=[qk_b[t]])
                        kb.dma("pool", kt_na.rearrange("(m p) n -> p m n", p=128)[:, :, cols], o1[:, 2:4, :], reads=[o1.b], writes=[qk_b[t]])
                        ck("A2")
                        for k in range(8):
                            mm(pB, pB[:, 0:512], hb[:, k, ts_], win[:, k, 1184:1696], [hb, win], start=(k == 0), stop=(k == 7))
                        for k in range(8):
                            mm(pB, pB[:, 512:768], hb[:, k, ts_], win[:, k, 1696:1952], [hb, win], start=(k == 0), stop=(k == 7))
                        for half in range(2):
                            rope(r5(pB[:, half * 256:(half + 1) * 256], 8, 8), r5(qkb[:, half * 256:(half + 1) * 256], 8, 8),
                                 r32, t, 8, 8, [pB], qkb)
                        cp("dve", vv, vv[:, 8:12, 0:64], pB[:, 512:768].rearrange("p (h d) -> p h d", h=4), [pB])
                        for k in range(4):
                            tp(pT, pT[:, k, :], qkb[:, k * 128:(k + 1) * 128], ident_b[:], [qkb, ident_b])
                        o1 = next_tq()
                        cp("act", o1, o1[:], pT[:, 0:4, :], [pT])
                        kb.dma("pool", qt_df.rearrange("(m p) n -> p m n", p=128)[:, :, cols], o1[:, 0:2, :], reads=[o1.b], writes=[qk_b[t]])
                        kb.dma("pool", kt_df.rearrange("(m p) n -> p m n", p=128)[:, :, cols], o1[:, 2:4, :], reads=[o1.b], writes=[qk_b[t]])
                        ck("A3")
                        for k in range(8):
                            mm(pC, pC[:, 0:512], hb[:, k, ts_], win[:, k, 1952:2464], [hb, win], start=(k == 0), stop=(k == 7))
                        act(sqs, sqs[:, 0:384], pC[:, 0:384], AF.Square, [pC])
                        red(st, st[:, 2:8], sqs[:, 0:384].rearrange("p (h d) -> p h d", h=6), ALU.add, [sqs])
                        rstd_from_ss(st, st[:, 2:8], st[:, 2:8], 64.0, [st])
                        g3 = gtmp[:, 0:384].rearrange("p (h d) -> p h d", h=6)
                        g32 = gtmp2[:, 0:384].rearrange("p (h d) -> p h d", h=6)
                        tt("dve", gtmp, g3, pC[:, 0:384].rearrange("p (h d) -> p h d", h=6),
                           st[:, 2:8].unsqueeze(2).to_broadcast([128, 6, 64]), ALU.mult, [pC, st])
                        tt("pool", gtmp2, g32, g3, ggq[:], ALU.mult, [gtmp, ggq])
                        rope(r5(gtmp2[:, 0:384], 6, 16), r5(qkb[:, 0:384], 6, 16), r64, t, 6, 16, [gtmp2], qkb)
                        cp("dve", vv, vv[:, 12:14, 0:64], pC[:, 384:512].rearrange("p (h d) -> p h d", h=2), [pC])
                        for k in range(3):
                            tp(pT, pT[:, k, :], qkb[:, k * 128:(k + 1) * 128], ident_b[:], [qkb, ident_b])
                        o1 = next_tq()
                        cp("act", o1, o1[:, 0:3, :], pT[:, 0:3, :], [pT])
                        kb.dma("pool", qt_gq.rearrange("(m p) n -> p m n", p=128)[:, :, cols], o1[:, 0:2, :], reads=[o1.b], writes=[qk_b[t]])
                        kb.dma("pool", kt_gq.rearrange("(m p) n -> p m n", p=128)[:, :, cols], o1[:, 2:3, :], reads=[o1.b], writes=[qk_b[t]])
                        kb.dma("pool", v1a[t * 128:(t + 1) * 128, :], vv[:].rearrange("p a b -> p (a b)"), reads=[vv.b], writes=[qk_b[t]])
            kb.barrier()
            if stop == "A":
                break
            cntB = [0]

            def attn_std(mixer, qt_d, kt_d, dk, nq, nk, kmap, vbase, nv, vmap, scale, diff=False):
                with contextlib.ExitStack() as es:
                    KT = sbuf(es, "b_KT", [128, nk, NT], BF16)
                    V1 = sbuf(es, "b_V1", [128, NTILE, nv, 65], BF16)
                    QT = [sbuf(es, "b_QT%d" % i, [128, nq, 512], BF16) for i in range(2)]
                    PT = [sbuf(es, "b_PT%d" % i, [128, 512], BF16) for i in range(3)]
                    osb = sbuf(es, "b_osb", [128, 4, nq, 64], F32)
                    osbb = sbuf(es, "b_osbb", [128, 4, 256], BF16)
                    rs = sbuf(es, "b_rs", [128, 4], F32)
                    otb = [sbuf(es, "b_otb%d" % i, [128, 2, 512], BF16) for i in range(2)]
                    stp = [psum(es, "b_st%d" % i, [128, 512]) for i in range(2)]
                    ops = [psum(es, "b_o%d" % i, [128, 512]) for i in range(4)]
                    tpp = psum(es, "b_tp", [128, 8, 128], BF16)
                    for m in range(nk):
                        kb.dma("sp", KT[0:dk, m, :], kt_d[m * dk:(m + 1) * dk, :], reads=qk_b, writes=[KT.b])
                    v4 = v1a.rearrange("(t p) (a b) -> p t a b", p=128, b=65)
                    for t0 in range(0, NTILE, 9):
                        t1 = min(NTILE, t0 + 9)
                        kb.dma("sp", V1[:, t0:t1, :, :], v4[:, t0:t1, vbase:vbase + nv, :], reads=qk_b, writes=[V1.b])
                    if diff:
                        lamt = sbuf(es, "b_lamt", [128, 4, 32], F32)
                        lp = sbuf(es, "b_lp", [128, 2, 32], F32)
                        ls = sbuf(es, "b_ls", [128, 2], F32)
                        neglam = sbuf(es, "b_neglam", [128, 1], F32)
                        gsub = sbuf(es, "b_gsub", [128, 64], F32)
                        dsb = sbuf(es, "b_dsb", [128, 4, 64], F32)
                        dsq = sbuf(es, "b_dsq", [128, 4, 64], F32)
                        dst = sbuf(es, "b_dst", [128, 4], F32)
                        for i in range(4):
                            kb.dma("sp", lamt[:, i, :], lamv[l, i].partition_broadcast(128), writes=[lamt.b])
                        kb.dma("sp", gsub[:], g_subln[l].partition_broadcast(128), writes=[gsub.b])
                        tt("dve", lp, lp[:, 0, :], lamt[:, 0, :], lamt[:, 1, :], ALU.mult, [lamt])
                        tt("dve", lp, lp[:, 1, :], lamt[:, 2, :], lamt[:, 3, :], ALU.mult, [lamt])
                        red(ls, ls[:, 0:2], lp[:], ALU.add, [lp])
                        act(ls, ls[:], ls[:], AF.Exp, [ls])
                        tt("dve", neglam, neglam[:, 0:1], ls[:, 1:2], ls[:, 0:1], ALU.subtract, [ls])
                        ts("dve", neglam, neglam[:], neglam[:], -lam_init, None, ALU.add, None, [neglam])
                        ts("dve", gsub, gsub[:], gsub[:], 1.0 - lam_init, None, ALU.mult, None, [gsub])
                    for bi, (c0, Tn, j) in enumerate(BLOCKS):
                        if j == 1 and l == DEPTH - 1:
                            continue
                        QTb = QT[bi % 2]
                        kb.dma("sp", QTb[0:dk, :, :Tn], qt_d.rearrange("(m p) n -> p m n", p=dk)[:, :, c0:c0 + Tn],
                               reads=qk_b, writes=[QTb.b])
                        kchunks = list(range(NTILE)) if j == 0 else [32, 33]
                        nqs = Tn // 128
                        for m in range(nq):
                            pend = None
                            for ci, kc in enumerate(kchunks):
                                sp_ = stp[cntB[0] % 2]
                                pt_ = PT[cntB[0] % 3]
                                cntB[0] += 1
                                mm(sp_, sp_[:, :Tn], KT[0:dk, kmap(m), kc * 128:(kc + 1) * 128], QTb[0:dk, m, :Tn], [KT, QTb])
                                if pend is not None:
                                    pend()
                                act(pt_, pt_[:, :Tn], sp_[:, :Tn], AF.Exp, [sp_], scale=scale)

                                def mk(ci=ci, kc=kc, pt_=pt_, m=m):
                                    def f():
                                        for qs in range(nqs):
                                            mm(ops[qs], ops[qs][:, 0:65], pt_[:, qs * 128:(qs + 1) * 128], V1[:, kc, vmap(m), :], [pt_, V1],
                                               start=(ci == 0), stop=(ci == len(kchunks) - 1))
                                    return f
                                pend = mk()
                            pend()
                            for qs in range(nqs):
                                recip(rs, rs[:, qs:qs + 1], ops[qs][:, 64:65], [ops[qs]])
                                if diff:
                                    ts("dve", osb, osb[:, qs, m, :], ops[qs][:, 0:64], rs[:, qs:qs + 1], None, ALU.mult, None, [ops[qs], rs])
                                else:
                                    ts("dve", osbb, osbb[:, qs, m * 64:(m + 1) * 64], ops[qs][:, 0:64], rs[:, qs:qs + 1], None, ALU.mult, None,
                                       [ops[qs], rs])
                        if diff:
                            for qs in range(nqs):
                                ov = osb[:, qs].rearrange("p (h i) d -> p h i d", i=2)
                                stt(dsb, dsb[:], ov[:, :, 1, :], neglam[:, 0:1], ov[:, :, 0, :], ALU.mult, ALU.add, [osb, neglam])
                                tt("pool", dsq, dsq[:], dsb[:], dsb[:], ALU.mult, [dsb])
                                red(dst, dst[:, 0:4], dsq[:], ALU.add, [dsq])
                                rstd_from_ss(dst, dst[:, 0:4], dst[:, 0:4], 64.0, [dst])
                                tt("dve", dsb, dsb[:], dsb[:], dst[:, 0:4].unsqueeze(2).to_broadcast([128, 4, 64]), ALU.mult, [dsb, dst])
                                tt("pool", osbb, osbb[:, qs, :].rearrange("p (h d) -> p h d", h=4), dsb[:],
                                   gsub[:].unsqueeze(1).to_broadcast([128, 4, 64]), ALU.mult, [dsb, gsub])
                        ot_ = otb[bi % 2]
                        for qs in range(nqs):
                            for c in range(2):
                                tp(tpp, tpp[:, qs * 2 + c, :], osbb[:, qs, c * 128:(c + 1) * 128], ident_b[:], [osbb, ident_b])
                        cp("act", ot_, ot_[:, :, :Tn].rearrange("p c (q n) -> p q c n", n=128),
                           tpp[:, 0:2 * nqs, :].rearrange("p (q c) n -> p q c n", c=2), [tpp])
                        kb.dma("pool", ota_view(c0, Tn)[:, 2 * mixer:2 * mixer + 2, :], ot_[:, :, :Tn], reads=[ot_.b],
                               writes=tr(ota_b, c0, Tn))
                kb.barrier()

            def attn_na():
                scale = 64.0 ** -0.5
                with contextlib.ExitStack() as es:
                    KT = sbuf(es, "n_KT", [128, 4, NT], BF16)
                    QT = sbuf(es, "n_QT", [128, 4, NT], BF16)
                    V1 = sbuf(es, "n_V1", [128, NTILE, 4, 65], BF16)
                    nb = [sbuf(es, "n_nb%d" % i, [128, 4, 5, 128], F32) for i in range(5)]
                    PT = [sbuf(es, "n_PT%d" % i, [128, 128], BF16) for i in range(3)]
                    tmpb = [sbuf(es, "n_tmp%d" % i, [128, 128], F32) for i in range(2)]
                    osbb = sbuf(es, "n_osbb", [128, 256], BF16)
                    rs = sbuf(es, "n_rs", [128, 1], F32)
                    otb = [sbuf(es, "n_otb%d" % i, [128, 2, 128], BF16) for i in range(2)]
                    stp = [psum(es, "n_st%d" % i, [128, 512]) for i in range(2)]
                    ops = [psum(es, "n_o%d" % i, [128, 512]) for i in range(2)]
                    tpp = psum(es, "n_tp", [128, 8, 128], BF16)
                    for m in range(4):
                        kb.dma("sp", KT[0:64, m, :], kt_na[m * 64:(m + 1) * 64, :], reads=qk_b, writes=[KT.b])
                        kb.dma("sp", QT[0:64, m, :], qt_na[m * 64:(m + 1) * 64, :], reads=qk_b, writes=[QT.b])
                    v4 = v1a.rearrange("(t p) (a b) -> p t a b", p=128, b=65)
                    for t0 in range(0, NTILE, 9):
                        t1 = min(NTILE, t0 + 9)
                        kb.dma("sp", V1[:, t0:t1, :, :], v4[:, t0:t1, 4:8, :], reads=qk_b, writes=[V1.b])
                    for cs in range(5):
                        kb.dma("sp", nb[cs][:].rearrange("p a b c -> p (a b c)"), nab[l, cs].rearrange("p a b c -> p (a b c)"),
                               writes=[nb[cs].b])
                    cnt = 0
                    for m in range(NTILE):
                        if m >= 32 and l == DEPTH - 1:
                            continue
                        if m < 32:
                            case = {0: 0, 1: 1, 30: 3, 31: 4}.get(m, 2)
                            k0 = min(max(m - 2, 0), 27)
                            chunks = [(k0 + i, i) for i in range(5)] + [(32, None), (33, None)]
                        else:
                            chunks = [(32, None), (33, None)]
                        for h in range(4):
                            o_ = ops[(m * 4 + h) % 2]
                            pend = None
                            for ci, (kc, li) in enumerate(chunks):
                                sp_ = stp[cnt % 2]
                                pt_ = PT[cnt % 3]
                                tb_ = tmpb[cnt % 2]
                                cnt += 1
                                mm(sp_, sp_[:, 0:128], KT[0:64, h, kc * 128:(kc + 1) * 128], QT[0:64, h, m * 128:(m + 1) * 128], [KT, QT])
                                if pend is not None:
                                    pend()
                                if li is not None:
                                    stt(tb_, tb_[:], sp_[:, 0:128], scale, nb[case][:, h, li, :], ALU.mult, ALU.add, [sp_, nb[case]])
                                    act(pt_, pt_[:], tb_[:], AF.Exp, [tb_])
                                else:
                                    act(pt_, pt_[:], sp_[:, 0:128], AF.Exp, [sp_], scale=scale)

                                def mkn(ci=ci, kc=kc, pt_=pt_, h=h, o_=o_, nch=len(chunks)):
                                    def f():
                                        mm(o_, o_[:, 0:65], pt_[:], V1[:, kc, h, :], [pt_, V1], start=(ci == 0), stop=(ci == nch - 1))
                                    return f
                                pend = mkn()
                            pend()
                            recip(rs, rs[:, 0:1], o_[:, 64:65], [o_])
                            ts("dve", osbb, osbb[:, h * 64:(h + 1) * 64], o_[:, 0:64], rs[:, 0:1], None, ALU.mult, None, [o_, rs])
                        ot_ = otb[m % 2]
                        for c in range(2):
                            tp(tpp, tpp[:, c, :], osbb[:, c * 128:(c + 1) * 128], ident_b[:], [osbb, ident_b])
                        cp("act", ot_, ot_[:], tpp[:, 0:2, :], [tpp])
                        kb.dma("pool", ota_view(m * 128, 128)[:, 2:4, :], ot_[:], reads=[ot_.b], writes=[ota_b[m]])
                kb.barrier()

            attn_std(0, qt_mla, kt_mla, 96, 4, 4, lambda m: m, 0, 4, lambda m: m, 96.0 ** -0.5)
            ck("B0")
            attn_na()
            ck("B1")
            attn_std(2, qt_df, kt_df, 32, 8, 8, lambda m: m, 8, 4, lambda m: m // 2, 32.0 ** -0.5, diff=True)
            ck("B2")
            attn_std(3, qt_gq, kt_gq, 64, 4, 2, lambda m: m // 2, 12, 2, lambda m: m // 2, 64.0 ** -0.5)
            if stop == "B":
                break
            with contextlib.ExitStack() as es:
                wg = sbuf(es, "c_wg", [128, 4, 8, D], BF16)
                wbr = sbuf(es, "c_wbr", [128, 4, 2, D], BF16)
                wo = sbuf(es, "c_wo", [128, 8, D], BF16)
                bg = sbuf(es, "c_bg", [128, 4, 8], F32)
                hb = sbuf(es, "c_hb", [128, 8, 512], BF16)
                ob = sbuf(es, "c_ob", [128, 8, 512], BF16)
                xb = sbuf(es, "c_xb", [128, 8, 512], F32)
                sig = [sbuf(es, "c_sig%d" % i, [128, 512], BF16) for i in range(2)]
                macc = [sbuf(es, "c_macc%d" % i, [128, 512], F32) for i in range(2)]
                tmpm = [sbuf(es, "c_tmpm%d" % i, [128, 512], F32) for i in range(2)]
                mrg = sbuf(es, "c_mrg", [128, 8, 512], BF16)
                pg = [psum(es, "c_pg%d" % i, [128, 512]) for i in range(2)]
                pb = [psum(es, "c_pb%d" % i, [128, 512]) for i in range(2)]
                py = [psum(es, "c_py%d" % i, [128, 512]) for i in range(2)]
                for i in range(4):
                    for k in range(8):
                        kb.dma("pool", wg[:, i, k, :], w_gate[l, i, k * 128:(k + 1) * 128, :], writes=[wg.b])
                    for k in range(2):
                        kb.dma("pool", wbr[:, i, k, :], w_branch[l, i, k * 128:(k + 1) * 128, :], writes=[wbr.b])
                for k in range(8):
                    kb.dma("pool", wo[:, k, :], w_out[l, k * 128:(k + 1) * 128, :], writes=[wo.b])
                kb.dma("sp", bg[:], b_gateT[l], writes=[bg.b])
                cntC = 0
                for bi, (c0, Tn, j) in enumerate(BLOCKS):
                    if j == 1 and l == DEPTH - 1:
                        continue
                    kb.dma("sp", hb[:, :, :Tn], hta_view(c0, Tn), reads=tr(hta_b, c0, Tn), writes=[hb.b])
                    kb.dma("sp", ob[:, :, :Tn], ota_view(c0, Tn), reads=tr(ota_b, c0, Tn), writes=[ob.b])
                    kb.dma("sp", xb[:, :, :Tn], xa_view(c0, Tn), reads=tr(xa_b, c0, Tn), writes=[xb.b])
                    for oc in range(8):
                        ocs = slice(oc * 128, (oc + 1) * 128)
                        ma = macc[oc % 2]
                        for i in range(4):
                            g_ = pg[cntC % 2]
                            b_ = pb[cntC % 2]
                            s_ = sig[cntC % 2]
                            t_ = tmpm[cntC % 2]
                            cntC += 1
                            for k in range(8):
                                mm(g_, g_[:, :Tn], wg[:, i, k, ocs], hb[:, k, :Tn], [wg, hb], start=(k == 0), stop=(k == 7))
                            act(s_, s_[:, :Tn], g_[:, :Tn], AF.Sigmoid, [g_, bg], bias=bg[:, i, oc:oc + 1])
                            for k in range(2):
                                mm(b_, b_[:, :Tn], wbr[:, i, k, ocs], ob[:, 2 * i + k, :Tn], [wbr, ob], start=(k == 0), stop=(k == 1))
                            if i == 0:
                                tt("dve", ma, ma[:, :Tn], b_[:, :Tn], s_[:, :Tn], ALU.mult, [b_, s_])
                            else:
                                tt("dve", t_, t_[:, :Tn], b_[:, :Tn], s_[:, :Tn], ALU.mult, [b_, s_])
                                if i < 3:
                                    tt("pool", ma, ma[:, :Tn], ma[:, :Tn], t_[:, :Tn], ALU.add, [ma, t_])
                                else:
                                    tt("pool", mrg, mrg[:, oc, :Tn], ma[:, :Tn], t_[:, :Tn], ALU.add, [ma, t_])
                    for oc in range(8):
                        ocs = slice(oc * 128, (oc + 1) * 128)
                        y_ = py[oc % 2]
                        for k in range(8):
                            mm(y_, y_[:, :Tn], wo[:, k, ocs], mrg[:, k, :Tn], [wo, mrg], start=(k == 0), stop=(k == 7))
                        stt(xb, xb[:, oc, :Tn], y_[:, :Tn], modT[:, 16 + oc, j:j + 1], xb[:, oc, :Tn], ALU.mult, ALU.add, [y_, modT, xb])
                    kb.dma("pool", xa_view(c0, Tn), xb[:, :, :Tn], reads=[xb.b], writes=tr(xa_b, c0, Tn))
            kb.barrier()
            if stop == "C":
                break

            with contextlib.ExitStack() as es:
                wq = sbuf(es, "d_wq", [128, 8, 2048], BF16)
                sk = sbuf(es, "d_sk", [128, 16, 128], F32)
                xb = sbuf(es, "d_xb", [128, 8, 256], F32)
                hb = sbuf(es, "d_hb", [128, 8, 256], BF16)
                sqk = [sbuf(es, "d_sq%d" % i, [128, 256], F32) for i in range(2)]
                tmpk = [sbuf(es, "d_tk%d" % i, [128, 256], F32) for i in range(2)]
                rstd = sbuf(es, "d_rstd", [128, 256], F32)
                qpc = [sbuf(es, "d_qpc%d" % i, [128, 256], F32) for i in range(2)]
                s_sb = sbuf(es, "d_s", [128, 2, 16, 128], F32)
                top16 = sbuf(es, "d_top", [128, 16, 16], F32)
                mr = sbuf(es, "d_mr", [128, 256], F32)
                best = sbuf(es, "d_best", [128, 8, 16], F32)
                eb = sbuf(es, "d_eb", [128, 8, 16], F32)
                sm = sbuf(es, "d_sm", [128, 6, 8], F32)
                tau = sbuf(es, "d_tau", [128, 2, 8], F32)
                nbias = sbuf(es, "d_nbias", [128, 2, 8], F32)
                cf = [sbuf(es, "d_cf%d" % i, [128, 16, 128], F32) for i in range(4)]
                cand = T(cf[0].t[:].rearrange("p (h a) (b c) -> p h a (b c)", h=8, c=16).rearrange("p h a (b c) -> p h (a b) c", c=16), "cand_alias")
                cand.b = cf[0].b
                pr = [sbuf(es, "d_pr%d" % i, [128, 16, 128], BF16) for i in range(4)]
                tw = [sbuf(es, "d_tw%d" % i, [128, 16, 128], BF16) for i in range(2)]
                Ws = [sbuf(es, "d_Ws%d" % i, [128, 2, 16, 128], BF16) for i in range(2)]
                uch = [sbuf(es, "d_uch%d" % i, [128, 8, 512], BF16) for i in range(2)]
                vch = [sbuf(es, "d_vch%d" % i, [128, 4, D], BF16) for i in range(2)]
                wts = [sbuf(es, "d_wts%d" % i, [128, 4, 256], BF16) for i in range(2)]
                gel = [sbuf(es, "d_gel%d" % i, [128, 256], BF16) for i in range(2)]
                cT = [sbuf(es, "d_cT%d" % i, [128, 256], BF16) for i in range(4)]
                yps = [psum(es, "d_y%d" % i, [128, 512]) for i in range(4)]
                pm1 = psum(es, "d_pm1", [128, 512])
                apsl = [psum(es, "d_a%d" % i, [128, 512]) for i in range(2)]
                wtp = psum(es, "d_wt", [128, 4, 256], BF16)
                for k in range(8):
                    kb.dma("pool", wq[:, k, :], w_pq[l, k * 128:(k + 1) * 128, :], writes=[wq.b])
                kb.dma("sp", sk[:].rearrange("p a b -> p (a b)"), skT[l].rearrange("p a b -> p (a b)"), writes=[sk.b])
                if l + 1 < NL:
                    convert_uv(l + 1)
                ub = ub2[l % 2]
                vb = vb2[l % 2]
                ub_b = ub_b2[l % 2]
                vb_b = vb_b2[l % 2]
                ubv = ub.rearrange("(k p) e -> p k e", p=128)
                cDl = [0]
                cUl = [0]
                for bi, (c0, Tn, j) in enumerate(PBLOCKS):
                    if j == 1 and l == DEPTH - 1:
                        continue
                    kb.dma("sp", xb[:], xa_view(c0, 256), reads=tr(xa_b, c0, 256), writes=[xb.b])
                    norm_mod(xb, hb, sqk, rstd, tmpk, pm1, 256,
                             lambda k: A2[:, k, j:j + 1], lambda k: modT[:, 24 + k, j:j + 1])
                    for c in range(16):
                        q_ = qpc[c % 2]
                        for k in range(8):
                            mm(pm1, pm1[:, 0:256], wq[:, k, c * 128:(c + 1) * 128], hb[:, k, :], [wq, hb], start=(k == 0), stop=(k == 7))
                        cp("act", q_, q_[:], pm1[:, 0:256], [pm1])
                        for tl in range(2):
                            col = 256 + tl * 128
                            mm(pm1, pm1[:, col:col + 128], q_[:, tl * 128:(tl + 1) * 128], sk[:, c, :], [q_, sk])
                        cp("dve", s_sb, s_sb[:, :, c, :], pm1[:, 256:512].rearrange("p (t n) -> p t n", t=2), [pm1])
                    for tl in range(2):
                        for c in range(16):
                            kb.op("dve", lambda: nc.vector.max(out=top16[:, c, 0:8], in_=s_sb[:, tl, c, :]), reads=[s_sb.b], writes=[top16.b])
                            kb.op("dve", lambda: nc.vector.match_replace(out=mr[:, 0:128], in_to_replace=top16[:, c, 0:8],
                                                                          in_values=s_sb[:, tl, c, :], imm_value=-1e30),
                                  reads=[s_sb.b, top16.b], writes=[mr.b])
                            kb.op("dve", lambda: nc.vector.max(out=top16[:, c, 8:16], in_=mr[:, 0:128]), reads=[mr.b], writes=[top16.b])
                        t4 = top16[:].rearrange("p (h a) k -> p h a k", a=2)
                        tt("dve", cand, cand[:], t4[:, :, 0, :].unsqueeze(3).to_broadcast([128, 8, 16, 16]),
                           t4[:, :, 1, :].unsqueeze(2).to_broadcast([128, 8, 16, 16]), ALU.add, [top16])
                        for h in range(8):
                            ch = cand[:, h].rearrange("p a b -> p (a b)")
                            kb.op("dve", lambda: nc.vector.max(out=best[:, h, 0:8], in_=ch), reads=[cand.b], writes=[best.b])
                            kb.op("dve", lambda: nc.vector.match_replace(out=mr[:, 0:256], in_to_replace=best[:, h, 0:8], in_values=ch,
                                                                          imm_value=-1e30),
                                  reads=[cand.b, best.b], writes=[mr.b])
                            kb.op("dve", lambda: nc.vector.max(out=best[:, h, 8:16], in_=mr[:, 0:256]), reads=[mr.b], writes=[best.b])
                        ts("dve", sm, sm[:, 0, :], best[:, :, 0], -1.0, None, ALU.mult, None, [best])
                        cp("dve", tau, tau[:, tl, :], best[:, :, 15], [best])
                        for h in range(8):
                            act(eb, eb[:, h, :], best[:, h, :], AF.Exp, [best, sm], bias=sm[:, 0, h:h + 1])
                        red(sm, sm[:, 1, :], eb[:], ALU.add, [eb])
                        act(sm, sm[:, 1, :], sm[:, 1, :], AF.Ln, [sm])
                        ts("dve", sm, sm[:, 2, :], t4[:, :, 0, 0], -1.0, None, ALU.mult, None, [top16])
                        stt(sm, sm[:, 3, :], t4[:, :, 1, 0], -1.0, sm[:, 1, :], ALU.mult, ALU.subtract, [top16, sm])
                        tt("dve", nbias, nbias[:, tl, :], sm[:, 0, :], sm[:, 1, :], ALU.subtract, [sm])
                    units = [(ig, tl, h) for ig in range(8) for tl in range(2) for h in range(8)]

                    def cand_of(n):
                        ig, tl, h = units[n]
                        isl = slice(ig * 16, (ig + 1) * 16)
                        c_ = cf[n % 4]
                        tt("dve", c_, c_[:], s_sb[:, tl, 2 * h, isl].unsqueeze(2).to_broadcast([128, 16, 128]),
                           s_sb[:, tl, 2 * h + 1, :].unsqueeze(1).to_broadcast([128, 16, 128]), ALU.add, [s_sb])

                    def exp_of(n):
                        ig, tl, h = units[n]
                        act(pr[n % 4], pr[n % 4][:], cf[n % 4][:], AF.Exp, [cf[n % 4], nbias], bias=nbias[:, tl, h:h + 1])

                    def acc_of(n):
                        ig, tl, h = units[n]
                        W_ = Ws[ig % 2]
                        c_ = cf[n % 4]
                        p_ = pr[n % 4]
                        w_ = tw[n % 2]
                        if h == 0:
                            stt(W_, W_[:, tl], c_[:], tau[:, tl, h:h + 1], p_[:], ALU.is_ge, ALU.mult, [c_, p_, tau])
                        else:
                            stt(w_, w_[:], c_[:], tau[:, tl, h:h + 1], p_[:], ALU.is_ge, ALU.mult, [c_, p_, tau])
                            tt("dve", W_, W_[:, tl], W_[:, tl], w_[:], ALU.add, [W_, w_])

                    cand_of(0)
                    cand_of(1)
                    pend = None
                    grp = None
                    for s_ in range(64 + 8):
                        if s_ + 1 < 64:
                            cand_of(2 * s_ + 2)
                            cand_of(2 * s_ + 3)
                        if s_ < 64:
                            exp_of(2 * s_)
                            exp_of(2 * s_ + 1)
                            acc_of(2 * s_)
                            acc_of(2 * s_ + 1)
                        if s_ >= 8:
                            cpair = s_ - 8
                            i_a = 2 * cpair
                            ig = i_a // 16
                            il = i_a % 16
                            W_ = Ws[ig % 2]
                            if i_a % 4 == 0:
                                u_ = uch[cUl[0] % 2]
                                v_ = vch[cUl[0] % 2]
                                ws_ = wts[cUl[0] % 2]
                                cUl[0] += 1
                                kb.dma("sp", u_[:], ubv[:, :, i_a * 128:(i_a + 4) * 128], reads=[ub_b], writes=[u_.b])
                                kb.dma("sp", v_[:], vb[i_a * 128:(i_a + 4) * 128, :].rearrange("(a p) d -> p a d", p=128), reads=[vb_b],
                                       writes=[v_.b])
                                for k4 in range(4):
                                    for tl in range(2):
                                        tp(wtp, wtp[:, k4, tl * 128:(tl + 1) * 128], W_[:, tl, il + k4, :], ident_b[:], [W_, ident_b])
                                cp("act", ws_, ws_[:], wtp[:], [wtp])
                                grp = (u_, v_, ws_)
                            u_, v_, ws_ = grp
                            for i in (i_a, i_a + 1):
                                ii = i % 4
                                a_ = apsl[i % 2]
                                for k in range(8):
                                    mm(a_, a_[:, 0:256], u_[:, k, ii * 128:(ii + 1) * 128], hb[:, k, :], [u_, hb], start=(k == 0), stop=(k == 7))
                            if pend is not None:
                                pend()
                            for i in (i_a, i_a + 1):
                                act(gel[i % 2], gel[i % 2][:], apsl[i % 2][:, 0:256], AF.Gelu, [apsl[i % 2]])
                            for i in (i_a, i_a + 1):
                                tt("dve", cT[i % 4], cT[i % 4][:], gel[i % 2][:], ws_[:, i % 4, :], ALU.mult, [gel[i % 2], ws_])

                            def mkv(i_a=i_a, v_=v_):
                                def f():
                                    for i in (i_a, i_a + 1):
                                        c2 = cT[i % 4]
                                        for oc in range(8):
                                            y_ = yps[oc // 2]
                                            mm(y_, y_[:, (oc % 2) * 256:(oc % 2) * 256 + 256], v_[:, i % 4, oc * 128:(oc + 1) * 128], c2[:],
                                               [v_, c2], start=(i == 0 and oc % 2 == 0), stop=(i == 127))
                                return f
                            pend = mkv()
                    pend()
                    for oc in range(8):
                        y_ = yps[oc // 2]
                        stt(xb, xb[:, oc, :], y_[:, (oc % 2) * 256:(oc % 2) * 256 + 256], modT[:, 40 + oc, j:j + 1], xb[:, oc, :],
                            ALU.mult, ALU.add, [y_, modT, xb])
                    kb.dma("sp", xa_view(c0, 256), xb[:], reads=[xb.b], writes=tr(xa_b, c0, 256))
                    ck("D0")
            kb.barrier()
            if stop == "D":
                break
          except StopBuild:
            break

        if stop is None:
            with contextlib.ExitStack() as es:
                fn = sbuf(es, "f_fn", [128, 8], F32)
                xb_ = [sbuf(es, "f_xb%d" % i, [128, 8, 512], F32) for i in range(2)]
                sqk = [sbuf(es, "f_sq%d" % i, [128, 512], F32) for i in range(2)]
                xn = sbuf(es, "f_xn", [128, 8, 512], F32)
                rstd = sbuf(es, "f_rstd", [128, 512], F32)
                yo = [sbuf(es, "f_yo%d" % i, [128, D], F32) for i in range(2)]
                ssp = psum(es, "f_ssp", [128, 512])
                pp = [psum(es, "f_pp%d" % i, [128, 1024]) for i in range(2)]
                kb.dma("sp", fn[:], fnormT[:, :], writes=[fn.b])
                for bi in range(8):
                    c0 = bi * 512
                    xb = xb_[bi % 2]
                    kb.dma("sp", xb[:], xa_view(c0, 512), reads=tr(xa_b, c0, 512), writes=[xb.b])
                    for k in range(8):
                        q_ = sqk[k % 2]
                        act(q_, q_[:], xb[:, k, :], AF.Square, [xb])
                        mm(ssp, ssp[:], ones_f[:], q_[:], [ones_f, q_], start=(k == 0), stop=(k == 7))
                    rstd_from_ss(rstd, rstd[:], ssp[:], float(D), [ssp])
                    for k in range(8):
                        stt(xn, xn[:, k, :], xb[:, k, :], fn[:, k:k + 1], rstd[:], ALU.mult, ALU.mult, [xb, fn, rstd])
                    for ti in range(4):
                        t = bi * 4 + ti
                        p = pp[t % 2]
                        o = yo[t % 2]
                        for k in range(8):
                            tp(p, p[:, k * 128:(k + 1) * 128], xn[:, k, ti * 128:(ti + 1) * 128], ident_f[:], [xn, ident_f])
                        cp("act" if t % 2 else "dve", o, o[:], p[:], [p])
                        kb.dma("sp", out[t * 128:(t + 1) * 128, :], o[:], reads=[o.b])

        kb.dead = False
        kb.barrier()
    return nc


def rope_tables():
    pos = np.arange(NLAT)
    rows = (pos // 64).astype(np.float32)
    cols = (pos % 64).astype(np.float32)

    def tab(dh):
        inv = np.power(np.float32(10000.0), -np.arange(0, dh, 2, dtype=np.float32) / np.float32(dh)).astype(np.float32)
        out = np.zeros((NT, 2, 2, dh // 2), np.float32)
        out[:, 0] = 1.0
        for a, p_ in enumerate((rows, cols)):
            ang = (p_[:, None] * inv[None, :]).astype(np.float32)
            out[:NLAT, 0, a] = np.cos(ang)
            out[:NLAT, 1, a] = np.sin(ang)
        return out.reshape(NT, -1)

    return tab(16), tab(32)


def na_bias_tables(rpb):
    Lr = rpb.shape[0]
    out = np.full((Lr, 5, 128, 4, 5, 128), NEG, np.float32)
    for case, m in enumerate((0, 1, 2, 30, 31)):
        k0 = min(max(m - 2, 0), 27)
        q = m * 128 + np.arange(128)
        qr, qc = q // 64, q % 64
        rs = np.clip(qr - 4, 0, 56)
        cs = np.clip(qc - 8, 0, 48)
        for i in range(5):
            key = (k0 + i) * 128 + np.arange(128)
            kr, kc_ = key // 64, key % 64
            inw = ((kr[:, None] >= rs[None, :]) & (kr[:, None] < rs[None, :] + 8)
                   & (kc_[:, None] >= cs[None, :]) & (kc_[:, None] < cs[None, :] + 16))
            ro = np.clip(kr[:, None] - qr[None, :] + 7, 0, 14)
            co = np.clip(kc_[:, None] - qc[None, :] + 15, 0, 30)
            for h in range(4):
                g = rpb[:, h][:, ro, co]
                out[:, case, :, h, i, :] = np.where(inw[None], g, np.float32(NEG))
    return out


def prep_inputs(inp):
    f = lambda a: np.ascontiguousarray(np.asarray(a, dtype=np.float32))
    L = DEPTH
    r32, r64 = rope_tables()
    shared = {
        "w_mod": f(inp["w_mod"]),
        "b_modT": f(inp["b_mod"].reshape(L, 48, 128).transpose(0, 2, 1)),
        "nmixT": f(inp["norm_mix"].reshape(L, 8, 128).transpose(0, 2, 1)),
        "nffnT": f(inp["norm_ffn"].reshape(L, 8, 128).transpose(0, 2, 1)),
        "fnormT": f(inp["final_norm"].reshape(8, 128).T),
        "w_in": f(inp["w_in"]),
        "g_mlaq": f(inp["mla_q_norm"]),
        "g_mlakv": f(inp["mla_kv_norm"]),
        "w_uq": f(inp["mla_w_uq"]),
        "w_ukv": f(inp["mla_w_ukv"]),
        "nab": f(na_bias_tables(np.asarray(inp["na_rpb"], np.float32))),
        "lamv": f(np.stack([inp["diff_lam_q1"], inp["diff_lam_k1"], inp["diff_lam_q2"], inp["diff_lam_k2"]], axis=1)),
        "g_subln": f(inp["diff_subln"]),
        "g_gq": f(inp["gqa_q_norm"]),
        "g_gk": f(inp["gqa_k_norm"]),
        "w_branch": f(inp["w_branch"]),
        "w_gate": f(inp["w_gate"]),
        "b_gateT": f(inp["b_gate"].reshape(L, 4, 8, 128).transpose(0, 3, 1, 2)),
        "w_out": f(inp["w_out"]),
        "w_pq": f(inp["peer_w_q"]),
        "skT": f(inp["peer_subkeys"].reshape(L, 16, 128, 128).transpose(0, 3, 1, 2)),
        "uT": f(np.asarray(inp["peer_u"]).transpose(0, 2, 1)),
        "pv": f(inp["peer_v"]),
        "rope32": f(r32),
        "rope64": f(r64),
    }
    maps = []
    for b in range(8):
        m = dict(shared)
        m["xin"] = f(np.concatenate([inp["x"][b], inp["ctx"][b]], axis=0))
        cc = np.stack([inp["c"][b], inp["c_ctx"]], axis=0).reshape(2, 8, 128).transpose(2, 1, 0).reshape(128, 16)
        m["cc"] = f(cc)
        maps.append(m)
    return maps


def kernel(**inputs):
    maps = prep_inputs(inputs)
    nc = build()
    res = run_bass_kernel_spmd(nc, maps, core_ids=list(range(8)))
    return np.stack([np.asarray(r["out"], dtype=np.float32) for r in res.results], axis=0)
```

```python
import math
import contextlib
import numpy as np
import concourse.bass as bass
import concourse.mybir as mybir
from concourse.bass_utils import run_bass_kernel_spmd

F32 = mybir.dt.float32
BF16 = mybir.dt.bfloat16
AF = mybir.ActivationFunctionType
ALU = mybir.AluOpType
AX = mybir.AxisListType

D = 1024
KC = 8
NLAT = 4096
NCTX = 256
NT = NLAT + NCTX
NTILE = NT // 128
DEPTH = 4
EPS = 1e-6
NEG = -30000.0
BLOCKS = [(i * 512, 512, 0) for i in range(8)] + [(4096, 256, 1)]
PBLOCKS = [(i * 256, 256, 0) for i in range(16)] + [(4096, 256, 1)]


class Ev:
    __slots__ = ("key", "val", "clk")

    def __init__(s, key, val, clk):
        s.key = key
        s.val = val
        s.clk = clk


class TB:
    __slots__ = ("name", "w", "r", "excl")

    def __init__(s, name="", excl=False):
        s.name = name
        s.w = None
        s.r = {}
        s.excl = excl


class KB:
    NS = {"sp": 24, "pool": 40, "act": 2}

    def __init__(s, nc, es):
        s.nc = nc
        s.eng = {"pe": nc.tensor, "act": nc.scalar, "dve": nc.vector, "pool": nc.gpsimd, "sp": nc.sync}
        s.semobj = {}
        s.ccnt = {}
        for e in ["pe", "act", "dve", "pool"]:
            s.semobj[e] = es.enter_context(nc.semaphore("cs_" + e))
            s.ccnt[e] = 0
        s.known = {e: {} for e in s.eng}
        s.dcnt = {}
        for q, n in s.NS.items():
            s.dcnt[q] = 0
            for j in range(n):
                s.semobj[(q, j)] = es.enter_context(nc.semaphore("ds_%s%d" % (q, j)))
        s.last_dma = {}
        s.ninstr = 0
        s.dead = False

    def _wait(s, e, deps):
        k = s.known[e]
        changed = False
        for ev in deps:
            if ev is None:
                continue
            if k.get(ev.key, 0) >= ev.val:
                continue
            s.eng[e].wait_ge(s.semobj[ev.key], ev.val)
            if not changed:
                k = dict(k)
                changed = True
            for kk, vv in ev.clk.items():
                if k.get(kk, 0) < vv:
                    k[kk] = vv
            k[ev.key] = ev.val
        if changed:
            s.known[e] = k

    def _deps(s, reads, writes):
        deps = []
        for b in reads:
            if b.w is not None:
                deps.append(b.w)
        for b in writes:
            if b.w is not None:
                deps.append(b.w)
            deps.extend(b.r.values())
        return deps

    def _upd(s, ev, reads, writes):
        for b in reads:
            o = b.r.get(ev.key)
            if o is None or o.val < ev.val:
                b.r[ev.key] = ev
        for b in writes:
            b.w = ev
            b.r = {}

    def op(s, e, fn, reads=(), writes=()):
        if s.dead:
            return None
        ex = [b for b in reads if b.excl]
        if ex:
            reads = [b for b in reads if not b.excl]
            writes = list(writes) + ex
        deps = s._deps(reads, writes)
        if e == "pe":
            deps = [d for d in deps if d.key != "pe"]
        s._wait(e, deps)
        ins = fn()
        s.ccnt[e] += 1
        ins.then_inc(s.semobj[e], 1)
        ev = Ev(e, s.ccnt[e], s.known[e])
        s._upd(ev, reads, writes)
        s.ninstr += 1
        return ev

    def dma(s, q, out, in_, reads=(), writes=(), in_barrier=True):
        if s.dead:
            return None
        i = s.dcnt[q]
        n = s.NS[q]
        j = i % n
        key = (q, j)
        prev = 16 * (i // n)
        val = prev + 16
        deps = s._deps(reads, writes)
        if prev > 0:
            deps.append(Ev(key, prev, {}))
        s._wait(q, deps)
        ins = s.eng[q].dma_start(out=out, in_=in_)
        ins.then_inc(s.semobj[key], 16)
        s.dcnt[q] += 1
        ev = Ev(key, val, s.known[q])
        if in_barrier:
            s.last_dma[key] = ev
        elif key in s.last_dma:
            del s.last_dma[key]
        s._upd(ev, reads, writes)
        s.ninstr += 1
        return ev

    def barrier(s, engines=("pe", "act", "dve", "pool", "sp")):
        if s.dead:
            return
        evs = [Ev(e, s.ccnt[e], {}) for e in ["pe", "act", "dve", "pool"] if s.ccnt[e] > 0]
        evs += list(s.last_dma.values())
        for e in engines:
            s._wait(e, evs)


class StopBuild(Exception):
    pass


class T:
    def __init__(s, t, name, excl=False):
        s.t = t
        s.b = TB(name, excl)

    def __getitem__(s, k):
        return s.t[k]


def build(NL=DEPTH, stop=None, dbg=()):
    nc = bass.Bass("TRN2", target_bir_lowering=False)

    def din(name, shape, dt=F32):
        return nc.dram_tensor(name, list(shape), dt, kind="ExternalInput").ap()

    def dscr(name, shape, dt):
        kind = "ExternalOutput" if name in dbg else "Internal"
        return nc.dram_tensor(name, list(shape), dt, kind=kind).ap()

    L = DEPTH
    xin = din("xin", [NT, D])
    cc = din("cc", [128, 16])
    w_mod = din("w_mod", [L, D, 6 * D])
    b_modT = din("b_modT", [L, 128, 48])
    nmixT = din("nmixT", [L, 128, 8])
    nffnT = din("nffnT", [L, 128, 8])
    fnormT = din("fnormT", [128, 8])
    w_in = din("w_in", [L, D, 2464])
    g_mlaq = din("g_mlaq", [L, 256])
    g_mlakv = din("g_mlakv", [L, 128])
    w_uq = din("w_uq", [L, 256, 384])
    w_ukv = din("w_ukv", [L, 128, 512])
    nab = din("nab", [L, 5, 128, 4, 5, 128])
    lamv = din("lamv", [L, 4, 32])
    g_subln = din("g_subln", [L, 64])
    g_gq = din("g_gq", [L, 64])
    g_gk = din("g_gk", [L, 64])
    w_branch = din("w_branch", [L, 4, 256, D])
    w_gate = din("w_gate", [L, 4, D, D])
    b_gateT = din("b_gateT", [L, 128, 4, 8])
    w_out = din("w_out", [L, D, D])
    w_pq = din("w_pq", [L, D, 2048])
    skT = din("skT", [L, 128, 16, 128])
    uT = din("uT", [L, D, 16384])
    pv = din("pv", [L, 16384, D])
    rope32 = din("rope32", [NT, 32])
    rope64 = din("rope64", [NT, 64])
    out = nc.dram_tensor("out", [NLAT, D], F32, kind="ExternalOutput").ap()

    xa = dscr("xa", [D, NT], F32)
    hta = dscr("hta", [D, NT], BF16)
    ota = dscr("ota", [D, NT], BF16)
    qt_mla = dscr("qt_mla", [4 * 96, NT], BF16)
    kt_mla = dscr("kt_mla", [4 * 96, NT], BF16)
    qt_na = dscr("qt_na", [256, NT], BF16)
    kt_na = dscr("kt_na", [256, NT], BF16)
    qt_df = dscr("qt_df", [256, NT], BF16)
    kt_df = dscr("kt_df", [256, NT], BF16)
    qt_gq = dscr("qt_gq", [256, NT], BF16)
    kt_gq = dscr("kt_gq", [128, NT], BF16)
    v1a = dscr("v1a", [NT, 14 * 65], BF16)
    ub2 = [dscr("ub%d" % i, [D, 16384], BF16) for i in range(2)]
    vb2 = [dscr("vb%d" % i, [16384, D], BF16) for i in range(2)]

    def tiles_tb(name):
        return [TB("%s%d" % (name, i)) for i in range(NTILE)]

    xa_b = tiles_tb("xa")
    hta_b = tiles_tb("hta")
    ota_b = tiles_tb("ota")
    qk_b = tiles_tb("qk")
    ub_b2 = [TB("ub0"), TB("ub1")]
    vb_b2 = [TB("vb0"), TB("vb1")]

    def tr(bl, start, n):
        return bl[start // 128:(start + n) // 128]

    es_top = contextlib.ExitStack()
    with es_top:
        kb = KB(nc, es_top)

        uid = [0]

        def sbuf(es, name, shape, dt):
            uid[0] += 1
            name = "%s_%d" % (name, uid[0])
            return T(es.enter_context(nc.sbuf_tensor(name, list(shape), dt)), name)

        def psum(es, name, shape, dt=F32):
            uid[0] += 1
            name = "%s_%d" % (name, uid[0])
            return T(es.enter_context(nc.psum_tensor(name, list(shape), dt)), name, True)

        def mm(o, oap, lhsT, rhs, reads, start=True, stop=True):
            kb.op("pe", lambda: nc.tensor.matmul(oap, lhsT=lhsT, rhs=rhs, start=start, stop=stop),
                  reads=[r.b for r in reads], writes=[o.b])

        def tp(o, oap, iap, ident, reads):
            kb.op("pe", lambda: nc.tensor.transpose(oap, iap, ident), reads=[r.b for r in reads], writes=[o.b])

        def act(o, oap, iap, func, reads, bias=None, scale=None):
            kw = {}
            if bias is not None:
                kw["bias"] = bias
            if scale is not None:
                kw["scale"] = scale
            kb.op("act", lambda: nc.scalar.activation(out=oap, in_=iap, func=func, **kw),
                  reads=[r.b for r in reads], writes=[o.b])

        def tt(e, o, oap, a, b, op, reads):
            en = nc.vector if e == "dve" else nc.gpsimd
            kb.op(e, lambda: en.tensor_tensor(out=oap, in0=a, in1=b, op=op), reads=[r.b for r in reads], writes=[o.b])

        def ts(e, o, oap, a, s1, s2, op0, op1, reads):
            en = nc.vector if e == "dve" else nc.gpsimd
            if op1 is None:
                kb.op(e, lambda: en.tensor_scalar(out=oap, in0=a, scalar1=s1, scalar2=None, op0=op0),
                      reads=[r.b for r in reads], writes=[o.b])
            else:
                kb.op(e, lambda: en.tensor_scalar(out=oap, in0=a, scalar1=s1, scalar2=s2, op0=op0, op1=op1),
                      reads=[r.b for r in reads], writes=[o.b])

        def stt(o, oap, a, sc, b, op0, op1, reads):
            kb.op("dve", lambda: nc.vector.scalar_tensor_tensor(out=oap, in0=a, scalar=sc, in1=b, op0=op0, op1=op1),
                  reads=[r.b for r in reads], writes=[o.b])

        def cp(e, o, oap, iap, reads):
            if e == "act":
                act(o, oap, iap, AF.Copy, reads)
            else:
                en = nc.vector if e == "dve" else nc.gpsimd
                kb.op(e, lambda: en.tensor_copy(out=oap, in_=iap), reads=[r.b for r in reads], writes=[o.b])

        def red(o, oap, iap, op, reads):
            kb.op("dve", lambda: nc.vector.tensor_reduce(out=oap, in_=iap, axis=AX.X, op=op),
                  reads=[r.b for r in reads], writes=[o.b])

        def recip(o, oap, iap, reads):
            kb.op("dve", lambda: nc.vector.reciprocal(out=oap, in_=iap), reads=[r.b for r in reads], writes=[o.b])

        def mset(e, o, oap, val):
            en = nc.vector if e == "dve" else nc.gpsimd
            kb.op(e, lambda: en.memset(oap, val), writes=[o.b])

        def ck(name):
            if stop == name:
                kb.dead = True

        def rstd_from_ss(o, oap, ssap, n, reads):
            ts("dve", o, oap, ssap, 1.0 / n, EPS, ALU.mult, ALU.add, reads)
            act(o, oap, oap, AF.Sqrt, [o])
            recip(o, oap, oap, [o])

        ident_f = sbuf(es_top, "ident_f", [128, 128], F32)
        ident_b = sbuf(es_top, "ident_b", [128, 128], BF16)
        ones_f = sbuf(es_top, "ones_f", [128, 128], F32)
        modT = sbuf(es_top, "modT", [128, 48, 2], F32)
        A1 = sbuf(es_top, "A1", [128, 8, 2], F32)
        A2 = sbuf(es_top, "A2", [128, 8, 2], F32)
        mset("pool", ident_f, ident_f[:], 1.0)
        kb.op("pool", lambda: nc.gpsimd.affine_select(out=ident_f[:], in_=ident_f[:], pattern=[[-1, 128]],
                                                      compare_op=ALU.is_equal, fill=0.0, base=0,
                                                      channel_multiplier=1),
              reads=[ident_f.b], writes=[ident_f.b])
        cp("dve", ident_b, ident_b[:], ident_f[:], [ident_f])
        mset("pool", ones_f, ones_f[:], 1.0)

        def convert_uv(lc):
            sset = lc % 2
            for k in range(8):
                for hf in range(2):
                    kb.dma("pool", ub2[sset][k * 128:(k + 1) * 128, hf * 8192:(hf + 1) * 8192],
                           uT[lc, k * 128:(k + 1) * 128, hf * 8192:(hf + 1) * 8192], writes=[ub_b2[sset]], in_barrier=False)
            src = pv[lc].rearrange("(g p r) d -> g p (r d)", p=128, r=8)
            dst = vb2[sset].rearrange("(g p r) d -> g p (r d)", p=128, r=8)
            for g in range(16):
                kb.dma("pool", dst[g], src[g], writes=[vb_b2[sset]], in_barrier=False)

        def xa_view(c0, n):
            return xa.rearrange("(k p) n -> p k n", p=128)[:, :, c0:c0 + n]

        def hta_view(c0, n):
            return hta.rearrange("(k p) n -> p k n", p=128)[:, :, c0:c0 + n]

        def ota_view(c0, n):
            return ota.rearrange("(k p) n -> p k n", p=128)[:, :, c0:c0 + n]

        if NL > 0:
            convert_uv(0)
        with contextlib.ExitStack() as es:
            xt = [sbuf(es, "i_xt%d" % i, [128, D], F32) for i in range(2)]
            xo = [sbuf(es, "i_xo%d" % i, [128, 8, 128], F32) for i in range(2)]
            pp = [psum(es, "i_pp%d" % i, [128, 1024], F32) for i in range(2)]
            for t in range(NTILE):
                a = xt[t % 2]
                o = xo[t % 2]
                p = pp[t % 2]
                kb.dma("sp", a[:], xin[t * 128:(t + 1) * 128, :], writes=[a.b])
                for k in range(8):
                    tp(p, p[:, k * 128:(k + 1) * 128], a[:, k * 128:(k + 1) * 128], ident_f[:], [a, ident_f])
                cp("act" if t % 2 else "dve", o, o[:].rearrange("p k n -> p (k n)"), p[:], [p])
                kb.dma("pool", xa_view(t * 128, 128), o[:], reads=[o.b], writes=[xa_b[t]])
        kb.barrier()
        if stop == "I":
            NL = 0

        def norm_mod(xb, hb, sqk, rstd, tmpk, ssp, Tn, Acol, Bcol):
            for k in range(8):
                q_ = sqk[k % 2]
                act(q_, q_[:, :Tn], xb[:, k, :Tn], AF.Square, [xb])
                mm(ssp, ssp[:, :Tn], ones_f[:], q_[:, :Tn], [ones_f, q_], start=(k == 0), stop=(k == 7))
            rstd_from_ss(rstd, rstd[:, :Tn], ssp[:, :Tn], float(D), [ssp])
            for k in range(8):
                t_ = tmpk[k % 2]
                tt("dve", t_, t_[:, :Tn], xb[:, k, :Tn], rstd[:, :Tn], ALU.mult, [xb, rstd])
                act(hb, hb[:, k, :Tn], t_[:, :Tn], AF.Identity, [t_, modT, A1, A2], bias=Bcol(k), scale=Acol(k))

        for l in range(NL):
          try:
            lam_init = 0.8 - 0.6 * math.exp(-0.3 * l)
            with contextlib.ExitStack() as es:
                cct = sbuf(es, "m_cc", [128, 16], F32)
                sc = sbuf(es, "m_sc", [128, 16], F32)
                wm = [sbuf(es, "m_w%d" % i, [128, 8, 768], F32) for i in range(2)]
                bm = sbuf(es, "m_b", [128, 48], F32)
                nm = sbuf(es, "m_nm", [128, 8], F32)
                nf = sbuf(es, "m_nf", [128, 8], F32)
                pm = psum(es, "m_p", [128, 96], F32)
                kb.dma("sp", cct[:], cc[:, :], writes=[cct.b])
                kb.dma("sp", bm[:], b_modT[l], writes=[bm.b])
                kb.dma("sp", nm[:], nmixT[l], writes=[nm.b])
                kb.dma("sp", nf[:], nffnT[l], writes=[nf.b])
                act(sc, sc[:], cct[:], AF.Silu, [cct])
                for blk in range(8):
                    w = wm[blk % 2]
                    for k in range(8):
                        kb.dma("sp", w[:, k, :], w_mod[l, k * 128:(k + 1) * 128, blk * 768:(blk + 1) * 768], writes=[w.b])
                    for cl in range(6):
                        c = blk * 6 + cl
                        for k in range(8):
                            mm(pm, pm[:, c * 2:c * 2 + 2], w[:, k, cl * 128:(cl + 1) * 128], sc[:, k * 2:k * 2 + 2], [w, sc],
                               start=(k == 0), stop=(k == 7))
                tt("dve", modT, modT[:], pm[:].rearrange("p (c j) -> p c j", j=2),
                   bm[:].unsqueeze(2).to_broadcast([128, 48, 2]), ALU.add, [pm, bm])
                stt(A1, A1[:], modT[:, 8:16, :], 1.0, nm[:].unsqueeze(2).to_broadcast([128, 8, 2]), ALU.add, ALU.mult, [modT, nm])
                stt(A2, A2[:], modT[:, 32:40, :], 1.0, nf[:].unsqueeze(2).to_broadcast([128, 8, 2]), ALU.add, ALU.mult, [modT, nf])
            kb.barrier()
            if stop == "mod":
                break

            with contextlib.ExitStack() as es:
                win = sbuf(es, "a_win", [128, 8, 2464], BF16)
                wuq = sbuf(es, "a_wuq", [128, 2, 384], BF16)
                wukv = sbuf(es, "a_wukv", [128, 512], BF16)
                gq = sbuf(es, "a_gq", [128, 256], F32)
                gkv = sbuf(es, "a_gkv", [128, 128], F32)
                ggq = sbuf(es, "a_ggq", [128, 6, 64], F32)
                r32 = sbuf(es, "a_r32", [128, NTILE, 32], F32)
                r64 = sbuf(es, "a_r64", [128, NTILE, 64], F32)
                xb_ = [sbuf(es, "a_xb%d" % i, [128, 8, 512], F32) for i in range(2)]
                hb_ = [sbuf(es, "a_hb%d" % i, [128, 8, 512], BF16) for i in range(2)]
                sq = [sbuf(es, "a_sq%d" % i, [128, 512], F32) for i in range(2)]
                tmpn = [sbuf(es, "a_tmpn%d" % i, [128, 512], F32) for i in range(2)]
                rstd = sbuf(es, "a_rstd", [128, 512], F32)
                sqs_2 = [sbuf(es, "a_sqs%d" % i_, [128, 384], F32) for i_ in range(2)]
                st_2 = [sbuf(es, "a_st%d" % i_, [128, 8], F32) for i_ in range(2)]
                cn_2 = [sbuf(es, "a_cn%d" % i_, [128, 384], BF16) for i_ in range(2)]
                cnT_2 = [sbuf(es, "a_cnT%d" % i_, [128, 3, 128], BF16) for i_ in range(2)]
                qf_2 = [sbuf(es, "a_qf%d" % i_, [128, 4, 96], BF16) for i_ in range(2)]
                kf_2 = [sbuf(es, "a_kf%d" % i_, [128, 4, 96], BF16) for i_ in range(2)]
                krr_2 = [sbuf(es, "a_krr%d" % i_, [128, 32], F32) for i_ in range(2)]
                ra_2 = [sbuf(es, "a_ra%d" % i_, [128, 512], F32) for i_ in range(2)]
                rb_2 = [sbuf(es, "a_rb%d" % i_, [128, 512], F32) for i_ in range(2)]
                qkb_2 = [sbuf(es, "a_qkb%d" % i_, [128, 512], BF16) for i_ in range(2)]
                gtmp_2 = [sbuf(es, "a_gtmp%d" % i_, [128, 384], F32) for i_ in range(2)]
                gtmp2_2 = [sbuf(es, "a_gtmp2%d" % i_, [128, 384], F32) for i_ in range(2)]
                v1 = [sbuf(es, "a_v1_%d" % i, [128, 14, 65], BF16) for i in range(2)]
                tq = [sbuf(es, "a_tq%d" % i, [128, 4, 128], BF16) for i in range(4)]
                ssp = psum(es, "a_ssp", [128, 512], F32)
                pA = psum(es, "a_pA", [128, 512], F32)
                pB = psum(es, "a_pB", [128, 1024], F32)
                pC = psum(es, "a_pC", [128, 512], F32)
                pU1 = psum(es, "a_pU1", [128, 512], F32)
                pU2 = psum(es, "a_pU2", [128, 512], F32)
                pT = psum(es, "a_pT", [128, 8, 128], BF16)

                for k in range(8):
                    kb.dma("pool", win[:, k, :], w_in[l, k * 128:(k + 1) * 128, :], writes=[win.b])
                for k in range(2):
                    kb.dma("pool", wuq[:, k, :], w_uq[l, k * 128:(k + 1) * 128, :], writes=[wuq.b])
                kb.dma("pool", wukv[:], w_ukv[l], writes=[wukv.b])
                kb.dma("sp", gq[:], g_mlaq[l].partition_broadcast(128), writes=[gq.b])
                kb.dma("sp", gkv[:], g_mlakv[l].partition_broadcast(128), writes=[gkv.b])
                for h in range(4):
                    kb.dma("sp", ggq[:, h, :], g_gq[l].partition_broadcast(128), writes=[ggq.b])
                for h in range(2):
                    kb.dma("sp", ggq[:, 4 + h, :], g_gk[l].partition_broadcast(128), writes=[ggq.b])
                kb.dma("sp", r32[:], rope32.rearrange("(t p) c -> p t c", p=128), writes=[r32.b])
                kb.dma("sp", r64[:], rope64.rearrange("(t p) c -> p t c", p=128), writes=[r64.b])
                for i in range(2):
                    mset("pool", v1[i], v1[i][:], 1.0)

                tqi = [0]

                def next_tq():
                    tqi[0] += 1
                    return tq[tqi[0] % 4]

                def rope(src5, dst5, rt, t, G, Fq, reads, dstT):
                    ra = ra_2[t % 2]
                    rb = rb_2[t % 2]
                    tab = rt[:, t, :].rearrange("p (a b f) -> p a b f", a=2, b=2)
                    C = tab[:, 0].unsqueeze(1).to_broadcast([128, G, 2, Fq])
                    S = tab[:, 1].unsqueeze(1).to_broadcast([128, G, 2, Fq])
                    n = G * 2 * Fq
                    rav = ra[:, 0:n].rearrange("p (g b f) -> p g b f", g=G, b=2)
                    rbv = rb[:, 0:n].rearrange("p (g b f) -> p g b f", g=G, b=2)
                    t1 = src5[:, :, :, 0, :]
                    t2 = src5[:, :, :, 1, :]
                    tt("dve", ra, rav, t1, C, ALU.mult, reads + [rt])
                    tt("dve", rb, rbv, t2, S, ALU.mult, reads + [rt])
                    tt("pool", dstT, dst5[:, :, :, 0, :], rav, rbv, ALU.subtract, [ra, rb])
                    tt("dve", ra, rav, t1, S, ALU.mult, reads + [rt])
                    tt("dve", rb, rbv, t2, C, ALU.mult, reads + [rt])
                    tt("pool", dstT, dst5[:, :, :, 1, :], rav, rbv, ALU.add, [ra, rb])

                def r5(ap, G, Fq):
                    if len(ap.shape) == 2:
                        return ap.rearrange("p (g a b f) -> p g a b f", g=G, a=2, b=2)
                    return ap.rearrange("p g (a b f) -> p g a b f", a=2, b=2)

                for bi, (c0, Tn, j) in enumerate(BLOCKS):
                    xb = xb_[bi % 2]
                    hb = hb_[bi % 2]
                    kb.dma("sp", xb[:, :, :Tn], xa_view(c0, Tn), reads=tr(xa_b, c0, Tn), writes=[xb.b])
                    norm_mod(xb, hb, sq, rstd, tmpn, ssp, Tn,
                             lambda k: A1[:, k, j:j + 1], lambda k: modT[:, 0 + k, j:j + 1])
                    kb.dma("pool", hta_view(c0, Tn), hb[:, :, :Tn], reads=[hb.b], writes=tr(hta_b, c0, Tn))
                    ck("A0")
                    for ti in range(Tn // 128):
                        t = c0 // 128 + ti
                        ts_ = slice(ti * 128, (ti + 1) * 128)
                        vv = v1[t % 2]
                        sqs, st, cn, cnT, qf, kf, krr, qkb, gtmp, gtmp2 = [x_[t % 2] for x_ in
                                                                          (sqs_2, st_2, cn_2, cnT_2, qf_2, kf_2, krr_2, qkb_2, gtmp_2, gtmp2_2)]
                        for k in range(8):
                            mm(pA, pA[:, 0:416], hb[:, k, ts_], win[:, k, 0:416], [hb, win], start=(k == 0), stop=(k == 7))
                        act(sqs, sqs[:, 0:384], pA[:, 0:384], AF.Square, [pA])
                        red(st, st[:, 0:1], sqs[:, 0:256], ALU.add, [sqs])
                        red(st, st[:, 1:2], sqs[:, 256:384], ALU.add, [sqs])
                        ts("dve", st, st[:, 0:1], st[:, 0:1], 1.0 / 256, EPS, ALU.mult, ALU.add, [st])
                        ts("dve", st, st[:, 1:2], st[:, 1:2], 1.0 / 128, EPS, ALU.mult, ALU.add, [st])
                        act(st, st[:, 0:2], st[:, 0:2], AF.Sqrt, [st])
                        recip(st, st[:, 0:2], st[:, 0:2], [st])
                        stt(cn, cn[:, 0:256], pA[:, 0:256], st[:, 0:1], gq[:], ALU.mult, ALU.mult, [pA, st, gq])
                        stt(cn, cn[:, 256:384], pA[:, 256:384], st[:, 1:2], gkv[:], ALU.mult, ALU.mult, [pA, st, gkv])
                        ck("A1a")
                        for k in range(3):
                            tp(pT, pT[:, k, :], cn[:, k * 128:(k + 1) * 128], ident_b[:], [cn, ident_b])
                        cp("act", cnT, cnT[:], pT[:, 0:3, :], [pT])
                        ck("A1b")
                        for k in range(2):
                            mm(pU1, pU1[:, 0:384], cnT[:, k, :], wuq[:, k, :], [cnT, wuq], start=(k == 0), stop=(k == 1))
                        mm(pU2, pU2[:, 0:512], cnT[:, 2, :], wukv[:], [cnT, wukv])
                        u1 = pU1[:, 0:384].rearrange("p (h d) -> p h d", h=4)
                        u2 = pU2[:, 0:512].rearrange("p (h d) -> p h d", h=4)
                        ck("A1c")
                        cp("act", qf, qf[:, :, 0:64], u1[:, :, 0:64], [pU1])
                        rope(r5(u1[:, :, 64:96], 4, 8), r5(qf[:, :, 64:96], 4, 8), r32, t, 4, 8, [pU1], qf)
                        ck("A1c1")
                        cp("act", kf, kf[:, :, 0:64], u2[:, :, 0:64], [pU2])
                        ck("A1c2")
                        rope(r5(pA[:, 384:416], 1, 8), r5(krr[:, :], 1, 8), r32, t, 1, 8, [pA], krr)
                        ck("A1c3")
                        cp("pool", kf, kf[:, :, 64:96], krr[:].unsqueeze(1).to_broadcast([128, 4, 32]), [krr])
                        ck("A1c4")
                        cp("dve", vv, vv[:, 0:4, 0:64], u2[:, :, 64:128], [pU2])
                        ck("A1d")
                        for h in range(4):
                            tp(pT, pT[0:96, h, :], qf[:, h, :], ident_b[:], [qf, ident_b])
                        for h in range(4):
                            tp(pT, pT[0:96, 4 + h, :], kf[:, h, :], ident_b[:], [kf, ident_b])
                        o1 = next_tq()
                        o2 = next_tq()
                        cp("act", o1, o1[0:96, :, :], pT[0:96, 0:4, :], [pT])
                        cp("dve", o2, o2[0:96, :, :], pT[0:96, 4:8, :], [pT])
                        cols = slice(t * 128, (t + 1) * 128)
                        ck("A1e")
                        kb.dma("pool", qt_mla.rearrange("(m p) n -> p m n", p=96)[:, :, cols], o1[0:96, :, :], reads=[o1.b], writes=[qk_b[t]])
                        kb.dma("pool", kt_mla.rearrange("(m p) n -> p m n", p=96)[:, :, cols], o2[0:96, :, :], reads=[o2.b], writes=[qk_b[t]])
                        ck("A1")
                        for k in range(8):
                            mm(pB, pB[:, 0:512], hb[:, k, ts_], win[:, k, 416:928], [hb, win], start=(k == 0), stop=(k == 7))
                        for k in range(8):
                            mm(pB, pB[:, 512:768], hb[:, k, ts_], win[:, k, 928:1184], [hb, win], start=(k == 0), stop=(k == 7))
                        cp("act", qkb, qkb[:], pB[:, 0:512], [pB])
                        cp("dve", vv, vv[:, 4:8, 0:64], pB[:, 512:768].rearrange("p (h d) -> p h d", h=4), [pB])
                        for k in range(4):
                            tp(pT, pT[:, k, :], qkb[:, k * 128:(k + 1) * 128], ident_b[:], [qkb, ident_b])
                        o1 = next_tq()
                        cp("act", o1, o1[:], pT[:, 0:4, :], [pT])
                        kb.dma("pool", qt_na.rearrange("(m p) n -> p m n", p=128)[:, :, cols], o1[:, 0:2, :], reads=[o1.b], writes=[qk_b[t]])
                        kb.dma("pool", kt_na.rearrange("(m p) n -> p m n", p=128)[:, :, cols], o1[:, 2:4, :], reads=[o1.b], writes=[qk_b[t]])
                        ck("A2")
                        for k in range(8):
                            mm(pB, pB[:, 0:512], hb[:, k, ts_], win[:, k, 1184:1696], [hb, win], start=(k == 0), stop=(k == 7))
                        for k in range(8):
                            mm(pB, pB[:, 512:768], hb[:, k, ts_], win[:, k, 1696:1952], [hb, win], start=(k == 0), stop=(k == 7))
                        for half in range(2):
                            rope(r5(pB[:, half * 256:(half + 1) * 256], 8, 8), r5(qkb[:, half * 256:(half + 1) * 256], 8, 8),
                                 r32, t, 8, 8, [pB], qkb)
                        cp("dve", vv, vv[:, 8:12, 0:64], pB[:, 512:768].rearrange("p (h d) -> p h d", h=4), [pB])
                        for k in range(4):
                            tp(pT, pT[:, k, :], qkb[:, k * 128:(k + 1) * 128], ident_b[:], [qkb, ident_b])
                        o1 = next_tq()
                        cp("act", o1, o1[:], pT[:, 0:4, :], [pT])
                        kb.dma("pool", qt_df.rearrange("(m p) n -> p m n", p=128)[:, :, cols], o1[:, 0:2, :], reads=[o1.b], writes=[qk_b[t]])
                        kb.dma("pool", kt_df.rearrange("(m p) n -> p m n", p=128)[:, :, cols], o1[:, 2:4, :], reads=[o1.b], writes=[qk_b[t]])
                        ck("A3")
                        for k in range(8):
                            mm(pC, pC[:, 0:512], hb[:, k, ts_], win[:, k, 1952:2464], [hb, win], start=(k == 0), stop=(k == 7))
                        act(sqs, sqs[:, 0:384], pC[:, 0:384], AF.Square, [pC])
                        red(st, st[:, 2:8], sqs[:, 0:384].rearrange("p (h d) -> p h d", h=6), ALU.add, [sqs])
                        rstd_from_ss(st, st[:, 2:8], st[:, 2:8], 64.0, [st])
                        g3 = gtmp[:, 0:384].rearrange("p (h d) -> p h d", h=6)
                        g32 = gtmp2[:, 0:384].rearrange("p (h d) -> p h d", h=6)
                        tt("dve", gtmp, g3, pC[:, 0:384].rearrange("p (h d) -> p h d", h=6),
                           st[:, 2:8].unsqueeze(2).to_broadcast([128, 6, 64]), ALU.mult, [pC, st])
                        tt("pool", gtmp2, g32, g3, ggq[:], ALU.mult, [gtmp, ggq])
                        rope(r5(gtmp2[:, 0:384], 6, 16), r5(qkb[:, 0:384], 6, 16), r64, t, 6, 16, [gtmp2], qkb)
                        cp("dve", vv, vv[:, 12:14, 0:64], pC[:, 384:512].rearrange("p (h d) -> p h d", h=2), [pC])
                        for k in range(3):
                            tp(pT, pT[:, k, :], qkb[:, k * 128:(k + 1) * 128], ident_b[:], [qkb, ident_b])
                        o1 = next_tq()
                        cp("act", o1, o1[:, 0:3, :], pT[:, 0:3, :], [pT])
                        kb.dma("pool", qt_gq.rearrange("(m p) n -> p m n", p=128)[:, :, cols], o1[:, 0:2, :], reads=[o1.b], writes=[qk_b[t]])
                        kb.dma("pool", kt_gq.rearrange("(m p) n -> p m n", p=128)[:, :, cols], o1[:, 2:3, :], reads=[o1.b], writes=[qk_b[t]])
                        kb.dma("pool", v1a[t * 128:(t + 1) * 128, :], vv[:].rearrange("p a b -> p (a b)"), reads=[vv.b], writes=[qk_b[t]])
            kb.barrier()
            if stop == "A":
                break
            cntB = [0]

            def attn_std(mixer, qt_d, kt_d, dk, nq, nk, kmap, vbase, nv, vmap, scale, diff=False):
                with contextlib.ExitStack() as es:
                    KT = sbuf(es, "b_KT", [128, nk, NT], BF16)
                    V1 = sbuf(es, "b_V1", [128, NTILE, nv, 65], BF16)
                    QT = [sbuf(es, "b_QT%d" % i, [128, nq, 512], BF16) for i in range(2)]
                    PT = [sbuf(es, "b_PT%d" % i, [128, 512], BF16) for i in range(3)]
                    osb = sbuf(es, "b_osb", [128, 4, nq, 64], F32)
                    osbb = sbuf(es, "b_osbb", [128, 4, 256], BF16)
                    rs = sbuf(es, "b_rs", [128, 4], F32)
                    otb = [sbuf(es, "b_otb%d" % i, [128, 2, 512], BF16) for i in range(2)]
                    stp = [psum(es, "b_st%d" % i, [128, 512]) for i in range(2)]
                    ops = [psum(es, "b_o%d" % i, [128, 512]) for i in range(4)]
                    tpp = psum(es, "b_tp", [128, 8, 128], BF16)
                    for m in range(nk):
                        kb.dma("sp", KT[0:dk, m, :], kt_d[m * dk:(m + 1) * dk, :], reads=qk_b, writes=[KT.b])
                    v4 = v1a.rearrange("(t p) (a b) -> p t a b", p=128, b=65)
                    for t0 in range(0, NTILE, 9):
                        t1 = min(NTILE, t0 + 9)
                        kb.dma("sp", V1[:, t0:t1, :, :], v4[:, t0:t1, vbase:vbase + nv, :], reads=qk_b, writes=[V1.b])
                    if diff:
                        lamt = sbuf(es, "b_lamt", [128, 4, 32], F32)
                        lp = sbuf(es, "b_lp", [128, 2, 32], F32)
                        ls = sbuf(es, "b_ls", [128, 2], F32)
                        neglam = sbuf(es, "b_neglam", [128, 1], F32)
                        gsub = sbuf(es, "b_gsub", [128, 64], F32)
                        dsb = sbuf(es, "b_dsb", [128, 4, 64], F32)
                        dsq = sbuf(es, "b_dsq", [128, 4, 64], F32)
                        dst = sbuf(es, "b_dst", [128, 4], F32)
                        for i in range(4):
                            kb.dma("sp", lamt[:, i, :], lamv[l, i].partition_broadcast(128), writes=[lamt.b])
                        kb.dma("sp", gsub[:], g_subln[l].partition_broadcast(128), writes=[gsub.b])
                        tt("dve", lp, lp[:, 0, :], lamt[:, 0, :], lamt[:, 1, :], ALU.mult, [lamt])
                        tt("dve", lp, lp[:, 1, :], lamt[:, 2, :], lamt[:, 3, :], ALU.mult, [lamt])
                        red(ls, ls[:, 0:2], lp[:], ALU.add, [lp])
                        act(ls, ls[:], ls[:], AF.Exp, [ls])
                        tt("dve", neglam, neglam[:, 0:1], ls[:, 1:2], ls[:, 0:1], ALU.subtract, [ls])
                        ts("dve", neglam, neglam[:], neglam[:], -lam_init, None, ALU.add, None, [neglam])
                        ts("dve", gsub, gsub[:], gsub[:], 1.0 - lam_init, None, ALU.mult, None, [gsub])
                    for bi, (c0, Tn, j) in enumerate(BLOCKS):
                        if j == 1 and l == DEPTH - 1:
                            continue
                        QTb = QT[bi % 2]
                        kb.dma("sp", QTb[0:dk, :, :Tn], qt_d.rearrange("(m p) n -> p m n", p=dk)[:, :, c0:c0 + Tn],
                               reads=qk_b, writes=[QTb.b])
                        kchunks = list(range(NTILE)) if j == 0 else [32, 33]
                        nqs = Tn // 128
                        for m in range(nq):
                            pend = None
                            for ci, kc in enumerate(kchunks):
                                sp_ = stp[cntB[0] % 2]
                                pt_ = PT[cntB[0] % 3]
                                cntB[0] += 1
                                mm(sp_, sp_[:, :Tn], KT[0:dk, kmap(m), kc * 128:(kc + 1) * 128], QTb[0:dk, m, :Tn], [KT, QTb])
                                if pend is not None:
                                    pend()
                                act(pt_, pt_[:, :Tn], sp_[:, :Tn], AF.Exp, [sp_], scale=scale)

                                def mk(ci=ci, kc=kc, pt_=pt_, m=m):
                                    def f():
                                        for qs in range(nqs):
                                            mm(ops[qs], ops[qs][:, 0:65], pt_[:, qs * 128:(qs + 1) * 128], V1[:, kc, vmap(m), :], [pt_, V1],
                                               start=(ci == 0), stop=(ci == len(kchunks) - 1))
                                    return f
                                pend = mk()
                            pend()
                            for qs in range(nqs):
                                recip(rs, rs[:, qs:qs + 1], ops[qs][:, 64:65], [ops[qs]])
                                if diff:
                                    ts("dve", osb, osb[:, qs, m, :], ops[qs][:, 0:64], rs[:, qs:qs + 1], None, ALU.mult, None, [ops[qs], rs])
                                else:
                                    ts("dve", osbb, osbb[:, qs, m * 64:(m + 1) * 64], ops[qs][:, 0:64], rs[:, qs:qs + 1], None, ALU.mult, None,
                                       [ops[qs], rs])
                        if diff:
                            for qs in range(nqs):
                                ov = osb[:, qs].rearrange("p (h i) d -> p h i d", i=2)
                                stt(dsb, dsb[:], ov[:, :, 1, :], neglam[:, 0:1], ov[:, :, 0, :], ALU.mult, ALU.add, [osb, neglam])
                                tt("pool", dsq, dsq[:], dsb[:], dsb[:], ALU.mult, [dsb])
                                red(dst, dst[:, 0:4], dsq[:], ALU.add, [dsq])
                                rstd_from_ss(dst, dst[:, 0:4], dst[:, 0:4], 64.0, [dst])
                                tt("dve", dsb, dsb[:], dsb[:], dst[:, 0:4].unsqueeze(2).to_broadcast([128, 4, 64]), ALU.mult, [dsb, dst])
                                tt("pool", osbb, osbb[:, qs, :].rearrange("p (h d) -> p h d", h=4), dsb[:],
                                   gsub[:].unsqueeze(1).to_broadcast([128, 4, 64]), ALU.mult, [dsb, gsub])
                        ot_ = otb[bi % 2]
                        for qs in range(nqs):
                            for c in range(2):
                                tp(tpp, tpp[:, qs * 2 + c, :], osbb[:, qs, c * 128:(c + 1) * 128], ident_b[:], [osbb, ident_b])
                        cp("act", ot_, ot_[:, :, :Tn].rearrange("p c (q n) -> p q c n", n=128),
                           tpp[:, 0:2 * nqs, :].rearrange("p (q c) n -> p q c n", c=2), [tpp])
                        kb.dma("pool", ota_view(c0, Tn)[:, 2 * mixer:2 * mixer + 2, :], ot_[:, :, :Tn], reads=[ot_.b],
                               writes=tr(ota_b, c0, Tn))
                kb.barrier()

            def attn_na():
                scale = 64.0 ** -0.5
                with contextlib.ExitStack() as es:
                    KT = sbuf(es, "n_KT", [128, 4, NT], BF16)
                    QT = sbuf(es, "n_QT", [128, 4, NT], BF16)
                    V1 = sbuf(es, "n_V1", [128, NTILE, 4, 65], BF16)
                    nb = [sbuf(es, "n_nb%d" % i, [128, 4, 5, 128], F32) for i in range(5)]
                    PT = [sbuf(es, "n_PT%d" % i, [128, 128], BF16) for i in range(3)]
                    tmpb = [sbuf(es, "n_tmp%d" % i, [128, 128], F32) for i in range(2)]
                    osbb = sbuf(es, "n_osbb", [128, 256], BF16)
                    rs = sbuf(es, "n_rs", [128, 1], F32)
                    otb = [sbuf(es, "n_otb%d" % i, [128, 2, 128], BF16) for i in range(2)]
                    stp = [psum(es, "n_st%d" % i, [128, 512]) for i in range(2)]
                    ops = [psum(es, "n_o%d" % i, [128, 512]) for i in range(2)]
                    tpp = psum(es, "n_tp", [128, 8, 128], BF16)
                    for m in range(4):
                        kb.dma("sp", KT[0:64, m, :], kt_na[m * 64:(m + 1) * 64, :], reads=qk_b, writes=[KT.b])
                        kb.dma("sp", QT[0:64, m, :], qt_na[m * 64:(m + 1) * 64, :], reads=qk_b, writes=[QT.b])
                    v4 = v1a.rearrange("(t p) (a b) -> p t a b", p=128, b=65)
                    for t0 in range(0, NTILE, 9):
                        t1 = min(NTILE, t0 + 9)
                        kb.dma("sp", V1[:, t0:t1, :, :], v4[:, t0:t1, 4:8, :], reads=qk_b, writes=[V1.b])
                    for cs in range(5):
                        kb.dma("sp", nb[cs][:].rearrange("p a b c -> p (a b c)"), nab[l, cs].rearrange("p a b c -> p (a b c)"),
                               writes=[nb[cs].b])
                    cnt = 0
                    for m in range(NTILE):
                        if m >= 32 and l == DEPTH - 1:
                            continue
                        if m < 32:
                            case = {0: 0, 1: 1, 30: 3, 31: 4}.get(m, 2)
                            k0 = min(max(m - 2, 0), 27)
                            chunks = [(k0 + i, i) for i in range(5)] + [(32, None), (33, None)]
                        else:
                            chunks = [(32, None), (33, None)]
                        for h in range(4):
                            o_ = ops[(m * 4 + h) % 2]
                            pend = None
                            for ci, (kc, li) in enumerate(chunks):
                                sp_ = stp[cnt % 2]
                                pt_ = PT[cnt % 3]
                                tb_ = tmpb[cnt % 2]
                                cnt += 1
                                mm(sp_, sp_[:, 0:128], KT[0:64, h, kc * 128:(kc + 1) * 128], QT[0:64, h, m * 128:(m + 1) * 128], [KT, QT])
                                if pend is not None:
                                    pend()
                                if li is not None:
                                    stt(tb_, tb_[:], sp_[:, 0:128], scale, nb[case][:, h, li, :], ALU.mult, ALU.add, [sp_, nb[case]])
                                    act(pt_, pt_[:], tb_[:], AF.Exp, [tb_])
                                else:
                                    act(pt_, pt_[:], sp_[:, 0:128], AF.Exp, [sp_], scale=scale)

                                def mkn(ci=ci, kc=kc, pt_=pt_, h=h, o_=o_, nch=len(chunks)):
                                    def f():
                                        mm(o_, o_[:, 0:65], pt_[:], V1[:, kc, h, :], [pt_, V1], start=(ci == 0), stop=(ci == nch - 1))
                                    return f
                                pend = mkn()
                            pend()
                            recip(rs, rs[:, 0:1], o_[:, 64:65], [o_])
                            ts("dve", osbb, osbb[:, h * 64:(h + 1) * 64], o_[:, 0:64], rs[:, 0:1], None, ALU.mult, None, [o_, rs])
                        ot_ = otb[m % 2]
                        for c in range(2):
                            tp(tpp, tpp[:, c, :], osbb[:, c * 128:(c + 1) * 128], ident_b[:], [osbb, ident_b])
                        cp("act", ot_, ot_[:], tpp[:, 0:2, :], [tpp])
                        kb.dma("pool", ota_view(m * 128, 128)[:, 2:4, :], ot_[:], reads=[ot_.b], writes=[ota_b[m]])
                kb.barrier()

            attn_std(0, qt_mla, kt_mla, 96, 4, 4, lambda m: m, 0, 4, lambda m: m, 96.0 ** -0.5)
            ck("B0")
            attn_na()
            ck("B1")
            attn_std(2, qt_df, kt_df, 32, 8, 8, lambda m: m, 8, 4, lambda m: m // 2, 32.0 ** -0.5, diff=True)
            ck("B2")
            attn_std(3, qt_gq, kt_gq, 64, 4, 2, lambda m: m // 2, 12, 2, lambda m: m // 2, 64.0 ** -0.5)
            if stop == "B":
                break
            with contextlib.ExitStack() as es:
                wg = sbuf(es, "c_wg", [128, 4, 8, D], BF16)
                wbr = sbuf(es, "c_wbr", [128, 4, 2, D], BF16)
                wo = sbuf(es, "c_wo", [128, 8, D], BF16)
                bg = sbuf(es, "c_bg", [128, 4, 8], F32)
                hb = sbuf(es, "c_hb", [128, 8, 512], BF16)
                ob = sbuf(es, "c_ob", [128, 8, 512], BF16)
                xb = sbuf(es, "c_xb", [128, 8, 512], F32)
                sig = [sbuf(es, "c_sig%d" % i, [128, 512], BF16) for i in range(2)]
                macc = [sbuf(es, "c_macc%d" % i, [128, 512], F32) for i in range(2)]
                tmpm = [sbuf(es, "c_tmpm%d" % i, [128, 512], F32) for i in range(2)]
                mrg = sbuf(es, "c_mrg", [128, 8, 512], BF16)
                pg = [psum(es, "c_pg%d" % i, [128, 512]) for i in range(2)]
                pb = [psum(es, "c_pb%d" % i, [128, 512]) for i in range(2)]
                py = [psum(es, "c_py%d" % i, [128, 512]) for i in range(2)]
                for i in range(4):
                    for k in range(8):
                        kb.dma("pool", wg[:, i, k, :], w_gate[l, i, k * 128:(k + 1) * 128, :], writes=[wg.b])
                    for k in range(2):
                        kb.dma("pool", wbr[:, i, k, :], w_branch[l, i, k * 128:(k + 1) * 128, :], writes=[wbr.b])
                for k in range(8):
                    kb.dma("pool", wo[:, k, :], w_out[l, k * 128:(k + 1) * 128, :], writes=[wo.b])
                kb.dma("sp", bg[:], b_gateT[l], writes=[bg.b])
                cntC = 0
                for bi, (c0, Tn, j) in enumerate(BLOCKS):
                    if j == 1 and l == DEPTH - 1:
                        continue
                    kb.dma("sp", hb[:, :, :Tn], hta_view(c0, Tn), reads=tr(hta_b, c0, Tn), writes=[hb.b])
                    kb.dma("sp", ob[:, :, :Tn], ota_view(c0, Tn), reads=tr(ota_b, c0, Tn), writes=[ob.b])
                    kb.dma("sp", xb[:, :, :Tn], xa_view(c0, Tn), reads=tr(xa_b, c0, Tn), writes=[xb.b])
                    for oc in range(8):
                        ocs = slice(oc * 128, (oc + 1) * 128)
                        ma = macc[oc % 2]
                        for i in range(4):
                            g_ = pg[cntC % 2]
                            b_ = pb[cntC % 2]
                            s_ = sig[cntC % 2]
                            t_ = tmpm[cntC % 2]
                            cntC += 1
                            for k in range(8):
                                mm(g_, g_[:, :Tn], wg[:, i, k, ocs], hb[:, k, :Tn], [wg, hb], start=(k == 0), stop=(k == 7))
                            act(s_, s_[:, :Tn], g_[:, :Tn], AF.Sigmoid, [g_, bg], bias=bg[:, i, oc:oc + 1])
                            for k in range(2):
                                mm(b_, b_[:, :Tn], wbr[:, i, k, ocs], ob[:, 2 * i + k, :Tn], [wbr, ob], start=(k == 0), stop=(k == 1))
                            if i == 0:
                                tt("dve", ma, ma[:, :Tn], b_[:, :Tn], s_[:, :Tn], ALU.mult, [b_, s_])
                            else:
                                tt("dve", t_, t_[:, :Tn], b_[:, :Tn], s_[:, :Tn], ALU.mult, [b_, s_])
                                if i < 3:
                                    tt("pool", ma, ma[:, :Tn], ma[:, :Tn], t_[:, :Tn], ALU.add, [ma, t_])
                                else:
                                    tt("pool", mrg, mrg[:, oc, :Tn], ma[:, :Tn], t_[:, :Tn], ALU.add, [ma, t_])
                    for oc in range(8):
                        ocs = slice(oc * 128, (oc + 1) * 128)
                        y_ = py[oc % 2]
                        for k in range(8):
                            mm(y_, y_[:, :Tn], wo[:, k, ocs], mrg[:, k, :Tn], [wo, mrg], start=(k == 0), stop=(k == 7))
                        stt(xb, xb[:, oc, :Tn], y_[:, :Tn], modT[:, 16 + oc, j:j + 1], xb[:, oc, :Tn], ALU.mult, ALU.add, [y_, modT, xb])
                    kb.dma("pool", xa_view(c0, Tn), xb[:, :, :Tn], reads=[xb.b], writes=tr(xa_b, c0, Tn))
            kb.barrier()
            if stop == "C":
                break

            with contextlib.ExitStack() as es:
                wq = sbuf(es, "d_wq", [128, 8, 2048], BF16)
                sk = sbuf(es, "d_sk", [128, 16, 128], F32)
                xb = sbuf(es, "d_xb", [128, 8, 256], F32)
                hb = sbuf(es, "d_hb", [128, 8, 256], BF16)
                sqk = [sbuf(es, "d_sq%d" % i, [128, 256], F32) for i in range(2)]
                tmpk = [sbuf(es, "d_tk%d" % i, [128, 256], F32) for i in range(2)]
                rstd = sbuf(es, "d_rstd", [128, 256], F32)
                qpc = [sbuf(es, "d_qpc%d" % i, [128, 256], F32) for i in range(2)]
                s_sb = sbuf(es, "d_s", [128, 2, 16, 128], F32)
                top16_2 = [sbuf(es, "d_top%d" % i, [128, 16, 16], F32) for i in range(2)]
                mr = sbuf(es, "d_mr", [128, 256], F32)
                best = sbuf(es, "d_best", [128, 8, 16], F32)
                eb = sbuf(es, "d_eb", [128, 8, 16], F32)
                sm = sbuf(es, "d_sm", [128, 6, 8], F32)
                tau = sbuf(es, "d_tau", [128, 2, 8], F32)
                nbias = sbuf(es, "d_nbias", [128, 2, 8], F32)
                cf = [sbuf(es, "d_cf%d" % i, [128, 16, 128], F32) for i in range(4)]
                cand = T(cf[0].t[:].rearrange("p (h a) (b c) -> p h a (b c)", h=8, c=16).rearrange("p h a (b c) -> p h (a b) c", c=16), "cand_alias")
                cand.b = cf[0].b
                pr = [sbuf(es, "d_pr%d" % i, [128, 16, 128], BF16) for i in range(4)]
                tw = [sbuf(es, "d_tw%d" % i, [128, 16, 128], BF16) for i in range(2)]
                Ws = [sbuf(es, "d_Ws%d" % i, [128, 2, 16, 128], BF16) for i in range(2)]
                uch = [sbuf(es, "d_uch%d" % i, [128, 8, 512], BF16) for i in range(2)]
                vch = [sbuf(es, "d_vch%d" % i, [128, 4, D], BF16) for i in range(2)]
                wts = [sbuf(es, "d_wts%d" % i, [128, 4, 256], BF16) for i in range(2)]
                gel = [sbuf(es, "d_gel%d" % i, [128, 256], BF16) for i in range(2)]
                cT = [sbuf(es, "d_cT%d" % i, [128, 256], BF16) for i in range(4)]
                yps = [psum(es, "d_y%d" % i, [128, 512]) for i in range(4)]
                pm1 = psum(es, "d_pm1", [128, 512])
                apsl = [psum(es, "d_a%d" % i, [128, 512]) for i in range(2)]
                wtp = psum(es, "d_wt", [128, 4, 256], BF16)
                for k in range(8):
                    kb.dma("pool", wq[:, k, :], w_pq[l, k * 128:(k + 1) * 128, :], writes=[wq.b])
                kb.dma("sp", sk[:].rearrange("p a b -> p (a b)"), skT[l].rearrange("p a b -> p (a b)"), writes=[sk.b])
                if l + 1 < NL:
                    convert_uv(l + 1)
                ub = ub2[l % 2]
                vb = vb2[l % 2]
                ub_b = ub_b2[l % 2]
                vb_b = vb_b2[l % 2]
                ubv = ub.rearrange("(k p) e -> p k e", p=128)
                cDl = [0]
                cUl = [0]
                for bi, (c0, Tn, j) in enumerate(PBLOCKS):
                    if j == 1 and l == DEPTH - 1:
                        continue
                    kb.dma("sp", xb[:], xa_view(c0, 256), reads=tr(xa_b, c0, 256), writes=[xb.b])
                    norm_mod(xb, hb, sqk, rstd, tmpk, pm1, 256,
                             lambda k: A2[:, k, j:j + 1], lambda k: modT[:, 24 + k, j:j + 1])
                    for c in range(16):
                        q_ = qpc[c % 2]
                        for k in range(8):
                            mm(pm1, pm1[:, 0:256], wq[:, k, c * 128:(c + 1) * 128], hb[:, k, :], [wq, hb], start=(k == 0), stop=(k == 7))
                        cp("act", q_, q_[:], pm1[:, 0:256], [pm1])
                        for tl in range(2):
                            col = 256 + tl * 128
                            mm(pm1, pm1[:, col:col + 128], q_[:, tl * 128:(tl + 1) * 128], sk[:, c, :], [q_, sk])
                        cp("dve", s_sb, s_sb[:, :, c, :], pm1[:, 256:512].rearrange("p (t n) -> p t n", t=2), [pm1])
                        for tl in range(2):
                            tk = top16_2[tl]
                            kb.op("dve", lambda: nc.vector.max(out=tk[:, c, 0:8], in_=s_sb[:, tl, c, :]), reads=[s_sb.b], writes=[tk.b])
                            kb.op("dve", lambda: nc.vector.match_replace(out=mr[:, 0:128], in_to_replace=tk[:, c, 0:8],
                                                                          in_values=s_sb[:, tl, c, :], imm_value=-1e30),
                                  reads=[s_sb.b, tk.b], writes=[mr.b])
                            kb.op("dve", lambda: nc.vector.max(out=tk[:, c, 8:16], in_=mr[:, 0:128]), reads=[mr.b], writes=[tk.b])
                    for tl in range(2):
                        top16 = top16_2[tl]
                        t4 = top16[:].rearrange("p (h a) k -> p h a k", a=2)
                        tt("dve", cand, cand[:], t4[:, :, 0, :].unsqueeze(3).to_broadcast([128, 8, 16, 16]),
                           t4[:, :, 1, :].unsqueeze(2).to_broadcast([128, 8, 16, 16]), ALU.add, [top16])
                        for h in range(8):
                            ch = cand[:, h].rearrange("p a b -> p (a b)")
                            kb.op("dve", lambda: nc.vector.max(out=best[:, h, 0:8], in_=ch), reads=[cand.b], writes=[best.b])
                            kb.op("dve", lambda: nc.vector.match_replace(out=mr[:, 0:256], in_to_replace=best[:, h, 0:8], in_values=ch,
                                                                          imm_value=-1e30),
                                  reads=[cand.b, best.b], writes=[mr.b])
                            kb.op("dve", lambda: nc.vector.max(out=best[:, h, 8:16], in_=mr[:, 0:256]), reads=[mr.b], writes=[best.b])
                        ts("dve", sm, sm[:, 0, :], best[:, :, 0], -1.0, None, ALU.mult, None, [best])
                        cp("dve", tau, tau[:, tl, :], best[:, :, 15], [best])
                        for h in range(8):
                            act(eb, eb[:, h, :], best[:, h, :], AF.Exp, [best, sm], bias=sm[:, 0, h:h + 1])
                        red(sm, sm[:, 1, :], eb[:], ALU.add, [eb])
                        act(sm, sm[:, 1, :], sm[:, 1, :], AF.Ln, [sm])
                        ts("dve", sm, sm[:, 2, :], t4[:, :, 0, 0], -1.0, None, ALU.mult, None, [top16])
                        stt(sm, sm[:, 3, :], t4[:, :, 1, 0], -1.0, sm[:, 1, :], ALU.mult, ALU.subtract, [top16, sm])
                        tt("dve", nbias, nbias[:, tl, :], sm[:, 0, :], sm[:, 1, :], ALU.subtract, [sm])
                    units = [(ig, tl, h) for ig in range(8) for tl in range(2) for h in range(8)]

                    def cand_of(n):
                        ig, tl, h = units[n]
                        isl = slice(ig * 16, (ig + 1) * 16)
                        c_ = cf[n % 4]
                        tt("dve", c_, c_[:], s_sb[:, tl, 2 * h, isl].unsqueeze(2).to_broadcast([128, 16, 128]),
                           s_sb[:, tl, 2 * h + 1, :].unsqueeze(1).to_broadcast([128, 16, 128]), ALU.add, [s_sb])

                    def exp_of(n):
                        ig, tl, h = units[n]
                        act(pr[n % 4], pr[n % 4][:], cf[n % 4][:], AF.Exp, [cf[n % 4], nbias], bias=nbias[:, tl, h:h + 1])

                    def acc_of(n):
                        ig, tl, h = units[n]
                        W_ = Ws[ig % 2]
                        c_ = cf[n % 4]
                        p_ = pr[n % 4]
                        w_ = tw[n % 2]
                        if h == 0:
                            stt(W_, W_[:, tl], c_[:], tau[:, tl, h:h + 1], p_[:], ALU.is_ge, ALU.mult, [c_, p_, tau])
                        else:
                            stt(w_, w_[:], c_[:], tau[:, tl, h:h + 1], p_[:], ALU.is_ge, ALU.mult, [c_, p_, tau])
                            tt("dve", W_, W_[:, tl], W_[:, tl], w_[:], ALU.add, [W_, w_])

                    cand_of(0)
                    cand_of(1)
                    pend = None
                    grp = None
                    for s_ in range(64 + 8):
                        if s_ + 1 < 64:
                            cand_of(2 * s_ + 2)
                            cand_of(2 * s_ + 3)
                        if s_ < 64:
                            exp_of(2 * s_)
                            exp_of(2 * s_ + 1)
                            acc_of(2 * s_)
                            acc_of(2 * s_ + 1)
                        if s_ >= 8:
                            cpair = s_ - 8
                            i_a = 2 * cpair
                            ig = i_a // 16
                            il = i_a % 16
                            W_ = Ws[ig % 2]
                            if i_a % 4 == 0:
                                u_ = uch[cUl[0] % 2]
                                v_ = vch[cUl[0] % 2]
                                ws_ = wts[cUl[0] % 2]
                                cUl[0] += 1
                                kb.dma("sp", u_[:], ubv[:, :, i_a * 128:(i_a + 4) * 128], reads=[ub_b], writes=[u_.b])
                                kb.dma("sp", v_[:], vb[i_a * 128:(i_a + 4) * 128, :].rearrange("(a p) d -> p a d", p=128), reads=[vb_b],
                                       writes=[v_.b])
                                for k4 in range(4):
                                    for tl in range(2):
                                        tp(wtp, wtp[:, k4, tl * 128:(tl + 1) * 128], W_[:, tl, il + k4, :], ident_b[:], [W_, ident_b])
                                cp("act", ws_, ws_[:], wtp[:], [wtp])
                                grp = (u_, v_, ws_)
                            u_, v_, ws_ = grp
                            for i in (i_a, i_a + 1):
                                ii = i % 4
                                a_ = apsl[i % 2]
                                for k in range(8):
                                    mm(a_, a_[:, 0:256], u_[:, k, ii * 128:(ii + 1) * 128], hb[:, k, :], [u_, hb], start=(k == 0), stop=(k == 7))
                            if pend is not None:
                                pend()
                            for i in (i_a, i_a + 1):
                                act(gel[i % 2], gel[i % 2][:], apsl[i % 2][:, 0:256], AF.Gelu, [apsl[i % 2]])
                            for i in (i_a, i_a + 1):
                                tt("dve", cT[i % 4], cT[i % 4][:], gel[i % 2][:], ws_[:, i % 4, :], ALU.mult, [gel[i % 2], ws_])

                            def mkv(i_a=i_a, v_=v_):
                                def f():
                                    for i in (i_a, i_a + 1):
                                        c2 = cT[i % 4]
                                        for oc in range(8):
                                            y_ = yps[oc // 2]
                                            mm(y_, y_[:, (oc % 2) * 256:(oc % 2) * 256 + 256], v_[:, i % 4, oc * 128:(oc + 1) * 128], c2[:],
                                               [v_, c2], start=(i == 0 and oc % 2 == 0), stop=(i == 127))
                                return f
                            pend = mkv()
                    pend()
                    for oc in range(8):
                        y_ = yps[oc // 2]
                        stt(xb, xb[:, oc, :], y_[:, (oc % 2) * 256:(oc % 2) * 256 + 256], modT[:, 40 + oc, j:j + 1], xb[:, oc, :],
                            ALU.mult, ALU.add, [y_, modT, xb])
                    kb.dma("sp", xa_view(c0, 256), xb[:], reads=[xb.b], writes=tr(xa_b, c0, 256))
                    ck("D0")
            kb.barrier()
            if stop == "D":
                break
          except StopBuild:
            break

        if stop is None:
            with contextlib.ExitStack() as es:
                fn = sbuf(es, "f_fn", [128, 8], F32)
                xb_ = [sbuf(es, "f_xb%d" % i, [128, 8, 512], F32) for i in range(2)]
                sqk = [sbuf(es, "f_sq%d" % i, [128, 512], F32) for i in range(2)]
                xn = sbuf(es, "f_xn", [128, 8, 512], F32)
                rstd = sbuf(es, "f_rstd", [128, 512], F32)
                yo = [sbuf(es, "f_yo%d" % i, [128, D], F32) for i in range(2)]
                ssp = psum(es, "f_ssp", [128, 512])
                pp = [psum(es, "f_pp%d" % i, [128, 1024]) for i in range(2)]
                kb.dma("sp", fn[:], fnormT[:, :], writes=[fn.b])
                for bi in range(8):
                    c0 = bi * 512
                    xb = xb_[bi % 2]
                    kb.dma("sp", xb[:], xa_view(c0, 512), reads=tr(xa_b, c0, 512), writes=[xb.b])
                    for k in range(8):
                        q_ = sqk[k % 2]
                        act(q_, q_[:], xb[:, k, :], AF.Square, [xb])
                        mm(ssp, ssp[:], ones_f[:], q_[:], [ones_f, q_], start=(k == 0), stop=(k == 7))
                    rstd_from_ss(rstd, rstd[:], ssp[:], float(D), [ssp])
                    for k in range(8):
                        stt(xn, xn[:, k, :], xb[:, k, :], fn[:, k:k + 1], rstd[:], ALU.mult, ALU.mult, [xb, fn, rstd])
                    for ti in range(4):
                        t = bi * 4 + ti
                        p = pp[t % 2]
                        o = yo[t % 2]
                        for k in range(8):
                            tp(p, p[:, k * 128:(k + 1) * 128], xn[:, k, ti * 128:(ti + 1) * 128], ident_f[:], [xn, ident_f])
                        cp("act" if t % 2 else "dve", o, o[:], p[:], [p])
                        kb.dma("sp", out[t * 128:(t + 1) * 128, :], o[:], reads=[o.b])

        kb.dead = False
        kb.barrier()
    return nc


def rope_tables():
    pos = np.arange(NLAT)
    rows = (pos // 64).astype(np.float32)
    cols = (pos % 64).astype(np.float32)

    def tab(dh):
        inv = np.power(np.float32(10000.0), -np.arange(0, dh, 2, dtype=np.float32) / np.float32(dh)).astype(np.float32)
        out = np.zeros((NT, 2, 2, dh // 2), np.float32)
        out[:, 0] = 1.0
        for a, p_ in enumerate((rows, cols)):
            ang = (p_[:, None] * inv[None, :]).astype(np.float32)
            out[:NLAT, 0, a] = np.cos(ang)
            out[:NLAT, 1, a] = np.sin(ang)
        return out.reshape(NT, -1)

    return tab(16), tab(32)


def na_bias_tables(rpb):
    Lr = rpb.shape[0]
    out = np.full((Lr, 5, 128, 4, 5, 128), NEG, np.float32)
    for case, m in enumerate((0, 1, 2, 30, 31)):
        k0 = min(max(m - 2, 0), 27)
        q = m * 128 + np.arange(128)
        qr, qc = q // 64, q % 64
        rs = np.clip(qr - 4, 0, 56)
        cs = np.clip(qc - 8, 0, 48)
        for i in range(5):
            key = (k0 + i) * 128 + np.arange(128)
            kr, kc_ = key // 64, key % 64
            inw = ((kr[:, None] >= rs[None, :]) & (kr[:, None] < rs[None, :] + 8)
                   & (kc_[:, None] >= cs[None, :]) & (kc_[:, None] < cs[None, :] + 16))
            ro = np.clip(kr[:, None] - qr[None, :] + 7, 0, 14)
            co = np.clip(kc_[:, None] - qc[None, :] + 15, 0, 30)
            for h in range(4):
                g = rpb[:, h][:, ro, co]
                out[:, case, :, h, i, :] = np.where(inw[None], g, np.float32(NEG))
    return out


def prep_inputs(inp):
    f = lambda a: np.ascontiguousarray(np.asarray(a, dtype=np.float32))
    L = DEPTH
    r32, r64 = rope_tables()
    shared = {
        "w_mod": f(inp["w_mod"]),
        "b_modT": f(inp["b_mod"].reshape(L, 48, 128).transpose(0, 2, 1)),
        "nmixT": f(inp["norm_mix"].reshape(L, 8, 128).transpose(0, 2, 1)),
        "nffnT": f(inp["norm_ffn"].reshape(L, 8, 128).transpose(0, 2, 1)),
        "fnormT": f(inp["final_norm"].reshape(8, 128).T),
        "w_in": f(inp["w_in"]),
        "g_mlaq": f(inp["mla_q_norm"]),
        "g_mlakv": f(inp["mla_kv_norm"]),
        "w_uq": f(inp["mla_w_uq"]),
        "w_ukv": f(inp["mla_w_ukv"]),
        "nab": f(na_bias_tables(np.asarray(inp["na_rpb"], np.float32))),
        "lamv": f(np.stack([inp["diff_lam_q1"], inp["diff_lam_k1"], inp["diff_lam_q2"], inp["diff_lam_k2"]], axis=1)),
        "g_subln": f(inp["diff_subln"]),
        "g_gq": f(inp["gqa_q_norm"]),
        "g_gk": f(inp["gqa_k_norm"]),
        "w_branch": f(inp["w_branch"]),
        "w_gate": f(inp["w_gate"]),
        "b_gateT": f(inp["b_gate"].reshape(L, 4, 8, 128).transpose(0, 3, 1, 2)),
        "w_out": f(inp["w_out"]),
        "w_pq": f(inp["peer_w_q"]),
        "skT": f(inp["peer_subkeys"].reshape(L, 16, 128, 128).transpose(0, 3, 1, 2)),
        "uT": f(np.asarray(inp["peer_u"]).transpose(0, 2, 1)),
        "pv": f(inp["peer_v"]),
        "rope32": f(r32),
        "rope64": f(r64),
    }
    maps = []
    for b in range(8):
        m = dict(shared)
        m["xin"] = f(np.concatenate([inp["x"][b], inp["ctx"][b]], axis=0))
        cc = np.stack([inp["c"][b], inp["c_ctx"]], axis=0).reshape(2, 8, 128).transpose(2, 1, 0).reshape(128, 16)
        m["cc"] = f(cc)
        maps.append(m)
    return maps


def kernel(**inputs):
    maps = prep_inputs(inputs)
    nc = build()
    res = run_bass_kernel_spmd(nc, maps, core_ids=list(range(8)))
    return np.stack([np.asarray(r["out"], dtype=np.float32) for r in res.results], axis=0)
```

```python
import math
import contextlib
import numpy as np
import concourse.bass as bass
import concourse.mybir as mybir
from concourse.bass_utils import run_bass_kernel_spmd

F32 = mybir.dt.float32
BF16 = mybir.dt.bfloat16
AF = mybir.ActivationFunctionType
ALU = mybir.AluOpType
AX = mybir.AxisListType

D = 1024
KC = 8
NLAT = 4096
NCTX = 256
NT = NLAT + NCTX
NTILE = NT // 128
DEPTH = 4
EPS = 1e-6
NEG = -30000.0
BLOCKS = [(i * 512, 512, 0) for i in range(8)] + [(4096, 256, 1)]
PBLOCKS = [(i * 256, 256, 0) for i in range(16)] + [(4096, 256, 1)]


class Ev:
    __slots__ = ("key", "val", "clk")

    def __init__(s, key, val, clk):
        s.key = key
        s.val = val
        s.clk = clk


class TB:
    __slots__ = ("name", "w", "r", "excl")

    def __init__(s, name="", excl=False):
        s.name = name
        s.w = None
        s.r = {}
        s.excl = excl


class KB:
    NS = {"sp": 24, "pool": 40, "act": 2}

    def __init__(s, nc, es):
        s.nc = nc
        s.eng = {"pe": nc.tensor, "act": nc.scalar, "dve": nc.vector, "pool": nc.gpsimd, "sp": nc.sync}
        s.semobj = {}
        s.ccnt = {}
        for e in ["pe", "act", "dve", "pool"]:
            s.semobj[e] = es.enter_context(nc.semaphore("cs_" + e))
            s.ccnt[e] = 0
        s.known = {e: {} for e in s.eng}
        s.dcnt = {}
        for q, n in s.NS.items():
            s.dcnt[q] = 0
            for j in range(n):
                s.semobj[(q, j)] = es.enter_context(nc.semaphore("ds_%s%d" % (q, j)))
        s.last_dma = {}
        s.ninstr = 0
        s.dead = False

    def _wait(s, e, deps):
        k = s.known[e]
        changed = False
        for ev in deps:
            if ev is None:
                continue
            if k.get(ev.key, 0) >= ev.val:
                continue
            s.eng[e].wait_ge(s.semobj[ev.key], ev.val)
            if not changed:
                k = dict(k)
                changed = True
            for kk, vv in ev.clk.items():
                if k.get(kk, 0) < vv:
                    k[kk] = vv
            k[ev.key] = ev.val
        if changed:
            s.known[e] = k

    def _deps(s, reads, writes):
        deps = []
        for b in reads:
            if b.w is not None:
                deps.append(b.w)
        for b in writes:
            if b.w is not None:
                deps.append(b.w)
            deps.extend(b.r.values())
        return deps

    def _upd(s, ev, reads, writes):
        for b in reads:
            o = b.r.get(ev.key)
            if o is None or o.val < ev.val:
                b.r[ev.key] = ev
        for b in writes:
            b.w = ev
            b.r = {}

    def op(s, e, fn, reads=(), writes=()):
        if s.dead:
            return None
        ex = [b for b in reads if b.excl]
        if ex:
            reads = [b for b in reads if not b.excl]
            writes = list(writes) + ex
        deps = s._deps(reads, writes)
        if e == "pe":
            deps = [d for d in deps if d.key != "pe"]
        s._wait(e, deps)
        ins = fn()
        s.ccnt[e] += 1
        ins.then_inc(s.semobj[e], 1)
        ev = Ev(e, s.ccnt[e], s.known[e])
        s._upd(ev, reads, writes)
        s.ninstr += 1
        return ev

    def dma(s, q, out, in_, reads=(), writes=(), in_barrier=True):
        if s.dead:
            return None
        i = s.dcnt[q]
        n = s.NS[q]
        j = i % n
        key = (q, j)
        prev = 16 * (i // n)
        val = prev + 16
        deps = s._deps(reads, writes)
        if prev > 0:
            deps.append(Ev(key, prev, {}))
        s._wait(q, deps)
        ins = s.eng[q].dma_start(out=out, in_=in_)
        ins.then_inc(s.semobj[key], 16)
        s.dcnt[q] += 1
        ev = Ev(key, val, s.known[q])
        if in_barrier:
            s.last_dma[key] = ev
        elif key in s.last_dma:
            del s.last_dma[key]
        s._upd(ev, reads, writes)
        s.ninstr += 1
        return ev

    def barrier(s, engines=("pe", "act", "dve", "pool", "sp")):
        if s.dead:
            return
        evs = [Ev(e, s.ccnt[e], {}) for e in ["pe", "act", "dve", "pool"] if s.ccnt[e] > 0]
        evs += list(s.last_dma.values())
        for e in engines:
            s._wait(e, evs)


class StopBuild(Exception):
    pass


class T:
    def __init__(s, t, name, excl=False):
        s.t = t
        s.b = TB(name, excl)

    def __getitem__(s, k):
        return s.t[k]


def build(NL=DEPTH, stop=None, dbg=()):
    nc = bass.Bass("TRN2", target_bir_lowering=False)

    def din(name, shape, dt=F32):
        return nc.dram_tensor(name, list(shape), dt, kind="ExternalInput").ap()

    def dscr(name, shape, dt):
        kind = "ExternalOutput" if name in dbg else "Internal"
        return nc.dram_tensor(name, list(shape), dt, kind=kind).ap()

    L = DEPTH
    xin = din("xin", [NT, D])
    cc = din("cc", [128, 16])
    w_mod = din("w_mod", [L, D, 6 * D])
    b_modT = din("b_modT", [L, 128, 48])
    nmixT = din("nmixT", [L, 128, 8])
    nffnT = din("nffnT", [L, 128, 8])
    fnormT = din("fnormT", [128, 8])
    w_in = din("w_in", [L, D, 2464])
    g_mlaq = din("g_mlaq", [L, 256])
    g_mlakv = din("g_mlakv", [L, 128])
    w_uq = din("w_uq", [L, 256, 384])
    w_ukv = din("w_ukv", [L, 128, 512])
    nab = din("nab", [L, 5, 128, 4, 5, 128])
    lamv = din("lamv", [L, 4, 32])
    g_subln = din("g_subln", [L, 64])
    g_gq = din("g_gq", [L, 64])
    g_gk = din("g_gk", [L, 64])
    w_branch = din("w_branch", [L, 4, 256, D])
    w_gate = din("w_gate", [L, 4, D, D])
    b_gateT = din("b_gateT", [L, 128, 4, 8])
    w_out = din("w_out", [L, D, D])
    w_pq = din("w_pq", [L, D, 2048])
    skT = din("skT", [L, 128, 16, 128])
    uT = din("uT", [L, D, 16384])
    pv = din("pv", [L, 16384, D])
    rope32 = din("rope32", [NT, 32])
    rope64 = din("rope64", [NT, 64])
    out = nc.dram_tensor("out", [NLAT, D], F32, kind="ExternalOutput").ap()

    xa = dscr("xa", [D, NT], F32)
    hta = dscr("hta", [D, NT], BF16)
    ota = dscr("ota", [D, NT], BF16)
    qt_mla = dscr("qt_mla", [4 * 96, NT], BF16)
    kt_mla = dscr("kt_mla", [4 * 96, NT], BF16)
    qt_na = dscr("qt_na", [256, NT], BF16)
    kt_na = dscr("kt_na", [256, NT], BF16)
    qt_df = dscr("qt_df", [256, NT], BF16)
    kt_df = dscr("kt_df", [256, NT], BF16)
    qt_gq = dscr("qt_gq", [256, NT], BF16)
    kt_gq = dscr("kt_gq", [128, NT], BF16)
    v1a = dscr("v1a", [NT, 14 * 65], BF16)
    ub2 = [dscr("ub%d" % i, [D, 16384], BF16) for i in range(2)]
    vb2 = [dscr("vb%d" % i, [16384, D], BF16) for i in range(2)]

    def tiles_tb(name):
        return [TB("%s%d" % (name, i)) for i in range(NTILE)]

    xa_b = tiles_tb("xa")
    hta_b = tiles_tb("hta")
    ota_b = tiles_tb("ota")
    qk_b = tiles_tb("qk")
    ub_b2 = [TB("ub0"), TB("ub1")]
    vb_b2 = [TB("vb0"), TB("vb1")]

    def tr(bl, start, n):
        return bl[start // 128:(start + n) // 128]

    es_top = contextlib.ExitStack()
    with es_top:
        kb = KB(nc, es_top)

        uid = [0]

        def sbuf(es, name, shape, dt):
            uid[0] += 1
            name = "%s_%d" % (name, uid[0])
            return T(es.enter_context(nc.sbuf_tensor(name, list(shape), dt)), name)

        def psum(es, name, shape, dt=F32):
            uid[0] += 1
            name = "%s_%d" % (name, uid[0])
            return T(es.enter_context(nc.psum_tensor(name, list(shape), dt)), name, True)

        def mm(o, oap, lhsT, rhs, reads, start=True, stop=True):
            kb.op("pe", lambda: nc.tensor.matmul(oap, lhsT=lhsT, rhs=rhs, start=start, stop=stop),
                  reads=[r.b for r in reads], writes=[o.b])

        def tp(o, oap, iap, ident, reads):
            kb.op("pe", lambda: nc.tensor.transpose(oap, iap, ident), reads=[r.b for r in reads], writes=[o.b])

        def act(o, oap, iap, func, reads, bias=None, scale=None):
            kw = {}
            if bias is not None:
                kw["bias"] = bias
            if scale is not None:
                kw["scale"] = scale
            kb.op("act", lambda: nc.scalar.activation(out=oap, in_=iap, func=func, **kw),
                  reads=[r.b for r in reads], writes=[o.b])

        def tt(e, o, oap, a, b, op, reads):
            en = nc.vector if e == "dve" else nc.gpsimd
            kb.op(e, lambda: en.tensor_tensor(out=oap, in0=a, in1=b, op=op), reads=[r.b for r in reads], writes=[o.b])

        def ts(e, o, oap, a, s1, s2, op0, op1, reads):
            en = nc.vector if e == "dve" else nc.gpsimd
            if op1 is None:
                kb.op(e, lambda: en.tensor_scalar(out=oap, in0=a, scalar1=s1, scalar2=None, op0=op0),
                      reads=[r.b for r in reads], writes=[o.b])
            else:
                kb.op(e, lambda: en.tensor_scalar(out=oap, in0=a, scalar1=s1, scalar2=s2, op0=op0, op1=op1),
                      reads=[r.b for r in reads], writes=[o.b])

        def stt(o, oap, a, sc, b, op0, op1, reads):
            kb.op("dve", lambda: nc.vector.scalar_tensor_tensor(out=oap, in0=a, scalar=sc, in1=b, op0=op0, op1=op1),
                  reads=[r.b for r in reads], writes=[o.b])

        def cp(e, o, oap, iap, reads):
            if e == "act":
                act(o, oap, iap, AF.Copy, reads)
            else:
                en = nc.vector if e == "dve" else nc.gpsimd
                kb.op(e, lambda: en.tensor_copy(out=oap, in_=iap), reads=[r.b for r in reads], writes=[o.b])

        def red(o, oap, iap, op, reads):
            kb.op("dve", lambda: nc.vector.tensor_reduce(out=oap, in_=iap, axis=AX.X, op=op),
                  reads=[r.b for r in reads], writes=[o.b])

        def recip(o, oap, iap, reads):
            kb.op("dve", lambda: nc.vector.reciprocal(out=oap, in_=iap), reads=[r.b for r in reads], writes=[o.b])

        def mset(e, o, oap, val):
            en = nc.vector if e == "dve" else nc.gpsimd
            kb.op(e, lambda: en.memset(oap, val), writes=[o.b])

        def ck(name):
            if stop == name:
                kb.dead = True

        def rstd_from_ss(o, oap, ssap, n, reads):
            ts("dve", o, oap, ssap, 1.0 / n, EPS, ALU.mult, ALU.add, reads)
            act(o, oap, oap, AF.Sqrt, [o])
            recip(o, oap, oap, [o])

        ident_f = sbuf(es_top, "ident_f", [128, 128], F32)
        ident_b = sbuf(es_top, "ident_b", [128, 128], BF16)
        ones_f = sbuf(es_top, "ones_f", [128, 128], F32)
        modT = sbuf(es_top, "modT", [128, 48, 2], F32)
        A1 = sbuf(es_top, "A1", [128, 8, 2], F32)
        A2 = sbuf(es_top, "A2", [128, 8, 2], F32)
        mset("pool", ident_f, ident_f[:], 1.0)
        kb.op("pool", lambda: nc.gpsimd.affine_select(out=ident_f[:], in_=ident_f[:], pattern=[[-1, 128]],
                                                      compare_op=ALU.is_equal, fill=0.0, base=0,
                                                      channel_multiplier=1),
              reads=[ident_f.b], writes=[ident_f.b])
        cp("dve", ident_b, ident_b[:], ident_f[:], [ident_f])
        mset("pool", ones_f, ones_f[:], 1.0)

        def convert_uv(lc):
            sset = lc % 2
            for k in range(8):
                for hf in range(2):
                    kb.dma("pool", ub2[sset][k * 128:(k + 1) * 128, hf * 8192:(hf + 1) * 8192],
                           uT[lc, k * 128:(k + 1) * 128, hf * 8192:(hf + 1) * 8192], writes=[ub_b2[sset]], in_barrier=False)
            src = pv[lc].rearrange("(g p r) d -> g p (r d)", p=128, r=8)
            dst = vb2[sset].rearrange("(g p r) d -> g p (r d)", p=128, r=8)
            for g in range(16):
                kb.dma("pool", dst[g], src[g], writes=[vb_b2[sset]], in_barrier=False)

        def xa_view(c0, n):
            return xa.rearrange("(k p) n -> p k n", p=128)[:, :, c0:c0 + n]

        def hta_view(c0, n):
            return hta.rearrange("(k p) n -> p k n", p=128)[:, :, c0:c0 + n]

        def ota_view(c0, n):
            return ota.rearrange("(k p) n -> p k n", p=128)[:, :, c0:c0 + n]

        if NL > 0:
            convert_uv(0)
        with contextlib.ExitStack() as es:
            xt = [sbuf(es, "i_xt%d" % i, [128, D], F32) for i in range(2)]
            xo = [sbuf(es, "i_xo%d" % i, [128, 8, 128], F32) for i in range(2)]
            pp = [psum(es, "i_pp%d" % i, [128, 1024], F32) for i in range(2)]
            for t in range(NTILE):
                a = xt[t % 2]
                o = xo[t % 2]
                p = pp[t % 2]
                kb.dma("sp", a[:], xin[t * 128:(t + 1) * 128, :], writes=[a.b])
                for k in range(8):
                    tp(p, p[:, k * 128:(k + 1) * 128], a[:, k * 128:(k + 1) * 128], ident_f[:], [a, ident_f])
                cp("act" if t % 2 else "dve", o, o[:].rearrange("p k n -> p (k n)"), p[:], [p])
                kb.dma("pool", xa_view(t * 128, 128), o[:], reads=[o.b], writes=[xa_b[t]])
        kb.barrier()
        if stop == "I":
            NL = 0

        def norm_mod(xb, hb, sqk, rstd, tmpk, ssp, Tn, Acol, Bcol):
            for k in range(8):
                q_ = sqk[k % 2]
                act(q_, q_[:, :Tn], xb[:, k, :Tn], AF.Square, [xb])
                mm(ssp, ssp[:, :Tn], ones_f[:], q_[:, :Tn], [ones_f, q_], start=(k == 0), stop=(k == 7))
            rstd_from_ss(rstd, rstd[:, :Tn], ssp[:, :Tn], float(D), [ssp])
            for k in range(8):
                t_ = tmpk[k % 2]
                tt("dve", t_, t_[:, :Tn], xb[:, k, :Tn], rstd[:, :Tn], ALU.mult, [xb, rstd])
                act(hb, hb[:, k, :Tn], t_[:, :Tn], AF.Identity, [t_, modT, A1, A2], bias=Bcol(k), scale=Acol(k))

        for l in range(NL):
          try:
            lam_init = 0.8 - 0.6 * math.exp(-0.3 * l)
            with contextlib.ExitStack() as es:
                cct = sbuf(es, "m_cc", [128, 16], F32)
                sc = sbuf(es, "m_sc", [128, 16], F32)
                wm = [sbuf(es, "m_w%d" % i, [128, 8, 768], F32) for i in range(2)]
                bm = sbuf(es, "m_b", [128, 48], F32)
                nm = sbuf(es, "m_nm", [128, 8], F32)
                nf = sbuf(es, "m_nf", [128, 8], F32)
                pm = psum(es, "m_p", [128, 96], F32)
                kb.dma("sp", cct[:], cc[:, :], writes=[cct.b])
                kb.dma("sp", bm[:], b_modT[l], writes=[bm.b])
                kb.dma("sp", nm[:], nmixT[l], writes=[nm.b])
                kb.dma("sp", nf[:], nffnT[l], writes=[nf.b])
                act(sc, sc[:], cct[:], AF.Silu, [cct])
                for blk in range(8):
                    w = wm[blk % 2]
                    for k in range(8):
                        kb.dma("sp", w[:, k, :], w_mod[l, k * 128:(k + 1) * 128, blk * 768:(blk + 1) * 768], writes=[w.b])
                    for cl in range(6):
                        c = blk * 6 + cl
                        for k in range(8):
                            mm(pm, pm[:, c * 2:c * 2 + 2], w[:, k, cl * 128:(cl + 1) * 128], sc[:, k * 2:k * 2 + 2], [w, sc],
                               start=(k == 0), stop=(k == 7))
                tt("dve", modT, modT[:], pm[:].rearrange("p (c j) -> p c j", j=2),
                   bm[:].unsqueeze(2).to_broadcast([128, 48, 2]), ALU.add, [pm, bm])
                stt(A1, A1[:], modT[:, 8:16, :], 1.0, nm[:].unsqueeze(2).to_broadcast([128, 8, 2]), ALU.add, ALU.mult, [modT, nm])
                stt(A2, A2[:], modT[:, 32:40, :], 1.0, nf[:].unsqueeze(2).to_broadcast([128, 8, 2]), ALU.add, ALU.mult, [modT, nf])
            kb.barrier()
            if stop == "mod":
                break

            with contextlib.ExitStack() as es:
                win = sbuf(es, "a_win", [128, 8, 2464], BF16)
                wuq = sbuf(es, "a_wuq", [128, 2, 384], BF16)
                wukv = sbuf(es, "a_wukv", [128, 512], BF16)
                gq = sbuf(es, "a_gq", [128, 256], F32)
                gkv = sbuf(es, "a_gkv", [128, 128], F32)
                ggq = sbuf(es, "a_ggq", [128, 6, 64], F32)
                r32 = sbuf(es, "a_r32", [128, NTILE, 32], F32)
                r64 = sbuf(es, "a_r64", [128, NTILE, 64], F32)
                xb_ = [sbuf(es, "a_xb%d" % i, [128, 8, 512], F32) for i in range(2)]
                hb_ = [sbuf(es, "a_hb%d" % i, [128, 8, 512], BF16) for i in range(2)]
                sq = [sbuf(es, "a_sq%d" % i, [128, 512], F32) for i in range(2)]
                tmpn = [sbuf(es, "a_tmpn%d" % i, [128, 512], F32) for i in range(2)]
                rstd = sbuf(es, "a_rstd", [128, 512], F32)
                sqs_2 = [sbuf(es, "a_sqs%d" % i_, [128, 384], F32) for i_ in range(2)]
                st_2 = [sbuf(es, "a_st%d" % i_, [128, 8], F32) for i_ in range(2)]
                cn_2 = [sbuf(es, "a_cn%d" % i_, [128, 384], BF16) for i_ in range(2)]
                cnT_2 = [sbuf(es, "a_cnT%d" % i_, [128, 3, 128], BF16) for i_ in range(2)]
                qf_2 = [sbuf(es, "a_qf%d" % i_, [128, 4, 96], BF16) for i_ in range(2)]
                kf_2 = [sbuf(es, "a_kf%d" % i_, [128, 4, 96], BF16) for i_ in range(2)]
                krr_2 = [sbuf(es, "a_krr%d" % i_, [128, 32], F32) for i_ in range(2)]
                ra_2 = [sbuf(es, "a_ra%d" % i_, [128, 512], F32) for i_ in range(2)]
                rb_2 = [sbuf(es, "a_rb%d" % i_, [128, 512], F32) for i_ in range(2)]
                qkb_2 = [sbuf(es, "a_qkb%d" % i_, [128, 512], BF16) for i_ in range(2)]
                gtmp_2 = [sbuf(es, "a_gtmp%d" % i_, [128, 384], F32) for i_ in range(2)]
                gtmp2_2 = [sbuf(es, "a_gtmp2%d" % i_, [128, 384], F32) for i_ in range(2)]
                v1 = [sbuf(es, "a_v1_%d" % i, [128, 14, 65], BF16) for i in range(2)]
                tq = [sbuf(es, "a_tq%d" % i, [128, 4, 128], BF16) for i in range(4)]
                ssp = psum(es, "a_ssp", [128, 512], F32)
                pA = psum(es, "a_pA", [128, 512], F32)
                pB = psum(es, "a_pB", [128, 1024], F32)
                pC = psum(es, "a_pC", [128, 512], F32)
                pU1 = psum(es, "a_pU1", [128, 512], F32)
                pU2 = psum(es, "a_pU2", [128, 512], F32)
                pT = psum(es, "a_pT", [128, 8, 128], BF16)

                for k in range(8):
                    kb.dma("pool", win[:, k, :], w_in[l, k * 128:(k + 1) * 128, :], writes=[win.b])
                for k in range(2):
                    kb.dma("pool", wuq[:, k, :], w_uq[l, k * 128:(k + 1) * 128, :], writes=[wuq.b])
                kb.dma("pool", wukv[:], w_ukv[l], writes=[wukv.b])
                kb.dma("sp", gq[:], g_mlaq[l].partition_broadcast(128), writes=[gq.b])
                kb.dma("sp", gkv[:], g_mlakv[l].partition_broadcast(128), writes=[gkv.b])
                for h in range(4):
                    kb.dma("sp", ggq[:, h, :], g_gq[l].partition_broadcast(128), writes=[ggq.b])
                for h in range(2):
                    kb.dma("sp", ggq[:, 4 + h, :], g_gk[l].partition_broadcast(128), writes=[ggq.b])
                kb.dma("sp", r32[:], rope32.rearrange("(t p) c -> p t c", p=128), writes=[r32.b])
                kb.dma("sp", r64[:], rope64.rearrange("(t p) c -> p t c", p=128), writes=[r64.b])
                for i in range(2):
                    mset("pool", v1[i], v1[i][:], 1.0)

                tqi = [0]

                def next_tq():
                    tqi[0] += 1
                    return tq[tqi[0] % 4]

                def rope(src5, dst5, rt, t, G, Fq, reads, dstT):
                    ra = ra_2[t % 2]
                    rb = rb_2[t % 2]
                    tab = rt[:, t, :].rearrange("p (a b f) -> p a b f", a=2, b=2)
                    C = tab[:, 0].unsqueeze(1).to_broadcast([128, G, 2, Fq])
                    S = tab[:, 1].unsqueeze(1).to_broadcast([128, G, 2, Fq])
                    n = G * 2 * Fq
                    rav = ra[:, 0:n].rearrange("p (g b f) -> p g b f", g=G, b=2)
                    rbv = rb[:, 0:n].rearrange("p (g b f) -> p g b f", g=G, b=2)
                    t1 = src5[:, :, :, 0, :]
                    t2 = src5[:, :, :, 1, :]
                    tt("dve", ra, rav, t1, C, ALU.mult, reads + [rt])
                    tt("dve", rb, rbv, t2, S, ALU.mult, reads + [rt])
                    tt("pool", dstT, dst5[:, :, :, 0, :], rav, rbv, ALU.subtract, [ra, rb])
                    tt("dve", ra, rav, t1, S, ALU.mult, reads + [rt])
                    tt("dve", rb, rbv, t2, C, ALU.mult, reads + [rt])
                    tt("pool", dstT, dst5[:, :, :, 1, :], rav, rbv, ALU.add, [ra, rb])

                def r5(ap, G, Fq):
                    if len(ap.shape) == 2:
                        return ap.rearrange("p (g a b f) -> p g a b f", g=G, a=2, b=2)
                    return ap.rearrange("p g (a b f) -> p g a b f", a=2, b=2)

                for bi, (c0, Tn, j) in enumerate(BLOCKS):
                    xb = xb_[bi % 2]
                    hb = hb_[bi % 2]
                    kb.dma("sp", xb[:, :, :Tn], xa_view(c0, Tn), reads=tr(xa_b, c0, Tn), writes=[xb.b])
                    norm_mod(xb, hb, sq, rstd, tmpn, ssp, Tn,
                             lambda k: A1[:, k, j:j + 1], lambda k: modT[:, 0 + k, j:j + 1])
                    kb.dma("pool", hta_view(c0, Tn), hb[:, :, :Tn], reads=[hb.b], writes=tr(hta_b, c0, Tn))
                    ck("A0")
                    for ti in range(Tn // 128):
                        t = c0 // 128 + ti
                        ts_ = slice(ti * 128, (ti + 1) * 128)
                        vv = v1[t % 2]
                        sqs, st, cn, cnT, qf, kf, krr, qkb, gtmp, gtmp2 = [x_[t % 2] for x_ in
                                                                          (sqs_2, st_2, cn_2, cnT_2, qf_2, kf_2, krr_2, qkb_2, gtmp_2, gtmp2_2)]
                        for k in range(8):
                            mm(pA, pA[:, 0:416], hb[:, k, ts_], win[:, k, 0:416], [hb, win], start=(k == 0), stop=(k == 7))
                        act(sqs, sqs[:, 0:384], pA[:, 0:384], AF.Square, [pA])
                        red(st, st[:, 0:1], sqs[:, 0:256], ALU.add, [sqs])
                        red(st, st[:, 1:2], sqs[:, 256:384], ALU.add, [sqs])
                        ts("dve", st, st[:, 0:1], st[:, 0:1], 1.0 / 256, EPS, ALU.mult, ALU.add, [st])
                        ts("dve", st, st[:, 1:2], st[:, 1:2], 1.0 / 128, EPS, ALU.mult, ALU.add, [st])
                        act(st, st[:, 0:2], st[:, 0:2], AF.Sqrt, [st])
                        recip(st, st[:, 0:2], st[:, 0:2], [st])
                        stt(cn, cn[:, 0:256], pA[:, 0:256], st[:, 0:1], gq[:], ALU.mult, ALU.mult, [pA, st, gq])
                        stt(cn, cn[:, 256:384], pA[:, 256:384], st[:, 1:2], gkv[:], ALU.mult, ALU.mult, [pA, st, gkv])
                        ck("A1a")
                        for k in range(3):
                            tp(pT, pT[:, k, :], cn[:, k * 128:(k + 1) * 128], ident_b[:], [cn, ident_b])
                        cp("act", cnT, cnT[:], pT[:, 0:3, :], [pT])
                        ck("A1b")
                        for k in range(2):
                            mm(pU1, pU1[:, 0:384], cnT[:, k, :], wuq[:, k, :], [cnT, wuq], start=(k == 0), stop=(k == 1))
                        mm(pU2, pU2[:, 0:512], cnT[:, 2, :], wukv[:], [cnT, wukv])
                        u1 = pU1[:, 0:384].rearrange("p (h d) -> p h d", h=4)
                        u2 = pU2[:, 0:512].rearrange("p (h d) -> p h d", h=4)
                        ck("A1c")
                        cp("act", qf, qf[:, :, 0:64], u1[:, :, 0:64], [pU1])
                        rope(r5(u1[:, :, 64:96], 4, 8), r5(qf[:, :, 64:96], 4, 8), r32, t, 4, 8, [pU1], qf)
                        ck("A1c1")
                        cp("act", kf, kf[:, :, 0:64], u2[:, :, 0:64], [pU2])
                        ck("A1c2")
                        rope(r5(pA[:, 384:416], 1, 8), r5(krr[:, :], 1, 8), r32, t, 1, 8, [pA], krr)
                        ck("A1c3")
                        cp("pool", kf, kf[:, :, 64:96], krr[:].unsqueeze(1).to_broadcast([128, 4, 32]), [krr])
                        ck("A1c4")
                        cp("dve", vv, vv[:, 0:4, 0:64], u2[:, :, 64:128], [pU2])
                        ck("A1d")
                        for h in range(4):
                            tp(pT, pT[0:96, h, :], qf[:, h, :], ident_b[:], [qf, ident_b])
                        for h in range(4):
                            tp(pT, pT[0:96, 4 + h, :], kf[:, h, :], ident_b[:], [kf, ident_b])
                        o1 = next_tq()
                        o2 = next_tq()
                        cp("act", o1, o1[0:96, :, :], pT[0:96, 0:4, :], [pT])
                        cp("dve", o2, o2[0:96, :, :], pT[0:96, 4:8, :], [pT])
                        cols = slice(t * 128, (t + 1) * 128)
                        ck("A1e")
                        kb.dma("pool", qt_mla.rearrange("(m p) n -> p m n", p=96)[:, :, cols], o1[0:96, :, :], reads=[o1.b], writes=[qk_b[t]])
                        kb.dma("pool", kt_mla.rearrange("(m p) n -> p m n", p=96)[:, :, cols], o2[0:96, :, :], reads=[o2.b], writes=[qk_b[t]])
                        ck("A1")
                        for k in range(8):
                            mm(pB, pB[:, 0:512], hb[:, k, ts_], win[:, k, 416:928], [hb, win], start=(k == 0), stop=(k == 7))
                        for k in range(8):
                            mm(pB, pB[:, 512:768], hb[:, k, ts_], win[:, k, 928:1184], [hb, win], start=(k == 0), stop=(k == 7))
                        cp("act", qkb, qkb[:], pB[:, 0:512], [pB])
                        cp("dve", vv, vv[:, 4:8, 0:64], pB[:, 512:768].rearrange("p (h d) -> p h d", h=4), [pB])
                        for k in range(4):
                            tp(pT, pT[:, k, :], qkb[:, k * 128:(k + 1) * 128], ident_b[:], [qkb, ident_b])
                        o1 = next_tq()
                        cp("act", o1, o1[:], pT[:, 0:4, :], [pT])
                        kb.dma("pool", qt_na.rearrange("(m p) n -> p m n", p=128)[:, :, cols], o1[:, 0:2, :], reads=[o1.b], writes=[qk_b[t]])
                        kb.dma("pool", kt_na.rearrange("(m p) n -> p m n", p=128)[:, :, cols], o1[:, 2:4, :], reads=[o1.b], writes=[qk_b[t]])
                        ck("A2")
                        for k in range(8):
                            mm(pB, pB[:, 0:512], hb[:, k, ts_], win[:, k, 1184:1696], [hb, win], start=(k == 0), stop=(k == 7))
                        for k in range(8):
                            mm(pB, pB[:, 512:768], hb[:, k, ts_], win[:, k, 1696:1952], [hb, win], start=(k == 0), stop=(k == 7))
                        for half in range(2):
                            rope(r5(pB[:, half * 256:(half + 1) * 256], 8, 8), r5(qkb[:, half * 256:(half + 1) * 256], 8, 8),
                                 r32, t, 8, 8, [pB], qkb)
                        cp("dve", vv, vv[:, 8:12, 0:64], pB[:, 512:768].rearrange("p (h d) -> p h d", h=4), [pB])
                        for k in range(4):
                            tp(pT, pT[:, k, :], qkb[:, k * 128:(k + 1) * 128], ident_b[:], [qkb, ident_b])
                        o1 = next_tq()
                        cp("act", o1, o1[:], pT[:, 0:4, :], [pT])
                        kb.dma("pool", qt_df.rearrange("(m p) n -> p m n", p=128)[:, :, cols], o1[:, 0:2, :], reads=[o1.b], writes=[qk_b[t]])
                        kb.dma("pool", kt_df.rearrange("(m p) n -> p m n", p=128)[:, :, cols], o1[:, 2:4, :], reads=[o1.b], writes=[qk_b[t]])
                        ck("A3")
                        for k in range(8):
                            mm(pC, pC[:, 0:512], hb[:, k, ts_], win[:, k, 1952:2464], [hb, win], start=(k == 0), stop=(k == 7))
                        act(sqs, sqs[:, 0:384], pC[:, 0:384], AF.Square, [pC])
                        red(st, st[:, 2:8], sqs[:, 0:384].rearrange("p (h d) -> p h d", h=6), ALU.add, [sqs])
                        rstd_from_ss(st, st[:, 2:8], st[:, 2:8], 64.0, [st])
                        g3 = gtmp[:, 0:384].rearrange("p (h d) -> p h d", h=6)
                        g32 = gtmp2[:, 0:384].rearrange("p (h d) -> p h d", h=6)
                        tt("dve", gtmp, g3, pC[:, 0:384].rearrange("p (h d) -> p h d", h=6),
                           st[:, 2:8].unsqueeze(2).to_broadcast([128, 6, 64]), ALU.mult, [pC, st])
                        tt("pool", gtmp2, g32, g3, ggq[:], ALU.mult, [gtmp, ggq])
                        rope(r5(gtmp2[:, 0:384], 6, 16), r5(qkb[:, 0:384], 6, 16), r64, t, 6, 16, [gtmp2], qkb)
                        cp("dve", vv, vv[:, 12:14, 0:64], pC[:, 384:512].rearrange("p (h d) -> p h d", h=2), [pC])
                        for k in range(3):
                            tp(pT, pT[:, k, :], qkb[:, k * 128:(k + 1) * 128], ident_b[:], [qkb, ident_b])
                        o1 = next_tq()
                        cp("act", o1, o1[:, 0:3, :], pT[:, 0:3, :], [pT])
                        kb.dma("pool", qt_gq.rearrange("(m p) n -> p m n", p=128)[:, :, cols], o1[:, 0:2, :], reads=[o1.b], writes=[qk_b[t]])
                        kb.dma("pool", kt_gq.rearrange("(m p) n -> p m n", p=128)[:, :, cols], o1[:, 2:3, :], reads=[o1.b], writes=[qk_b[t]])
                        kb.dma("pool", v1a[t * 128:(t + 1) * 128, :], vv[:].rearrange("p a b -> p (a b)"), reads=[vv.b], writes=[qk_b[t]])
            kb.barrier()
            if stop == "A":
                break
            cntB = [0]

            def attn_std(mixer, qt_d, kt_d, dk, nq, nk, kmap, vbase, nv, vmap, scale, diff=False):
                with contextlib.ExitStack() as es:
                    KT = sbuf(es, "b_KT", [128, nk, NT], BF16)
                    V1 = sbuf(es, "b_V1", [128, NTILE, nv, 65], BF16)
                    QT = [sbuf(es, "b_QT%d" % i, [128, nq, 512], BF16) for i in range(2)]
                    PT = [sbuf(es, "b_PT%d" % i, [128, 512], BF16) for i in range(3)]
                    osb = sbuf(es, "b_osb", [128, 4, nq, 64], F32)
                    osbb = sbuf(es, "b_osbb", [128, 4, 256], BF16)
                    rs = sbuf(es, "b_rs", [128, 4], F32)
                    otb = [sbuf(es, "b_otb%d" % i, [128, 2, 512], BF16) for i in range(2)]
                    stp = [psum(es, "b_st%d" % i, [128, 512]) for i in range(2)]
                    ops = [psum(es, "b_o%d" % i, [128, 512]) for i in range(4)]
                    tpp = psum(es, "b_tp", [128, 8, 128], BF16)
                    for m in range(nk):
                        kb.dma("sp", KT[0:dk, m, :], kt_d[m * dk:(m + 1) * dk, :], reads=qk_b, writes=[KT.b])
                    v4 = v1a.rearrange("(t p) (a b) -> p t a b", p=128, b=65)
                    for t0 in range(0, NTILE, 9):
                        t1 = min(NTILE, t0 + 9)
                        kb.dma("sp", V1[:, t0:t1, :, :], v4[:, t0:t1, vbase:vbase + nv, :], reads=qk_b, writes=[V1.b])
                    if diff:
                        lamt = sbuf(es, "b_lamt", [128, 4, 32], F32)
                        lp = sbuf(es, "b_lp", [128, 2, 32], F32)
                        ls = sbuf(es, "b_ls", [128, 2], F32)
                        neglam = sbuf(es, "b_neglam", [128, 1], F32)
                        gsub = sbuf(es, "b_gsub", [128, 64], F32)
                        dsb = sbuf(es, "b_dsb", [128, 4, 64], F32)
                        dsq = sbuf(es, "b_dsq", [128, 4, 64], F32)
                        dst = sbuf(es, "b_dst", [128, 4], F32)
                        for i in range(4):
                            kb.dma("sp", lamt[:, i, :], lamv[l, i].partition_broadcast(128), writes=[lamt.b])
                        kb.dma("sp", gsub[:], g_subln[l].partition_broadcast(128), writes=[gsub.b])
                        tt("dve", lp, lp[:, 0, :], lamt[:, 0, :], lamt[:, 1, :], ALU.mult, [lamt])
                        tt("dve", lp, lp[:, 1, :], lamt[:, 2, :], lamt[:, 3, :], ALU.mult, [lamt])
                        red(ls, ls[:, 0:2], lp[:], ALU.add, [lp])
                        act(ls, ls[:], ls[:], AF.Exp, [ls])
                        tt("dve", neglam, neglam[:, 0:1], ls[:, 1:2], ls[:, 0:1], ALU.subtract, [ls])
                        ts("dve", neglam, neglam[:], neglam[:], -lam_init, None, ALU.add, None, [neglam])
                        ts("dve", gsub, gsub[:], gsub[:], 1.0 - lam_init, None, ALU.mult, None, [gsub])
                    for bi, (c0, Tn, j) in enumerate(BLOCKS):
                        if j == 1 and l == DEPTH - 1:
                            continue
                        QTb = QT[bi % 2]
                        kb.dma("sp", QTb[0:dk, :, :Tn], qt_d.rearrange("(m p) n -> p m n", p=dk)[:, :, c0:c0 + Tn],
                               reads=qk_b, writes=[QTb.b])
                        kchunks = list(range(NTILE)) if j == 0 else [32, 33]
                        nqs = Tn // 128
                        for m in range(nq):
                            pend = None
                            for ci, kc in enumerate(kchunks):
                                sp_ = stp[cntB[0] % 2]
                                pt_ = PT[cntB[0] % 3]
                                cntB[0] += 1
                                mm(sp_, sp_[:, :Tn], KT[0:dk, kmap(m), kc * 128:(kc + 1) * 128], QTb[0:dk, m, :Tn], [KT, QTb])
                                if pend is not None:
                                    pend()
                                act(pt_, pt_[:, :Tn], sp_[:, :Tn], AF.Exp, [sp_], scale=scale)

                                def mk(ci=ci, kc=kc, pt_=pt_, m=m):
                                    def f():
                                        for qs in range(nqs):
                                            mm(ops[qs], ops[qs][:, 0:65], pt_[:, qs * 128:(qs + 1) * 128], V1[:, kc, vmap(m), :], [pt_, V1],
                                               start=(ci == 0), stop=(ci == len(kchunks) - 1))
                                    return f
                                pend = mk()
                            pend()
                            for qs in range(nqs):
                                recip(rs, rs[:, qs:qs + 1], ops[qs][:, 64:65], [ops[qs]])
                                if diff:
                                    ts("dve", osb, osb[:, qs, m, :], ops[qs][:, 0:64], rs[:, qs:qs + 1], None, ALU.mult, None, [ops[qs], rs])
                                else:
                                    ts("dve", osbb, osbb[:, qs, m * 64:(m + 1) * 64], ops[qs][:, 0:64], rs[:, qs:qs + 1], None, ALU.mult, None,
                                       [ops[qs], rs])
                        if diff:
                            for qs in range(nqs):
                                ov = osb[:, qs].rearrange("p (h i) d -> p h i d", i=2)
                                stt(dsb, dsb[:], ov[:, :, 1, :], neglam[:, 0:1], ov[:, :, 0, :], ALU.mult, ALU.add, [osb, neglam])
                                tt("pool", dsq, dsq[:], dsb[:], dsb[:], ALU.mult, [dsb])
                                red(dst, dst[:, 0:4], dsq[:], ALU.add, [dsq])
                                rstd_from_ss(dst, dst[:, 0:4], dst[:, 0:4], 64.0, [dst])
                                tt("dve", dsb, dsb[:], dsb[:], dst[:, 0:4].unsqueeze(2).to_broadcast([128, 4, 64]), ALU.mult, [dsb, dst])
                                tt("pool", osbb, osbb[:, qs, :].rearrange("p (h d) -> p h d", h=4), dsb[:],
                                   gsub[:].unsqueeze(1).to_broadcast([128, 4, 64]), ALU.mult, [dsb, gsub])
                        ot_ = otb[bi % 2]
                        for qs in range(nqs):
                            for c in range(2):
                                tp(tpp, tpp[:, qs * 2 + c, :], osbb[:, qs, c * 128:(c + 1) * 128], ident_b[:], [osbb, ident_b])
                        cp("act", ot_, ot_[:, :, :Tn].rearrange("p c (q n) -> p q c n", n=128),
                           tpp[:, 0:2 * nqs, :].rearrange("p (q c) n -> p q c n", c=2), [tpp])
                        kb.dma("pool", ota_view(c0, Tn)[:, 2 * mixer:2 * mixer + 2, :], ot_[:, :, :Tn], reads=[ot_.b],
                               writes=tr(ota_b, c0, Tn))
                kb.barrier()

            def attn_na():
                scale = 64.0 ** -0.5
                with contextlib.ExitStack() as es:
                    KT = sbuf(es, "n_KT", [128, 4, NT], BF16)
                    QT = sbuf(es, "n_QT", [128, 4, NT], BF16)
                    V1 = sbuf(es, "n_V1", [128, NTILE, 4, 65], BF16)
                    nb = [sbuf(es, "n_nb%d" % i, [128, 4, 5, 128], F32) for i in range(5)]
                    PT = [sbuf(es, "n_PT%d" % i, [128, 128], BF16) for i in range(3)]
                    tmpb = [sbuf(es, "n_tmp%d" % i, [128, 128], F32) for i in range(2)]
                    osbb = sbuf(es, "n_osbb", [128, 256], BF16)
                    rs = sbuf(es, "n_rs", [128, 1], F32)
                    otb = [sbuf(es, "n_otb%d" % i, [128, 2, 128], BF16) for i in range(2)]
                    stp = [psum(es, "n_st%d" % i, [128, 512]) for i in range(2)]
                    ops = [psum(es, "n_o%d" % i, [128, 512]) for i in range(2)]
                    tpp = psum(es, "n_tp", [128, 8, 128], BF16)
                    for m in range(4):
                        kb.dma("sp", KT[0:64, m, :], kt_na[m * 64:(m + 1) * 64, :], reads=qk_b, writes=[KT.b])
                        kb.dma("sp", QT[0:64, m, :], qt_na[m * 64:(m + 1) * 64, :], reads=qk_b, writes=[QT.b])
                    v4 = v1a.rearrange("(t p) (a b) -> p t a b", p=128, b=65)
                    for t0 in range(0, NTILE, 9):
                        t1 = min(NTILE, t0 + 9)
                        kb.dma("sp", V1[:, t0:t1, :, :], v4[:, t0:t1, 4:8, :], reads=qk_b, writes=[V1.b])
                    for cs in range(5):
                        kb.dma("sp", nb[cs][:].rearrange("p a b c -> p (a b c)"), nab[l, cs].rearrange("p a b c -> p (a b c)"),
                               writes=[nb[cs].b])
                    cnt = 0
                    for m in range(NTILE):
                        if m >= 32 and l == DEPTH - 1:
                            continue
                        if m < 32:
                            case = {0: 0, 1: 1, 30: 3, 31: 4}.get(m, 2)
                            k0 = min(max(m - 2, 0), 27)
                            chunks = [(k0 + i, i) for i in range(5)] + [(32, None), (33, None)]
                        else:
                            chunks = [(32, None), (33, None)]
                        for h in range(4):
                            o_ = ops[(m * 4 + h) % 2]
                            pend = None
                            for ci, (kc, li) in enumerate(chunks):
                                sp_ = stp[cnt % 2]
                                pt_ = PT[cnt % 3]
                                tb_ = tmpb[cnt % 2]
                                cnt += 1
                                mm(sp_, sp_[:, 0:128], KT[0:64, h, kc * 128:(kc + 1) * 128], QT[0:64, h, m * 128:(m + 1) * 128], [KT, QT])
                                if pend is not None:
                                    pend()
                                if li is not None:
                                    stt(tb_, tb_[:], sp_[:, 0:128], scale, nb[case][:, h, li, :], ALU.mult, ALU.add, [sp_, nb[case]])
                                    act(pt_, pt_[:], tb_[:], AF.Exp, [tb_])
                                else:
                                    act(pt_, pt_[:], sp_[:, 0:128], AF.Exp, [sp_], scale=scale)

                                def mkn(ci=ci, kc=kc, pt_=pt_, h=h, o_=o_, nch=len(chunks)):
                                    def f():
                                        mm(o_, o_[:, 0:65], pt_[:], V1[:, kc, h, :], [pt_, V1], start=(ci == 0), stop=(ci == nch - 1))
                                    return f
                                pend = mkn()
                            pend()
                            recip(rs, rs[:, 0:1], o_[:, 64:65], [o_])
                            ts("dve", osbb, osbb[:, h * 64:(h + 1) * 64], o_[:, 0:64], rs[:, 0:1], None, ALU.mult, None, [o_, rs])
                        ot_ = otb[m % 2]
                        for c in range(2):
                            tp(tpp, tpp[:, c, :], osbb[:, c * 128:(c + 1) * 128], ident_b[:], [osbb, ident_b])
                        cp("act", ot_, ot_[:], tpp[:, 0:2, :], [tpp])
                        kb.dma("pool", ota_view(m * 128, 128)[:, 2:4, :], ot_[:], reads=[ot_.b], writes=[ota_b[m]])
                kb.barrier()

            attn_std(0, qt_mla, kt_mla, 96, 4, 4, lambda m: m, 0, 4, lambda m: m, 96.0 ** -0.5)
            ck("B0")
            attn_na()
            ck("B1")
            attn_std(2, qt_df, kt_df, 32, 8, 8, lambda m: m, 8, 4, lambda m: m // 2, 32.0 ** -0.5, diff=True)
            ck("B2")
            attn_std(3, qt_gq, kt_gq, 64, 4, 2, lambda m: m // 2, 12, 2, lambda m: m // 2, 64.0 ** -0.5)
            if stop == "B":
                break
            with contextlib.ExitStack() as es:
                wg = sbuf(es, "c_wg", [128, 4, 8, D], BF16)
                wbr = sbuf(es, "c_wbr", [128, 4, 2, D], BF16)
                wo = sbuf(es, "c_wo", [128, 8, D], BF16)
                bg = sbuf(es, "c_bg", [128, 4, 8], F32)
                hb = sbuf(es, "c_hb", [128, 8, 512], BF16)
                ob = sbuf(es, "c_ob", [128, 8, 512], BF16)
                xb = sbuf(es, "c_xb", [128, 8, 512], F32)
                sig = [sbuf(es, "c_sig%d" % i, [128, 512], BF16) for i in range(2)]
                macc = [sbuf(es, "c_macc%d" % i, [128, 512], F32) for i in range(2)]
                tmpm = [sbuf(es, "c_tmpm%d" % i, [128, 512], F32) for i in range(2)]
                mrg = sbuf(es, "c_mrg", [128, 8, 512], BF16)
                pg = [psum(es, "c_pg%d" % i, [128, 512]) for i in range(2)]
                pb = [psum(es, "c_pb%d" % i, [128, 512]) for i in range(2)]
                py = [psum(es, "c_py%d" % i, [128, 512]) for i in range(2)]
                for i in range(4):
                    for k in range(8):
                        kb.dma("pool", wg[:, i, k, :], w_gate[l, i, k * 128:(k + 1) * 128, :], writes=[wg.b])
                    for k in range(2):
                        kb.dma("pool", wbr[:, i, k, :], w_branch[l, i, k * 128:(k + 1) * 128, :], writes=[wbr.b])
                for k in range(8):
                    kb.dma("pool", wo[:, k, :], w_out[l, k * 128:(k + 1) * 128, :], writes=[wo.b])
                kb.dma("sp", bg[:], b_gateT[l], writes=[bg.b])
                cntC = 0
                for bi, (c0, Tn, j) in enumerate(BLOCKS):
                    if j == 1 and l == DEPTH - 1:
                        continue
                    kb.dma("sp", hb[:, :, :Tn], hta_view(c0, Tn), reads=tr(hta_b, c0, Tn), writes=[hb.b])
                    kb.dma("sp", ob[:, :, :Tn], ota_view(c0, Tn), reads=tr(ota_b, c0, Tn), writes=[ob.b])
                    kb.dma("sp", xb[:, :, :Tn], xa_view(c0, Tn), reads=tr(xa_b, c0, Tn), writes=[xb.b])
                    for oc in range(8):
                        ocs = slice(oc * 128, (oc + 1) * 128)
                        ma = macc[oc % 2]
                        for i in range(4):
                            g_ = pg[cntC % 2]
                            b_ = pb[cntC % 2]
                            s_ = sig[cntC % 2]
                            t_ = tmpm[cntC % 2]
                            cntC += 1
                            for k in range(8):
                                mm(g_, g_[:, :Tn], wg[:, i, k, ocs], hb[:, k, :Tn], [wg, hb], start=(k == 0), stop=(k == 7))
                            act(s_, s_[:, :Tn], g_[:, :Tn], AF.Sigmoid, [g_, bg], bias=bg[:, i, oc:oc + 1])
                            for k in range(2):
                                mm(b_, b_[:, :Tn], wbr[:, i, k, ocs], ob[:, 2 * i + k, :Tn], [wbr, ob], start=(k == 0), stop=(k == 1))
                            if i == 0:
                                tt("dve", ma, ma[:, :Tn], b_[:, :Tn], s_[:, :Tn], ALU.mult, [b_, s_])
                            else:
                                tt("dve", t_, t_[:, :Tn], b_[:, :Tn], s_[:, :Tn], ALU.mult, [b_, s_])
                                if i < 3:
                                    tt("pool", ma, ma[:, :Tn], ma[:, :Tn], t_[:, :Tn], ALU.add, [ma, t_])
                                else:
                                    tt("pool", mrg, mrg[:, oc, :Tn], ma[:, :Tn], t_[:, :Tn], ALU.add, [ma, t_])
                    for oc in range(8):
                        ocs = slice(oc * 128, (oc + 1) * 128)
                        y_ = py[oc % 2]
                        for k in range(8):
                            mm(y_, y_[:, :Tn], wo[:, k, ocs], mrg[:, k, :Tn], [wo, mrg], start=(k == 0), stop=(k == 7))
                        stt(xb, xb[:, oc, :Tn], y_[:, :Tn], modT[:, 16 + oc, j:j + 1], xb[:, oc, :Tn], ALU.mult, ALU.add, [y_, modT, xb])
                    kb.dma("pool", xa_view(c0, Tn), xb[:, :, :Tn], reads=[xb.b], writes=tr(xa_b, c0, Tn))
            kb.barrier()
            if stop == "C":
                break

            with contextlib.ExitStack() as es:
                wq = sbuf(es, "d_wq", [128, 8, 2048], BF16)
                sk = sbuf(es, "d_sk", [128, 16, 128], F32)
                xb = sbuf(es, "d_xb", [128, 8, 256], F32)
                hb = sbuf(es, "d_hb", [128, 8, 256], BF16)
                sqk = [sbuf(es, "d_sq%d" % i, [128, 256], F32) for i in range(2)]
                tmpk = [sbuf(es, "d_tk%d" % i, [128, 256], F32) for i in range(2)]
                rstd = sbuf(es, "d_rstd", [128, 256], F32)
                qpc = [sbuf(es, "d_qpc%d" % i, [128, 256], F32) for i in range(2)]
                s_sb = sbuf(es, "d_s", [128, 2, 16, 128], F32)
                top16_2 = [sbuf(es, "d_top%d" % i, [128, 16, 16], F32) for i in range(2)]
                mr = sbuf(es, "d_mr", [128, 256], F32)
                best = sbuf(es, "d_best", [128, 8, 16], F32)
                eb = sbuf(es, "d_eb", [128, 8, 16], F32)
                sm = sbuf(es, "d_sm", [128, 6, 8], F32)
                tau = sbuf(es, "d_tau", [128, 2, 8], F32)
                nbias = sbuf(es, "d_nbias", [128, 2, 8], F32)
                cf = [sbuf(es, "d_cf%d" % i, [128, 16, 128], F32) for i in range(4)]
                cand = T(cf[0].t[:].rearrange("p (h a) (b c) -> p h a (b c)", h=8, c=16).rearrange("p h a (b c) -> p h (a b) c", c=16), "cand_alias")
                cand.b = cf[0].b
                pr = [sbuf(es, "d_pr%d" % i, [128, 16, 128], BF16) for i in range(4)]
                tw = [sbuf(es, "d_tw%d" % i, [128, 16, 128], BF16) for i in range(2)]
                Ws = [sbuf(es, "d_Ws%d" % i, [128, 2, 16, 128], BF16) for i in range(2)]
                uch = [sbuf(es, "d_uch%d" % i, [128, 8, 512], BF16) for i in range(2)]
                vch = [sbuf(es, "d_vch%d" % i, [128, 4, D], BF16) for i in range(2)]
                wts = [sbuf(es, "d_wts%d" % i, [128, 4, 256], BF16) for i in range(2)]
                gel = [sbuf(es, "d_gel%d" % i, [128, 256], BF16) for i in range(2)]
                cT = [sbuf(es, "d_cT%d" % i, [128, 256], BF16) for i in range(4)]
                yps = [psum(es, "d_y%d" % i, [128, 512]) for i in range(4)]
                pm1 = psum(es, "d_pm1", [128, 512])
                apsl = [psum(es, "d_a%d" % i, [128, 512]) for i in range(2)]
                wtp = psum(es, "d_wt", [128, 4, 256], BF16)
                for k in range(8):
                    kb.dma("pool", wq[:, k, :], w_pq[l, k * 128:(k + 1) * 128, :], writes=[wq.b])
                kb.dma("sp", sk[:].rearrange("p a b -> p (a b)"), skT[l].rearrange("p a b -> p (a b)"), writes=[sk.b])
                if l + 1 < NL:
                    convert_uv(l + 1)
                ub = ub2[l % 2]
                vb = vb2[l % 2]
                ub_b = ub_b2[l % 2]
                vb_b = vb_b2[l % 2]
                ubv = ub.rearrange("(k p) e -> p k e", p=128)
                cDl = [0]
                cUl = [0]
                for bi, (c0, Tn, j) in enumerate(PBLOCKS):
                    if j == 1 and l == DEPTH - 1:
                        continue
                    kb.dma("sp", xb[:], xa_view(c0, 256), reads=tr(xa_b, c0, 256), writes=[xb.b])
                    norm_mod(xb, hb, sqk, rstd, tmpk, pm1, 256,
                             lambda k: A2[:, k, j:j + 1], lambda k: modT[:, 24 + k, j:j + 1])
                    for c in range(16):
                        q_ = qpc[c % 2]
                        for k in range(8):
                            mm(pm1, pm1[:, 0:256], wq[:, k, c * 128:(c + 1) * 128], hb[:, k, :], [wq, hb], start=(k == 0), stop=(k == 7))
                        cp("act", q_, q_[:], pm1[:, 0:256], [pm1])
                        for tl in range(2):
                            col = 256 + tl * 128
                            mm(pm1, pm1[:, col:col + 128], q_[:, tl * 128:(tl + 1) * 128], sk[:, c, :], [q_, sk])
                        cp("act", s_sb, s_sb[:, :, c, :], pm1[:, 256:512].rearrange("p (t n) -> p t n", t=2), [pm1])
                        for tl in range(2):
                            tk = top16_2[tl]
                            kb.op("dve", lambda: nc.vector.max(out=tk[:, c, 0:8], in_=s_sb[:, tl, c, :]), reads=[s_sb.b], writes=[tk.b])
                            kb.op("dve", lambda: nc.vector.match_replace(out=mr[:, 0:128], in_to_replace=tk[:, c, 0:8],
                                                                          in_values=s_sb[:, tl, c, :], imm_value=-1e30),
                                  reads=[s_sb.b, tk.b], writes=[mr.b])
                            kb.op("dve", lambda: nc.vector.max(out=tk[:, c, 8:16], in_=mr[:, 0:128]), reads=[mr.b], writes=[tk.b])
                    for tl in range(2):
                        top16 = top16_2[tl]
                        t4 = top16[:].rearrange("p (h a) k -> p h a k", a=2)
                        tt("dve", cand, cand[:], t4[:, :, 0, :].unsqueeze(3).to_broadcast([128, 8, 16, 16]),
                           t4[:, :, 1, :].unsqueeze(2).to_broadcast([128, 8, 16, 16]), ALU.add, [top16])
                        for h in range(8):
                            ch = cand[:, h].rearrange("p a b -> p (a b)")
                            kb.op("dve", lambda: nc.vector.max(out=best[:, h, 0:8], in_=ch), reads=[cand.b], writes=[best.b])
                            kb.op("dve", lambda: nc.vector.match_replace(out=mr[:, 0:256], in_to_replace=best[:, h, 0:8], in_values=ch,
                                                                          imm_value=-1e30),
                                  reads=[cand.b, best.b], writes=[mr.b])
                            kb.op("dve", lambda: nc.vector.max(out=best[:, h, 8:16], in_=mr[:, 0:256]), reads=[mr.b], writes=[best.b])
                        ts("dve", sm, sm[:, 0, :], best[:, :, 0], -1.0, None, ALU.mult, None, [best])
                        cp("dve", tau, tau[:, tl, :], best[:, :, 15], [best])
                        for h in range(8):
                            act(eb, eb[:, h, :], best[:, h, :], AF.Exp, [best, sm], bias=sm[:, 0, h:h + 1])
                        red(sm, sm[:, 1, :], eb[:], ALU.add, [eb])
                        act(sm, sm[:, 1, :], sm[:, 1, :], AF.Ln, [sm])
                        ts("dve", sm, sm[:, 2, :], t4[:, :, 0, 0], -1.0, None, ALU.mult, None, [top16])
                        stt(sm, sm[:, 3, :], t4[:, :, 1, 0], -1.0, sm[:, 1, :], ALU.mult, ALU.subtract, [top16, sm])
                        tt("dve", nbias, nbias[:, tl, :], sm[:, 0, :], sm[:, 1, :], ALU.subtract, [sm])
                    units = [(ig, tl, h) for ig in range(8) for tl in range(2) for h in range(8)]

                    def cand_of(n):
                        ig, tl, h = units[n]
                        isl = slice(ig * 16, (ig + 1) * 16)
                        c_ = cf[n % 4]
                        tt("dve", c_, c_[:], s_sb[:, tl, 2 * h, isl].unsqueeze(2).to_broadcast([128, 16, 128]),
                           s_sb[:, tl, 2 * h + 1, :].unsqueeze(1).to_broadcast([128, 16, 128]), ALU.add, [s_sb])

                    def exp_of(n):
                        ig, tl, h = units[n]
                        act(pr[n % 4], pr[n % 4][:], cf[n % 4][:], AF.Exp, [cf[n % 4], nbias], bias=nbias[:, tl, h:h + 1])

                    def acc_of(n):
                        ig, tl, h = units[n]
                        W_ = Ws[ig % 2]
                        c_ = cf[n % 4]
                        p_ = pr[n % 4]
                        w_ = tw[n % 2]
                        if h == 0:
                            stt(W_, W_[:, tl], c_[:], tau[:, tl, h:h + 1], p_[:], ALU.is_ge, ALU.mult, [c_, p_, tau])
                        else:
                            stt(w_, w_[:], c_[:], tau[:, tl, h:h + 1], p_[:], ALU.is_ge, ALU.mult, [c_, p_, tau])
                            tt("dve", W_, W_[:, tl], W_[:, tl], w_[:], ALU.add, [W_, w_])

                    cand_of(0)
                    cand_of(1)
                    pend = None
                    grp = None
                    for s_ in range(64 + 8):
                        if s_ + 1 < 64:
                            cand_of(2 * s_ + 2)
                            cand_of(2 * s_ + 3)
                        if s_ < 64:
                            exp_of(2 * s_)
                            exp_of(2 * s_ + 1)
                            acc_of(2 * s_)
                            acc_of(2 * s_ + 1)
                        if s_ >= 8:
                            cpair = s_ - 8
                            i_a = 2 * cpair
                            ig = i_a // 16
                            il = i_a % 16
                            W_ = Ws[ig % 2]
                            if i_a % 4 == 0:
                                u_ = uch[cUl[0] % 2]
                                v_ = vch[cUl[0] % 2]
                                ws_ = wts[cUl[0] % 2]
                                cUl[0] += 1
                                kb.dma("sp", u_[:], ubv[:, :, i_a * 128:(i_a + 4) * 128], reads=[ub_b], writes=[u_.b])
                                kb.dma("sp", v_[:], vb[i_a * 128:(i_a + 4) * 128, :].rearrange("(a p) d -> p a d", p=128), reads=[vb_b],
                                       writes=[v_.b])
                                for k4 in range(4):
                                    for tl in range(2):
                                        tp(wtp, wtp[:, k4, tl * 128:(tl + 1) * 128], W_[:, tl, il + k4, :], ident_b[:], [W_, ident_b])
                                cp("act", ws_, ws_[:], wtp[:], [wtp])
                                grp = (u_, v_, ws_)
                            u_, v_, ws_ = grp
                            for i in (i_a, i_a + 1):
                                ii = i % 4
                                a_ = apsl[i % 2]
                                for k in range(8):
                                    mm(a_, a_[:, 0:256], u_[:, k, ii * 128:(ii + 1) * 128], hb[:, k, :], [u_, hb], start=(k == 0), stop=(k == 7))
                            if pend is not None:
                                pend()
                            for i in (i_a, i_a + 1):
                                act(gel[i % 2], gel[i % 2][:], apsl[i % 2][:, 0:256], AF.Gelu, [apsl[i % 2]])
                            for i in (i_a, i_a + 1):
                                tt("dve", cT[i % 4], cT[i % 4][:], gel[i % 2][:], ws_[:, i % 4, :], ALU.mult, [gel[i % 2], ws_])

                            def mkv(i_a=i_a, v_=v_):
                                def f():
                                    for i in (i_a, i_a + 1):
                                        c2 = cT[i % 4]
                                        for oc in range(8):
                                            y_ = yps[oc // 2]
                                            mm(y_, y_[:, (oc % 2) * 256:(oc % 2) * 256 + 256], v_[:, i % 4, oc * 128:(oc + 1) * 128], c2[:],
                                               [v_, c2], start=(i == 0 and oc % 2 == 0), stop=(i == 127))
                                return f
                            pend = mkv()
                    pend()
                    for oc in range(8):
                        y_ = yps[oc // 2]
                        stt(xb, xb[:, oc, :], y_[:, (oc % 2) * 256:(oc % 2) * 256 + 256], modT[:, 40 + oc, j:j + 1], xb[:, oc, :],
                            ALU.mult, ALU.add, [y_, modT, xb])
                    kb.dma("sp", xa_view(c0, 256), xb[:], reads=[xb.b], writes=tr(xa_b, c0, 256))
                    ck("D0")
            kb.barrier()
            if stop == "D":
                break
          except StopBuild:
            break

        if stop is None:
            with contextlib.ExitStack() as es:
                fn = sbuf(es, "f_fn", [128, 8], F32)
                xb_ = [sbuf(es, "f_xb%d" % i, [128, 8, 512], F32) for i in range(2)]
                sqk = [sbuf(es, "f_sq%d" % i, [128, 512], F32) for i in range(2)]
                xn = sbuf(es, "f_xn", [128, 8, 512], F32)
                rstd = sbuf(es, "f_rstd", [128, 512], F32)
                yo = [sbuf(es, "f_yo%d" % i, [128, D], F32) for i in range(2)]
                ssp = psum(es, "f_ssp", [128, 512])
                pp = [psum(es, "f_pp%d" % i, [128, 1024]) for i in range(2)]
                kb.dma("sp", fn[:], fnormT[:, :], writes=[fn.b])
                for bi in range(8):
                    c0 = bi * 512
                    xb = xb_[bi % 2]
                    kb.dma("sp", xb[:], xa_view(c0, 512), reads=tr(xa_b, c0, 512), writes=[xb.b])
                    for k in range(8):
                        q_ = sqk[k % 2]
                        act(q_, q_[:], xb[:, k, :], AF.Square, [xb])
                        mm(ssp, ssp[:], ones_f[:], q_[:], [ones_f, q_], start=(k == 0), stop=(k == 7))
                    rstd_from_ss(rstd, rstd[:], ssp[:], float(D), [ssp])
                    for k in range(8):
                        stt(xn, xn[:, k, :], xb[:, k, :], fn[:, k:k + 1], rstd[:], ALU.mult, ALU.mult, [xb, fn, rstd])
                    for ti in range(4):
                        t = bi * 4 + ti
                        p = pp[t % 2]
                        o = yo[t % 2]
                        for k in range(8):
                            tp(p, p[:, k * 128:(k + 1) * 128], xn[:, k, ti * 128:(ti + 1) * 128], ident_f[:], [xn, ident_f])
                        cp("act" if t % 2 else "dve", o, o[:], p[:], [p])
                        kb.dma("sp", out[t * 128:(t + 1) * 128, :], o[:], reads=[o.b])

        kb.dead = False
        kb.barrier()
    return nc


def rope_tables():
    pos = np.arange(NLAT)
    rows = (pos // 64).astype(np.float32)
    cols = (pos % 64).astype(np.float32)

    def tab(dh):
        inv = np.power(np.float32(10000.0), -np.arange(0, dh, 2, dtype=np.float32) / np.float32(dh)).astype(np.float32)
        out = np.zeros((NT, 2, 2, dh // 2), np.float32)
        out[:, 0] = 1.0
        for a, p_ in enumerate((rows, cols)):
            ang = (p_[:, None] * inv[None, :]).astype(np.float32)
            out[:NLAT, 0, a] = np.cos(ang)
            out[:NLAT, 1, a] = np.sin(ang)
        return out.reshape(NT, -1)

    return tab(16), tab(32)


def na_bias_tables(rpb):
    Lr = rpb.shape[0]
    out = np.full((Lr, 5, 128, 4, 5, 128), NEG, np.float32)
    for case, m in enumerate((0, 1, 2, 30, 31)):
        k0 = min(max(m - 2, 0), 27)
        q = m * 128 + np.arange(128)
        qr, qc = q // 64, q % 64
        rs = np.clip(qr - 4, 0, 56)
        cs = np.clip(qc - 8, 0, 48)
        for i in range(5):
            key = (k0 + i) * 128 + np.arange(128)
            kr, kc_ = key // 64, key % 64
            inw = ((kr[:, None] >= rs[None, :]) & (kr[:, None] < rs[None, :] + 8)
                   & (kc_[:, None] >= cs[None, :]) & (kc_[:, None] < cs[None, :] + 16))
            ro = np.clip(kr[:, None] - qr[None, :] + 7, 0, 14)
            co = np.clip(kc_[:, None] - qc[None, :] + 15, 0, 30)
            for h in range(4):
                g = rpb[:, h][:, ro, co]
                out[:, case, :, h, i, :] = np.where(inw[None], g, np.float32(NEG))
    return out


def prep_inputs(inp):
    f = lambda a: np.ascontiguousarray(np.asarray(a, dtype=np.float32))
    L = DEPTH
    r32, r64 = rope_tables()
    shared = {
        "w_mod": f(inp["w_mod"]),
        "b_modT": f(inp["b_mod"].reshape(L, 48, 128).transpose(0, 2, 1)),
        "nmixT": f(inp["norm_mix"].reshape(L, 8, 128).transpose(0, 2, 1)),
        "nffnT": f(inp["norm_ffn"].reshape(L, 8, 128).transpose(0, 2, 1)),
        "fnormT": f(inp["final_norm"].reshape(8, 128).T),
        "w_in": f(inp["w_in"]),
        "g_mlaq": f(inp["mla_q_norm"]),
        "g_mlakv": f(inp["mla_kv_norm"]),
        "w_uq": f(inp["mla_w_uq"]),
        "w_ukv": f(inp["mla_w_ukv"]),
        "nab": f(na_bias_tables(np.asarray(inp["na_rpb"], np.float32))),
        "lamv": f(np.stack([inp["diff_lam_q1"], inp["diff_lam_k1"], inp["diff_lam_q2"], inp["diff_lam_k2"]], axis=1)),
        "g_subln": f(inp["diff_subln"]),
        "g_gq": f(inp["gqa_q_norm"]),
        "g_gk": f(inp["gqa_k_norm"]),
        "w_branch": f(inp["w_branch"]),
        "w_gate": f(inp["w_gate"]),
        "b_gateT": f(inp["b_gate"].reshape(L, 4, 8, 128).transpose(0, 3, 1, 2)),
        "w_out": f(inp["w_out"]),
        "w_pq": f(inp["peer_w_q"]),
        "skT": f(inp["peer_subkeys"].reshape(L, 16, 128, 128).transpose(0, 3, 1, 2)),
        "uT": f(np.asarray(inp["peer_u"]).transpose(0, 2, 1)),
        "pv": f(inp["peer_v"]),
        "rope32": f(r32),
        "rope64": f(r64),
    }
    maps = []
    for b in range(8):
        m = dict(shared)
        m["xin"] = f(np.concatenate([inp["x"][b], inp["ctx"][b]], axis=0))
        cc = np.stack([inp["c"][b], inp["c_ctx"]], axis=0).reshape(2, 8, 128).transpose(2, 1, 0).reshape(128, 16)
        m["cc"] = f(cc)
        maps.append(m)
    return maps


def kernel(**inputs):
    maps = prep_inputs(inputs)
    nc = build()
    res = run_bass_kernel_spmd(nc, maps, core_ids=list(range(8)))
    return np.stack([np.asarray(r["out"], dtype=np.float32) for r in res.results], axis=0)
```
